# Optimizing a Trainium2 kernel written in Bass

```python
import math
import jax, jax.numpy as jnp
from jax import lax
import numpy as np

D_MODEL = 1024
BATCH = 8
SEQ = 4096
DEPTH = 4

N_MIXERS = 2
N_HEADS = 8
QK_HEAD_DIM = 128
V_HEAD_DIM = 128
KV_RANK = 256
IDX_HEADS = 8
IDX_HEAD_DIM = 64
TOPK_MAX = 256
Q_BLOCK = 128
A_IN = N_HEADS * QK_HEAD_DIM + KV_RANK + IDX_HEADS * IDX_HEAD_DIM + IDX_HEAD_DIM + IDX_HEADS
GMLP_HALF = 2 * D_MODEL
GMLP_GROUPS = 8
GMLP_GROUP_DIM = GMLP_HALF // GMLP_GROUPS
CHUNK = 128
D_FF = -(-8 * D_MODEL // (3 * 256)) * 256
N_MOD = 6
EPS = 1e-6
N_A = (DEPTH + 1) // 2
N_B = DEPTH // 2

kernel_name = 'hybrid_dsa_gmlp_swiglu_adaln'


def rmsnorm(x, g):
    xf = x.astype(jnp.float32)
    y = xf * lax.rsqrt(jnp.mean(xf * xf, axis=-1, keepdims=True) + EPS)
    return (y * g.astype(jnp.float32)).astype(x.dtype)


def layernorm(x, g, b):
    xf = x.astype(jnp.float32)
    mu = jnp.mean(xf, axis=-1, keepdims=True)
    xc = xf - mu
    var = jnp.mean(xc * xc, axis=-1, keepdims=True)
    return (xc * lax.rsqrt(var + EPS) * g.astype(jnp.float32) + b.astype(jnp.float32)).astype(x.dtype)


def modulate(h, shift, scale):
    return h * (1 + scale[:, None, :]) + shift[:, None, :]


def dsa_mixer(h, w_in, g_kv, g_kidx, b_kidx, w_uk, w_uv, w_o):
    B, T, _ = h.shape
    proj = h @ w_in
    o1 = N_HEADS * QK_HEAD_DIM
    o2 = o1 + KV_RANK
    o3 = o2 + IDX_HEADS * IDX_HEAD_DIM
    o4 = o3 + IDX_HEAD_DIM
    q = proj[..., :o1].reshape(B, T, N_HEADS, QK_HEAD_DIM)
    c_kv = rmsnorm(proj[..., o1:o2], g_kv)
    q_idx = proj[..., o2:o3].reshape(B, T, IDX_HEADS, IDX_HEAD_DIM)
    k_idx = layernorm(proj[..., o3:o4], g_kidx, b_kidx)
    w_idx = proj[..., o4:] * (IDX_HEADS ** -0.5 * IDX_HEAD_DIM ** -0.5)
    q_lat = jnp.einsum('bthd,hdr->bthr', q, w_uk) * (QK_HEAD_DIM ** -0.5)
    top_k = min(TOPK_MAX, T // 4)
    nb = T // Q_BLOCK
    key_pos = jnp.arange(T)

    def to_blocks(a):
        return a.reshape(B, nb, Q_BLOCK, *a.shape[2:]).swapaxes(0, 1)

    def block(args):
        qi_b, wi_b, ql_b, start = args
        q_pos = start + jnp.arange(Q_BLOCK)
        causal = key_pos[None, :] <= q_pos[:, None]
        logits = jnp.einsum('bqhd,bsd->bqhs', qi_b, k_idx).astype(jnp.float32)
        score = jnp.einsum('bqhs,bqh->bqs', jax.nn.relu(logits), wi_b.astype(jnp.float32))
        score = jnp.where(causal[None], score, -jnp.inf)
        _, idx = lax.top_k(score, top_k)
        valid = idx <= q_pos[None, :, None]
        c_sel = jax.vmap(lambda cb, ib: cb[ib])(c_kv, idx)
        att = jnp.einsum('bqhr,bqkr->bqhk', ql_b, c_sel).astype(jnp.float32)
        att = jnp.where(valid[:, :, None, :], att, -jnp.inf)
        p = jax.nn.softmax(att, axis=-1).astype(c_sel.dtype)
        return jnp.einsum('bqhk,bqkr->bqhr', p, c_sel)

    starts = jnp.arange(nb, dtype=jnp.int32) * Q_BLOCK
    o_lat = lax.map(block, (to_blocks(q_idx), to_blocks(w_idx), to_blocks(q_lat), starts))
    o_lat = o_lat.swapaxes(0, 1).reshape(B, T, N_HEADS, KV_RANK)
    o = jnp.einsum('bthr,hrd->bthd', o_lat, w_uv).reshape(B, T, N_HEADS * V_HEAD_DIM)
    return o @ w_o


def gmlp_mixer(h, w_in, ln_g, ln_b, w_s, b_s, w_out):
    B, T, _ = h.shape
    z = jax.nn.gelu(h @ w_in, approximate=False)
    u = z[..., :GMLP_HALF]
    v = layernorm(z[..., GMLP_HALF:], ln_g, ln_b)
    nc = T // CHUNK
    v = v.reshape(B, nc, CHUNK, GMLP_GROUPS, GMLP_GROUP_DIM)
    w_causal = w_s * jnp.tril(jnp.ones((CHUNK, CHUNK), dtype=w_s.dtype))
    mixed = jnp.einsum('gts,bcsge->bctge', w_causal, v) + b_s.T[:, :, None]
    y = u * mixed.reshape(B, T, GMLP_HALF)
    return y @ w_out


def swiglu(h, w_gate, w_up, w_down):
    return (jax.nn.silu(h @ w_gate) * (h @ w_up)) @ w_down


def setup_inputs(seed: int = 0) -> dict:
    key = jax.random.key(seed)
    ks = jax.random.split(key, 24)
    f32 = jnp.float32
    nrm = lambda k, shape, s: jax.random.normal(k, shape, f32) * s
    D = D_MODEL
    return {
        'x': nrm(ks[0], (BATCH, SEQ, D), 1.0),
        'c': nrm(ks[1], (BATCH, D), 1.0),
        'mod_w': nrm(ks[2], (DEPTH, D, N_MOD * D), 0.5 * D ** -0.5),
        'mod_b': nrm(ks[3], (DEPTH, N_MOD * D), 0.02),
        'norm_mix_g': 1.0 + nrm(ks[4], (DEPTH, D), 0.02),
        'norm_ffn_g': 1.0 + nrm(ks[5], (DEPTH, D), 0.02),
        'a_w_in': nrm(ks[6], (N_A, D, A_IN), D ** -0.5),
        'a_g_kv': 1.0 + nrm(ks[7], (N_A, KV_RANK), 0.02),
        'a_g_kidx': 1.0 + nrm(ks[8], (N_A, IDX_HEAD_DIM), 0.02),
        'a_b_kidx': nrm(ks[9], (N_A, IDX_HEAD_DIM), 0.02),
        'a_w_uk': nrm(ks[10], (N_A, N_HEADS, QK_HEAD_DIM, KV_RANK), QK_HEAD_DIM ** -0.5),
        'a_w_uv': nrm(ks[11], (N_A, N_HEADS, KV_RANK, V_HEAD_DIM), KV_RANK ** -0.5),
        'a_w_o': nrm(ks[12], (N_A, N_HEADS * V_HEAD_DIM, D), (N_HEADS * V_HEAD_DIM) ** -0.5),
        'b_w_in': nrm(ks[13], (N_B, D, 2 * GMLP_HALF), D ** -0.5),
        'b_ln_g': 1.0 + nrm(ks[14], (N_B, GMLP_HALF), 0.02),
        'b_ln_b': nrm(ks[15], (N_B, GMLP_HALF), 0.02),
        'b_w_s': nrm(ks[16], (N_B, GMLP_GROUPS, CHUNK, CHUNK), 0.5 * CHUNK ** -0.5),
        'b_b_s': 1.0 + nrm(ks[17], (N_B, GMLP_GROUPS, CHUNK), 0.1),
        'b_w_out': nrm(ks[18], (N_B, GMLP_HALF, D), GMLP_HALF ** -0.5),
        'ffn_w_gate': nrm(ks[19], (DEPTH, D, D_FF), D ** -0.5),
        'ffn_w_up': nrm(ks[20], (DEPTH, D, D_FF), D ** -0.5),
        'ffn_w_down': nrm(ks[21], (DEPTH, D_FF, D), D_FF ** -0.5),
        'final_g': 1.0 + nrm(ks[22], (D,), 0.02),
    }


def reference(x, c, mod_w, mod_b, norm_mix_g, norm_ffn_g, a_w_in, a_g_kv, a_g_kidx, a_b_kidx,
              a_w_uk, a_w_uv, a_w_o, b_w_in, b_ln_g, b_ln_b, b_w_s, b_b_s, b_w_out,
              ffn_w_gate, ffn_w_up, ffn_w_down, final_g):
    c_act = jax.nn.silu(c)
    for layer in range(DEPTH):
        mod = c_act @ mod_w[layer] + mod_b[layer]
        sh1, sc1, g1, sh2, sc2, g2 = jnp.split(mod, N_MOD, axis=-1)
        h = modulate(rmsnorm(x, norm_mix_g[layer]), sh1, sc1)
        j = layer // N_MIXERS
        if layer % N_MIXERS == 0:
            y = dsa_mixer(h, a_w_in[j], a_g_kv[j], a_g_kidx[j], a_b_kidx[j],
                          a_w_uk[j], a_w_uv[j], a_w_o[j])
        else:
            y = gmlp_mixer(h, b_w_in[j], b_ln_g[j], b_ln_b[j], b_w_s[j], b_b_s[j], b_w_out[j])
        x = x + g1[:, None, :] * y
        h = modulate(rmsnorm(x, norm_ffn_g[layer]), sh2, sc2)
        x = x + g2[:, None, :] * swiglu(h, ffn_w_gate[layer], ffn_w_up[layer], ffn_w_down[layer])
    return rmsnorm(x, final_g)
```

```python
import numpy as np
from contextlib import ExitStack
import concourse.bass as bass
import concourse.mybir as mybir
from concourse.bass_utils import run_bass_kernel_spmd

F32 = mybir.dt.float32
BF16 = mybir.dt.bfloat16
ALU = mybir.AluOpType
AF = mybir.ActivationFunctionType
AX = mybir.AxisListType

T = 4096
D = 1024
NT = 32
DFF = 2816
NFC = 22
A_IN = 1864
TOPK = 256
NIT = 13
EPS = 1e-6
NEG = -1.0e30


class Buf:
    __slots__ = ("w", "r")

    def __init__(self):
        self.w = None
        self.r = {}


class KB:
    def __init__(self, nc, es):
        self.nc = nc
        self.es = es
        self.eng = dict(pe=nc.tensor, dve=nc.vector, act=nc.scalar, pool=nc.gpsimd, sp=nc.sync)
        self.sems = {}
        self.cnt = {}
        self.seen = {e: {} for e in self.eng}
        for e in self.eng:
            self.sems[e] = es.enter_context(nc.semaphore("s_" + e))
            self.cnt[e] = 0
        self.dq = {}
        self.dqi = {}
        for q, n in (("sp", 20), ("pool", 10)):
            self.dq[q] = []
            self.dqi[q] = 0
            for i in range(n):
                k = "d_%s%d" % (q, i)
                self.sems[k] = es.enter_context(nc.semaphore(k))
                self.cnt[k] = 0
                self.dq[q].append(k)
        self.nbank = 0

    def _wait(self, e, key, val):
        if val <= 0 or self.seen[e].get(key, 0) >= val:
            return
        self.eng[e].wait_ge(self.sems[key], val)
        self.seen[e][key] = val

    def _deps(self, e, reads, writes, strict=False):
        for b in reads:
            if b.w is not None:
                k, v = b.w
                if strict or k != e or e != "pe":
                    self._wait(e, k, v)
        for b in writes:
            if b.w is not None:
                k, v = b.w
                if strict or k != e:
                    self._wait(e, k, v)
            for k, v in b.r.items():
                if strict or k != e:
                    self._wait(e, k, v)

    def op(self, e, fn, reads=(), writes=()):
        self._deps(e, reads, writes)
        inst = fn(self.eng[e])
        self.cnt[e] += 1
        inst.then_inc(self.sems[e], 1)
        v = self.cnt[e]
        for b in reads:
            b.r[e] = v
        for b in writes:
            b.w = (e, v)
            b.r = {}

    def dma(self, q, out, in_, reads=(), writes=()):
        pool = self.dq[q]
        key = pool[self.dqi[q] % len(pool)]
        self.dqi[q] += 1
        self._wait(q, key, self.cnt[key])
        self._deps(q, reads, writes, strict=True)
        inst = self.eng[q].dma_start(out=out, in_=in_)
        self.cnt[key] += 16
        inst.then_inc(self.sems[key], 16)
        v = self.cnt[key]
        for b in reads:
            b.r[key] = v
        for b in writes:
            b.w = (key, v)
            b.r = {}

    def barrier(self):
        keys = list(self.sems.keys())
        for e in self.eng:
            for k in keys:
                if k != e:
                    self._wait(e, k, self.cnt[k])


def _build(phases):
    nc = bass.Bass("TRN2", target_bir_lowering=False)
    es = ExitStack()
    kb = KB(nc, es)

    def dram(name, shape, kind="ExternalInput", dt=F32):
        return nc.dram_tensor(name, list(shape), dt, kind=kind).ap()

    dx = dram("x", [T, D])
    dout = dram("out", [T, D], kind="ExternalOutput")
    dxs = dram("xs", [T, D], kind="Internal")
    dgs = dram("gsc", [8, 128, D], kind="Internal")
    d_cT = dram("cT", [128, 8])
    d_modw = dram("mod_w", [4, D, 6 * D])
    d_modb = dram("mod_b", [4, 6 * D])
    d_gmix = dram("gmixT", [128, 32])
    d_gffn = dram("gffnT", [128, 32])
    d_fg = dram("final_g", [1, D])
    d_awin = dram("a_w_in", [2, D, A_IN])
    d_agkv = dram("a_g_kv", [2, 256])
    d_agki = dram("a_g_kidx", [2, 64])
    d_abki = dram("a_b_kidx", [2, 64])
    d_awuk = dram("a_w_uk", [2, 8, 128, 256])
    d_awuv = dram("a_w_uv", [2, 8, 256, 128])
    d_awo = dram("a_w_o", [2, D, D])
    d_bwin = dram("b_w_in", [2, D, 4096])
    d_blng = dram("b_ln_g", [2, 2048])
    d_blnb = dram("b_ln_b", [2, 2048])
    d_bwsT = dram("b_w_sT", [2, 128, 8 * 128])
    d_bbs = dram("b_b_s", [2, 8 * 128])
    d_bwout = dram("b_w_out", [2, 2048, D])
    d_fwg = dram("ffn_w_gate", [4, D, DFF])
    d_fwu = dram("ffn_w_up", [4, D, DFF])
    d_fwd = dram("ffn_w_down", [4, DFF, D])

    uid = [0]

    def sb(stack, name, shape, dt):
        uid[0] += 1
        return stack.enter_context(nc.sbuf_tensor("%s_%d" % (name, uid[0]), list(shape), dt))

    ident_f = sb(es, "ident_f", [128, 128], F32)
    ident_b = sb(es, "ident_b", [128, 128], BF16)
    mhalf = sb(es, "mhalf", [128, 1], F32)
    modT = sb(es, "modT", [128, 4, 48], F32)
    amix = sb(es, "amix", [128, 4, 8], F32)
    affn = sb(es, "affn", [128, 4, 8], F32)
    gmixT = sb(es, "gmixT_s", [128, 32], F32)
    gffnT = sb(es, "gffnT_s", [128, 32], F32)
    B_const = Buf()
    B_modT = Buf()
    banks = [es.enter_context(nc.psum_tensor("ps%d" % i, [128, 512], F32)) for i in range(8)]
    bankb = [Buf() for _ in range(8)]

    nb2 = [0]

    def pbank():
        i = kb.nbank % 5
        kb.nbank += 1
        return banks[i], bankb[i]

    def pbank2():
        i = 4 + nb2[0] % 2
        nb2[0] += 1
        return banks[i], bankb[i]

    TB = banks[7][:].bitcast(BF16)
    TBb = bankb[7]

    kb.op("pool", lambda e: e.memset(ident_f[:], 1.0), writes=[B_const])
    kb.op("pool", lambda e: e.memset(mhalf[:], -0.5), writes=[B_const])
    kb.op("pool", lambda e: e.affine_select(out=ident_f[:], in_=ident_f[:], pattern=[[-1, 128]],
                                            compare_op=ALU.is_equal, fill=0.0, base=0, channel_multiplier=1),
          reads=[B_const], writes=[B_const])
    kb.op("dve", lambda e: e.tensor_copy(out=ident_b[:], in_=ident_f[:]), reads=[B_const], writes=[B_const])

    def rstd_from_ss(stk_rs, ss, B_ss, n, B_rs):
        kb.op("pool", lambda e: e.tensor_scalar(out=stk_rs, in0=ss, scalar1=float(n) * EPS, scalar2=None,
                                                op0=ALU.add), reads=[B_ss], writes=[B_rs])
        kb.op("pool", lambda e: e.tensor_tensor(out=stk_rs, in0=stk_rs, in1=mhalf[:], op=ALU.pow),
              reads=[B_rs, B_const], writes=[B_rs])

    def phase_mod():
        ps_ = ExitStack()
        cT = sb(ps_, "cT_s", [128, 8], F32)
        cact = sb(ps_, "cact", [128, 8], F32)
        crep = sb(ps_, "crep", [128, 8, 128], F32)
        ones1 = sb(ps_, "ones1", [1, 128], F32)
        mb = sb(ps_, "mb", [1, 6 * D], F32)
        mw = [sb(ps_, "mw%d" % i, [128, 3072], F32) for i in range(2)]
        mwb = [Buf(), Buf()]
        mh = sb(ps_, "mh", [128, 3072], F32)
        dtmp = sb(ps_, "dtmp", [128, 24, 128], F32)
        B_c, B_mb, B_mh, B_dt = Buf(), Buf(), Buf(), Buf()
        kb.dma("sp", cT[:], d_cT[:, :], writes=[B_c])
        kb.dma("sp", gmixT[:], d_gmix[:, :], writes=[B_modT])
        kb.dma("sp", gffnT[:], d_gffn[:, :], writes=[B_modT])
        kb.op("act", lambda e: e.activation(out=cact[:], in_=cT[:], func=AF.Silu), reads=[B_c], writes=[B_c])
        kb.op("pool", lambda e: e.memset(ones1[:], 1.0), writes=[B_c])
        for kc in range(8):
            kb.op("dve", lambda e, kc=kc: e.tensor_copy(out=crep[:, kc, :],
                                                        in_=cact[:, kc:kc + 1].to_broadcast([128, 128])),
                  reads=[B_c], writes=[B_c])
        li = 0
        for l in range(4):
            kb.dma("sp", mb[:], d_modb[l:l + 1, :], writes=[B_mb])
            for half in range(2):
                acc = [(banks[b_], bankb[b_]) for b_ in range(6)]
                for kc in range(8):
                    t_, tb = mw[li % 2], mwb[li % 2]
                    li += 1
                    kb.dma("sp", t_[:], d_modw[l, kc * 128:(kc + 1) * 128, half * 3072:(half + 1) * 3072],
                           writes=[tb])
                    for blk in range(6):
                        pt, pb = acc[blk]
                        if kc == 0:
                            kb.op("pe", lambda e, pt=pt, blk=blk, half=half: e.matmul(
                                pt[:], lhsT=ones1[0:1, :], rhs=mb[0:1, half * 3072 + blk * 512: half * 3072 + (blk + 1) * 512],
                                start=True, stop=False), reads=[B_c, B_mb], writes=[pb])
                        kb.op("pe", lambda e, pt=pt, blk=blk, kc=kc, t_=t_: e.matmul(
                            pt[:], lhsT=crep[:, kc, :], rhs=t_[:, blk * 512:(blk + 1) * 512],
                            start=False, stop=(kc == 7)), reads=[B_c, tb], writes=[pb])
                for blk in range(6):
                    pt, pb = acc[blk]
                    kb.op("dve" if blk % 2 == 0 else "act",
                          (lambda e, pt=pt, blk=blk: e.tensor_copy(out=mh[:, blk * 512:(blk + 1) * 512], in_=pt[:]))
                          if blk % 2 == 0 else
                          (lambda e, pt=pt, blk=blk: e.activation(out=mh[:, blk * 512:(blk + 1) * 512], in_=pt[:],
                                                                  func=AF.Identity)),
                          reads=[pb], writes=[B_mh])
                kb.dma("sp", dgs[l * 2 + half, :, :], mh[:, 2048:3072], reads=[B_mh], writes=[B_gs[l * 2 + half]])
                kb.op("dve", lambda e: e.tensor_tensor(
                    out=dtmp[:], in0=mh[:].rearrange("p (c j) -> p c j", j=128),
                    in1=ident_f[:].unsqueeze(1).to_broadcast([128, 24, 128]), op=ALU.mult),
                    reads=[B_mh, B_const], writes=[B_dt])
                kb.op("dve", lambda e, l=l, half=half: e.tensor_reduce(
                    out=modT[:, l, half * 24:(half + 1) * 24], in_=dtmp[:], axis=AX.X, op=ALU.add),
                    reads=[B_dt], writes=[B_modT])
            kb.op("dve", lambda e, l=l: e.scalar_tensor_tensor(
                out=amix[:, l, :], in0=modT[:, l, 8:16], scalar=1.0, in1=gmixT[:, l * 8:(l + 1) * 8],
                op0=ALU.add, op1=ALU.mult), reads=[B_modT], writes=[B_modT])
            kb.op("dve", lambda e, l=l: e.scalar_tensor_tensor(
                out=affn[:, l, :], in0=modT[:, l, 32:40], scalar=1.0, in1=gffnT[:, l * 8:(l + 1) * 8],
                op0=ALU.add, op1=ALU.mult), reads=[B_modT], writes=[B_modT])
        kb.barrier()
        ps_.close()

    B_gs = [Buf() for _ in range(8)]
    B_xs = [Buf() for _ in range(NT)]

    def load_x(src, i, xt, xb):
        kb.dma("sp", xt[:], src[i * 128:(i + 1) * 128, :], reads=[B_xs[i]] if src is dxs else [], writes=[xb])

    def norm_tile(xt, xb, junk, B_junk, ss, rs, B_st, xsb, B_xsb):
        kb.op("act", lambda e: e.activation(out=junk, in_=xt[:], func=AF.Square, accum_out=ss),
              reads=[xb], writes=[B_junk, B_st])
        rstd_from_ss(rs, ss, B_st, float(D), B_st)
        kb.op("dve", lambda e: e.tensor_scalar(out=xsb, in0=xt[:], scalar1=rs, scalar2=32.0, op0=ALU.mult,
                                               op1=ALU.mult), reads=[xb, B_st], writes=[B_xsb])

    def make_hT(xsb_t, B_xsb, hT_dst, B_hT, A, S):
        for kc in range(8):
            kb.op("pe", lambda e, kc=kc: e.transpose(out=TB[:, kc * 128:(kc + 1) * 128],
                                                     in_=xsb_t[:, kc * 128:(kc + 1) * 128], identity=ident_b[:]),
                  reads=[B_xsb, B_const], writes=[TBb])
        for kc in range(8):
            kb.op("act", lambda e, kc=kc: e.activation(out=hT_dst(kc), in_=TB[:, kc * 128:(kc + 1) * 128],
                                                       func=AF.Identity, scale=A[:, kc:kc + 1], bias=S[:, kc:kc + 1]),
                  reads=[TBb, B_modT], writes=[B_hT])

    def residual_store(ypairs, xt, xb, gb, B_gb, tmps, dst, dstb):
        for dh, (pt, pb) in enumerate(ypairs):
            tmp, tb = tmps[dh]
            kb.op("dve", lambda e, pt=pt, dh=dh, tmp=tmp: e.tensor_tensor(
                out=tmp[:], in0=pt[:], in1=gb[:, dh * 512:(dh + 1) * 512], op=ALU.mult),
                reads=[pb, B_gb], writes=[tb])
            kb.op("pool", lambda e, dh=dh, tmp=tmp: e.tensor_tensor(
                out=xt[:, dh * 512:(dh + 1) * 512], in0=xt[:, dh * 512:(dh + 1) * 512], in1=tmp[:], op=ALU.add),
                reads=[tb, xb], writes=[xb])
        kb.dma("sp", dst, xt[:], reads=[xb], writes=[dstb])

    def phase_ffn(l, src):
        ps_ = ExitStack()
        wg = sb(ps_, "wg", [128, 8, DFF], BF16)
        wu = sb(ps_, "wu", [128, 8, DFF], BF16)
        wd = sb(ps_, "wd", [128, NFC, D], BF16)
        gb = sb(ps_, "gb", [128, D], F32)
        xts = [sb(ps_, "xt%d" % i, [128, D], F32) for i in range(4)]
        xbs = [Buf() for _ in range(4)]
        junk = sb(ps_, "junk", [128, D], BF16)
        st = sb(ps_, "st", [128, 4], F32)
        xsb = [sb(ps_, "xsb%d" % i, [128, D], BF16) for i in range(2)]
        hT = [sb(ps_, "hT%d" % i, [128, 8, 256], BF16) for i in range(2)]
        actT = sb(ps_, "actT", [128, NFC, 256], BF16)
        sg = [sb(ps_, "sg%d" % i, [128, 256], F32) for i in range(2)]
        tmps = [(sb(ps_, "tmp%d" % i, [128, 512], F32), Buf()) for i in range(2)]
        B_w, B_gb, B_junk = Buf(), Buf(), Buf()
        B_st = [Buf(), Buf()]
        B_xsb = [Buf(), Buf()]
        B_hT = [Buf(), Buf()]
        B_sg = [Buf(), Buf()]
        B_act = [Buf() for _ in range(NFC)]
        for kc in range(8):
            kb.dma("pool", wg[:, kc, :], d_fwg[l, kc * 128:(kc + 1) * 128, :], writes=[B_w])
            kb.dma("pool", wu[:, kc, :], d_fwu[l, kc * 128:(kc + 1) * 128, :], writes=[B_w])
        for fc in range(NFC):
            kb.dma("pool", wd[:, fc, :], d_fwd[l, fc * 128:(fc + 1) * 128, :], writes=[B_w])
        kb.dma("sp", gb[:], dgs[l * 2 + 1, :, :], reads=[B_gs[l * 2 + 1]], writes=[B_gb])
        A = affn[:, l, :]
        S = modT[:, l, 24:32]
        NG = NT // 2

        def prep_a(g):
            for tl in range(2):
                i = g * 2 + tl
                load_x(src, i, xts[i % 4], xbs[i % 4])

        def prep_b(g):
            for tl in range(2):
                i = g * 2 + tl
                k = i % 2
                norm_tile(xts[i % 4], xbs[i % 4], junk[:], B_junk, st[:, 2 * k:2 * k + 1], st[:, 2 * k + 1:2 * k + 2],
                          B_st[k], xsb[k][:], B_xsb[k])
                make_hT(xsb[k], B_xsb[k], lambda kc, tl=tl, g=g: hT[g % 2][:, kc, tl * 128:(tl + 1) * 128],
                        B_hT[g % 2], A, S)

        prep_a(0)
        prep_b(0)
        for g in range(NG):
            h = hT[g % 2]
            hb = B_hT[g % 2]
            if g + 1 < NG:
                prep_a(g + 1)
            for fc in range(NFC):
                pt, pb = pbank()
                for slot, w in ((0, wg), (1, wu)):
                    for kc in range(8):
                        kb.op("pe", lambda e, pt=pt, slot=slot, w=w, kc=kc, fc=fc: e.matmul(
                            pt[:, slot * 256:(slot + 1) * 256], lhsT=w[:, kc, fc * 128:(fc + 1) * 128],
                            rhs=h[:, kc, :], start=(kc == 0), stop=(kc == 7)), reads=[B_w, hb], writes=[pb])
                s_ = sg[fc % 2]
                sbf = B_sg[fc % 2]
                kb.op("act", lambda e, pt=pt, s_=s_: e.activation(out=s_[:], in_=pt[:, 0:256], func=AF.Silu),
                      reads=[pb], writes=[sbf])
                kb.op("dve", lambda e, pt=pt, s_=s_, fc=fc: e.tensor_tensor(
                    out=actT[:, fc, :], in0=pt[:, 256:512], in1=s_[:], op=ALU.mult),
                    reads=[pb, sbf], writes=[B_act[fc]])
                if fc == 6 and g + 1 < NG:
                    prep_b(g + 1)
            for tl in range(2):
                i = g * 2 + tl
                ys = []
                for dh in range(2):
                    pt, pb = pbank()
                    for fc in range(NFC):
                        kb.op("pe", lambda e, pt=pt, fc=fc, tl=tl, dh=dh: e.matmul(
                            pt[:], lhsT=actT[:, fc, tl * 128:(tl + 1) * 128], rhs=wd[:, fc, dh * 512:(dh + 1) * 512],
                            start=(fc == 0), stop=(fc == NFC - 1)), reads=[B_w, B_act[fc]], writes=[pb])
                    ys.append((pt, pb))
                residual_store(ys, xts[i % 4], xbs[i % 4], gb, B_gb, tmps, dxs[i * 128:(i + 1) * 128, :], B_xs[i])
        kb.barrier()
        ps_.close()

    def phase_final(src):
        ps_ = ExitStack()
        fg = sb(ps_, "fg", [128, D], F32)
        xts = [sb(ps_, "fxt%d" % i, [128, D], F32) for i in range(3)]
        xbs = [Buf() for _ in range(3)]
        ots = [sb(ps_, "fot%d" % i, [128, D], F32) for i in range(2)]
        obs = [Buf() for _ in range(2)]
        junk = sb(ps_, "fjunk", [128, D], BF16)
        st = sb(ps_, "fst", [128, 4], F32)
        B_st = [Buf(), Buf()]
        B_fg, B_junk, B_o = Buf(), Buf(), Buf()
        kb.dma("sp", fg[:], d_fg[0:1, :].to_broadcast([128, D]), writes=[B_fg])
        kb.op("dve", lambda e: e.tensor_scalar(out=fg[:], in0=fg[:], scalar1=32.0, scalar2=None, op0=ALU.mult),
              reads=[B_fg], writes=[B_fg])
        for i in range(NT):
            k = i % 2
            xt, xb = xts[i % 3], xbs[i % 3]
            load_x(src, i, xt, xb)
            ss, rs = st[:, 2 * k:2 * k + 1], st[:, 2 * k + 1:2 * k + 2]
            kb.op("act", lambda e, xt=xt, ss=ss: e.activation(out=junk[:], in_=xt[:], func=AF.Square, accum_out=ss),
                  reads=[xb], writes=[B_junk, B_st[k]])
            rstd_from_ss(rs, ss, B_st[k], float(D), B_st[k])
            kb.op("dve", lambda e, xt=xt, rs=rs, k=k: e.scalar_tensor_tensor(
                out=ots[k][:], in0=xt[:], scalar=rs, in1=fg[:], op0=ALU.mult, op1=ALU.mult),
                reads=[xb, B_st[k], B_fg], writes=[obs[k]])
            kb.dma("sp", dout[i * 128:(i + 1) * 128, :], ots[k][:], reads=[obs[k]], writes=[B_o])
        kb.barrier()
        ps_.close()

    def phase_gmlp(l, j, src):
        ps_ = ExitStack()
        win = sb(ps_, "bwin", [128, 8, 4096], BF16)
        wout = sb(ps_, "bwout", [128, 16, D], BF16)
        wcf = sb(ps_, "wcf", [128, 8, 128], F32)
        wcT = sb(ps_, "wcT", [128, 8, 128], BF16)
        lng = sb(ps_, "lng", [128, 2048], F32)
        lnb = sb(ps_, "lnb", [128, 2048], F32)
        bsb = sb(ps_, "bsb", [128, 8, 128], F32)
        gb = sb(ps_, "ggb", [128, D], F32)
        xts = [sb(ps_, "gxt%d" % i, [128, D], F32) for i in range(4)]
        xbs = [Buf() for _ in range(4)]
        junk = sb(ps_, "gjunk", [128, D], BF16)
        st = sb(ps_, "gst", [128, 4], F32)
        xsb = [sb(ps_, "gxsb%d" % i, [128, D], BF16) for i in range(2)]
        hT = [sb(ps_, "ghT%d" % i, [128, 8, 256], BF16) for i in range(2)]
        uT = sb(ps_, "uT", [128, 16, 256], F32)
        vr = [sb(ps_, "vr0", [128, 2048], F32)] * 2
        vt = [sb(ps_, "vt%d" % i, [128, 2048], BF16) for i in range(2)]
        bst = sb(ps_, "bst", [128, 2, 4, 6], F32)
        lst = sb(ps_, "lst", [128, 2, 4], F32)
        ypT = sb(ps_, "ypT", [128, 16, 256], BF16)
        ytm = [sb(ps_, "ytm%d" % i, [128, 512], F32) for i in range(2)]
        tmps = [(sb(ps_, "gtmp%d" % i, [128, 512], F32), Buf()) for i in range(2)]
        B_w, B_gb, B_junk, B_k = Buf(), Buf(), Buf(), Buf()
        B_st = [Buf(), Buf()]
        B_xsb = [Buf(), Buf()]
        B_hT = [Buf(), Buf()]
        B_u = [Buf() for _ in range(16)]
        B_vr = [Buf()] * 2
        B_vt = [Buf(), Buf()]
        B_ls = [Buf(), Buf()]
        B_yp = [[Buf() for _ in range(4)] for _ in range(2)]
        B_ytm = [Buf(), Buf()]
        for kc in range(8):
            kb.dma("pool", win[:, kc, :], d_bwin[j, kc * 128:(kc + 1) * 128, :], writes=[B_w])
        for ec in range(16):
            kb.dma("pool", wout[:, ec, :], d_bwout[j, ec * 128:(ec + 1) * 128, :], writes=[B_w])
        kb.dma("sp", wcf[:], d_bwsT[j, :, :].rearrange("s (g t) -> s g t", g=8), writes=[B_k])
        kb.dma("sp", lng[:], d_blng[j:j + 1, :].to_broadcast([128, 2048]), writes=[B_k])
        kb.dma("sp", lnb[:], d_blnb[j:j + 1, :].to_broadcast([128, 2048]), writes=[B_k])
        kb.dma("sp", bsb[:], d_bbs[j:j + 1, :].to_broadcast([128, 1024]).rearrange("p (g t) -> p g t", g=8),
               writes=[B_k])
        kb.dma("sp", gb[:], dgs[l * 2, :, :], reads=[B_gs[l * 2]], writes=[B_gb])
        kb.op("pool", lambda e: e.affine_select(out=wcf[:], in_=wcf[:], pattern=[[0, 8], [1, 128]],
                                                compare_op=ALU.is_ge, fill=0.0, base=0, channel_multiplier=-1),
              reads=[B_k], writes=[B_k])
        kb.op("dve", lambda e: e.tensor_copy(out=wcT[:], in_=wcf[:]), reads=[B_k], writes=[B_k])
        A = amix[:, l, :]
        S = modT[:, l, 0:8]
        NG = NT // 2

        def prep_a(g):
            for tl in range(2):
                i = g * 2 + tl
                load_x(src, i, xts[i % 4], xbs[i % 4])

        def prep_b(g):
            for tl in range(2):
                i = g * 2 + tl
                k = i % 2
                norm_tile(xts[i % 4], xbs[i % 4], junk[:], B_junk, st[:, 2 * k:2 * k + 1], st[:, 2 * k + 1:2 * k + 2],
                          B_st[k], xsb[k][:], B_xsb[k])
                make_hT(xsb[k], B_xsb[k], lambda kc, tl=tl, g=g: hT[g % 2][:, kc, tl * 128:(tl + 1) * 128],
                        B_hT[g % 2], A, S)

        prep_a(0)
        prep_b(0)
        for g in range(NG):
            h = hT[g % 2]
            hb = B_hT[g % 2]
            if g + 1 < NG:
                prep_a(g + 1)
            for ec in range(16):
                if ec % 2 == 0:
                    pt, pb = pbank()
                half = ec % 2
                for kc in range(8):
                    kb.op("pe", lambda e, pt=pt, half=half, kc=kc, ec=ec: e.matmul(
                        pt[:, half * 256:(half + 1) * 256], lhsT=win[:, kc, ec * 128:(ec + 1) * 128],
                        rhs=h[:, kc, :], start=(kc == 0), stop=(kc == 7)), reads=[B_w, hb], writes=[pb])
                if ec % 2 == 1:
                    kb.op("act", lambda e, pt=pt, ec=ec: e.activation(
                        out=uT[:, ec - 1:ec + 1, :].rearrange("p a t -> p (a t)"), in_=pt[:], func=AF.Gelu),
                        reads=[pb], writes=[B_u[ec - 1], B_u[ec]])
            for tl in range(2):
                for cb in range(4):
                    pt, pb = pbank()
                    for kc in range(8):
                        kb.op("pe", lambda e, pt=pt, kc=kc, cb=cb, tl=tl: e.matmul(
                            pt[:], lhsT=h[:, kc, tl * 128:(tl + 1) * 128],
                            rhs=win[:, kc, 2048 + cb * 512:2048 + (cb + 1) * 512],
                            start=(kc == 0), stop=(kc == 7)), reads=[B_w, hb], writes=[pb])
                    kb.op("act", lambda e, pt=pt, cb=cb, tl=tl: e.activation(
                        out=vr[tl][:, cb * 512:(cb + 1) * 512], in_=pt[:], func=AF.Gelu),
                        reads=[pb], writes=[B_vr[tl]])
                    kb.op("dve", lambda e, cb=cb, tl=tl: e.bn_stats(out=bst[:, tl, cb, :],
                                                                    in_=vr[tl][:, cb * 512:(cb + 1) * 512]),
                          reads=[B_vr[tl]], writes=[B_ls[tl]])
                kb.op("dve", lambda e, tl=tl: e.bn_aggr(out=lst[:, tl, 0:2],
                                                        in_=bst[:, tl, :, :].rearrange("p a b -> p (a b)")),
                      reads=[B_ls[tl]], writes=[B_ls[tl]])
                kb.op("pool", lambda e, tl=tl: e.tensor_scalar(out=lst[:, tl, 2:3], in0=lst[:, tl, 1:2], scalar1=EPS,
                                                               scalar2=None, op0=ALU.add),
                      reads=[B_ls[tl]], writes=[B_ls[tl]])
                kb.op("pool", lambda e, tl=tl: e.tensor_tensor(out=lst[:, tl, 2:3], in0=lst[:, tl, 2:3], in1=mhalf[:],
                                                               op=ALU.pow),
                      reads=[B_ls[tl], B_const], writes=[B_ls[tl]])
                kb.op("dve", lambda e, tl=tl: e.tensor_scalar(out=vr[tl][:], in0=vr[tl][:], scalar1=lst[:, tl, 0:1],
                                                              scalar2=lst[:, tl, 2:3], op0=ALU.subtract, op1=ALU.mult),
                      reads=[B_ls[tl], B_vr[tl]], writes=[B_vr[tl]])
                kb.op("pool", lambda e, tl=tl: e.tensor_tensor(out=vr[tl][:], in0=vr[tl][:], in1=lng[:], op=ALU.mult),
                      reads=[B_vr[tl], B_k], writes=[B_vr[tl]])
                kb.op("pool", lambda e, tl=tl: e.tensor_tensor(out=vt[tl][:], in0=vr[tl][:], in1=lnb[:], op=ALU.add),
                      reads=[B_vr[tl], B_k], writes=[B_vt[tl]])
            if g + 1 < NG:
                prep_b(g + 1)
            for tl in range(2):
                i = g * 2 + tl
                for q4 in range(4):
                    pt, pb = pbank()
                    for a in range(4):
                        ec = q4 * 4 + a
                        kb.op("pe", lambda e, pt=pt, a=a, ec=ec, tl=tl: e.matmul(
                            pt[:, a * 128:(a + 1) * 128], lhsT=vt[tl][:, ec * 128:(ec + 1) * 128],
                            rhs=wcT[:, ec // 2, :], start=True, stop=True), reads=[B_vt[tl], B_k], writes=[pb])
                    ym, ymb = ytm[q4 % 2], B_ytm[q4 % 2]
                    kb.op("dve", lambda e, pt=pt, q4=q4, ym=ym: e.tensor_tensor(
                        out=ym[:].rearrange("p (g r t) -> p g r t", g=2, r=2),
                        in0=pt[:].rearrange("p (g r t) -> p g r t", g=2, r=2),
                        in1=bsb[:, q4 * 2:q4 * 2 + 2, :].unsqueeze(2).to_broadcast([128, 2, 2, 128]), op=ALU.add),
                        reads=[pb, B_k], writes=[ymb])
                    kb.op("dve", lambda e, q4=q4, tl=tl, ym=ym: e.tensor_tensor(
                        out=ypT[:, q4 * 4:(q4 + 1) * 4, tl * 128:(tl + 1) * 128],
                        in0=ym[:].rearrange("p (a t) -> p a t", a=4),
                        in1=uT[:, q4 * 4:(q4 + 1) * 4, tl * 128:(tl + 1) * 128], op=ALU.mult),
                        reads=[ymb] + [B_u[q4 * 4 + a] for a in range(4)], writes=[B_yp[tl][q4]])
                ys = []
                for dh in range(2):
                    pt, pb = pbank()
                    for ec in range(16):
                        kb.op("pe", lambda e, pt=pt, ec=ec, tl=tl, dh=dh: e.matmul(
                            pt[:], lhsT=ypT[:, ec, tl * 128:(tl + 1) * 128], rhs=wout[:, ec, dh * 512:(dh + 1) * 512],
                            start=(ec == 0), stop=(ec == 15)), reads=[B_w, B_yp[tl][ec // 4]], writes=[pb])
                    ys.append((pt, pb))
                residual_store(ys, xts[i % 4], xbs[i % 4], gb, B_gb, tmps, dxs[i * 128:(i + 1) * 128, :], B_xs[i])
        kb.barrier()
        ps_.close()

    def phase_dsa(l, j, src, ntiles=NT):
        ps_ = ExitStack()
        win = sb(ps_, "awin", [128, 8, A_IN], BF16)
        wuk = sb(ps_, "awuk", [128, 8, 256], BF16)
        wuv = sb(ps_, "awuv", [128, 8, 2, 128], BF16)
        wo = sb(ps_, "awo", [128, 8, D], BF16)
        gkv = sb(ps_, "gkv", [128, 256], F32)
        gki = sb(ps_, "gki", [128, 64], F32)
        bki = sb(ps_, "bki", [128, 64], F32)
        cmask = sb(ps_, "cmask", [128, 128], F32)
        pw2 = sb(ps_, "pw2", [128, NIT + 1], F32)
        ckvT = sb(ps_, "ckvT", [128, 2, T], BF16)
        ckve = sb(ps_, "ckve", [128, NT, 258], BF16)
        kiT = [sb(ps_, "kiT%d" % i_, [128, T], BF16) for i_ in range(2)]
        xts = [sb(ps_, "axt%d" % i, [128, D], F32) for i in range(3)]
        xbs = [Buf() for _ in range(3)]
        junk = sb(ps_, "ajunk", [128, D], BF16)
        st = sb(ps_, "ast", [128, 16], F32)
        xsb = sb(ps_, "axsb", [128, D], BF16)
        hT = sb(ps_, "ahT", [128, 8, 128], BF16)
        qT = sb(ps_, "qT", [128, 8, 128], BF16)
        qiT = sb(ps_, "qiT", [128, 4, 128], BF16)
        qlT = [sb(ps_, "qlT%d" % i, [128, 2, 1024], BF16) for i in range(3)]
        ctm = sb(ps_, "ctm", [128, 256], BF16)
        ktm = sb(ps_, "ktm", [128, 64], F32)
        kd = sb(ps_, "kd", [128, 2, 128], BF16)
        wi = sb(ps_, "wi", [128, 8], F32)
        dg = sb(ps_, "dg", [128, 8, 128], BF16)
        scores = [sb(ps_, "score%d" % i_, [128, T], F32) for i_ in range(2)]
        B_scs = [Buf(), Buf()]
        NR = 4
        Rh = [sb(ps_, "Rh%d" % i, [128, 512], BF16) for i in range(NR)]
        B_Rh = [Buf() for _ in range(NR)]
        nms = [sb(ps_, "nm%d" % i_, [128, T], BF16) for i_ in range(2)]
        B_nms = [Buf(), Buf()]
        ident4 = sb(ps_, "ident4", [128, 4, 128], BF16)
        thb = sb(ps_, "thb", [128, 8 + NIT + 8], F32)
        PT = [sb(ps_, "PT%d" % i, [128, 512], BF16) for i in range(3)]
        B_PT = [Buf() for _ in range(3)]
        ol = sb(ps_, "ol", [128, 8, 256], BF16)
        olT = sb(ps_, "olT", [128, 16, 128], BF16)
        oT = sb(ps_, "oT", [128, 8, 128], BF16)
        rden = sb(ps_, "rden", [128, 8], F32)
        B_w, B_k, B_gb, B_junk, B_st, B_xsb, B_hT = Buf(), Buf(), Buf(), Buf(), Buf(), Buf(), Buf()
        B_q, B_qi, B_ct, B_kt, B_kd, B_wi, B_dg, B_sc, B_nm, B_th = (Buf() for _ in range(10))
        B_ql = [Buf(), Buf(), Buf()]
        B_ckvT = [Buf() for _ in range(NT)]
        B_ckve = [Buf() for _ in range(NT)]
        B_kiT = [Buf() for _ in range(NT)]
        B_ol, B_olT, B_oT, B_rd = Buf(), Buf(), Buf(), Buf()
        for kc in range(8):
            kb.dma("pool", win[:, kc, :], d_awin[j, kc * 128:(kc + 1) * 128, :], writes=[B_w])
        kb.dma("pool", wuk[:], d_awuk[j].rearrange("h d r -> d h r"), writes=[B_w])
        kb.dma("pool", wuv[:], d_awuv[j].rearrange("h (c p) d -> p h c d", p=128), writes=[B_w])
        kb.dma("sp", gkv[:], d_agkv[j:j + 1, :].to_broadcast([128, 256]), writes=[B_k])
        kb.op("dve", lambda e: e.tensor_scalar(out=gkv[:], in0=gkv[:], scalar1=16.0, scalar2=None, op0=ALU.mult),
              reads=[B_k], writes=[B_k])
        kb.dma("sp", gki[:], d_agki[j:j + 1, :].to_broadcast([128, 64]), writes=[B_k])
        kb.dma("sp", bki[:], d_abki[j:j + 1, :].to_broadcast([128, 64]), writes=[B_k])
        kb.dma("sp", scores[1][:, 0:D], dgs[l * 2, :, :], reads=[B_gs[l * 2]], writes=[B_scs[1]])
        for kc in range(8):
            slot = scores[0][:, (kc % 4) * D:(kc % 4 + 1) * D]
            kb.dma("sp", slot, d_awo[j, kc * 128:(kc + 1) * 128, :], writes=[B_scs[0]])
            kb.op("dve", lambda e, kc=kc, slot=slot: e.tensor_tensor(out=wo[:, kc, :], in0=slot, in1=scores[1][:, 0:D],
                                                                     op=ALU.mult),
                  reads=[B_scs[0], B_scs[1]], writes=[B_w])
        kb.op("pool", lambda e: e.memset(cmask[:], 0.0), writes=[B_k])
        kb.op("pool", lambda e: e.memset(kd[:], 0.0), writes=[B_kd])
        kb.op("pool", lambda e: e.affine_select(out=cmask[:], in_=cmask[:], pattern=[[-1, 128]], compare_op=ALU.is_ge,
                                                fill=NEG, base=0, channel_multiplier=1), reads=[B_k], writes=[B_k])
        for k in range(NIT + 1):
            kb.op("pool", lambda e, k=k: e.memset(pw2[:, k:k + 1], 2.0 ** (-(k + 1))), writes=[B_k])
        kb.op("pool", lambda e: e.memset(ckve[:], 1.0), writes=B_ckve)
        kb.op("pool", lambda e: e.tensor_copy(out=ident4[:], in_=ident_b[:].unsqueeze(1).to_broadcast([128, 4, 128])),
              reads=[B_const], writes=[B_k])
        A = amix[:, l, :]
        S = modT[:, l, 0:8]
        SC_W = float(8 ** -0.5 * 64 ** -0.5)
        m8, lo_, w_, t_, c_, g_, thr = (thb[:, 0:8], thb[:, 8:9], thb[:, 9:10], thb[:, 10:11], thb[:, 11:12],
                                        thb[:, 12:13], thb[:, 13:14])
        wk = thb[:, 14:14 + NIT + 1]

        def stage_a(i):
            xt, xb = xts[i % 3], xbs[i % 3]
            score, B_sc = scores[i % 2], B_scs[i % 2]
            load_x(src, i, xt, xb)
            norm_tile(xt, xb, junk[:], B_junk, st[:, 0:1], st[:, 1:2], B_st, xsb[:], B_xsb)
            make_hT(xsb, B_xsb, lambda kc: hT[:, kc, :], B_hT, A, S)
            for grp in range(3):
                pt, pb = pbank()
                for a in range(4):
                    ch = grp * 4 + a
                    c0 = ch * 128 if ch < 8 else 1280 + (ch - 8) * 128
                    for kc in range(8):
                        kb.op("pe", lambda e, pt=pt, a=a, c0=c0, kc=kc: e.matmul(
                            pt[:, a * 128:(a + 1) * 128], lhsT=win[:, kc, c0:c0 + 128], rhs=hT[:, kc, :],
                            start=(kc == 0), stop=(kc == 7)), reads=[B_w, B_hT], writes=[pb])
                if grp < 2:
                    kb.op("act", lambda e, pt=pt, grp=grp: e.activation(
                        out=qT[:, grp * 4:(grp + 1) * 4, :].rearrange("p a t -> p (a t)"), in_=pt[:], func=AF.Identity),
                        reads=[pb], writes=[B_q])
                else:
                    kb.op("act", lambda e, pt=pt: e.activation(
                        out=qiT[:].rearrange("p a t -> p (a t)"), in_=pt[:], func=AF.Identity),
                        reads=[pb], writes=[B_qi])
            pt, pb = pbank()
            for kc in range(8):
                kb.op("pe", lambda e, pt=pt, kc=kc: e.matmul(pt[:, 0:256], lhsT=hT[:, kc, :], rhs=win[:, kc, 1024:1280],
                                                             start=(kc == 0), stop=(kc == 7)),
                      reads=[B_w, B_hT], writes=[pb])
            for kc in range(8):
                kb.op("pe", lambda e, pt=pt, kc=kc: e.matmul(pt[:, 256:328], lhsT=hT[:, kc, :], rhs=win[:, kc, 1792:1864],
                                                             start=(kc == 0), stop=(kc == 7)),
                      reads=[B_w, B_hT], writes=[pb])
            kb.op("act", lambda e, pt=pt: e.activation(out=junk[:, 0:256], in_=pt[:, 0:256], func=AF.Square,
                                                       accum_out=st[:, 2:3]), reads=[pb], writes=[B_junk, B_ct])
            rstd_from_ss(st[:, 3:4], st[:, 2:3], B_ct, 256.0, B_ct)
            kb.op("dve", lambda e, pt=pt: e.scalar_tensor_tensor(out=ckve[:, i, 0:256], in0=pt[:, 0:256],
                                                                 scalar=st[:, 3:4], in1=gkv[:], op0=ALU.mult,
                                                                 op1=ALU.mult),
                  reads=[pb, B_ct, B_k], writes=[B_ckve[i]])
            kb.op("dve", lambda e, pt=pt: e.bn_stats(out=st[:, 4:10], in_=pt[:, 256:320]), reads=[pb], writes=[B_kt])
            kb.op("dve", lambda e: e.bn_aggr(out=st[:, 10:12], in_=st[:, 4:10]), reads=[B_kt], writes=[B_kt])
            kb.op("pool", lambda e: e.tensor_scalar(out=st[:, 12:13], in0=st[:, 11:12], scalar1=EPS, scalar2=None,
                                                    op0=ALU.add), reads=[B_kt], writes=[B_kt])
            kb.op("pool", lambda e: e.tensor_tensor(out=st[:, 12:13], in0=st[:, 12:13], in1=mhalf[:], op=ALU.pow),
                  reads=[B_kt, B_const], writes=[B_kt])
            kb.op("dve", lambda e, pt=pt: e.tensor_scalar(out=ktm[:], in0=pt[:, 256:320], scalar1=st[:, 10:11],
                                                          scalar2=st[:, 12:13], op0=ALU.subtract, op1=ALU.mult),
                  reads=[pb, B_kt], writes=[B_kt])
            kb.op("dve", lambda e: e.tensor_tensor(out=ktm[:], in0=ktm[:], in1=gki[:], op=ALU.mult),
                  reads=[B_kt, B_k], writes=[B_kt])
            for a_ in range(2):
                kb.op("dve", lambda e, a_=a_: e.tensor_tensor(out=kd[:, a_, a_ * 64:(a_ + 1) * 64], in0=ktm[:],
                                                              in1=bki[:], op=ALU.add),
                      reads=[B_kt, B_k], writes=[B_kd])
            kb.op("dve", lambda e, pt=pt: e.tensor_scalar(out=wi[:], in0=pt[:, 320:328], scalar1=SC_W, scalar2=None,
                                                          op0=ALU.mult), reads=[pb], writes=[B_wi])
            for h in range(8):
                kb.op("dve", lambda e, h=h: e.tensor_scalar(out=dg[:, h, :], in0=ident_b[:], scalar1=wi[:, h:h + 1],
                                                            scalar2=None, op0=ALU.mult),
                      reads=[B_wi, B_const], writes=[B_dg])
            for rc in range(2):
                kb.op("pe", lambda e, rc=rc: e.transpose(out=TB[:, rc * 128:(rc + 1) * 128],
                                                         in_=ckve[:, i, rc * 128:(rc + 1) * 128], identity=ident_b[:]),
                      reads=[B_ckve[i], B_const], writes=[TBb])
            for a_ in range(2):
                kb.op("pe", lambda e, a_=a_: e.transpose(out=TB[:, 256 + a_ * 128:384 + a_ * 128], in_=kd[:, a_, :],
                                                         identity=ident_b[:]), reads=[B_kd, B_const], writes=[TBb])
            kb.op("dve", lambda e: e.tensor_copy(out=ckvT[:, :, i * 128:(i + 1) * 128],
                                                 in_=TB[:, 0:256].rearrange("p (c t) -> p c t", c=2)),
                  reads=[TBb], writes=[B_ckvT[i]])
            for a_ in range(2):
                kb.op("dve", lambda e, a_=a_: e.tensor_copy(out=kiT[a_][:, i * 128:(i + 1) * 128],
                                                            in_=TB[:, 256 + a_ * 128:384 + a_ * 128]),
                      reads=[TBb], writes=[B_kiT[i]])
            ql, qlb = qlT[i % 3], B_ql[i % 3]
            for rc in range(2):
                pts = [pbank(), pbank()]
                for h in range(8):
                    pt2, pb2 = pts[h // 4]
                    kb.op("pe", lambda e, pt2=pt2, h=h, rc=rc: e.matmul(
                        pt2[:, (h % 4) * 128:(h % 4 + 1) * 128], lhsT=wuk[:, h, rc * 128:(rc + 1) * 128],
                        rhs=qT[:, h, :], start=True, stop=True), reads=[B_w, B_q], writes=[pb2])
                for hg in range(2):
                    pt2, pb2 = pts[hg]
                    kb.op("act", lambda e, pt2=pt2, hg=hg, rc=rc, ql=ql: e.activation(
                        out=ql[:, rc, hg * 512:(hg + 1) * 512], in_=pt2[:], func=AF.Identity,
                        scale=float(128 ** -0.5)), reads=[pb2], writes=[qlb])
        def idx_gen(i):
            score, B_sc = scores[i % 2], B_scs[i % 2]
            n = (i + 1) * 128
            rr = 0
            for c0 in range(0, n, 512):
                wd_ = min(512, n - c0)
                kblk = [B_kiT[b] for b in range(c0 // 128, (c0 + wd_) // 128)]
                rl = []
                pt3, pb3 = banks[7], bankb[7]

                def hsum(h, rl=rl, pt3=pt3, pb3=pb3, wd_=wd_):
                    R, Rb = rl[h]
                    kb.op("pe", lambda e: e.matmul(pt3[:, 0:wd_], lhsT=dg[:, h, :], rhs=R[:, 0:wd_],
                                                   start=(h == 0), stop=(h == 7)), reads=[B_dg, Rb], writes=[pb3])
                for h in range(8):
                    pt2, pb2 = banks[6], bankb[6]
                    p0 = (h % 2) * 64
                    kb.op("pe", lambda e, pt2=pt2, h=h, p0=p0, c0=c0, wd_=wd_: e.matmul(
                        pt2[:, 0:wd_], lhsT=qiT[:, h // 2, :], rhs=kiT[h % 2][:, c0:c0 + wd_],
                        start=True, stop=True), reads=[B_qi] + kblk, writes=[pb2])
                    R, Rb = Rh[rr % NR], B_Rh[rr % NR]
                    rr += 1
                    kb.op("act", lambda e, pt2=pt2, R=R, wd_=wd_: e.activation(out=R[:, 0:wd_], in_=pt2[:, 0:wd_],
                                                                               func=AF.Relu),
                          reads=[pb2], writes=[Rb])
                    rl.append((R, Rb))
                    if h >= 2:
                        hsum(h - 2)
                    yield
                hsum(6)
                hsum(7)
                if c0 + wd_ == n:
                    if wd_ > 128:
                        kb.op("dve", lambda e, pt3=pt3, c0=c0, wd_=wd_: e.tensor_copy(
                            out=score[:, c0:c0 + wd_ - 128], in_=pt3[:, 0:wd_ - 128]), reads=[pb3], writes=[B_sc])
                    kb.op("dve", lambda e, pt3=pt3, c0=c0, wd_=wd_: e.tensor_tensor(
                        out=score[:, n - 128:n], in0=pt3[:, wd_ - 128:wd_], in1=cmask[:], op=ALU.add),
                        reads=[pb3, B_k], writes=[B_sc])
                else:
                    kb.op("dve", lambda e, pt3=pt3, c0=c0, wd_=wd_: e.tensor_copy(
                        out=score[:, c0:c0 + wd_], in_=pt3[:, 0:wd_]), reads=[pb3], writes=[B_sc])

        def t1_gen(i):
            n = (i + 1) * 128
            nm, B_nm = nms[i % 2], B_nms[i % 2]
            score, B_sc = scores[i % 2], B_scs[i % 2]
            if i < 2:
                kb.op("dve", lambda e: e.memset(thr, -1.0e29), writes=[B_th])
            else:
                kb.op("dve", lambda e: e.max(out=m8, in_=score[:, 0:n]), reads=[B_sc], writes=[B_th])
                yield
                kb.op("dve", lambda e: e.tensor_reduce(out=lo_, in_=score[:, 0:n - 128], axis=AX.X, op=ALU.min),
                      reads=[B_sc], writes=[B_th])
                kb.op("dve", lambda e: e.tensor_tensor(out=w_, in0=thb[:, 7:8], in1=lo_, op=ALU.subtract),
                      reads=[B_th], writes=[B_th])
                kb.op("dve", lambda e: e.tensor_scalar(out=wk, in0=pw2[:], scalar1=w_, scalar2=None, op0=ALU.mult),
                      reads=[B_th, B_k], writes=[B_th])
                kb.op("dve", lambda e: e.tensor_tensor(out=t_, in0=lo_, in1=thb[:, 14:15], op=ALU.add),
                      reads=[B_th], writes=[B_th])
                yield
                for k in range(NIT):
                    kb.op("dve", lambda e: e.tensor_scalar(out=nm[:, 0:n], in0=score[:, 0:n], scalar1=t_, scalar2=0.0,
                                                           op0=ALU.is_ge, op1=ALU.add, accum_out=c_),
                          reads=[B_sc, B_th], writes=[B_nm, B_th])
                    kb.op("dve", lambda e: e.tensor_scalar(out=g_, in0=c_, scalar1=float(TOPK) - 0.5, scalar2=0.5,
                                                           op0=ALU.is_ge, op1=ALU.subtract), reads=[B_th], writes=[B_th])
                    kb.op("dve", lambda e, k=k: e.scalar_tensor_tensor(out=t_, in0=g_, scalar=thb[:, 14 + k:15 + k],
                                                                       in1=t_, op0=ALU.mult, op1=ALU.add),
                          reads=[B_th], writes=[B_th])
                    yield
                kb.op("dve", lambda e: e.tensor_tensor(out=thr, in0=t_, in1=thb[:, 14 + NIT:15 + NIT], op=ALU.subtract),
                      reads=[B_th], writes=[B_th])
            kb.op("dve", lambda e: e.tensor_scalar(out=nm[:, 0:n], in0=score[:, 0:n], scalar1=thr, scalar2=-30000.0,
                                                   op0=ALU.is_lt, op1=ALU.mult), reads=[B_sc, B_th], writes=[B_nm])
            yield

        def stage_b(i, tick=None, drain=None):
            xt, xb = xts[i % 3], xbs[i % 3]
            ql, qlb = qlT[i % 3], B_ql[i % 3]
            nm_i, B_nm_i = nms[i % 2], B_nms[i % 2]
            pi = 0
            for hg in range(2):
                accs = [(banks[b_], bankb[b_]) for b_ in range(4)]
                def qk(sbk, hg=hg):
                    pt, pb = pbank2()
                    for rc in range(2):
                        kb.op("pe", lambda e, pt=pt, rc=rc: e.matmul(
                            pt[:], lhsT=ckvT[:, rc, sbk * 128:(sbk + 1) * 128], rhs=ql[:, rc, hg * 512:(hg + 1) * 512],
                            start=(rc == 0), stop=False), reads=[B_ckvT[sbk], qlb], writes=[pb])
                    kb.op("pe", lambda e, pt=pt: e.matmul(
                        pt[:], lhsT=nm_i[:, sbk * 128:(sbk + 1) * 128], rhs=ident4[:].rearrange("p a t -> p (a t)"),
                        start=False, stop=True), reads=[B_nm_i, B_k], writes=[pb])
                    return pt, pb

                cur = qk(0)
                for sbk in range(i + 1):
                    pt, pb = cur
                    P_, Pb = PT[pi % 3], B_PT[pi % 3]
                    pi += 1
                    kb.op("act", lambda e, pt=pt, P_=P_: e.activation(out=P_[:], in_=pt[:], func=AF.Exp),
                          reads=[pb], writes=[Pb])
                    if sbk + 1 <= i:
                        cur = qk(sbk + 1)
                    if tick is not None:
                        tick()
                    for a in range(4):
                        at, ab = accs[a]
                        kb.op("pe", lambda e, at=at, a=a, P_=P_, sbk=sbk: e.matmul(
                            at[:, 0:257], lhsT=P_[:, a * 128:(a + 1) * 128], rhs=ckve[:, sbk, 0:257],
                            start=(sbk == 0), stop=(sbk == i)), reads=[Pb, B_ckve[sbk]], writes=[ab])
                for a in range(4):
                    h = hg * 4 + a
                    at, ab = accs[a]
                    kb.op("dve", lambda e, at=at, h=h: e.reciprocal(out=rden[:, h:h + 1], in_=at[:, 256:257]),
                          reads=[ab], writes=[B_rd])
                    kb.op("dve", lambda e, at=at, h=h: e.tensor_scalar(out=ol[:, h, :], in0=at[:, 0:256],
                                                                       scalar1=rden[:, h:h + 1], scalar2=None,
                                                                       op0=ALU.mult), reads=[ab, B_rd], writes=[B_ol])
            if drain is not None:
                drain()
            for half in range(2):
                for a in range(8):
                    blk = half * 8 + a
                    h, rc = blk // 2, blk % 2
                    kb.op("pe", lambda e, a=a, h=h, rc=rc: e.transpose(out=TB[:, a * 128:(a + 1) * 128],
                                                                       in_=ol[:, h, rc * 128:(rc + 1) * 128],
                                                                       identity=ident_b[:]),
                          reads=[B_ol, B_const], writes=[TBb])
                kb.op("dve", lambda e, half=half: e.tensor_copy(
                    out=olT[:, half * 8:(half + 1) * 8, :], in_=TB[:].rearrange("p (a t) -> p a t", a=8)),
                    reads=[TBb], writes=[B_olT])
            for hg in range(2):
                pt, pb = pbank()
                for a in range(4):
                    h = hg * 4 + a
                    for rc in range(2):
                        kb.op("pe", lambda e, pt=pt, a=a, h=h, rc=rc: e.matmul(
                            pt[:, a * 128:(a + 1) * 128], lhsT=wuv[:, h, rc, :], rhs=olT[:, h * 2 + rc, :],
                            start=(rc == 0), stop=(rc == 1)), reads=[B_w, B_olT], writes=[pb])
                kb.op("act", lambda e, pt=pt, hg=hg: e.activation(
                    out=oT[:, hg * 4:(hg + 1) * 4, :].rearrange("p a t -> p (a t)"), in_=pt[:], func=AF.Identity),
                    reads=[pb], writes=[B_oT])
            ys = []
            for dh in range(2):
                pt, pb = pbank()
                for h in range(8):
                    kb.op("pe", lambda e, pt=pt, h=h, dh=dh: e.matmul(
                        pt[:], lhsT=oT[:, h, :], rhs=wo[:, h, dh * 512:(dh + 1) * 512],
                        start=(h == 0), stop=(h == 7)), reads=[B_w, B_oT], writes=[pb])
                ys.append((pt, pb))
            for dh, (pt, pb) in enumerate(ys):
                kb.op("dve", lambda e, pt=pt, dh=dh: e.tensor_tensor(
                    out=xt[:, dh * 512:(dh + 1) * 512], in0=pt[:], in1=xt[:, dh * 512:(dh + 1) * 512], op=ALU.add),
                    reads=[pb, xb], writes=[xb])
            kb.dma("sp", dxs[i * 128:(i + 1) * 128, :], xt[:], reads=[xb], writes=[B_xs[i]])

        def run_all(g):
            for _ in g:
                pass

        stage_a(0)
        run_all(idx_gen(0))
        for i in range(ntiles):
            if i + 1 < ntiles:
                stage_a(i + 1)
            gi = idx_gen(i + 1) if i + 1 < ntiles else iter(())
            gt = t1_gen(i)
            nloop = max(1, 2 * i)
            t_every = max(1, nloop // 16)
            cnt = [0]

            def tick(gi=gi, gt=gt, cnt=cnt, t_every=t_every):
                next(gi, None)
                cnt[0] += 1
                if cnt[0] % t_every == 0:
                    next(gt, None)

            def drain(gi=gi, gt=gt):
                run_all(gi)
                run_all(gt)
            if i >= 1:
                stage_b(i - 1, tick, drain)
            else:
                drain()
        stage_b(ntiles - 1)
        kb.barrier()
        ps_.close()

    src = dx
    for ph in phases:
        if ph[0] == "mod":
            phase_mod()
        elif ph[0] == "dsa":
            phase_dsa(ph[1], ph[1] // 2, src, *(ph[2:]))
            src = dxs
        elif ph[0] == "gmlp":
            phase_gmlp(ph[1], ph[1] // 2, src)
            src = dxs
        elif ph[0] == "ffn":
            phase_ffn(ph[1], src)
            src = dxs
        elif ph[0] == "final":
            phase_final(src)
        elif ph[0] == "dump":
            B_o = Buf()
            for i in range(NT):
                kb.dma("sp", dout[i * 128:(i + 1) * 128, :], dxs[i * 128:(i + 1) * 128, :], reads=[B_xs[i]], writes=[B_o])
        elif ph[0] == "dbgmod":
            ddbg = dram("dbg", [128, 4 * 48 + 64], kind="ExternalOutput")
            kb.dma("sp", ddbg[:, 0:192], modT[:].rearrange("p l c -> p (l c)"), reads=[B_modT])
            kb.dma("sp", ddbg[:, 192:224], amix[:].rearrange("p l c -> p (l c)"), reads=[B_modT])
            kb.dma("sp", ddbg[:, 224:256], affn[:].rearrange("p l c -> p (l c)"), reads=[B_modT])
    kb.barrier()
    es.close()
    return nc


FULL = [("mod",)]
for _l in range(4):
    FULL.append(("dsa" if _l % 2 == 0 else "gmlp", _l))
    FULL.append(("ffn", _l))
FULL.append(("final",))


def _core_inputs(inp, b):
    f = np.float32
    c = np.ascontiguousarray
    m = {
        "x": c(inp["x"][b], dtype=f),
        "cT": c(np.asarray(inp["c"][b], dtype=f).reshape(8, 128).T),
        "mod_w": c(inp["mod_w"], dtype=f),
        "mod_b": c(inp["mod_b"], dtype=f),
        "gmixT": c(np.asarray(inp["norm_mix_g"], dtype=f).reshape(4, 8, 128).transpose(2, 0, 1).reshape(128, 32)),
        "gffnT": c(np.asarray(inp["norm_ffn_g"], dtype=f).reshape(4, 8, 128).transpose(2, 0, 1).reshape(128, 32)),
        "final_g": c(np.asarray(inp["final_g"], dtype=f).reshape(1, D)),
        "a_w_in": c(inp["a_w_in"], dtype=f),
        "a_g_kv": c(inp["a_g_kv"], dtype=f),
        "a_g_kidx": c(inp["a_g_kidx"], dtype=f),
        "a_b_kidx": c(inp["a_b_kidx"], dtype=f),
        "a_w_uk": c(inp["a_w_uk"], dtype=f),
        "a_w_uv": c(inp["a_w_uv"], dtype=f),
        "a_w_o": c(inp["a_w_o"], dtype=f),
        "b_w_in": c(inp["b_w_in"], dtype=f),
        "b_ln_g": c(inp["b_ln_g"], dtype=f),
        "b_ln_b": c(inp["b_ln_b"], dtype=f),
        "b_w_sT": c(np.asarray(inp["b_w_s"], dtype=f).transpose(0, 3, 1, 2).reshape(2, 128, 1024)),
        "b_b_s": c(np.asarray(inp["b_b_s"], dtype=f).reshape(2, 1024)),
        "b_w_out": c(inp["b_w_out"], dtype=f),
        "ffn_w_gate": c(inp["ffn_w_gate"], dtype=f),
        "ffn_w_up": c(inp["ffn_w_up"], dtype=f),
        "ffn_w_down": c(inp["ffn_w_down"], dtype=f),
    }
    return m


def kernel(**inputs):
    inp = {k: np.asarray(v) for k, v in inputs.items()}
    nc = _build(FULL)
    in_maps = [_core_inputs(inp, b) for b in range(8)]
    res = run_bass_kernel_spmd(nc, in_maps, core_ids=list(range(8)))
    out = np.stack([np.asarray(r["out"], dtype=np.float32) for r in res.results], axis=0)
    return out
```

```python
import numpy as np
from contextlib import ExitStack
import concourse.bass as bass
import concourse.mybir as mybir
from concourse.bass_utils import run_bass_kernel_spmd

F32 = mybir.dt.float32
BF16 = mybir.dt.bfloat16
ALU = mybir.AluOpType
AF = mybir.ActivationFunctionType
AX = mybir.AxisListType

T = 4096
D = 1024
NT = 32
DFF = 2816
NFC = 22
A_IN = 1864
TOPK = 256
NIT = 13
EPS = 1e-6
NEG = -1.0e30


class Buf:
    __slots__ = ("w", "r")

    def __init__(self):
        self.w = None
        self.r = {}


class KB:
    def __init__(self, nc, es):
        self.nc = nc
        self.es = es
        self.eng = dict(pe=nc.tensor, dve=nc.vector, act=nc.scalar, pool=nc.gpsimd, sp=nc.sync)
        self.sems = {}
        self.cnt = {}
        self.seen = {e: {} for e in self.eng}
        for e in self.eng:
            self.sems[e] = es.enter_context(nc.semaphore("s_" + e))
            self.cnt[e] = 0
        self.dq = {}
        self.dqi = {}
        for q, n in (("sp", 20), ("pool", 10), ("bg", 24)):
            self.dq[q] = []
            self.dqi[q] = 0
            for i in range(n):
                k = "d_%s%d" % (q, i)
                self.sems[k] = es.enter_context(nc.semaphore(k))
                self.cnt[k] = 0
                self.dq[q].append(k)
        self.nbank = 0

    def _wait(self, e, key, val):
        if val <= 0 or self.seen[e].get(key, 0) >= val:
            return
        self.eng[e].wait_ge(self.sems[key], val)
        self.seen[e][key] = val

    def _deps(self, e, reads, writes, strict=False):
        for b in reads:
            if b.w is not None:
                k, v = b.w
                if strict or k != e or e != "pe":
                    self._wait(e, k, v)
        for b in writes:
            if b.w is not None:
                k, v = b.w
                if strict or k != e:
                    self._wait(e, k, v)
            for k, v in b.r.items():
                if strict or k != e:
                    self._wait(e, k, v)

    def op(self, e, fn, reads=(), writes=()):
        self._deps(e, reads, writes)
        inst = fn(self.eng[e])
        self.cnt[e] += 1
        inst.then_inc(self.sems[e], 1)
        v = self.cnt[e]
        for b in reads:
            b.r[e] = v
        for b in writes:
            b.w = (e, v)
            b.r = {}

    def dma(self, q, out, in_, reads=(), writes=(), sems=None):
        pool = self.dq[sems or q]
        key = pool[self.dqi[sems or q] % len(pool)]
        self.dqi[sems or q] += 1
        self._wait(q, key, self.cnt[key])
        self._deps(q, reads, writes, strict=True)
        inst = self.eng[q].dma_start(out=out, in_=in_)
        self.cnt[key] += 16
        inst.then_inc(self.sems[key], 16)
        v = self.cnt[key]
        for b in reads:
            b.r[key] = v
        for b in writes:
            b.w = (key, v)
            b.r = {}

    def barrier(self):
        keys = list(self.sems.keys())
        for e in self.eng:
            for k in keys:
                if k != e:
                    self._wait(e, k, self.cnt[k])


def _build(phases):
    nc = bass.Bass("TRN2", target_bir_lowering=False)
    es = ExitStack()
    kb = KB(nc, es)

    def dram(name, shape, kind="ExternalInput", dt=F32):
        return nc.dram_tensor(name, list(shape), dt, kind=kind).ap()

    dx = dram("x", [T, D])
    dout = dram("out", [T, D], kind="ExternalOutput")
    dxs = dram("xs", [T, D], kind="Internal")
    dgs = dram("gsc", [8, 128, D], kind="Internal")
    d_cT = dram("cT", [128, 8])
    d_modw = dram("mod_w", [4, D, 6 * D])
    d_modb = dram("mod_b", [4, 6 * D])
    d_gmix = dram("gmixT", [128, 32])
    d_gffn = dram("gffnT", [128, 32])
    d_fg = dram("final_g", [1, D])
    d_awin = dram("a_w_in", [2, D, A_IN])
    d_agkv = dram("a_g_kv", [2, 256])
    d_agki = dram("a_g_kidx", [2, 64])
    d_abki = dram("a_b_kidx", [2, 64])
    d_awuk = dram("a_w_uk", [2, 8, 128, 256])
    d_awuv = dram("a_w_uv", [2, 8, 256, 128])
    d_awo = dram("a_w_o", [2, D, D])
    d_bwin = dram("b_w_in", [2, D, 4096])
    d_blng = dram("b_ln_g", [2, 2048])
    d_blnb = dram("b_ln_b", [2, 2048])
    d_bwsT = dram("b_w_sT", [2, 128, 8 * 128])
    d_bbs = dram("b_b_s", [2, 8 * 128])
    d_bwout = dram("b_w_out", [2, 2048, D])
    d_fwg = dram("ffn_w_gate", [4, D, DFF])
    d_fwu = dram("ffn_w_up", [4, D, DFF])
    d_fwd = dram("ffn_w_down", [4, DFF, D])

    uid = [0]

    w16 = {}
    B_w16 = {}

    def bg_cast(name, src_ap, shape):
        t = dram("w16_" + name, shape, kind="Internal", dt=BF16)
        w16[name] = t
        B_w16[name] = Buf()
        kb.dma("pool", t, src_ap, writes=[B_w16[name]], sems="bg")

    def bg_cast_all():
        for l_ in range(4):
            bg_cast("fwg%d" % l_, d_fwg[l_], [D, DFF])
            bg_cast("fwu%d" % l_, d_fwu[l_], [D, DFF])
            bg_cast("fwd%d" % l_, d_fwd[l_], [DFF, D])
            if l_ == 0:
                bg_cast("bwin0", d_bwin[0], [D, 4096])
                bg_cast("bwout0", d_bwout[0], [2048, D])
            if l_ == 1:
                bg_cast("awin1", d_awin[1], [D, A_IN])
                bg_cast("awuk1", d_awuk[1].rearrange("h d r -> (h d) r"), [8 * 128, 256])
                bg_cast("awuv1", d_awuv[1].rearrange("h r d -> (h r) d"), [8 * 256, 128])
            if l_ == 2:
                bg_cast("bwin1", d_bwin[1], [D, 4096])
                bg_cast("bwout1", d_bwout[1], [2048, D])

    def sb(stack, name, shape, dt):
        uid[0] += 1
        return stack.enter_context(nc.sbuf_tensor("%s_%d" % (name, uid[0]), list(shape), dt))

    ident_f = sb(es, "ident_f", [128, 128], F32)
    ident_b = sb(es, "ident_b", [128, 128], BF16)
    mhalf = sb(es, "mhalf", [128, 1], F32)
    modT = sb(es, "modT", [128, 4, 48], F32)
    amix = sb(es, "amix", [128, 4, 8], F32)
    affn = sb(es, "affn", [128, 4, 8], F32)
    gmixT = sb(es, "gmixT_s", [128, 32], F32)
    gffnT = sb(es, "gffnT_s", [128, 32], F32)
    B_const = Buf()
    B_modT = Buf()
    banks = [es.enter_context(nc.psum_tensor("ps%d" % i, [128, 512], F32)) for i in range(8)]
    bankb = [Buf() for _ in range(8)]

    nb2 = [0]

    def pbank():
        i = kb.nbank % 5
        kb.nbank += 1
        return banks[i], bankb[i]

    def pbank2():
        i = 4 + nb2[0] % 2
        nb2[0] += 1
        return banks[i], bankb[i]

    TB = banks[7][:].bitcast(BF16)
    TBb = bankb[7]

    kb.op("pool", lambda e: e.memset(ident_f[:], 1.0), writes=[B_const])
    kb.op("pool", lambda e: e.memset(mhalf[:], -0.5), writes=[B_const])
    kb.op("pool", lambda e: e.affine_select(out=ident_f[:], in_=ident_f[:], pattern=[[-1, 128]],
                                            compare_op=ALU.is_equal, fill=0.0, base=0, channel_multiplier=1),
          reads=[B_const], writes=[B_const])
    kb.op("dve", lambda e: e.tensor_copy(out=ident_b[:], in_=ident_f[:]), reads=[B_const], writes=[B_const])

    def rstd_from_ss(stk_rs, ss, B_ss, n, B_rs):
        kb.op("pool", lambda e: e.tensor_scalar(out=stk_rs, in0=ss, scalar1=float(n) * EPS, scalar2=None,
                                                op0=ALU.add), reads=[B_ss], writes=[B_rs])
        kb.op("pool", lambda e: e.tensor_tensor(out=stk_rs, in0=stk_rs, in1=mhalf[:], op=ALU.pow),
              reads=[B_rs, B_const], writes=[B_rs])

    def phase_mod():
        ps_ = ExitStack()
        cT = sb(ps_, "cT_s", [128, 8], F32)
        cact = sb(ps_, "cact", [128, 8], F32)
        crep = sb(ps_, "crep", [128, 8, 128], F32)
        ones1 = sb(ps_, "ones1", [1, 128], F32)
        mb = sb(ps_, "mb", [1, 6 * D], F32)
        mw = [sb(ps_, "mw%d" % i, [128, 3072], F32) for i in range(2)]
        mwb = [Buf(), Buf()]
        mh = sb(ps_, "mh", [128, 3072], F32)
        dtmp = sb(ps_, "dtmp", [128, 24, 128], F32)
        B_c, B_mb, B_mh, B_dt = Buf(), Buf(), Buf(), Buf()
        kb.dma("sp", cT[:], d_cT[:, :], writes=[B_c])
        kb.dma("sp", gmixT[:], d_gmix[:, :], writes=[B_modT])
        kb.dma("sp", gffnT[:], d_gffn[:, :], writes=[B_modT])
        kb.op("act", lambda e: e.activation(out=cact[:], in_=cT[:], func=AF.Silu), reads=[B_c], writes=[B_c])
        kb.op("pool", lambda e: e.memset(ones1[:], 1.0), writes=[B_c])
        for kc in range(8):
            kb.op("dve", lambda e, kc=kc: e.tensor_copy(out=crep[:, kc, :],
                                                        in_=cact[:, kc:kc + 1].to_broadcast([128, 128])),
                  reads=[B_c], writes=[B_c])
        li = 0
        for l in range(4):
            kb.dma("sp", mb[:], d_modb[l:l + 1, :], writes=[B_mb])
            for half in range(2):
                acc = [(banks[b_], bankb[b_]) for b_ in range(6)]
                for kc in range(8):
                    t_, tb = mw[li % 2], mwb[li % 2]
                    li += 1
                    kb.dma("sp", t_[:], d_modw[l, kc * 128:(kc + 1) * 128, half * 3072:(half + 1) * 3072],
                           writes=[tb])
                    for blk in range(6):
                        pt, pb = acc[blk]
                        if kc == 0:
                            kb.op("pe", lambda e, pt=pt, blk=blk, half=half: e.matmul(
                                pt[:], lhsT=ones1[0:1, :], rhs=mb[0:1, half * 3072 + blk * 512: half * 3072 + (blk + 1) * 512],
                                start=True, stop=False), reads=[B_c, B_mb], writes=[pb])
                        kb.op("pe", lambda e, pt=pt, blk=blk, kc=kc, t_=t_: e.matmul(
                            pt[:], lhsT=crep[:, kc, :], rhs=t_[:, blk * 512:(blk + 1) * 512],
                            start=False, stop=(kc == 7)), reads=[B_c, tb], writes=[pb])
                for blk in range(6):
                    pt, pb = acc[blk]
                    kb.op("dve" if blk % 2 == 0 else "act",
                          (lambda e, pt=pt, blk=blk: e.tensor_copy(out=mh[:, blk * 512:(blk + 1) * 512], in_=pt[:]))
                          if blk % 2 == 0 else
                          (lambda e, pt=pt, blk=blk: e.activation(out=mh[:, blk * 512:(blk + 1) * 512], in_=pt[:],
                                                                  func=AF.Identity)),
                          reads=[pb], writes=[B_mh])
                kb.dma("sp", dgs[l * 2 + half, :, :], mh[:, 2048:3072], reads=[B_mh], writes=[B_gs[l * 2 + half]])
                kb.op("dve", lambda e: e.tensor_tensor(
                    out=dtmp[:], in0=mh[:].rearrange("p (c j) -> p c j", j=128),
                    in1=ident_f[:].unsqueeze(1).to_broadcast([128, 24, 128]), op=ALU.mult),
                    reads=[B_mh, B_const], writes=[B_dt])
                kb.op("dve", lambda e, l=l, half=half: e.tensor_reduce(
                    out=modT[:, l, half * 24:(half + 1) * 24], in_=dtmp[:], axis=AX.X, op=ALU.add),
                    reads=[B_dt], writes=[B_modT])
            kb.op("dve", lambda e, l=l: e.scalar_tensor_tensor(
                out=amix[:, l, :], in0=modT[:, l, 8:16], scalar=1.0, in1=gmixT[:, l * 8:(l + 1) * 8],
                op0=ALU.add, op1=ALU.mult), reads=[B_modT], writes=[B_modT])
            kb.op("dve", lambda e, l=l: e.scalar_tensor_tensor(
                out=affn[:, l, :], in0=modT[:, l, 32:40], scalar=1.0, in1=gffnT[:, l * 8:(l + 1) * 8],
                op0=ALU.add, op1=ALU.mult), reads=[B_modT], writes=[B_modT])
        kb.barrier()
        ps_.close()

    B_gs = [Buf() for _ in range(8)]
    B_xs = [Buf() for _ in range(NT)]

    def load_x(src, i, xt, xb):
        kb.dma("sp", xt[:], src[i * 128:(i + 1) * 128, :], reads=[B_xs[i]] if src is dxs else [], writes=[xb])

    def norm_tile(xt, xb, junk, B_junk, ss, rs, B_st, xsb, B_xsb):
        kb.op("act", lambda e: e.activation(out=junk, in_=xt[:], func=AF.Square, accum_out=ss),
              reads=[xb], writes=[B_junk, B_st])
        rstd_from_ss(rs, ss, B_st, float(D), B_st)
        kb.op("dve", lambda e: e.tensor_scalar(out=xsb, in0=xt[:], scalar1=rs, scalar2=32.0, op0=ALU.mult,
                                               op1=ALU.mult), reads=[xb, B_st], writes=[B_xsb])

    def make_hT(xsb_t, B_xsb, hT_dst, B_hT, A, S):
        for kc in range(8):
            kb.op("pe", lambda e, kc=kc: e.transpose(out=TB[:, kc * 128:(kc + 1) * 128],
                                                     in_=xsb_t[:, kc * 128:(kc + 1) * 128], identity=ident_b[:]),
                  reads=[B_xsb, B_const], writes=[TBb])
        for kc in range(8):
            kb.op("act", lambda e, kc=kc: e.activation(out=hT_dst(kc), in_=TB[:, kc * 128:(kc + 1) * 128],
                                                       func=AF.Identity, scale=A[:, kc:kc + 1], bias=S[:, kc:kc + 1]),
                  reads=[TBb, B_modT], writes=[B_hT])

    def residual_store(ypairs, xt, xb, gb, B_gb, tmps, dst, dstb):
        for dh, (pt, pb) in enumerate(ypairs):
            tmp, tb = tmps[dh]
            kb.op("dve", lambda e, pt=pt, dh=dh, tmp=tmp: e.tensor_tensor(
                out=tmp[:], in0=pt[:], in1=gb[:, dh * 512:(dh + 1) * 512], op=ALU.mult),
                reads=[pb, B_gb], writes=[tb])
            kb.op("pool", lambda e, dh=dh, tmp=tmp: e.tensor_tensor(
                out=xt[:, dh * 512:(dh + 1) * 512], in0=xt[:, dh * 512:(dh + 1) * 512], in1=tmp[:], op=ALU.add),
                reads=[tb, xb], writes=[xb])
        kb.dma("sp", dst, xt[:], reads=[xb], writes=[dstb])

    def phase_ffn(l, src):
        ps_ = ExitStack()
        wg = sb(ps_, "wg", [128, 8, DFF], BF16)
        wu = sb(ps_, "wu", [128, 8, DFF], BF16)
        wd = sb(ps_, "wd", [128, NFC, D], BF16)
        gb = sb(ps_, "gb", [128, D], F32)
        xts = [sb(ps_, "xt%d" % i, [128, D], F32) for i in range(4)]
        xbs = [Buf() for _ in range(4)]
        junk = sb(ps_, "junk", [128, D], BF16)
        st = sb(ps_, "st", [128, 4], F32)
        xsb = [sb(ps_, "xsb%d" % i, [128, D], BF16) for i in range(2)]
        hT = [sb(ps_, "hT%d" % i, [128, 8, 256], BF16) for i in range(2)]
        actT = sb(ps_, "actT", [128, NFC, 256], BF16)
        sg = [sb(ps_, "sg%d" % i, [128, 256], F32) for i in range(2)]
        tmps = [(sb(ps_, "tmp%d" % i, [128, 512], F32), Buf()) for i in range(2)]
        B_w, B_gb, B_junk = Buf(), Buf(), Buf()
        B_st = [Buf(), Buf()]
        B_xsb = [Buf(), Buf()]
        B_hT = [Buf(), Buf()]
        B_sg = [Buf(), Buf()]
        B_act = [Buf() for _ in range(NFC)]
        B_wg = [Buf() for _ in range(8)]
        B_wu = [Buf() for _ in range(8)]
        B_wd = [Buf() for _ in range(NFC)]
        for kc in range(8):
            kb.dma("sp", wg[:, kc, :], w16["fwg%d" % l][kc * 128:(kc + 1) * 128, :], reads=[B_w16["fwg%d" % l]],
                   writes=[B_wg[kc]])
        for kc in range(8):
            kb.dma("sp", wu[:, kc, :], w16["fwu%d" % l][kc * 128:(kc + 1) * 128, :], reads=[B_w16["fwu%d" % l]],
                   writes=[B_wu[kc]])
        for fc in range(NFC):
            kb.dma("sp", wd[:, fc, :], w16["fwd%d" % l][fc * 128:(fc + 1) * 128, :], reads=[B_w16["fwd%d" % l]],
                   writes=[B_wd[fc]])
        kb.dma("sp", gb[:], dgs[l * 2 + 1, :, :], reads=[B_gs[l * 2 + 1]], writes=[B_gb])
        A = affn[:, l, :]
        S = modT[:, l, 24:32]
        NG = NT // 2

        def prep_a(g):
            for tl in range(2):
                i = g * 2 + tl
                load_x(src, i, xts[i % 4], xbs[i % 4])

        def prep_b(g):
            for tl in range(2):
                i = g * 2 + tl
                k = i % 2
                norm_tile(xts[i % 4], xbs[i % 4], junk[:], B_junk, st[:, 2 * k:2 * k + 1], st[:, 2 * k + 1:2 * k + 2],
                          B_st[k], xsb[k][:], B_xsb[k])
                make_hT(xsb[k], B_xsb[k], lambda kc, tl=tl, g=g: hT[g % 2][:, kc, tl * 128:(tl + 1) * 128],
                        B_hT[g % 2], A, S)

        prep_a(0)
        prep_b(0)
        for g in range(NG):
            h = hT[g % 2]
            hb = B_hT[g % 2]
            if g + 1 < NG:
                prep_a(g + 1)
            for fc in range(NFC):
                pt, pb = pbank()
                for slot, w in ((0, wg), (1, wu)):
                    for kc in range(8):
                        kb.op("pe", lambda e, pt=pt, slot=slot, w=w, kc=kc, fc=fc: e.matmul(
                            pt[:, slot * 256:(slot + 1) * 256], lhsT=w[:, kc, fc * 128:(fc + 1) * 128],
                            rhs=h[:, kc, :], start=(kc == 0), stop=(kc == 7)),
                            reads=[(B_wg if slot == 0 else B_wu)[kc], hb], writes=[pb])
                s_ = sg[fc % 2]
                sbf = B_sg[fc % 2]
                kb.op("act", lambda e, pt=pt, s_=s_: e.activation(out=s_[:], in_=pt[:, 0:256], func=AF.Silu),
                      reads=[pb], writes=[sbf])
                kb.op("dve", lambda e, pt=pt, s_=s_, fc=fc: e.tensor_tensor(
                    out=actT[:, fc, :], in0=pt[:, 256:512], in1=s_[:], op=ALU.mult),
                    reads=[pb, sbf], writes=[B_act[fc]])
                if fc == 6 and g + 1 < NG:
                    prep_b(g + 1)
            for tl in range(2):
                i = g * 2 + tl
                ys = []
                for dh in range(2):
                    pt, pb = pbank()
                    for fc in range(NFC):
                        kb.op("pe", lambda e, pt=pt, fc=fc, tl=tl, dh=dh: e.matmul(
                            pt[:], lhsT=actT[:, fc, tl * 128:(tl + 1) * 128], rhs=wd[:, fc, dh * 512:(dh + 1) * 512],
                            start=(fc == 0), stop=(fc == NFC - 1)), reads=[B_wd[fc], B_act[fc]], writes=[pb])
                    ys.append((pt, pb))
                residual_store(ys, xts[i % 4], xbs[i % 4], gb, B_gb, tmps, dxs[i * 128:(i + 1) * 128, :], B_xs[i])
        kb.barrier()
        ps_.close()

    def phase_final(src):
        ps_ = ExitStack()
        fg = sb(ps_, "fg", [128, D], F32)
        xts = [sb(ps_, "fxt%d" % i, [128, D], F32) for i in range(3)]
        xbs = [Buf() for _ in range(3)]
        ots = [sb(ps_, "fot%d" % i, [128, D], F32) for i in range(2)]
        obs = [Buf() for _ in range(2)]
        junk = sb(ps_, "fjunk", [128, D], BF16)
        st = sb(ps_, "fst", [128, 4], F32)
        B_st = [Buf(), Buf()]
        B_fg, B_junk, B_o = Buf(), Buf(), Buf()
        kb.dma("sp", fg[:], d_fg[0:1, :].to_broadcast([128, D]), writes=[B_fg])
        kb.op("dve", lambda e: e.tensor_scalar(out=fg[:], in0=fg[:], scalar1=32.0, scalar2=None, op0=ALU.mult),
              reads=[B_fg], writes=[B_fg])
        for i in range(NT):
            k = i % 2
            xt, xb = xts[i % 3], xbs[i % 3]
            load_x(src, i, xt, xb)
            ss, rs = st[:, 2 * k:2 * k + 1], st[:, 2 * k + 1:2 * k + 2]
            kb.op("act", lambda e, xt=xt, ss=ss: e.activation(out=junk[:], in_=xt[:], func=AF.Square, accum_out=ss),
                  reads=[xb], writes=[B_junk, B_st[k]])
            rstd_from_ss(rs, ss, B_st[k], float(D), B_st[k])
            kb.op("dve", lambda e, xt=xt, rs=rs, k=k: e.scalar_tensor_tensor(
                out=ots[k][:], in0=xt[:], scalar=rs, in1=fg[:], op0=ALU.mult, op1=ALU.mult),
                reads=[xb, B_st[k], B_fg], writes=[obs[k]])
            kb.dma("sp", dout[i * 128:(i + 1) * 128, :], ots[k][:], reads=[obs[k]], writes=[B_o])
        kb.barrier()
        ps_.close()

    def phase_gmlp(l, j, src):
        ps_ = ExitStack()
        win = sb(ps_, "bwin", [128, 8, 4096], BF16)
        wout = sb(ps_, "bwout", [128, 16, D], BF16)
        wcf = sb(ps_, "wcf", [128, 8, 128], F32)
        wcT = sb(ps_, "wcT", [128, 8, 128], BF16)
        lng = sb(ps_, "lng", [128, 2048], F32)
        lnb = sb(ps_, "lnb", [128, 2048], F32)
        bsb = sb(ps_, "bsb", [128, 8, 128], F32)
        gb = sb(ps_, "ggb", [128, D], F32)
        xts = [sb(ps_, "gxt%d" % i, [128, D], F32) for i in range(4)]
        xbs = [Buf() for _ in range(4)]
        junk = sb(ps_, "gjunk", [128, D], BF16)
        st = sb(ps_, "gst", [128, 4], F32)
        xsb = [sb(ps_, "gxsb%d" % i, [128, D], BF16) for i in range(2)]
        hT = [sb(ps_, "ghT%d" % i, [128, 8, 256], BF16) for i in range(2)]
        uT = sb(ps_, "uT", [128, 16, 256], F32)
        vr = [sb(ps_, "vr0", [128, 2048], F32)] * 2
        vt = [sb(ps_, "vt%d" % i, [128, 2048], BF16) for i in range(2)]
        bst = sb(ps_, "bst", [128, 2, 4, 6], F32)
        lst = sb(ps_, "lst", [128, 2, 4], F32)
        ypT = sb(ps_, "ypT", [128, 16, 256], BF16)
        ytm = [sb(ps_, "ytm%d" % i, [128, 512], F32) for i in range(2)]
        tmps = [(sb(ps_, "gtmp%d" % i, [128, 512], F32), Buf()) for i in range(2)]
        B_w, B_gb, B_junk, B_k = Buf(), Buf(), Buf(), Buf()
        B_st = [Buf(), Buf()]
        B_xsb = [Buf(), Buf()]
        B_hT = [Buf(), Buf()]
        B_u = [Buf() for _ in range(16)]
        B_vr = [Buf()] * 2
        B_vt = [Buf(), Buf()]
        B_ls = [Buf(), Buf()]
        B_yp = [[Buf() for _ in range(4)] for _ in range(2)]
        B_ytm = [Buf(), Buf()]
        for kc in range(8):
            kb.dma("sp", win[:, kc, :], w16["bwin%d" % j][kc * 128:(kc + 1) * 128, :], reads=[B_w16["bwin%d" % j]],
                   writes=[B_w])
        for ec in range(16):
            kb.dma("sp", wout[:, ec, :], w16["bwout%d" % j][ec * 128:(ec + 1) * 128, :], reads=[B_w16["bwout%d" % j]],
                   writes=[B_w])
        kb.dma("sp", wcf[:], d_bwsT[j, :, :].rearrange("s (g t) -> s g t", g=8), writes=[B_k])
        kb.dma("sp", lng[:], d_blng[j:j + 1, :].to_broadcast([128, 2048]), writes=[B_k])
        kb.dma("sp", lnb[:], d_blnb[j:j + 1, :].to_broadcast([128, 2048]), writes=[B_k])
        kb.dma("sp", bsb[:], d_bbs[j:j + 1, :].to_broadcast([128, 1024]).rearrange("p (g t) -> p g t", g=8),
               writes=[B_k])
        kb.dma("sp", gb[:], dgs[l * 2, :, :], reads=[B_gs[l * 2]], writes=[B_gb])
        kb.op("pool", lambda e: e.affine_select(out=wcf[:], in_=wcf[:], pattern=[[0, 8], [1, 128]],
                                                compare_op=ALU.is_ge, fill=0.0, base=0, channel_multiplier=-1),
              reads=[B_k], writes=[B_k])
        kb.op("dve", lambda e: e.tensor_copy(out=wcT[:], in_=wcf[:]), reads=[B_k], writes=[B_k])
        A = amix[:, l, :]
        S = modT[:, l, 0:8]
        NG = NT // 2

        def prep_a(g):
            for tl in range(2):
                i = g * 2 + tl
                load_x(src, i, xts[i % 4], xbs[i % 4])

        def prep_b(g):
            for tl in range(2):
                i = g * 2 + tl
                k = i % 2
                norm_tile(xts[i % 4], xbs[i % 4], junk[:], B_junk, st[:, 2 * k:2 * k + 1], st[:, 2 * k + 1:2 * k + 2],
                          B_st[k], xsb[k][:], B_xsb[k])
                make_hT(xsb[k], B_xsb[k], lambda kc, tl=tl, g=g: hT[g % 2][:, kc, tl * 128:(tl + 1) * 128],
                        B_hT[g % 2], A, S)

        prep_a(0)
        prep_b(0)
        for g in range(NG):
            h = hT[g % 2]
            hb = B_hT[g % 2]
            if g + 1 < NG:
                prep_a(g + 1)
            for ec in range(16):
                if ec % 2 == 0:
                    pt, pb = pbank()
                half = ec % 2
                for kc in range(8):
                    kb.op("pe", lambda e, pt=pt, half=half, kc=kc, ec=ec: e.matmul(
                        pt[:, half * 256:(half + 1) * 256], lhsT=win[:, kc, ec * 128:(ec + 1) * 128],
                        rhs=h[:, kc, :], start=(kc == 0), stop=(kc == 7)), reads=[B_w, hb], writes=[pb])
                if ec % 2 == 1:
                    kb.op("act", lambda e, pt=pt, ec=ec: e.activation(
                        out=uT[:, ec - 1:ec + 1, :].rearrange("p a t -> p (a t)"), in_=pt[:], func=AF.Gelu),
                        reads=[pb], writes=[B_u[ec - 1], B_u[ec]])
            for tl in range(2):
                for cb in range(4):
                    pt, pb = pbank()
                    for kc in range(8):
                        kb.op("pe", lambda e, pt=pt, kc=kc, cb=cb, tl=tl: e.matmul(
                            pt[:], lhsT=h[:, kc, tl * 128:(tl + 1) * 128],
                            rhs=win[:, kc, 2048 + cb * 512:2048 + (cb + 1) * 512],
                            start=(kc == 0), stop=(kc == 7)), reads=[B_w, hb], writes=[pb])
                    kb.op("act", lambda e, pt=pt, cb=cb, tl=tl: e.activation(
                        out=vr[tl][:, cb * 512:(cb + 1) * 512], in_=pt[:], func=AF.Gelu),
                        reads=[pb], writes=[B_vr[tl]])
                    kb.op("dve", lambda e, cb=cb, tl=tl: e.bn_stats(out=bst[:, tl, cb, :],
                                                                    in_=vr[tl][:, cb * 512:(cb + 1) * 512]),
                          reads=[B_vr[tl]], writes=[B_ls[tl]])
                kb.op("dve", lambda e, tl=tl: e.bn_aggr(out=lst[:, tl, 0:2],
                                                        in_=bst[:, tl, :, :].rearrange("p a b -> p (a b)")),
                      reads=[B_ls[tl]], writes=[B_ls[tl]])
                kb.op("pool", lambda e, tl=tl: e.tensor_scalar(out=lst[:, tl, 2:3], in0=lst[:, tl, 1:2], scalar1=EPS,
                                                               scalar2=None, op0=ALU.add),
                      reads=[B_ls[tl]], writes=[B_ls[tl]])
                kb.op("pool", lambda e, tl=tl: e.tensor_tensor(out=lst[:, tl, 2:3], in0=lst[:, tl, 2:3], in1=mhalf[:],
                                                               op=ALU.pow),
                      reads=[B_ls[tl], B_const], writes=[B_ls[tl]])
                kb.op("dve", lambda e, tl=tl: e.tensor_scalar(out=vr[tl][:], in0=vr[tl][:], scalar1=lst[:, tl, 0:1],
                                                              scalar2=lst[:, tl, 2:3], op0=ALU.subtract, op1=ALU.mult),
                      reads=[B_ls[tl], B_vr[tl]], writes=[B_vr[tl]])
                kb.op("pool", lambda e, tl=tl: e.tensor_tensor(out=vr[tl][:], in0=vr[tl][:], in1=lng[:], op=ALU.mult),
                      reads=[B_vr[tl], B_k], writes=[B_vr[tl]])
                kb.op("pool", lambda e, tl=tl: e.tensor_tensor(out=vt[tl][:], in0=vr[tl][:], in1=lnb[:], op=ALU.add),
                      reads=[B_vr[tl], B_k], writes=[B_vt[tl]])
            if g + 1 < NG:
                prep_b(g + 1)
            for tl in range(2):
                i = g * 2 + tl
                for q4 in range(4):
                    pt, pb = pbank()
                    for a in range(4):
                        ec = q4 * 4 + a
                        kb.op("pe", lambda e, pt=pt, a=a, ec=ec, tl=tl: e.matmul(
                            pt[:, a * 128:(a + 1) * 128], lhsT=vt[tl][:, ec * 128:(ec + 1) * 128],
                            rhs=wcT[:, ec // 2, :], start=True, stop=True), reads=[B_vt[tl], B_k], writes=[pb])
                    ym, ymb = ytm[q4 % 2], B_ytm[q4 % 2]
                    kb.op("dve", lambda e, pt=pt, q4=q4, ym=ym: e.tensor_tensor(
                        out=ym[:].rearrange("p (g r t) -> p g r t", g=2, r=2),
                        in0=pt[:].rearrange("p (g r t) -> p g r t", g=2, r=2),
                        in1=bsb[:, q4 * 2:q4 * 2 + 2, :].unsqueeze(2).to_broadcast([128, 2, 2, 128]), op=ALU.add),
                        reads=[pb, B_k], writes=[ymb])
                    kb.op("dve", lambda e, q4=q4, tl=tl, ym=ym: e.tensor_tensor(
                        out=ypT[:, q4 * 4:(q4 + 1) * 4, tl * 128:(tl + 1) * 128],
                        in0=ym[:].rearrange("p (a t) -> p a t", a=4),
                        in1=uT[:, q4 * 4:(q4 + 1) * 4, tl * 128:(tl + 1) * 128], op=ALU.mult),
                        reads=[ymb] + [B_u[q4 * 4 + a] for a in range(4)], writes=[B_yp[tl][q4]])
                ys = []
                for dh in range(2):
                    pt, pb = pbank()
                    for ec in range(16):
                        kb.op("pe", lambda e, pt=pt, ec=ec, tl=tl, dh=dh: e.matmul(
                            pt[:], lhsT=ypT[:, ec, tl * 128:(tl + 1) * 128], rhs=wout[:, ec, dh * 512:(dh + 1) * 512],
                            start=(ec == 0), stop=(ec == 15)), reads=[B_w, B_yp[tl][ec // 4]], writes=[pb])
                    ys.append((pt, pb))
                residual_store(ys, xts[i % 4], xbs[i % 4], gb, B_gb, tmps, dxs[i * 128:(i + 1) * 128, :], B_xs[i])
        kb.barrier()
        ps_.close()

    def phase_dsa(l, j, src, ntiles=NT):
        ps_ = ExitStack()
        win = sb(ps_, "awin", [128, 8, A_IN], BF16)
        wuk = sb(ps_, "awuk", [128, 8, 256], BF16)
        wuv = sb(ps_, "awuv", [128, 8, 2, 128], BF16)
        wo = sb(ps_, "awo", [128, 8, D], BF16)
        gkv = sb(ps_, "gkv", [128, 256], F32)
        gki = sb(ps_, "gki", [128, 64], F32)
        bki = sb(ps_, "bki", [128, 64], F32)
        cmask = sb(ps_, "cmask", [128, 128], F32)
        pw2 = sb(ps_, "pw2", [128, NIT + 1], F32)
        ckvT = sb(ps_, "ckvT", [128, 2, T], BF16)
        ckve = sb(ps_, "ckve", [128, NT, 258], BF16)
        kiT = [sb(ps_, "kiT%d" % i_, [128, T], BF16) for i_ in range(2)]
        xts = [sb(ps_, "axt%d" % i, [128, D], F32) for i in range(3)]
        xbs = [Buf() for _ in range(3)]
        junk = sb(ps_, "ajunk", [128, D], BF16)
        st = sb(ps_, "ast", [128, 16], F32)
        xsb = sb(ps_, "axsb", [128, D], BF16)
        hT = sb(ps_, "ahT", [128, 8, 128], BF16)
        qT = sb(ps_, "qT", [128, 8, 128], BF16)
        qiT = sb(ps_, "qiT", [128, 4, 128], BF16)
        qlT = [sb(ps_, "qlT%d" % i, [128, 2, 1024], BF16) for i in range(3)]
        ctm = sb(ps_, "ctm", [128, 256], BF16)
        ktm = sb(ps_, "ktm", [128, 64], F32)
        kd = sb(ps_, "kd", [128, 2, 128], BF16)
        wi = sb(ps_, "wi", [128, 8], F32)
        dg = sb(ps_, "dg", [128, 8, 128], BF16)
        scores = [sb(ps_, "score%d" % i_, [128, T], F32) for i_ in range(2)]
        B_scs = [Buf(), Buf()]
        NR = 4
        Rh = [sb(ps_, "Rh%d" % i, [128, 512], BF16) for i in range(NR)]
        B_Rh = [Buf() for _ in range(NR)]
        nms = [sb(ps_, "nm%d" % i_, [128, T], BF16) for i_ in range(2)]
        B_nms = [Buf(), Buf()]
        ident4 = sb(ps_, "ident4", [128, 4, 128], BF16)
        thb = sb(ps_, "thb", [128, 8 + NIT + 8], F32)
        PT = [sb(ps_, "PT%d" % i, [128, 512], BF16) for i in range(3)]
        B_PT = [Buf() for _ in range(3)]
        ol = sb(ps_, "ol", [128, 8, 256], BF16)
        olT = sb(ps_, "olT", [128, 16, 128], BF16)
        oT = sb(ps_, "oT", [128, 8, 128], BF16)
        rden = sb(ps_, "rden", [128, 8], F32)
        B_w, B_k, B_gb, B_junk, B_st, B_xsb, B_hT = Buf(), Buf(), Buf(), Buf(), Buf(), Buf(), Buf()
        B_q, B_qi, B_ct, B_kt, B_kd, B_wi, B_dg, B_sc, B_nm, B_th = (Buf() for _ in range(10))
        B_ql = [Buf(), Buf(), Buf()]
        B_ckvT = [Buf() for _ in range(NT)]
        B_ckve = [Buf() for _ in range(NT)]
        B_kiT = [Buf() for _ in range(NT)]
        B_ol, B_olT, B_oT, B_rd = Buf(), Buf(), Buf(), Buf()
        if j == 0:
            for kc in range(8):
                kb.dma("pool", win[:, kc, :], d_awin[j, kc * 128:(kc + 1) * 128, :], writes=[B_w])
            kb.dma("pool", wuk[:], d_awuk[j].rearrange("h d r -> d h r"), writes=[B_w])
            kb.dma("pool", wuv[:], d_awuv[j].rearrange("h (c p) d -> p h c d", p=128), writes=[B_w])
            bg_cast_all()
        else:
            for kc in range(8):
                kb.dma("sp", win[:, kc, :], w16["awin1"][kc * 128:(kc + 1) * 128, :], reads=[B_w16["awin1"]],
                       writes=[B_w])
            kb.dma("sp", wuk[:], w16["awuk1"].rearrange("(h d) r -> d h r", h=8), reads=[B_w16["awuk1"]], writes=[B_w])
            kb.dma("sp", wuv[:], w16["awuv1"].rearrange("(h c p) d -> p h c d", h=8, p=128), reads=[B_w16["awuv1"]],
                   writes=[B_w])
        kb.dma("sp", gkv[:], d_agkv[j:j + 1, :].to_broadcast([128, 256]), writes=[B_k])
        kb.op("dve", lambda e: e.tensor_scalar(out=gkv[:], in0=gkv[:], scalar1=16.0, scalar2=None, op0=ALU.mult),
              reads=[B_k], writes=[B_k])
        kb.dma("sp", gki[:], d_agki[j:j + 1, :].to_broadcast([128, 64]), writes=[B_k])
        kb.dma("sp", bki[:], d_abki[j:j + 1, :].to_broadcast([128, 64]), writes=[B_k])
        kb.dma("sp", scores[1][:, 0:D], dgs[l * 2, :, :], reads=[B_gs[l * 2]], writes=[B_scs[1]])
        for kc in range(8):
            slot = scores[0][:, (kc % 4) * D:(kc % 4 + 1) * D]
            kb.dma("sp", slot, d_awo[j, kc * 128:(kc + 1) * 128, :], writes=[B_scs[0]])
            kb.op("dve", lambda e, kc=kc, slot=slot: e.tensor_tensor(out=wo[:, kc, :], in0=slot, in1=scores[1][:, 0:D],
                                                                     op=ALU.mult),
                  reads=[B_scs[0], B_scs[1]], writes=[B_w])
        kb.op("pool", lambda e: e.memset(cmask[:], 0.0), writes=[B_k])
        kb.op("pool", lambda e: e.memset(kd[:], 0.0), writes=[B_kd])
        kb.op("pool", lambda e: e.affine_select(out=cmask[:], in_=cmask[:], pattern=[[-1, 128]], compare_op=ALU.is_ge,
                                                fill=NEG, base=0, channel_multiplier=1), reads=[B_k], writes=[B_k])
        for k in range(NIT + 1):
            kb.op("pool", lambda e, k=k: e.memset(pw2[:, k:k + 1], 2.0 ** (-(k + 1))), writes=[B_k])
        kb.op("pool", lambda e: e.memset(ckve[:], 1.0), writes=B_ckve)
        kb.op("pool", lambda e: e.tensor_copy(out=ident4[:], in_=ident_b[:].unsqueeze(1).to_broadcast([128, 4, 128])),
              reads=[B_const], writes=[B_k])
        A = amix[:, l, :]
        S = modT[:, l, 0:8]
        SC_W = float(8 ** -0.5 * 64 ** -0.5)
        m8, lo_, w_, t_, c_, g_, thr = (thb[:, 0:8], thb[:, 8:9], thb[:, 9:10], thb[:, 10:11], thb[:, 11:12],
                                        thb[:, 12:13], thb[:, 13:14])
        wk = thb[:, 14:14 + NIT + 1]

        def stage_a(i):
            xt, xb = xts[i % 3], xbs[i % 3]
            score, B_sc = scores[i % 2], B_scs[i % 2]
            load_x(src, i, xt, xb)
            norm_tile(xt, xb, junk[:], B_junk, st[:, 0:1], st[:, 1:2], B_st, xsb[:], B_xsb)
            make_hT(xsb, B_xsb, lambda kc: hT[:, kc, :], B_hT, A, S)
            for grp in range(3):
                pt, pb = pbank()
                for a in range(4):
                    ch = grp * 4 + a
                    c0 = ch * 128 if ch < 8 else 1280 + (ch - 8) * 128
                    for kc in range(8):
                        kb.op("pe", lambda e, pt=pt, a=a, c0=c0, kc=kc: e.matmul(
                            pt[:, a * 128:(a + 1) * 128], lhsT=win[:, kc, c0:c0 + 128], rhs=hT[:, kc, :],
                            start=(kc == 0), stop=(kc == 7)), reads=[B_w, B_hT], writes=[pb])
                if grp < 2:
                    kb.op("act", lambda e, pt=pt, grp=grp: e.activation(
                        out=qT[:, grp * 4:(grp + 1) * 4, :].rearrange("p a t -> p (a t)"), in_=pt[:], func=AF.Identity),
                        reads=[pb], writes=[B_q])
                else:
                    kb.op("act", lambda e, pt=pt: e.activation(
                        out=qiT[:].rearrange("p a t -> p (a t)"), in_=pt[:], func=AF.Identity),
                        reads=[pb], writes=[B_qi])
            pt, pb = pbank()
            for kc in range(8):
                kb.op("pe", lambda e, pt=pt, kc=kc: e.matmul(pt[:, 0:256], lhsT=hT[:, kc, :], rhs=win[:, kc, 1024:1280],
                                                             start=(kc == 0), stop=(kc == 7)),
                      reads=[B_w, B_hT], writes=[pb])
            for kc in range(8):
                kb.op("pe", lambda e, pt=pt, kc=kc: e.matmul(pt[:, 256:328], lhsT=hT[:, kc, :], rhs=win[:, kc, 1792:1864],
                                                             start=(kc == 0), stop=(kc == 7)),
                      reads=[B_w, B_hT], writes=[pb])
            kb.op("act", lambda e, pt=pt: e.activation(out=junk[:, 0:256], in_=pt[:, 0:256], func=AF.Square,
                                                       accum_out=st[:, 2:3]), reads=[pb], writes=[B_junk, B_ct])
            rstd_from_ss(st[:, 3:4], st[:, 2:3], B_ct, 256.0, B_ct)
            kb.op("dve", lambda e, pt=pt: e.scalar_tensor_tensor(out=ckve[:, i, 0:256], in0=pt[:, 0:256],
                                                                 scalar=st[:, 3:4], in1=gkv[:], op0=ALU.mult,
                                                                 op1=ALU.mult),
                  reads=[pb, B_ct, B_k], writes=[B_ckve[i]])
            kb.op("dve", lambda e, pt=pt: e.bn_stats(out=st[:, 4:10], in_=pt[:, 256:320]), reads=[pb], writes=[B_kt])
            kb.op("dve", lambda e: e.bn_aggr(out=st[:, 10:12], in_=st[:, 4:10]), reads=[B_kt], writes=[B_kt])
            kb.op("pool", lambda e: e.tensor_scalar(out=st[:, 12:13], in0=st[:, 11:12], scalar1=EPS, scalar2=None,
                                                    op0=ALU.add), reads=[B_kt], writes=[B_kt])
            kb.op("pool", lambda e: e.tensor_tensor(out=st[:, 12:13], in0=st[:, 12:13], in1=mhalf[:], op=ALU.pow),
                  reads=[B_kt, B_const], writes=[B_kt])
            kb.op("dve", lambda e, pt=pt: e.tensor_scalar(out=ktm[:], in0=pt[:, 256:320], scalar1=st[:, 10:11],
                                                          scalar2=st[:, 12:13], op0=ALU.subtract, op1=ALU.mult),
                  reads=[pb, B_kt], writes=[B_kt])
            kb.op("dve", lambda e: e.tensor_tensor(out=ktm[:], in0=ktm[:], in1=gki[:], op=ALU.mult),
                  reads=[B_kt, B_k], writes=[B_kt])
            for a_ in range(2):
                kb.op("dve", lambda e, a_=a_: e.tensor_tensor(out=kd[:, a_, a_ * 64:(a_ + 1) * 64], in0=ktm[:],
                                                              in1=bki[:], op=ALU.add),
                      reads=[B_kt, B_k], writes=[B_kd])
            kb.op("dve", lambda e, pt=pt: e.tensor_scalar(out=wi[:], in0=pt[:, 320:328], scalar1=SC_W, scalar2=None,
                                                          op0=ALU.mult), reads=[pb], writes=[B_wi])
            for h in range(8):
                kb.op("dve", lambda e, h=h: e.tensor_scalar(out=dg[:, h, :], in0=ident_b[:], scalar1=wi[:, h:h + 1],
                                                            scalar2=None, op0=ALU.mult),
                      reads=[B_wi, B_const], writes=[B_dg])
            for rc in range(2):
                kb.op("pe", lambda e, rc=rc: e.transpose(out=TB[:, rc * 128:(rc + 1) * 128],
                                                         in_=ckve[:, i, rc * 128:(rc + 1) * 128], identity=ident_b[:]),
                      reads=[B_ckve[i], B_const], writes=[TBb])
            for a_ in range(2):
                kb.op("pe", lambda e, a_=a_: e.transpose(out=TB[:, 256 + a_ * 128:384 + a_ * 128], in_=kd[:, a_, :],
                                                         identity=ident_b[:]), reads=[B_kd, B_const], writes=[TBb])
            kb.op("dve", lambda e: e.tensor_copy(out=ckvT[:, :, i * 128:(i + 1) * 128],
                                                 in_=TB[:, 0:256].rearrange("p (c t) -> p c t", c=2)),
                  reads=[TBb], writes=[B_ckvT[i]])
            for a_ in range(2):
                kb.op("dve", lambda e, a_=a_: e.tensor_copy(out=kiT[a_][:, i * 128:(i + 1) * 128],
                                                            in_=TB[:, 256 + a_ * 128:384 + a_ * 128]),
                      reads=[TBb], writes=[B_kiT[i]])
            ql, qlb = qlT[i % 3], B_ql[i % 3]
            for rc in range(2):
                pts = [pbank(), pbank()]
                for h in range(8):
                    pt2, pb2 = pts[h // 4]
                    kb.op("pe", lambda e, pt2=pt2, h=h, rc=rc: e.matmul(
                        pt2[:, (h % 4) * 128:(h % 4 + 1) * 128], lhsT=wuk[:, h, rc * 128:(rc + 1) * 128],
                        rhs=qT[:, h, :], start=True, stop=True), reads=[B_w, B_q], writes=[pb2])
                for hg in range(2):
                    pt2, pb2 = pts[hg]
                    kb.op("act", lambda e, pt2=pt2, hg=hg, rc=rc, ql=ql: e.activation(
                        out=ql[:, rc, hg * 512:(hg + 1) * 512], in_=pt2[:], func=AF.Identity,
                        scale=float(128 ** -0.5)), reads=[pb2], writes=[qlb])
        def idx_gen(i):
            score, B_sc = scores[i % 2], B_scs[i % 2]
            n = (i + 1) * 128
            rr = 0
            for c0 in range(0, n, 512):
                wd_ = min(512, n - c0)
                kblk = [B_kiT[b] for b in range(c0 // 128, (c0 + wd_) // 128)]
                rl = []
                pt3, pb3 = banks[7], bankb[7]

                def hsum(h, rl=rl, pt3=pt3, pb3=pb3, wd_=wd_):
                    R, Rb = rl[h]
                    kb.op("pe", lambda e: e.matmul(pt3[:, 0:wd_], lhsT=dg[:, h, :], rhs=R[:, 0:wd_],
                                                   start=(h == 0), stop=(h == 7)), reads=[B_dg, Rb], writes=[pb3])
                for h in range(8):
                    pt2, pb2 = banks[6], bankb[6]
                    p0 = (h % 2) * 64
                    kb.op("pe", lambda e, pt2=pt2, h=h, p0=p0, c0=c0, wd_=wd_: e.matmul(
                        pt2[:, 0:wd_], lhsT=qiT[:, h // 2, :], rhs=kiT[h % 2][:, c0:c0 + wd_],
                        start=True, stop=True), reads=[B_qi] + kblk, writes=[pb2])
                    R, Rb = Rh[rr % NR], B_Rh[rr % NR]
                    rr += 1
                    kb.op("act", lambda e, pt2=pt2, R=R, wd_=wd_: e.activation(out=R[:, 0:wd_], in_=pt2[:, 0:wd_],
                                                                               func=AF.Relu),
                          reads=[pb2], writes=[Rb])
                    rl.append((R, Rb))
                    if h >= 2:
                        hsum(h - 2)
                    yield
                hsum(6)
                hsum(7)
                if c0 + wd_ == n:
                    if wd_ > 128:
                        kb.op("dve", lambda e, pt3=pt3, c0=c0, wd_=wd_: e.tensor_copy(
                            out=score[:, c0:c0 + wd_ - 128], in_=pt3[:, 0:wd_ - 128]), reads=[pb3], writes=[B_sc])
                    kb.op("dve", lambda e, pt3=pt3, c0=c0, wd_=wd_: e.tensor_tensor(
                        out=score[:, n - 128:n], in0=pt3[:, wd_ - 128:wd_], in1=cmask[:], op=ALU.add),
                        reads=[pb3, B_k], writes=[B_sc])
                else:
                    kb.op("dve", lambda e, pt3=pt3, c0=c0, wd_=wd_: e.tensor_copy(
                        out=score[:, c0:c0 + wd_], in_=pt3[:, 0:wd_]), reads=[pb3], writes=[B_sc])

        def t1_gen(i):
            n = (i + 1) * 128
            nm, B_nm = nms[i % 2], B_nms[i % 2]
            score, B_sc = scores[i % 2], B_scs[i % 2]
            if i < 2:
                kb.op("dve", lambda e: e.memset(thr, -1.0e29), writes=[B_th])
            else:
                kb.op("dve", lambda e: e.max(out=m8, in_=score[:, 0:n]), reads=[B_sc], writes=[B_th])
                yield
                kb.op("dve", lambda e: e.tensor_reduce(out=lo_, in_=score[:, 0:n - 128], axis=AX.X, op=ALU.min),
                      reads=[B_sc], writes=[B_th])
                kb.op("dve", lambda e: e.tensor_tensor(out=w_, in0=thb[:, 7:8], in1=lo_, op=ALU.subtract),
                      reads=[B_th], writes=[B_th])
                kb.op("dve", lambda e: e.tensor_scalar(out=wk, in0=pw2[:], scalar1=w_, scalar2=None, op0=ALU.mult),
                      reads=[B_th, B_k], writes=[B_th])
                kb.op("dve", lambda e: e.tensor_tensor(out=t_, in0=lo_, in1=thb[:, 14:15], op=ALU.add),
                      reads=[B_th], writes=[B_th])
                yield
                for k in range(NIT):
                    kb.op("dve", lambda e: e.tensor_scalar(out=nm[:, 0:n], in0=score[:, 0:n], scalar1=t_, scalar2=0.0,
                                                           op0=ALU.is_ge, op1=ALU.add, accum_out=c_),
                          reads=[B_sc, B_th], writes=[B_nm, B_th])
                    kb.op("dve", lambda e: e.tensor_scalar(out=g_, in0=c_, scalar1=float(TOPK) - 0.5, scalar2=0.5,
                                                           op0=ALU.is_ge, op1=ALU.subtract), reads=[B_th], writes=[B_th])
                    kb.op("dve", lambda e, k=k: e.scalar_tensor_tensor(out=t_, in0=g_, scalar=thb[:, 14 + k:15 + k],
                                                                       in1=t_, op0=ALU.mult, op1=ALU.add),
                          reads=[B_th], writes=[B_th])
                    yield
                kb.op("dve", lambda e: e.tensor_tensor(out=thr, in0=t_, in1=thb[:, 14 + NIT:15 + NIT], op=ALU.subtract),
                      reads=[B_th], writes=[B_th])
            kb.op("dve", lambda e: e.tensor_scalar(out=nm[:, 0:n], in0=score[:, 0:n], scalar1=thr, scalar2=-30000.0,
                                                   op0=ALU.is_lt, op1=ALU.mult), reads=[B_sc, B_th], writes=[B_nm])
            yield

        def stage_b(i, tick=None, drain=None):
            xt, xb = xts[i % 3], xbs[i % 3]
            ql, qlb = qlT[i % 3], B_ql[i % 3]
            nm_i, B_nm_i = nms[i % 2], B_nms[i % 2]
            pi = 0
            for hg in range(2):
                accs = [(banks[b_], bankb[b_]) for b_ in range(4)]
                def qk(sbk, hg=hg):
                    pt, pb = pbank2()
                    for rc in range(2):
                        kb.op("pe", lambda e, pt=pt, rc=rc: e.matmul(
                            pt[:], lhsT=ckvT[:, rc, sbk * 128:(sbk + 1) * 128], rhs=ql[:, rc, hg * 512:(hg + 1) * 512],
                            start=(rc == 0), stop=False), reads=[B_ckvT[sbk], qlb], writes=[pb])
                    kb.op("pe", lambda e, pt=pt: e.matmul(
                        pt[:], lhsT=nm_i[:, sbk * 128:(sbk + 1) * 128], rhs=ident4[:].rearrange("p a t -> p (a t)"),
                        start=False, stop=True), reads=[B_nm_i, B_k], writes=[pb])
                    return pt, pb

                cur = qk(0)
                for sbk in range(i + 1):
                    pt, pb = cur
                    P_, Pb = PT[pi % 3], B_PT[pi % 3]
                    pi += 1
                    kb.op("act", lambda e, pt=pt, P_=P_: e.activation(out=P_[:], in_=pt[:], func=AF.Exp),
                          reads=[pb], writes=[Pb])
                    if sbk + 1 <= i:
                        cur = qk(sbk + 1)
                    if tick is not None:
                        tick()
                    for a in range(4):
                        at, ab = accs[a]
                        kb.op("pe", lambda e, at=at, a=a, P_=P_, sbk=sbk: e.matmul(
                            at[:, 0:257], lhsT=P_[:, a * 128:(a + 1) * 128], rhs=ckve[:, sbk, 0:257],
                            start=(sbk == 0), stop=(sbk == i)), reads=[Pb, B_ckve[sbk]], writes=[ab])
                for a in range(4):
                    h = hg * 4 + a
                    at, ab = accs[a]
                    kb.op("dve", lambda e, at=at, h=h: e.reciprocal(out=rden[:, h:h + 1], in_=at[:, 256:257]),
                          reads=[ab], writes=[B_rd])
                    kb.op("dve", lambda e, at=at, h=h: e.tensor_scalar(out=ol[:, h, :], in0=at[:, 0:256],
                                                                       scalar1=rden[:, h:h + 1], scalar2=None,
                                                                       op0=ALU.mult), reads=[ab, B_rd], writes=[B_ol])
            if drain is not None:
                drain()
            for half in range(2):
                for a in range(8):
                    blk = half * 8 + a
                    h, rc = blk // 2, blk % 2
                    kb.op("pe", lambda e, a=a, h=h, rc=rc: e.transpose(out=TB[:, a * 128:(a + 1) * 128],
                                                                       in_=ol[:, h, rc * 128:(rc + 1) * 128],
                                                                       identity=ident_b[:]),
                          reads=[B_ol, B_const], writes=[TBb])
                kb.op("dve", lambda e, half=half: e.tensor_copy(
                    out=olT[:, half * 8:(half + 1) * 8, :], in_=TB[:].rearrange("p (a t) -> p a t", a=8)),
                    reads=[TBb], writes=[B_olT])
            for hg in range(2):
                pt, pb = pbank()
                for a in range(4):
                    h = hg * 4 + a
                    for rc in range(2):
                        kb.op("pe", lambda e, pt=pt, a=a, h=h, rc=rc: e.matmul(
                            pt[:, a * 128:(a + 1) * 128], lhsT=wuv[:, h, rc, :], rhs=olT[:, h * 2 + rc, :],
                            start=(rc == 0), stop=(rc == 1)), reads=[B_w, B_olT], writes=[pb])
                kb.op("act", lambda e, pt=pt, hg=hg: e.activation(
                    out=oT[:, hg * 4:(hg + 1) * 4, :].rearrange("p a t -> p (a t)"), in_=pt[:], func=AF.Identity),
                    reads=[pb], writes=[B_oT])
            ys = []
            for dh in range(2):
                pt, pb = pbank()
                for h in range(8):
                    kb.op("pe", lambda e, pt=pt, h=h, dh=dh: e.matmul(
                        pt[:], lhsT=oT[:, h, :], rhs=wo[:, h, dh * 512:(dh + 1) * 512],
                        start=(h == 0), stop=(h == 7)), reads=[B_w, B_oT], writes=[pb])
                ys.append((pt, pb))
            for dh, (pt, pb) in enumerate(ys):
                kb.op("dve", lambda e, pt=pt, dh=dh: e.tensor_tensor(
                    out=xt[:, dh * 512:(dh + 1) * 512], in0=pt[:], in1=xt[:, dh * 512:(dh + 1) * 512], op=ALU.add),
                    reads=[pb, xb], writes=[xb])
            kb.dma("sp", dxs[i * 128:(i + 1) * 128, :], xt[:], reads=[xb], writes=[B_xs[i]])

        def run_all(g):
            for _ in g:
                pass

        stage_a(0)
        run_all(idx_gen(0))
        for i in range(ntiles):
            if i + 1 < ntiles:
                stage_a(i + 1)
            gi = idx_gen(i + 1) if i + 1 < ntiles else iter(())
            gt = t1_gen(i)
            nloop = max(1, 2 * i)
            t_every = max(1, nloop // 16)
            cnt = [0]

            def tick(gi=gi, gt=gt, cnt=cnt, t_every=t_every):
                next(gi, None)
                cnt[0] += 1
                if cnt[0] % t_every == 0:
                    next(gt, None)

            def drain(gi=gi, gt=gt):
                run_all(gi)
                run_all(gt)
            if i >= 1:
                stage_b(i - 1, tick, drain)
            else:
                drain()
        stage_b(ntiles - 1)
        kb.barrier()
        ps_.close()

    src = dx
    for ph in phases:
        if ph[0] == "mod":
            phase_mod()
        elif ph[0] == "dsa":
            phase_dsa(ph[1], ph[1] // 2, src, *(ph[2:]))
            src = dxs
        elif ph[0] == "gmlp":
            phase_gmlp(ph[1], ph[1] // 2, src)
            src = dxs
        elif ph[0] == "ffn":
            phase_ffn(ph[1], src)
            src = dxs
        elif ph[0] == "final":
            phase_final(src)
        elif ph[0] == "dump":
            B_o = Buf()
            for i in range(NT):
                kb.dma("sp", dout[i * 128:(i + 1) * 128, :], dxs[i * 128:(i + 1) * 128, :], reads=[B_xs[i]], writes=[B_o])
        elif ph[0] == "dbgmod":
            ddbg = dram("dbg", [128, 4 * 48 + 64], kind="ExternalOutput")
            kb.dma("sp", ddbg[:, 0:192], modT[:].rearrange("p l c -> p (l c)"), reads=[B_modT])
            kb.dma("sp", ddbg[:, 192:224], amix[:].rearrange("p l c -> p (l c)"), reads=[B_modT])
            kb.dma("sp", ddbg[:, 224:256], affn[:].rearrange("p l c -> p (l c)"), reads=[B_modT])
    kb.barrier()
    es.close()
    return nc


FULL = [("mod",)]
for _l in range(4):
    FULL.append(("dsa" if _l % 2 == 0 else "gmlp", _l))
    FULL.append(("ffn", _l))
FULL.append(("final",))


def _core_inputs(inp, b):
    f = np.float32
    c = np.ascontiguousarray
    m = {
        "x": c(inp["x"][b], dtype=f),
        "cT": c(np.asarray(inp["c"][b], dtype=f).reshape(8, 128).T),
        "mod_w": c(inp["mod_w"], dtype=f),
        "mod_b": c(inp["mod_b"], dtype=f),
        "gmixT": c(np.asarray(inp["norm_mix_g"], dtype=f).reshape(4, 8, 128).transpose(2, 0, 1).reshape(128, 32)),
        "gffnT": c(np.asarray(inp["norm_ffn_g"], dtype=f).reshape(4, 8, 128).transpose(2, 0, 1).reshape(128, 32)),
        "final_g": c(np.asarray(inp["final_g"], dtype=f).reshape(1, D)),
        "a_w_in": c(inp["a_w_in"], dtype=f),
        "a_g_kv": c(inp["a_g_kv"], dtype=f),
        "a_g_kidx": c(inp["a_g_kidx"], dtype=f),
        "a_b_kidx": c(inp["a_b_kidx"], dtype=f),
        "a_w_uk": c(inp["a_w_uk"], dtype=f),
        "a_w_uv": c(inp["a_w_uv"], dtype=f),
        "a_w_o": c(inp["a_w_o"], dtype=f),
        "b_w_in": c(inp["b_w_in"], dtype=f),
        "b_ln_g": c(inp["b_ln_g"], dtype=f),
        "b_ln_b": c(inp["b_ln_b"], dtype=f),
        "b_w_sT": c(np.asarray(inp["b_w_s"], dtype=f).transpose(0, 3, 1, 2).reshape(2, 128, 1024)),
        "b_b_s": c(np.asarray(inp["b_b_s"], dtype=f).reshape(2, 1024)),
        "b_w_out": c(inp["b_w_out"], dtype=f),
        "ffn_w_gate": c(inp["ffn_w_gate"], dtype=f),
        "ffn_w_up": c(inp["ffn_w_up"], dtype=f),
        "ffn_w_down": c(inp["ffn_w_down"], dtype=f),
    }
    return m


def kernel(**inputs):
    inp = {k: np.asarray(v) for k, v in inputs.items()}
    nc = _build(FULL)
    in_maps = [_core_inputs(inp, b) for b in range(8)]
    res = run_bass_kernel_spmd(nc, in_maps, core_ids=list(range(8)))
    out = np.stack([np.asarray(r["out"], dtype=np.float32) for r in res.results], axis=0)
    return out
```

```python
import numpy as np
from contextlib import ExitStack
import concourse.bass as bass
import concourse.mybir as mybir
from concourse.bass_utils import run_bass_kernel_spmd

F32 = mybir.dt.float32
BF16 = mybir.dt.bfloat16
ALU = mybir.AluOpType
AF = mybir.ActivationFunctionType
AX = mybir.AxisListType

T = 4096
D = 1024
NT = 32
DFF = 2816
NFC = 22
A_IN = 1864
TOPK = 256
NIT = 13
EPS = 1e-6
NEG = -1.0e30


class Buf:
    __slots__ = ("w", "r")

    def __init__(self):
        self.w = None
        self.r = {}


class KB:
    def __init__(self, nc, es):
        self.nc = nc
        self.es = es
        self.eng = dict(pe=nc.tensor, dve=nc.vector, act=nc.scalar, pool=nc.gpsimd, sp=nc.sync)
        self.sems = {}
        self.cnt = {}
        self.seen = {e: {} for e in self.eng}
        for e in self.eng:
            self.sems[e] = es.enter_context(nc.semaphore("s_" + e))
            self.cnt[e] = 0
        self.dq = {}
        self.dqi = {}
        for q, n in (("sp", 20), ("pool", 10), ("bg", 3)):
            self.dq[q] = []
            self.dqi[q] = 0
            for i in range(n):
                k = "d_%s%d" % (q, i)
                self.sems[k] = es.enter_context(nc.semaphore(k))
                self.cnt[k] = 0
                self.dq[q].append(k)
        self.nbank = 0

    def _wait(self, e, key, val):
        if val <= 0 or self.seen[e].get(key, 0) >= val:
            return
        self.eng[e].wait_ge(self.sems[key], val)
        self.seen[e][key] = val

    def _deps(self, e, reads, writes, strict=False):
        for b in reads:
            if b.w is not None:
                k, v = b.w
                if strict or k != e or e != "pe":
                    self._wait(e, k, v)
        for b in writes:
            if b.w is not None:
                k, v = b.w
                if strict or k != e:
                    self._wait(e, k, v)
            for k, v in b.r.items():
                if strict or k != e:
                    self._wait(e, k, v)

    def op(self, e, fn, reads=(), writes=()):
        self._deps(e, reads, writes)
        inst = fn(self.eng[e])
        self.cnt[e] += 1
        inst.then_inc(self.sems[e], 1)
        v = self.cnt[e]
        for b in reads:
            b.r[e] = v
        for b in writes:
            b.w = (e, v)
            b.r = {}

    def dma(self, q, out, in_, reads=(), writes=(), sems=None):
        pool = self.dq[sems or q]
        key = pool[self.dqi[sems or q] % len(pool)]
        self.dqi[sems or q] += 1
        self._wait(q, key, self.cnt[key])
        self._deps(q, reads, writes, strict=True)
        inst = self.eng[q].dma_start(out=out, in_=in_)
        self.cnt[key] += 16
        inst.then_inc(self.sems[key], 16)
        v = self.cnt[key]
        for b in reads:
            b.r[key] = v
        for b in writes:
            b.w = (key, v)
            b.r = {}

    def barrier(self):
        keys = list(self.sems.keys())
        for e in self.eng:
            for k in keys:
                if k != e:
                    self._wait(e, k, self.cnt[k])


def _build(phases):
    nc = bass.Bass("TRN2", target_bir_lowering=False)
    es = ExitStack()
    kb = KB(nc, es)

    def dram(name, shape, kind="ExternalInput", dt=F32):
        return nc.dram_tensor(name, list(shape), dt, kind=kind).ap()

    dx = dram("x", [T, D])
    dout = dram("out", [T, D], kind="ExternalOutput")
    dxs = dram("xs", [T, D], kind="Internal")
    dgs = dram("gsc", [8, 128, D], kind="Internal")
    d_cT = dram("cT", [128, 8])
    d_modw = dram("mod_w", [4, D, 6 * D])
    d_modb = dram("mod_b", [4, 6 * D])
    d_gmix = dram("gmixT", [128, 32])
    d_gffn = dram("gffnT", [128, 32])
    d_fg = dram("final_g", [1, D])
    d_awin = dram("a_w_in", [2, D, A_IN])
    d_agkv = dram("a_g_kv", [2, 256])
    d_agki = dram("a_g_kidx", [2, 64])
    d_abki = dram("a_b_kidx", [2, 64])
    d_awuk = dram("a_w_uk", [2, 8, 128, 256])
    d_awuv = dram("a_w_uv", [2, 8, 256, 128])
    d_awo = dram("a_w_o", [2, D, D])
    d_bwin = dram("b_w_in", [2, D, 4096])
    d_blng = dram("b_ln_g", [2, 2048])
    d_blnb = dram("b_ln_b", [2, 2048])
    d_bwsT = dram("b_w_sT", [2, 128, 8 * 128])
    d_bbs = dram("b_b_s", [2, 8 * 128])
    d_bwout = dram("b_w_out", [2, 2048, D])
    d_fwg = dram("ffn_w_gate", [4, D, DFF])
    d_fwu = dram("ffn_w_up", [4, D, DFF])
    d_fwd = dram("ffn_w_down", [4, DFF, D])

    uid = [0]

    w16 = {}
    B_w16 = {}

    bg_pending = []

    def bg_cast(name, src_ap, shape):
        t = dram("w16_" + name, shape, kind="Internal", dt=BF16)
        w16[name] = t
        B_w16[name] = Buf()
        bg_pending.append(lambda: kb.dma("pool", t, src_ap, writes=[B_w16[name]], sems="bg"))

    def bg_issue(n=1):
        for _ in range(n):
            if bg_pending:
                bg_pending.pop(0)()

    def bg_cast_all():
        for l_ in range(4):
            bg_cast("fwg%d" % l_, d_fwg[l_], [D, DFF])
            bg_cast("fwu%d" % l_, d_fwu[l_], [D, DFF])
            bg_cast("fwd%d" % l_, d_fwd[l_], [DFF, D])
            if l_ == 0:
                bg_cast("bwin0", d_bwin[0], [D, 4096])
                bg_cast("bwout0", d_bwout[0], [2048, D])
            if l_ == 1:
                bg_cast("awin1", d_awin[1], [D, A_IN])
                bg_cast("awuk1", d_awuk[1].rearrange("h d r -> (h d) r"), [8 * 128, 256])
                bg_cast("awuv1", d_awuv[1].rearrange("h r d -> (h r) d"), [8 * 256, 128])
            if l_ == 2:
                bg_cast("bwin1", d_bwin[1], [D, 4096])
                bg_cast("bwout1", d_bwout[1], [2048, D])

    def sb(stack, name, shape, dt):
        uid[0] += 1
        return stack.enter_context(nc.sbuf_tensor("%s_%d" % (name, uid[0]), list(shape), dt))

    ident_f = sb(es, "ident_f", [128, 128], F32)
    ident_b = sb(es, "ident_b", [128, 128], BF16)
    mhalf = sb(es, "mhalf", [128, 1], F32)
    modT = sb(es, "modT", [128, 4, 48], F32)
    amix = sb(es, "amix", [128, 4, 8], F32)
    affn = sb(es, "affn", [128, 4, 8], F32)
    gmixT = sb(es, "gmixT_s", [128, 32], F32)
    gffnT = sb(es, "gffnT_s", [128, 32], F32)
    B_const = Buf()
    B_modT = Buf()
    banks = [es.enter_context(nc.psum_tensor("ps%d" % i, [128, 512], F32)) for i in range(8)]
    bankb = [Buf() for _ in range(8)]

    nb2 = [0]

    def pbank():
        i = kb.nbank % 5
        kb.nbank += 1
        return banks[i], bankb[i]

    def pbank2():
        i = 4 + nb2[0] % 2
        nb2[0] += 1
        return banks[i], bankb[i]

    TB = banks[7][:].bitcast(BF16)
    TBb = bankb[7]

    kb.op("pool", lambda e: e.memset(ident_f[:], 1.0), writes=[B_const])
    kb.op("pool", lambda e: e.memset(mhalf[:], -0.5), writes=[B_const])
    kb.op("pool", lambda e: e.affine_select(out=ident_f[:], in_=ident_f[:], pattern=[[-1, 128]],
                                            compare_op=ALU.is_equal, fill=0.0, base=0, channel_multiplier=1),
          reads=[B_const], writes=[B_const])
    kb.op("dve", lambda e: e.tensor_copy(out=ident_b[:], in_=ident_f[:]), reads=[B_const], writes=[B_const])

    def rstd_from_ss(stk_rs, ss, B_ss, n, B_rs):
        kb.op("pool", lambda e: e.tensor_scalar(out=stk_rs, in0=ss, scalar1=float(n) * EPS, scalar2=None,
                                                op0=ALU.add), reads=[B_ss], writes=[B_rs])
        kb.op("pool", lambda e: e.tensor_tensor(out=stk_rs, in0=stk_rs, in1=mhalf[:], op=ALU.pow),
              reads=[B_rs, B_const], writes=[B_rs])

    def phase_mod():
        ps_ = ExitStack()
        cT = sb(ps_, "cT_s", [128, 8], F32)
        cact = sb(ps_, "cact", [128, 8], F32)
        crep = sb(ps_, "crep", [128, 8, 128], F32)
        ones1 = sb(ps_, "ones1", [1, 128], F32)
        mb = sb(ps_, "mb", [1, 6 * D], F32)
        mw = [sb(ps_, "mw%d" % i, [128, 3072], F32) for i in range(2)]
        mwb = [Buf(), Buf()]
        mh = sb(ps_, "mh", [128, 3072], F32)
        dtmp = sb(ps_, "dtmp", [128, 24, 128], F32)
        B_c, B_mb, B_mh, B_dt = Buf(), Buf(), Buf(), Buf()
        kb.dma("sp", cT[:], d_cT[:, :], writes=[B_c])
        kb.dma("sp", gmixT[:], d_gmix[:, :], writes=[B_modT])
        kb.dma("sp", gffnT[:], d_gffn[:, :], writes=[B_modT])
        kb.op("act", lambda e: e.activation(out=cact[:], in_=cT[:], func=AF.Silu), reads=[B_c], writes=[B_c])
        kb.op("pool", lambda e: e.memset(ones1[:], 1.0), writes=[B_c])
        for kc in range(8):
            kb.op("dve", lambda e, kc=kc: e.tensor_copy(out=crep[:, kc, :],
                                                        in_=cact[:, kc:kc + 1].to_broadcast([128, 128])),
                  reads=[B_c], writes=[B_c])
        li = 0
        for l in range(4):
            kb.dma("sp", mb[:], d_modb[l:l + 1, :], writes=[B_mb])
            for half in range(2):
                acc = [(banks[b_], bankb[b_]) for b_ in range(6)]
                for kc in range(8):
                    t_, tb = mw[li % 2], mwb[li % 2]
                    li += 1
                    kb.dma("sp", t_[:], d_modw[l, kc * 128:(kc + 1) * 128, half * 3072:(half + 1) * 3072],
                           writes=[tb])
                    for blk in range(6):
                        pt, pb = acc[blk]
                        if kc == 0:
                            kb.op("pe", lambda e, pt=pt, blk=blk, half=half: e.matmul(
                                pt[:], lhsT=ones1[0:1, :], rhs=mb[0:1, half * 3072 + blk * 512: half * 3072 + (blk + 1) * 512],
                                start=True, stop=False), reads=[B_c, B_mb], writes=[pb])
                        kb.op("pe", lambda e, pt=pt, blk=blk, kc=kc, t_=t_: e.matmul(
                            pt[:], lhsT=crep[:, kc, :], rhs=t_[:, blk * 512:(blk + 1) * 512],
                            start=False, stop=(kc == 7)), reads=[B_c, tb], writes=[pb])
                for blk in range(6):
                    pt, pb = acc[blk]
                    kb.op("dve" if blk % 2 == 0 else "act",
                          (lambda e, pt=pt, blk=blk: e.tensor_copy(out=mh[:, blk * 512:(blk + 1) * 512], in_=pt[:]))
                          if blk % 2 == 0 else
                          (lambda e, pt=pt, blk=blk: e.activation(out=mh[:, blk * 512:(blk + 1) * 512], in_=pt[:],
                                                                  func=AF.Identity)),
                          reads=[pb], writes=[B_mh])
                kb.dma("sp", dgs[l * 2 + half, :, :], mh[:, 2048:3072], reads=[B_mh], writes=[B_gs[l * 2 + half]])
                kb.op("dve", lambda e: e.tensor_tensor(
                    out=dtmp[:], in0=mh[:].rearrange("p (c j) -> p c j", j=128),
                    in1=ident_f[:].unsqueeze(1).to_broadcast([128, 24, 128]), op=ALU.mult),
                    reads=[B_mh, B_const], writes=[B_dt])
                kb.op("dve", lambda e, l=l, half=half: e.tensor_reduce(
                    out=modT[:, l, half * 24:(half + 1) * 24], in_=dtmp[:], axis=AX.X, op=ALU.add),
                    reads=[B_dt], writes=[B_modT])
            kb.op("dve", lambda e, l=l: e.scalar_tensor_tensor(
                out=amix[:, l, :], in0=modT[:, l, 8:16], scalar=1.0, in1=gmixT[:, l * 8:(l + 1) * 8],
                op0=ALU.add, op1=ALU.mult), reads=[B_modT], writes=[B_modT])
            kb.op("dve", lambda e, l=l: e.scalar_tensor_tensor(
                out=affn[:, l, :], in0=modT[:, l, 32:40], scalar=1.0, in1=gffnT[:, l * 8:(l + 1) * 8],
                op0=ALU.add, op1=ALU.mult), reads=[B_modT], writes=[B_modT])
        kb.barrier()
        ps_.close()

    B_gs = [Buf() for _ in range(8)]
    B_xs = [Buf() for _ in range(NT)]

    def load_x(src, i, xt, xb):
        kb.dma("sp", xt[:], src[i * 128:(i + 1) * 128, :], reads=[B_xs[i]] if src is dxs else [], writes=[xb])

    def norm_tile(xt, xb, junk, B_junk, ss, rs, B_st, xsb, B_xsb):
        kb.op("act", lambda e: e.activation(out=junk, in_=xt[:], func=AF.Square, accum_out=ss),
              reads=[xb], writes=[B_junk, B_st])
        rstd_from_ss(rs, ss, B_st, float(D), B_st)
        kb.op("dve", lambda e: e.tensor_scalar(out=xsb, in0=xt[:], scalar1=rs, scalar2=32.0, op0=ALU.mult,
                                               op1=ALU.mult), reads=[xb, B_st], writes=[B_xsb])

    def make_hT(xsb_t, B_xsb, hT_dst, B_hT, A, S):
        for kc in range(8):
            kb.op("pe", lambda e, kc=kc: e.transpose(out=TB[:, kc * 128:(kc + 1) * 128],
                                                     in_=xsb_t[:, kc * 128:(kc + 1) * 128], identity=ident_b[:]),
                  reads=[B_xsb, B_const], writes=[TBb])
        for kc in range(8):
            kb.op("act", lambda e, kc=kc: e.activation(out=hT_dst(kc), in_=TB[:, kc * 128:(kc + 1) * 128],
                                                       func=AF.Identity, scale=A[:, kc:kc + 1], bias=S[:, kc:kc + 1]),
                  reads=[TBb, B_modT], writes=[B_hT])

    def residual_store(ypairs, xt, xb, gb, B_gb, tmps, dst, dstb):
        for dh, (pt, pb) in enumerate(ypairs):
            tmp, tb = tmps[dh]
            kb.op("dve", lambda e, pt=pt, dh=dh, tmp=tmp: e.tensor_tensor(
                out=tmp[:], in0=pt[:], in1=gb[:, dh * 512:(dh + 1) * 512], op=ALU.mult),
                reads=[pb, B_gb], writes=[tb])
            kb.op("pool", lambda e, dh=dh, tmp=tmp: e.tensor_tensor(
                out=xt[:, dh * 512:(dh + 1) * 512], in0=xt[:, dh * 512:(dh + 1) * 512], in1=tmp[:], op=ALU.add),
                reads=[tb, xb], writes=[xb])
        kb.dma("sp", dst, xt[:], reads=[xb], writes=[dstb])

    def phase_ffn(l, src):
        ps_ = ExitStack()
        wg = sb(ps_, "wg", [128, 8, DFF], BF16)
        wu = sb(ps_, "wu", [128, 8, DFF], BF16)
        wd = sb(ps_, "wd", [128, NFC, D], BF16)
        gb = sb(ps_, "gb", [128, D], F32)
        xts = [sb(ps_, "xt%d" % i, [128, D], F32) for i in range(4)]
        xbs = [Buf() for _ in range(4)]
        junk = sb(ps_, "junk", [128, D], BF16)
        st = sb(ps_, "st", [128, 4], F32)
        xsb = [sb(ps_, "xsb%d" % i, [128, D], BF16) for i in range(2)]
        hT = [sb(ps_, "hT%d" % i, [128, 8, 256], BF16) for i in range(2)]
        actT = sb(ps_, "actT", [128, NFC, 256], BF16)
        sg = [sb(ps_, "sg%d" % i, [128, 256], F32) for i in range(2)]
        tmps = [(sb(ps_, "tmp%d" % i, [128, 512], F32), Buf()) for i in range(2)]
        B_w, B_gb, B_junk = Buf(), Buf(), Buf()
        B_st = [Buf(), Buf()]
        B_xsb = [Buf(), Buf()]
        B_hT = [Buf(), Buf()]
        B_sg = [Buf(), Buf()]
        B_act = [Buf() for _ in range(NFC)]
        B_wg = [Buf() for _ in range(8)]
        B_wu = [Buf() for _ in range(8)]
        B_wd = [Buf() for _ in range(NFC)]
        for kc in range(8):
            kb.dma("sp", wg[:, kc, :], w16["fwg%d" % l][kc * 128:(kc + 1) * 128, :], reads=[B_w16["fwg%d" % l]],
                   writes=[B_wg[kc]])
        for kc in range(8):
            kb.dma("sp", wu[:, kc, :], w16["fwu%d" % l][kc * 128:(kc + 1) * 128, :], reads=[B_w16["fwu%d" % l]],
                   writes=[B_wu[kc]])
        for fc in range(NFC):
            kb.dma("sp", wd[:, fc, :], w16["fwd%d" % l][fc * 128:(fc + 1) * 128, :], reads=[B_w16["fwd%d" % l]],
                   writes=[B_wd[fc]])
        kb.dma("sp", gb[:], dgs[l * 2 + 1, :, :], reads=[B_gs[l * 2 + 1]], writes=[B_gb])
        A = affn[:, l, :]
        S = modT[:, l, 24:32]
        NG = NT // 2

        def prep_a(g):
            for tl in range(2):
                i = g * 2 + tl
                load_x(src, i, xts[i % 4], xbs[i % 4])

        def prep_b(g):
            for tl in range(2):
                i = g * 2 + tl
                k = i % 2
                norm_tile(xts[i % 4], xbs[i % 4], junk[:], B_junk, st[:, 2 * k:2 * k + 1], st[:, 2 * k + 1:2 * k + 2],
                          B_st[k], xsb[k][:], B_xsb[k])
                make_hT(xsb[k], B_xsb[k], lambda kc, tl=tl, g=g: hT[g % 2][:, kc, tl * 128:(tl + 1) * 128],
                        B_hT[g % 2], A, S)

        prep_a(0)
        prep_b(0)
        for g in range(NG):
            h = hT[g % 2]
            hb = B_hT[g % 2]
            if g + 1 < NG:
                prep_a(g + 1)
            for fc in range(NFC):
                pt, pb = pbank()
                for slot, w in ((0, wg), (1, wu)):
                    for kc in range(8):
                        kb.op("pe", lambda e, pt=pt, slot=slot, w=w, kc=kc, fc=fc: e.matmul(
                            pt[:, slot * 256:(slot + 1) * 256], lhsT=w[:, kc, fc * 128:(fc + 1) * 128],
                            rhs=h[:, kc, :], start=(kc == 0), stop=(kc == 7)),
                            reads=[(B_wg if slot == 0 else B_wu)[kc], hb], writes=[pb])
                s_ = sg[fc % 2]
                sbf = B_sg[fc % 2]
                kb.op("act", lambda e, pt=pt, s_=s_: e.activation(out=s_[:], in_=pt[:, 0:256], func=AF.Silu),
                      reads=[pb], writes=[sbf])
                kb.op("dve", lambda e, pt=pt, s_=s_, fc=fc: e.tensor_tensor(
                    out=actT[:, fc, :], in0=pt[:, 256:512], in1=s_[:], op=ALU.mult),
                    reads=[pb, sbf], writes=[B_act[fc]])
                if fc == 6 and g + 1 < NG:
                    prep_b(g + 1)
            for tl in range(2):
                i = g * 2 + tl
                ys = []
                for dh in range(2):
                    pt, pb = pbank()
                    for fc in range(NFC):
                        kb.op("pe", lambda e, pt=pt, fc=fc, tl=tl, dh=dh: e.matmul(
                            pt[:], lhsT=actT[:, fc, tl * 128:(tl + 1) * 128], rhs=wd[:, fc, dh * 512:(dh + 1) * 512],
                            start=(fc == 0), stop=(fc == NFC - 1)), reads=[B_wd[fc], B_act[fc]], writes=[pb])
                    ys.append((pt, pb))
                residual_store(ys, xts[i % 4], xbs[i % 4], gb, B_gb, tmps, dxs[i * 128:(i + 1) * 128, :], B_xs[i])
        kb.barrier()
        ps_.close()

    def phase_final(src):
        ps_ = ExitStack()
        fg = sb(ps_, "fg", [128, D], F32)
        xts = [sb(ps_, "fxt%d" % i, [128, D], F32) for i in range(3)]
        xbs = [Buf() for _ in range(3)]
        ots = [sb(ps_, "fot%d" % i, [128, D], F32) for i in range(2)]
        obs = [Buf() for _ in range(2)]
        junk = sb(ps_, "fjunk", [128, D], BF16)
        st = sb(ps_, "fst", [128, 4], F32)
        B_st = [Buf(), Buf()]
        B_fg, B_junk, B_o = Buf(), Buf(), Buf()
        kb.dma("sp", fg[:], d_fg[0:1, :].to_broadcast([128, D]), writes=[B_fg])
        kb.op("dve", lambda e: e.tensor_scalar(out=fg[:], in0=fg[:], scalar1=32.0, scalar2=None, op0=ALU.mult),
              reads=[B_fg], writes=[B_fg])
        for i in range(NT):
            k = i % 2
            xt, xb = xts[i % 3], xbs[i % 3]
            load_x(src, i, xt, xb)
            ss, rs = st[:, 2 * k:2 * k + 1], st[:, 2 * k + 1:2 * k + 2]
            kb.op("act", lambda e, xt=xt, ss=ss: e.activation(out=junk[:], in_=xt[:], func=AF.Square, accum_out=ss),
                  reads=[xb], writes=[B_junk, B_st[k]])
            rstd_from_ss(rs, ss, B_st[k], float(D), B_st[k])
            kb.op("dve", lambda e, xt=xt, rs=rs, k=k: e.scalar_tensor_tensor(
                out=ots[k][:], in0=xt[:], scalar=rs, in1=fg[:], op0=ALU.mult, op1=ALU.mult),
                reads=[xb, B_st[k], B_fg], writes=[obs[k]])
            kb.dma("sp", dout[i * 128:(i + 1) * 128, :], ots[k][:], reads=[obs[k]], writes=[B_o])
        kb.barrier()
        ps_.close()

    def phase_gmlp(l, j, src):
        ps_ = ExitStack()
        win = sb(ps_, "bwin", [128, 8, 4096], BF16)
        wout = sb(ps_, "bwout", [128, 16, D], BF16)
        wcf = sb(ps_, "wcf", [128, 8, 128], F32)
        wcT = sb(ps_, "wcT", [128, 8, 128], BF16)
        lng = sb(ps_, "lng", [128, 2048], F32)
        lnb = sb(ps_, "lnb", [128, 2048], F32)
        bsb = sb(ps_, "bsb", [128, 8, 128], F32)
        gb = sb(ps_, "ggb", [128, D], F32)
        xts = [sb(ps_, "gxt%d" % i, [128, D], F32) for i in range(4)]
        xbs = [Buf() for _ in range(4)]
        junk = sb(ps_, "gjunk", [128, D], BF16)
        st = sb(ps_, "gst", [128, 4], F32)
        xsb = [sb(ps_, "gxsb%d" % i, [128, D], BF16) for i in range(2)]
        hT = [sb(ps_, "ghT%d" % i, [128, 8, 256], BF16) for i in range(2)]
        uT = sb(ps_, "uT", [128, 16, 256], F32)
        vr = [sb(ps_, "vr0", [128, 2048], F32)] * 2
        vt = [sb(ps_, "vt%d" % i, [128, 2048], BF16) for i in range(2)]
        bst = sb(ps_, "bst", [128, 2, 4, 6], F32)
        lst = sb(ps_, "lst", [128, 2, 4], F32)
        ypT = sb(ps_, "ypT", [128, 16, 256], BF16)
        ytm = [sb(ps_, "ytm%d" % i, [128, 512], F32) for i in range(2)]
        tmps = [(sb(ps_, "gtmp%d" % i, [128, 512], F32), Buf()) for i in range(2)]
        B_w, B_gb, B_junk, B_k = Buf(), Buf(), Buf(), Buf()
        B_st = [Buf(), Buf()]
        B_xsb = [Buf(), Buf()]
        B_hT = [Buf(), Buf()]
        B_u = [Buf() for _ in range(16)]
        B_vr = [Buf()] * 2
        B_vt = [Buf(), Buf()]
        B_ls = [Buf(), Buf()]
        B_yp = [[Buf() for _ in range(4)] for _ in range(2)]
        B_ytm = [Buf(), Buf()]
        for kc in range(8):
            kb.dma("sp", win[:, kc, :], w16["bwin%d" % j][kc * 128:(kc + 1) * 128, :], reads=[B_w16["bwin%d" % j]],
                   writes=[B_w])
        for ec in range(16):
            kb.dma("sp", wout[:, ec, :], w16["bwout%d" % j][ec * 128:(ec + 1) * 128, :], reads=[B_w16["bwout%d" % j]],
                   writes=[B_w])
        kb.dma("sp", wcf[:], d_bwsT[j, :, :].rearrange("s (g t) -> s g t", g=8), writes=[B_k])
        kb.dma("sp", lng[:], d_blng[j:j + 1, :].to_broadcast([128, 2048]), writes=[B_k])
        kb.dma("sp", lnb[:], d_blnb[j:j + 1, :].to_broadcast([128, 2048]), writes=[B_k])
        kb.dma("sp", bsb[:], d_bbs[j:j + 1, :].to_broadcast([128, 1024]).rearrange("p (g t) -> p g t", g=8),
               writes=[B_k])
        kb.dma("sp", gb[:], dgs[l * 2, :, :], reads=[B_gs[l * 2]], writes=[B_gb])
        kb.op("pool", lambda e: e.affine_select(out=wcf[:], in_=wcf[:], pattern=[[0, 8], [1, 128]],
                                                compare_op=ALU.is_ge, fill=0.0, base=0, channel_multiplier=-1),
              reads=[B_k], writes=[B_k])
        kb.op("dve", lambda e: e.tensor_copy(out=wcT[:], in_=wcf[:]), reads=[B_k], writes=[B_k])
        A = amix[:, l, :]
        S = modT[:, l, 0:8]
        NG = NT // 2

        def prep_a(g):
            for tl in range(2):
                i = g * 2 + tl
                load_x(src, i, xts[i % 4], xbs[i % 4])

        def prep_b(g):
            for tl in range(2):
                i = g * 2 + tl
                k = i % 2
                norm_tile(xts[i % 4], xbs[i % 4], junk[:], B_junk, st[:, 2 * k:2 * k + 1], st[:, 2 * k + 1:2 * k + 2],
                          B_st[k], xsb[k][:], B_xsb[k])
                make_hT(xsb[k], B_xsb[k], lambda kc, tl=tl, g=g: hT[g % 2][:, kc, tl * 128:(tl + 1) * 128],
                        B_hT[g % 2], A, S)

        prep_a(0)
        prep_b(0)
        for g in range(NG):
            h = hT[g % 2]
            hb = B_hT[g % 2]
            if g + 1 < NG:
                prep_a(g + 1)
            for ec in range(16):
                if ec % 2 == 0:
                    pt, pb = pbank()
                half = ec % 2
                for kc in range(8):
                    kb.op("pe", lambda e, pt=pt, half=half, kc=kc, ec=ec: e.matmul(
                        pt[:, half * 256:(half + 1) * 256], lhsT=win[:, kc, ec * 128:(ec + 1) * 128],
                        rhs=h[:, kc, :], start=(kc == 0), stop=(kc == 7)), reads=[B_w, hb], writes=[pb])
                if ec % 2 == 1:
                    kb.op("act", lambda e, pt=pt, ec=ec: e.activation(
                        out=uT[:, ec - 1:ec + 1, :].rearrange("p a t -> p (a t)"), in_=pt[:], func=AF.Gelu),
                        reads=[pb], writes=[B_u[ec - 1], B_u[ec]])
            for tl in range(2):
                for cb in range(4):
                    pt, pb = pbank()
                    for kc in range(8):
                        kb.op("pe", lambda e, pt=pt, kc=kc, cb=cb, tl=tl: e.matmul(
                            pt[:], lhsT=h[:, kc, tl * 128:(tl + 1) * 128],
                            rhs=win[:, kc, 2048 + cb * 512:2048 + (cb + 1) * 512],
                            start=(kc == 0), stop=(kc == 7)), reads=[B_w, hb], writes=[pb])
                    kb.op("act", lambda e, pt=pt, cb=cb, tl=tl: e.activation(
                        out=vr[tl][:, cb * 512:(cb + 1) * 512], in_=pt[:], func=AF.Gelu),
                        reads=[pb], writes=[B_vr[tl]])
                    kb.op("dve", lambda e, cb=cb, tl=tl: e.bn_stats(out=bst[:, tl, cb, :],
                                                                    in_=vr[tl][:, cb * 512:(cb + 1) * 512]),
                          reads=[B_vr[tl]], writes=[B_ls[tl]])
                kb.op("dve", lambda e, tl=tl: e.bn_aggr(out=lst[:, tl, 0:2],
                                                        in_=bst[:, tl, :, :].rearrange("p a b -> p (a b)")),
                      reads=[B_ls[tl]], writes=[B_ls[tl]])
                kb.op("pool", lambda e, tl=tl: e.tensor_scalar(out=lst[:, tl, 2:3], in0=lst[:, tl, 1:2], scalar1=EPS,
                                                               scalar2=None, op0=ALU.add),
                      reads=[B_ls[tl]], writes=[B_ls[tl]])
                kb.op("pool", lambda e, tl=tl: e.tensor_tensor(out=lst[:, tl, 2:3], in0=lst[:, tl, 2:3], in1=mhalf[:],
                                                               op=ALU.pow),
                      reads=[B_ls[tl], B_const], writes=[B_ls[tl]])
                kb.op("dve", lambda e, tl=tl: e.tensor_scalar(out=vr[tl][:], in0=vr[tl][:], scalar1=lst[:, tl, 0:1],
                                                              scalar2=lst[:, tl, 2:3], op0=ALU.subtract, op1=ALU.mult),
                      reads=[B_ls[tl], B_vr[tl]], writes=[B_vr[tl]])
                kb.op("pool", lambda e, tl=tl: e.tensor_tensor(out=vr[tl][:], in0=vr[tl][:], in1=lng[:], op=ALU.mult),
                      reads=[B_vr[tl], B_k], writes=[B_vr[tl]])
                kb.op("pool", lambda e, tl=tl: e.tensor_tensor(out=vt[tl][:], in0=vr[tl][:], in1=lnb[:], op=ALU.add),
                      reads=[B_vr[tl], B_k], writes=[B_vt[tl]])
            if g + 1 < NG:
                prep_b(g + 1)
            for tl in range(2):
                i = g * 2 + tl
                for q4 in range(4):
                    pt, pb = pbank()
                    for a in range(4):
                        ec = q4 * 4 + a
                        kb.op("pe", lambda e, pt=pt, a=a, ec=ec, tl=tl: e.matmul(
                            pt[:, a * 128:(a + 1) * 128], lhsT=vt[tl][:, ec * 128:(ec + 1) * 128],
                            rhs=wcT[:, ec // 2, :], start=True, stop=True), reads=[B_vt[tl], B_k], writes=[pb])
                    ym, ymb = ytm[q4 % 2], B_ytm[q4 % 2]
                    kb.op("dve", lambda e, pt=pt, q4=q4, ym=ym: e.tensor_tensor(
                        out=ym[:].rearrange("p (g r t) -> p g r t", g=2, r=2),
                        in0=pt[:].rearrange("p (g r t) -> p g r t", g=2, r=2),
                        in1=bsb[:, q4 * 2:q4 * 2 + 2, :].unsqueeze(2).to_broadcast([128, 2, 2, 128]), op=ALU.add),
                        reads=[pb, B_k], writes=[ymb])
                    kb.op("dve", lambda e, q4=q4, tl=tl, ym=ym: e.tensor_tensor(
                        out=ypT[:, q4 * 4:(q4 + 1) * 4, tl * 128:(tl + 1) * 128],
                        in0=ym[:].rearrange("p (a t) -> p a t", a=4),
                        in1=uT[:, q4 * 4:(q4 + 1) * 4, tl * 128:(tl + 1) * 128], op=ALU.mult),
                        reads=[ymb] + [B_u[q4 * 4 + a] for a in range(4)], writes=[B_yp[tl][q4]])
                ys = []
                for dh in range(2):
                    pt, pb = pbank()
                    for ec in range(16):
                        kb.op("pe", lambda e, pt=pt, ec=ec, tl=tl, dh=dh: e.matmul(
                            pt[:], lhsT=ypT[:, ec, tl * 128:(tl + 1) * 128], rhs=wout[:, ec, dh * 512:(dh + 1) * 512],
                            start=(ec == 0), stop=(ec == 15)), reads=[B_w, B_yp[tl][ec // 4]], writes=[pb])
                    ys.append((pt, pb))
                residual_store(ys, xts[i % 4], xbs[i % 4], gb, B_gb, tmps, dxs[i * 128:(i + 1) * 128, :], B_xs[i])
        kb.barrier()
        ps_.close()

    def phase_dsa(l, j, src, ntiles=NT):
        ps_ = ExitStack()
        win = sb(ps_, "awin", [128, 8, A_IN], BF16)
        wuk = sb(ps_, "awuk", [128, 8, 256], BF16)
        wuv = sb(ps_, "awuv", [128, 8, 2, 128], BF16)
        wo = sb(ps_, "awo", [128, 8, D], BF16)
        gkv = sb(ps_, "gkv", [128, 256], F32)
        gki = sb(ps_, "gki", [128, 64], F32)
        bki = sb(ps_, "bki", [128, 64], F32)
        cmask = sb(ps_, "cmask", [128, 128], F32)
        pw2 = sb(ps_, "pw2", [128, NIT + 1], F32)
        ckvT = sb(ps_, "ckvT", [128, 2, T], BF16)
        ckve = sb(ps_, "ckve", [128, NT, 258], BF16)
        kiT = [sb(ps_, "kiT%d" % i_, [128, T], BF16) for i_ in range(2)]
        xts = [sb(ps_, "axt%d" % i, [128, D], F32) for i in range(3)]
        xbs = [Buf() for _ in range(3)]
        junk = sb(ps_, "ajunk", [128, D], BF16)
        st = sb(ps_, "ast", [128, 16], F32)
        xsb = sb(ps_, "axsb", [128, D], BF16)
        hT = sb(ps_, "ahT", [128, 8, 128], BF16)
        qT = sb(ps_, "qT", [128, 8, 128], BF16)
        qiT = sb(ps_, "qiT", [128, 4, 128], BF16)
        qlT = [sb(ps_, "qlT%d" % i, [128, 2, 1024], BF16) for i in range(3)]
        ctm = sb(ps_, "ctm", [128, 256], BF16)
        ktm = sb(ps_, "ktm", [128, 64], F32)
        kd = sb(ps_, "kd", [128, 2, 128], BF16)
        wi = sb(ps_, "wi", [128, 8], F32)
        dg = sb(ps_, "dg", [128, 8, 128], BF16)
        scores = [sb(ps_, "score%d" % i_, [128, T], F32) for i_ in range(2)]
        B_scs = [Buf(), Buf()]
        NR = 4
        Rh = [sb(ps_, "Rh%d" % i, [128, 512], BF16) for i in range(NR)]
        B_Rh = [Buf() for _ in range(NR)]
        nms = [sb(ps_, "nm%d" % i_, [128, T], BF16) for i_ in range(2)]
        B_nms = [Buf(), Buf()]
        ident4 = sb(ps_, "ident4", [128, 4, 128], BF16)
        thb = sb(ps_, "thb", [128, 8 + NIT + 8], F32)
        PT = [sb(ps_, "PT%d" % i, [128, 512], BF16) for i in range(3)]
        B_PT = [Buf() for _ in range(3)]
        ol = sb(ps_, "ol", [128, 8, 256], BF16)
        olT = sb(ps_, "olT", [128, 16, 128], BF16)
        oT = sb(ps_, "oT", [128, 8, 128], BF16)
        rden = sb(ps_, "rden", [128, 8], F32)
        B_w, B_k, B_gb, B_junk, B_st, B_xsb, B_hT = Buf(), Buf(), Buf(), Buf(), Buf(), Buf(), Buf()
        B_q, B_qi, B_ct, B_kt, B_kd, B_wi, B_dg, B_sc, B_nm, B_th = (Buf() for _ in range(10))
        B_ql = [Buf(), Buf(), Buf()]
        B_ckvT = [Buf() for _ in range(NT)]
        B_ckve = [Buf() for _ in range(NT)]
        B_kiT = [Buf() for _ in range(NT)]
        B_ol, B_olT, B_oT, B_rd = Buf(), Buf(), Buf(), Buf()
        if j == 0:
            for kc in range(8):
                kb.dma("pool", win[:, kc, :], d_awin[j, kc * 128:(kc + 1) * 128, :], writes=[B_w])
            kb.dma("pool", wuk[:], d_awuk[j].rearrange("h d r -> d h r"), writes=[B_w])
            kb.dma("pool", wuv[:], d_awuv[j].rearrange("h (c p) d -> p h c d", p=128), writes=[B_w])
            bg_cast_all()
        else:
            for kc in range(8):
                kb.dma("sp", win[:, kc, :], w16["awin1"][kc * 128:(kc + 1) * 128, :], reads=[B_w16["awin1"]],
                       writes=[B_w])
            kb.dma("sp", wuk[:], w16["awuk1"].rearrange("(h d) r -> d h r", h=8), reads=[B_w16["awuk1"]], writes=[B_w])
            kb.dma("sp", wuv[:], w16["awuv1"].rearrange("(h c p) d -> p h c d", h=8, p=128), reads=[B_w16["awuv1"]],
                   writes=[B_w])
        kb.dma("sp", gkv[:], d_agkv[j:j + 1, :].to_broadcast([128, 256]), writes=[B_k])
        kb.op("dve", lambda e: e.tensor_scalar(out=gkv[:], in0=gkv[:], scalar1=16.0, scalar2=None, op0=ALU.mult),
              reads=[B_k], writes=[B_k])
        kb.dma("sp", gki[:], d_agki[j:j + 1, :].to_broadcast([128, 64]), writes=[B_k])
        kb.dma("sp", bki[:], d_abki[j:j + 1, :].to_broadcast([128, 64]), writes=[B_k])
        kb.dma("sp", scores[1][:, 0:D], dgs[l * 2, :, :], reads=[B_gs[l * 2]], writes=[B_scs[1]])
        for kc in range(8):
            slot = scores[0][:, (kc % 4) * D:(kc % 4 + 1) * D]
            kb.dma("sp", slot, d_awo[j, kc * 128:(kc + 1) * 128, :], writes=[B_scs[0]])
            kb.op("dve", lambda e, kc=kc, slot=slot: e.tensor_tensor(out=wo[:, kc, :], in0=slot, in1=scores[1][:, 0:D],
                                                                     op=ALU.mult),
                  reads=[B_scs[0], B_scs[1]], writes=[B_w])
        kb.op("pool", lambda e: e.memset(cmask[:], 0.0), writes=[B_k])
        kb.op("pool", lambda e: e.memset(kd[:], 0.0), writes=[B_kd])
        kb.op("pool", lambda e: e.affine_select(out=cmask[:], in_=cmask[:], pattern=[[-1, 128]], compare_op=ALU.is_ge,
                                                fill=NEG, base=0, channel_multiplier=1), reads=[B_k], writes=[B_k])
        for k in range(NIT + 1):
            kb.op("pool", lambda e, k=k: e.memset(pw2[:, k:k + 1], 2.0 ** (-(k + 1))), writes=[B_k])
        kb.op("pool", lambda e: e.memset(ckve[:], 1.0), writes=B_ckve)
        kb.op("pool", lambda e: e.tensor_copy(out=ident4[:], in_=ident_b[:].unsqueeze(1).to_broadcast([128, 4, 128])),
              reads=[B_const], writes=[B_k])
        A = amix[:, l, :]
        S = modT[:, l, 0:8]
        SC_W = float(8 ** -0.5 * 64 ** -0.5)
        m8, lo_, w_, t_, c_, g_, thr = (thb[:, 0:8], thb[:, 8:9], thb[:, 9:10], thb[:, 10:11], thb[:, 11:12],
                                        thb[:, 12:13], thb[:, 13:14])
        wk = thb[:, 14:14 + NIT + 1]

        def stage_a(i):
            xt, xb = xts[i % 3], xbs[i % 3]
            score, B_sc = scores[i % 2], B_scs[i % 2]
            load_x(src, i, xt, xb)
            norm_tile(xt, xb, junk[:], B_junk, st[:, 0:1], st[:, 1:2], B_st, xsb[:], B_xsb)
            make_hT(xsb, B_xsb, lambda kc: hT[:, kc, :], B_hT, A, S)
            for grp in range(3):
                pt, pb = pbank()
                for a in range(4):
                    ch = grp * 4 + a
                    c0 = ch * 128 if ch < 8 else 1280 + (ch - 8) * 128
                    for kc in range(8):
                        kb.op("pe", lambda e, pt=pt, a=a, c0=c0, kc=kc: e.matmul(
                            pt[:, a * 128:(a + 1) * 128], lhsT=win[:, kc, c0:c0 + 128], rhs=hT[:, kc, :],
                            start=(kc == 0), stop=(kc == 7)), reads=[B_w, B_hT], writes=[pb])
                if grp < 2:
                    kb.op("act", lambda e, pt=pt, grp=grp: e.activation(
                        out=qT[:, grp * 4:(grp + 1) * 4, :].rearrange("p a t -> p (a t)"), in_=pt[:], func=AF.Identity),
                        reads=[pb], writes=[B_q])
                else:
                    kb.op("act", lambda e, pt=pt: e.activation(
                        out=qiT[:].rearrange("p a t -> p (a t)"), in_=pt[:], func=AF.Identity),
                        reads=[pb], writes=[B_qi])
            pt, pb = pbank()
            for kc in range(8):
                kb.op("pe", lambda e, pt=pt, kc=kc: e.matmul(pt[:, 0:256], lhsT=hT[:, kc, :], rhs=win[:, kc, 1024:1280],
                                                             start=(kc == 0), stop=(kc == 7)),
                      reads=[B_w, B_hT], writes=[pb])
            for kc in range(8):
                kb.op("pe", lambda e, pt=pt, kc=kc: e.matmul(pt[:, 256:328], lhsT=hT[:, kc, :], rhs=win[:, kc, 1792:1864],
                                                             start=(kc == 0), stop=(kc == 7)),
                      reads=[B_w, B_hT], writes=[pb])
            kb.op("act", lambda e, pt=pt: e.activation(out=junk[:, 0:256], in_=pt[:, 0:256], func=AF.Square,
                                                       accum_out=st[:, 2:3]), reads=[pb], writes=[B_junk, B_ct])
            rstd_from_ss(st[:, 3:4], st[:, 2:3], B_ct, 256.0, B_ct)
            kb.op("dve", lambda e, pt=pt: e.scalar_tensor_tensor(out=ckve[:, i, 0:256], in0=pt[:, 0:256],
                                                                 scalar=st[:, 3:4], in1=gkv[:], op0=ALU.mult,
                                                                 op1=ALU.mult),
                  reads=[pb, B_ct, B_k], writes=[B_ckve[i]])
            kb.op("dve", lambda e, pt=pt: e.bn_stats(out=st[:, 4:10], in_=pt[:, 256:320]), reads=[pb], writes=[B_kt])
            kb.op("dve", lambda e: e.bn_aggr(out=st[:, 10:12], in_=st[:, 4:10]), reads=[B_kt], writes=[B_kt])
            kb.op("pool", lambda e: e.tensor_scalar(out=st[:, 12:13], in0=st[:, 11:12], scalar1=EPS, scalar2=None,
                                                    op0=ALU.add), reads=[B_kt], writes=[B_kt])
            kb.op("pool", lambda e: e.tensor_tensor(out=st[:, 12:13], in0=st[:, 12:13], in1=mhalf[:], op=ALU.pow),
                  reads=[B_kt, B_const], writes=[B_kt])
            kb.op("dve", lambda e, pt=pt: e.tensor_scalar(out=ktm[:], in0=pt[:, 256:320], scalar1=st[:, 10:11],
                                                          scalar2=st[:, 12:13], op0=ALU.subtract, op1=ALU.mult),
                  reads=[pb, B_kt], writes=[B_kt])
            kb.op("dve", lambda e: e.tensor_tensor(out=ktm[:], in0=ktm[:], in1=gki[:], op=ALU.mult),
                  reads=[B_kt, B_k], writes=[B_kt])
            for a_ in range(2):
                kb.op("dve", lambda e, a_=a_: e.tensor_tensor(out=kd[:, a_, a_ * 64:(a_ + 1) * 64], in0=ktm[:],
                                                              in1=bki[:], op=ALU.add),
                      reads=[B_kt, B_k], writes=[B_kd])
            kb.op("dve", lambda e, pt=pt: e.tensor_scalar(out=wi[:], in0=pt[:, 320:328], scalar1=SC_W, scalar2=None,
                                                          op0=ALU.mult), reads=[pb], writes=[B_wi])
            for h in range(8):
                kb.op("dve", lambda e, h=h: e.tensor_scalar(out=dg[:, h, :], in0=ident_b[:], scalar1=wi[:, h:h + 1],
                                                            scalar2=None, op0=ALU.mult),
                      reads=[B_wi, B_const], writes=[B_dg])
            for rc in range(2):
                kb.op("pe", lambda e, rc=rc: e.transpose(out=TB[:, rc * 128:(rc + 1) * 128],
                                                         in_=ckve[:, i, rc * 128:(rc + 1) * 128], identity=ident_b[:]),
                      reads=[B_ckve[i], B_const], writes=[TBb])
            for a_ in range(2):
                kb.op("pe", lambda e, a_=a_: e.transpose(out=TB[:, 256 + a_ * 128:384 + a_ * 128], in_=kd[:, a_, :],
                                                         identity=ident_b[:]), reads=[B_kd, B_const], writes=[TBb])
            kb.op("dve", lambda e: e.tensor_copy(out=ckvT[:, :, i * 128:(i + 1) * 128],
                                                 in_=TB[:, 0:256].rearrange("p (c t) -> p c t", c=2)),
                  reads=[TBb], writes=[B_ckvT[i]])
            for a_ in range(2):
                kb.op("dve", lambda e, a_=a_: e.tensor_copy(out=kiT[a_][:, i * 128:(i + 1) * 128],
                                                            in_=TB[:, 256 + a_ * 128:384 + a_ * 128]),
                      reads=[TBb], writes=[B_kiT[i]])
            ql, qlb = qlT[i % 3], B_ql[i % 3]
            for rc in range(2):
                pts = [pbank(), pbank()]
                for h in range(8):
                    pt2, pb2 = pts[h // 4]
                    kb.op("pe", lambda e, pt2=pt2, h=h, rc=rc: e.matmul(
                        pt2[:, (h % 4) * 128:(h % 4 + 1) * 128], lhsT=wuk[:, h, rc * 128:(rc + 1) * 128],
                        rhs=qT[:, h, :], start=True, stop=True), reads=[B_w, B_q], writes=[pb2])
                for hg in range(2):
                    pt2, pb2 = pts[hg]
                    kb.op("act", lambda e, pt2=pt2, hg=hg, rc=rc, ql=ql: e.activation(
                        out=ql[:, rc, hg * 512:(hg + 1) * 512], in_=pt2[:], func=AF.Identity,
                        scale=float(128 ** -0.5)), reads=[pb2], writes=[qlb])
        def idx_gen(i):
            score, B_sc = scores[i % 2], B_scs[i % 2]
            n = (i + 1) * 128
            rr = 0
            for c0 in range(0, n, 512):
                wd_ = min(512, n - c0)
                kblk = [B_kiT[b] for b in range(c0 // 128, (c0 + wd_) // 128)]
                rl = []
                pt3, pb3 = banks[7], bankb[7]

                def hsum(h, rl=rl, pt3=pt3, pb3=pb3, wd_=wd_):
                    R, Rb = rl[h]
                    kb.op("pe", lambda e: e.matmul(pt3[:, 0:wd_], lhsT=dg[:, h, :], rhs=R[:, 0:wd_],
                                                   start=(h == 0), stop=(h == 7)), reads=[B_dg, Rb], writes=[pb3])
                for h in range(8):
                    pt2, pb2 = banks[6], bankb[6]
                    p0 = (h % 2) * 64
                    kb.op("pe", lambda e, pt2=pt2, h=h, p0=p0, c0=c0, wd_=wd_: e.matmul(
                        pt2[:, 0:wd_], lhsT=qiT[:, h // 2, :], rhs=kiT[h % 2][:, c0:c0 + wd_],
                        start=True, stop=True), reads=[B_qi] + kblk, writes=[pb2])
                    R, Rb = Rh[rr % NR], B_Rh[rr % NR]
                    rr += 1
                    kb.op("act", lambda e, pt2=pt2, R=R, wd_=wd_: e.activation(out=R[:, 0:wd_], in_=pt2[:, 0:wd_],
                                                                               func=AF.Relu),
                          reads=[pb2], writes=[Rb])
                    rl.append((R, Rb))
                    if h >= 2:
                        hsum(h - 2)
                    yield
                hsum(6)
                hsum(7)
                if c0 + wd_ == n:
                    if wd_ > 128:
                        kb.op("dve", lambda e, pt3=pt3, c0=c0, wd_=wd_: e.tensor_copy(
                            out=score[:, c0:c0 + wd_ - 128], in_=pt3[:, 0:wd_ - 128]), reads=[pb3], writes=[B_sc])
                    kb.op("dve", lambda e, pt3=pt3, c0=c0, wd_=wd_: e.tensor_tensor(
                        out=score[:, n - 128:n], in0=pt3[:, wd_ - 128:wd_], in1=cmask[:], op=ALU.add),
                        reads=[pb3, B_k], writes=[B_sc])
                else:
                    kb.op("dve", lambda e, pt3=pt3, c0=c0, wd_=wd_: e.tensor_copy(
                        out=score[:, c0:c0 + wd_], in_=pt3[:, 0:wd_]), reads=[pb3], writes=[B_sc])

        def t1_gen(i):
            n = (i + 1) * 128
            nm, B_nm = nms[i % 2], B_nms[i % 2]
            score, B_sc = scores[i % 2], B_scs[i % 2]
            if i < 2:
                kb.op("dve", lambda e: e.memset(thr, -1.0e29), writes=[B_th])
            else:
                kb.op("dve", lambda e: e.max(out=m8, in_=score[:, 0:n]), reads=[B_sc], writes=[B_th])
                yield
                kb.op("dve", lambda e: e.tensor_reduce(out=lo_, in_=score[:, 0:n - 128], axis=AX.X, op=ALU.min),
                      reads=[B_sc], writes=[B_th])
                kb.op("dve", lambda e: e.tensor_tensor(out=w_, in0=thb[:, 7:8], in1=lo_, op=ALU.subtract),
                      reads=[B_th], writes=[B_th])
                kb.op("dve", lambda e: e.tensor_scalar(out=wk, in0=pw2[:], scalar1=w_, scalar2=None, op0=ALU.mult),
                      reads=[B_th, B_k], writes=[B_th])
                kb.op("dve", lambda e: e.tensor_tensor(out=t_, in0=lo_, in1=thb[:, 14:15], op=ALU.add),
                      reads=[B_th], writes=[B_th])
                yield
                for k in range(NIT):
                    kb.op("dve", lambda e: e.tensor_scalar(out=nm[:, 0:n], in0=score[:, 0:n], scalar1=t_, scalar2=0.0,
                                                           op0=ALU.is_ge, op1=ALU.add, accum_out=c_),
                          reads=[B_sc, B_th], writes=[B_nm, B_th])
                    kb.op("dve", lambda e: e.tensor_scalar(out=g_, in0=c_, scalar1=float(TOPK) - 0.5, scalar2=0.5,
                                                           op0=ALU.is_ge, op1=ALU.subtract), reads=[B_th], writes=[B_th])
                    kb.op("dve", lambda e, k=k: e.scalar_tensor_tensor(out=t_, in0=g_, scalar=thb[:, 14 + k:15 + k],
                                                                       in1=t_, op0=ALU.mult, op1=ALU.add),
                          reads=[B_th], writes=[B_th])
                    yield
                kb.op("dve", lambda e: e.tensor_tensor(out=thr, in0=t_, in1=thb[:, 14 + NIT:15 + NIT], op=ALU.subtract),
                      reads=[B_th], writes=[B_th])
            kb.op("dve", lambda e: e.tensor_scalar(out=nm[:, 0:n], in0=score[:, 0:n], scalar1=thr, scalar2=-30000.0,
                                                   op0=ALU.is_lt, op1=ALU.mult), reads=[B_sc, B_th], writes=[B_nm])
            yield

        def stage_b(i, tick=None, drain=None):
            xt, xb = xts[i % 3], xbs[i % 3]
            ql, qlb = qlT[i % 3], B_ql[i % 3]
            nm_i, B_nm_i = nms[i % 2], B_nms[i % 2]
            pi = 0
            for hg in range(2):
                accs = [(banks[b_], bankb[b_]) for b_ in range(4)]
                def qk(sbk, hg=hg):
                    pt, pb = pbank2()
                    for rc in range(2):
                        kb.op("pe", lambda e, pt=pt, rc=rc: e.matmul(
                            pt[:], lhsT=ckvT[:, rc, sbk * 128:(sbk + 1) * 128], rhs=ql[:, rc, hg * 512:(hg + 1) * 512],
                            start=(rc == 0), stop=False), reads=[B_ckvT[sbk], qlb], writes=[pb])
                    kb.op("pe", lambda e, pt=pt: e.matmul(
                        pt[:], lhsT=nm_i[:, sbk * 128:(sbk + 1) * 128], rhs=ident4[:].rearrange("p a t -> p (a t)"),
                        start=False, stop=True), reads=[B_nm_i, B_k], writes=[pb])
                    return pt, pb

                cur = qk(0)
                for sbk in range(i + 1):
                    pt, pb = cur
                    P_, Pb = PT[pi % 3], B_PT[pi % 3]
                    pi += 1
                    kb.op("act", lambda e, pt=pt, P_=P_: e.activation(out=P_[:], in_=pt[:], func=AF.Exp),
                          reads=[pb], writes=[Pb])
                    if sbk + 1 <= i:
                        cur = qk(sbk + 1)
                    if tick is not None:
                        tick()
                    for a in range(4):
                        at, ab = accs[a]
                        kb.op("pe", lambda e, at=at, a=a, P_=P_, sbk=sbk: e.matmul(
                            at[:, 0:257], lhsT=P_[:, a * 128:(a + 1) * 128], rhs=ckve[:, sbk, 0:257],
                            start=(sbk == 0), stop=(sbk == i)), reads=[Pb, B_ckve[sbk]], writes=[ab])
                for a in range(4):
                    h = hg * 4 + a
                    at, ab = accs[a]
                    kb.op("dve", lambda e, at=at, h=h: e.reciprocal(out=rden[:, h:h + 1], in_=at[:, 256:257]),
                          reads=[ab], writes=[B_rd])
                    kb.op("dve", lambda e, at=at, h=h: e.tensor_scalar(out=ol[:, h, :], in0=at[:, 0:256],
                                                                       scalar1=rden[:, h:h + 1], scalar2=None,
                                                                       op0=ALU.mult), reads=[ab, B_rd], writes=[B_ol])
            if drain is not None:
                drain()
            for half in range(2):
                for a in range(8):
                    blk = half * 8 + a
                    h, rc = blk // 2, blk % 2
                    kb.op("pe", lambda e, a=a, h=h, rc=rc: e.transpose(out=TB[:, a * 128:(a + 1) * 128],
                                                                       in_=ol[:, h, rc * 128:(rc + 1) * 128],
                                                                       identity=ident_b[:]),
                          reads=[B_ol, B_const], writes=[TBb])
                kb.op("dve", lambda e, half=half: e.tensor_copy(
                    out=olT[:, half * 8:(half + 1) * 8, :], in_=TB[:].rearrange("p (a t) -> p a t", a=8)),
                    reads=[TBb], writes=[B_olT])
            for hg in range(2):
                pt, pb = pbank()
                for a in range(4):
                    h = hg * 4 + a
                    for rc in range(2):
                        kb.op("pe", lambda e, pt=pt, a=a, h=h, rc=rc: e.matmul(
                            pt[:, a * 128:(a + 1) * 128], lhsT=wuv[:, h, rc, :], rhs=olT[:, h * 2 + rc, :],
                            start=(rc == 0), stop=(rc == 1)), reads=[B_w, B_olT], writes=[pb])
                kb.op("act", lambda e, pt=pt, hg=hg: e.activation(
                    out=oT[:, hg * 4:(hg + 1) * 4, :].rearrange("p a t -> p (a t)"), in_=pt[:], func=AF.Identity),
                    reads=[pb], writes=[B_oT])
            ys = []
            for dh in range(2):
                pt, pb = pbank()
                for h in range(8):
                    kb.op("pe", lambda e, pt=pt, h=h, dh=dh: e.matmul(
                        pt[:], lhsT=oT[:, h, :], rhs=wo[:, h, dh * 512:(dh + 1) * 512],
                        start=(h == 0), stop=(h == 7)), reads=[B_w, B_oT], writes=[pb])
                ys.append((pt, pb))
            for dh, (pt, pb) in enumerate(ys):
                kb.op("dve", lambda e, pt=pt, dh=dh: e.tensor_tensor(
                    out=xt[:, dh * 512:(dh + 1) * 512], in0=pt[:], in1=xt[:, dh * 512:(dh + 1) * 512], op=ALU.add),
                    reads=[pb, xb], writes=[xb])
            kb.dma("sp", dxs[i * 128:(i + 1) * 128, :], xt[:], reads=[xb], writes=[B_xs[i]])

        def run_all(g):
            for _ in g:
                pass

        stage_a(0)
        run_all(idx_gen(0))
        for i in range(ntiles):
            if i + 1 < ntiles:
                stage_a(i + 1)
            gi = idx_gen(i + 1) if i + 1 < ntiles else iter(())
            gt = t1_gen(i)
            nloop = max(1, 2 * i)
            t_every = max(1, nloop // 16)
            cnt = [0]

            def tick(gi=gi, gt=gt, cnt=cnt, t_every=t_every):
                next(gi, None)
                cnt[0] += 1
                if cnt[0] % t_every == 0:
                    next(gt, None)

            def drain(gi=gi, gt=gt):
                run_all(gi)
                run_all(gt)
            if i >= 1:
                stage_b(i - 1, tick, drain)
            else:
                drain()
            if i >= 2:
                bg_issue(1)
        stage_b(ntiles - 1)
        bg_issue(1000)
        kb.barrier()
        ps_.close()

    src = dx
    for ph in phases:
        if ph[0] == "mod":
            phase_mod()
        elif ph[0] == "dsa":
            phase_dsa(ph[1], ph[1] // 2, src, *(ph[2:]))
            src = dxs
        elif ph[0] == "gmlp":
            phase_gmlp(ph[1], ph[1] // 2, src)
            src = dxs
        elif ph[0] == "ffn":
            phase_ffn(ph[1], src)
            src = dxs
        elif ph[0] == "final":
            phase_final(src)
        elif ph[0] == "dump":
            B_o = Buf()
            for i in range(NT):
                kb.dma("sp", dout[i * 128:(i + 1) * 128, :], dxs[i * 128:(i + 1) * 128, :], reads=[B_xs[i]], writes=[B_o])
        elif ph[0] == "dbgmod":
            ddbg = dram("dbg", [128, 4 * 48 + 64], kind="ExternalOutput")
            kb.dma("sp", ddbg[:, 0:192], modT[:].rearrange("p l c -> p (l c)"), reads=[B_modT])
            kb.dma("sp", ddbg[:, 192:224], amix[:].rearrange("p l c -> p (l c)"), reads=[B_modT])
            kb.dma("sp", ddbg[:, 224:256], affn[:].rearrange("p l c -> p (l c)"), reads=[B_modT])
    kb.barrier()
    es.close()
    return nc


FULL = [("mod",)]
for _l in range(4):
    FULL.append(("dsa" if _l % 2 == 0 else "gmlp", _l))
    FULL.append(("ffn", _l))
FULL.append(("final",))


def _core_inputs(inp, b):
    f = np.float32
    c = np.ascontiguousarray
    m = {
        "x": c(inp["x"][b], dtype=f),
        "cT": c(np.asarray(inp["c"][b], dtype=f).reshape(8, 128).T),
        "mod_w": c(inp["mod_w"], dtype=f),
        "mod_b": c(inp["mod_b"], dtype=f),
        "gmixT": c(np.asarray(inp["norm_mix_g"], dtype=f).reshape(4, 8, 128).transpose(2, 0, 1).reshape(128, 32)),
        "gffnT": c(np.asarray(inp["norm_ffn_g"], dtype=f).reshape(4, 8, 128).transpose(2, 0, 1).reshape(128, 32)),
        "final_g": c(np.asarray(inp["final_g"], dtype=f).reshape(1, D)),
        "a_w_in": c(inp["a_w_in"], dtype=f),
        "a_g_kv": c(inp["a_g_kv"], dtype=f),
        "a_g_kidx": c(inp["a_g_kidx"], dtype=f),
        "a_b_kidx": c(inp["a_b_kidx"], dtype=f),
        "a_w_uk": c(inp["a_w_uk"], dtype=f),
        "a_w_uv": c(inp["a_w_uv"], dtype=f),
        "a_w_o": c(inp["a_w_o"], dtype=f),
        "b_w_in": c(inp["b_w_in"], dtype=f),
        "b_ln_g": c(inp["b_ln_g"], dtype=f),
        "b_ln_b": c(inp["b_ln_b"], dtype=f),
        "b_w_sT": c(np.asarray(inp["b_w_s"], dtype=f).transpose(0, 3, 1, 2).reshape(2, 128, 1024)),
        "b_b_s": c(np.asarray(inp["b_b_s"], dtype=f).reshape(2, 1024)),
        "b_w_out": c(inp["b_w_out"], dtype=f),
        "ffn_w_gate": c(inp["ffn_w_gate"], dtype=f),
        "ffn_w_up": c(inp["ffn_w_up"], dtype=f),
        "ffn_w_down": c(inp["ffn_w_down"], dtype=f),
    }
    return m


def kernel(**inputs):
    inp = {k: np.asarray(v) for k, v in inputs.items()}
    nc = _build(FULL)
    in_maps = [_core_inputs(inp, b) for b in range(8)]
    res = run_bass_kernel_spmd(nc, in_maps, core_ids=list(range(8)))
    out = np.stack([np.asarray(r["out"], dtype=np.float32) for r in res.results], axis=0)
    return out
```

```python
import numpy as np
from contextlib import ExitStack
import concourse.bass as bass
import concourse.mybir as mybir
from concourse.bass_utils import run_bass_kernel_spmd

F32 = mybir.dt.float32
BF16 = mybir.dt.bfloat16
ALU = mybir.AluOpType
AF = mybir.ActivationFunctionType
AX = mybir.AxisListType

T = 4096
D = 1024
NT = 32
DFF = 2816
NFC = 22
A_IN = 1864
TOPK = 256
NIT = 13
EPS = 1e-6
NEG = -1.0e30


class Buf:
    __slots__ = ("w", "r")

    def __init__(self):
        self.w = None
        self.r = {}


class KB:
    def __init__(self, nc, es):
        self.nc = nc
        self.es = es
        self.eng = dict(pe=nc.tensor, dve=nc.vector, act=nc.scalar, pool=nc.gpsimd, sp=nc.sync)
        self.sems = {}
        self.cnt = {}
        self.seen = {e: {} for e in self.eng}
        for e in self.eng:
            self.sems[e] = es.enter_context(nc.semaphore("s_" + e))
            self.cnt[e] = 0
        self.dq = {}
        self.dqi = {}
        for q, n in (("sp", 20), ("pool", 10), ("bg", 3)):
            self.dq[q] = []
            self.dqi[q] = 0
            for i in range(n):
                k = "d_%s%d" % (q, i)
                self.sems[k] = es.enter_context(nc.semaphore(k))
                self.cnt[k] = 0
                self.dq[q].append(k)
        self.nbank = 0

    def _wait(self, e, key, val):
        if val <= 0 or self.seen[e].get(key, 0) >= val:
            return
        self.eng[e].wait_ge(self.sems[key], val)
        self.seen[e][key] = val

    def _deps(self, e, reads, writes, strict=False):
        for b in reads:
            if b.w is not None:
                k, v = b.w
                if strict or k != e or e != "pe":
                    self._wait(e, k, v)
        for b in writes:
            if b.w is not None:
                k, v = b.w
                if strict or k != e:
                    self._wait(e, k, v)
            for k, v in b.r.items():
                if strict or k != e:
                    self._wait(e, k, v)

    def op(self, e, fn, reads=(), writes=()):
        self._deps(e, reads, writes)
        inst = fn(self.eng[e])
        self.cnt[e] += 1
        inst.then_inc(self.sems[e], 1)
        v = self.cnt[e]
        for b in reads:
            b.r[e] = v
        for b in writes:
            b.w = (e, v)
            b.r = {}

    def dma(self, q, out, in_, reads=(), writes=(), sems=None):
        pool = self.dq[sems or q]
        key = pool[self.dqi[sems or q] % len(pool)]
        self.dqi[sems or q] += 1
        self._wait(q, key, self.cnt[key])
        self._deps(q, reads, writes, strict=True)
        inst = self.eng[q].dma_start(out=out, in_=in_)
        self.cnt[key] += 16
        inst.then_inc(self.sems[key], 16)
        v = self.cnt[key]
        for b in reads:
            b.r[key] = v
        for b in writes:
            b.w = (key, v)
            b.r = {}

    def barrier(self):
        keys = list(self.sems.keys())
        for e in self.eng:
            for k in keys:
                if k != e:
                    self._wait(e, k, self.cnt[k])


def _build(phases):
    nc = bass.Bass("TRN2", target_bir_lowering=False)
    es = ExitStack()
    kb = KB(nc, es)

    def dram(name, shape, kind="ExternalInput", dt=F32):
        return nc.dram_tensor(name, list(shape), dt, kind=kind).ap()

    dx = dram("x", [T, D])
    dout = dram("out", [T, D], kind="ExternalOutput")
    dxs = dram("xs", [T, D], kind="Internal")
    dgs = dram("gsc", [8, 128, D], kind="Internal")
    d_cT = dram("cT", [128, 8])
    d_modw = dram("mod_w", [4, D, 6 * D])
    d_modb = dram("mod_b", [4, 6 * D])
    d_gmix = dram("gmixT", [128, 32])
    d_gffn = dram("gffnT", [128, 32])
    d_fg = dram("final_g", [1, D])
    d_awin = dram("a_w_in", [2, D, A_IN])
    d_agkv = dram("a_g_kv", [2, 256])
    d_agki = dram("a_g_kidx", [2, 64])
    d_abki = dram("a_b_kidx", [2, 64])
    d_awuk = dram("a_w_uk", [2, 8, 128, 256])
    d_awuv = dram("a_w_uv", [2, 8, 256, 128])
    d_awo = dram("a_w_o", [2, D, D])
    d_bwin = dram("b_w_in", [2, D, 4096])
    d_blng = dram("b_ln_g", [2, 2048])
    d_blnb = dram("b_ln_b", [2, 2048])
    d_bwsT = dram("b_w_sT", [2, 128, 8 * 128])
    d_bbs = dram("b_b_s", [2, 8 * 128])
    d_bwout = dram("b_w_out", [2, 2048, D])
    d_fwg = dram("ffn_w_gate", [4, D, DFF])
    d_fwu = dram("ffn_w_up", [4, D, DFF])
    d_fwd = dram("ffn_w_down", [4, DFF, D])

    uid = [0]

    w16 = {}
    B_w16 = {}

    bg_pending = []

    def bg_cast(name, src_ap, shape):
        t = dram("w16_" + name, shape, kind="Internal", dt=BF16)
        w16[name] = t
        B_w16[name] = Buf()
        bg_pending.append(lambda: kb.dma("pool", t, src_ap, writes=[B_w16[name]], sems="bg"))

    def bg_issue(n=1):
        for _ in range(n):
            if bg_pending:
                bg_pending.pop(0)()

    def bg_cast_all():
        for l_ in range(4):
            bg_cast("fwg%d" % l_, d_fwg[l_], [D, DFF])
            bg_cast("fwu%d" % l_, d_fwu[l_], [D, DFF])
            bg_cast("fwd%d" % l_, d_fwd[l_], [DFF, D])
            if l_ == 0:
                bg_cast("bwin0", d_bwin[0], [D, 4096])
                bg_cast("bwout0", d_bwout[0], [2048, D])
            if l_ == 1:
                bg_cast("awin1", d_awin[1], [D, A_IN])
                bg_cast("awuk1", d_awuk[1].rearrange("h d r -> (h d) r"), [8 * 128, 256])
                bg_cast("awuv1", d_awuv[1].rearrange("h r d -> (h r) d"), [8 * 256, 128])
            if l_ == 2:
                bg_cast("bwin1", d_bwin[1], [D, 4096])
                bg_cast("bwout1", d_bwout[1], [2048, D])

    def sb(stack, name, shape, dt):
        uid[0] += 1
        return stack.enter_context(nc.sbuf_tensor("%s_%d" % (name, uid[0]), list(shape), dt))

    ident_f = sb(es, "ident_f", [128, 128], F32)
    ident_b = sb(es, "ident_b", [128, 128], BF16)
    mhalf = sb(es, "mhalf", [128, 1], F32)
    modT = sb(es, "modT", [128, 4, 48], F32)
    amix = sb(es, "amix", [128, 4, 8], F32)
    affn = sb(es, "affn", [128, 4, 8], F32)
    gmixT = sb(es, "gmixT_s", [128, 32], F32)
    gffnT = sb(es, "gffnT_s", [128, 32], F32)
    B_const = Buf()
    B_modT = Buf()
    banks = [es.enter_context(nc.psum_tensor("ps%d" % i, [128, 512], F32)) for i in range(8)]
    bankb = [Buf() for _ in range(8)]

    nb2 = [0]

    def pbank():
        i = kb.nbank % 5
        kb.nbank += 1
        return banks[i], bankb[i]

    def pbank2():
        i = 4 + nb2[0] % 2
        nb2[0] += 1
        return banks[i], bankb[i]

    TB = banks[7][:].bitcast(BF16)
    TBb = bankb[7]

    kb.op("pool", lambda e: e.memset(ident_f[:], 1.0), writes=[B_const])
    kb.op("pool", lambda e: e.memset(mhalf[:], -0.5), writes=[B_const])
    kb.op("pool", lambda e: e.affine_select(out=ident_f[:], in_=ident_f[:], pattern=[[-1, 128]],
                                            compare_op=ALU.is_equal, fill=0.0, base=0, channel_multiplier=1),
          reads=[B_const], writes=[B_const])
    kb.op("dve", lambda e: e.tensor_copy(out=ident_b[:], in_=ident_f[:]), reads=[B_const], writes=[B_const])

    def rstd_from_ss(stk_rs, ss, B_ss, n, B_rs):
        kb.op("pool", lambda e: e.tensor_scalar(out=stk_rs, in0=ss, scalar1=float(n) * EPS, scalar2=None,
                                                op0=ALU.add), reads=[B_ss], writes=[B_rs])
        kb.op("pool", lambda e: e.tensor_tensor(out=stk_rs, in0=stk_rs, in1=mhalf[:], op=ALU.pow),
              reads=[B_rs, B_const], writes=[B_rs])

    def phase_mod():
        ps_ = ExitStack()
        cT = sb(ps_, "cT_s", [128, 8], F32)
        cact = sb(ps_, "cact", [128, 8], F32)
        crep = sb(ps_, "crep", [128, 8, 128], F32)
        ones1 = sb(ps_, "ones1", [1, 128], F32)
        mb = sb(ps_, "mb", [1, 6 * D], F32)
        mw = [sb(ps_, "mw%d" % i, [128, 3072], F32) for i in range(2)]
        mwb = [Buf(), Buf()]
        mh = sb(ps_, "mh", [128, 3072], F32)
        dtmp = sb(ps_, "dtmp", [128, 24, 128], F32)
        B_c, B_mb, B_mh, B_dt = Buf(), Buf(), Buf(), Buf()
        kb.dma("sp", cT[:], d_cT[:, :], writes=[B_c])
        kb.dma("sp", gmixT[:], d_gmix[:, :], writes=[B_modT])
        kb.dma("sp", gffnT[:], d_gffn[:, :], writes=[B_modT])
        kb.op("act", lambda e: e.activation(out=cact[:], in_=cT[:], func=AF.Silu), reads=[B_c], writes=[B_c])
        kb.op("pool", lambda e: e.memset(ones1[:], 1.0), writes=[B_c])
        for kc in range(8):
            kb.op("dve", lambda e, kc=kc: e.tensor_copy(out=crep[:, kc, :],
                                                        in_=cact[:, kc:kc + 1].to_broadcast([128, 128])),
                  reads=[B_c], writes=[B_c])
        li = 0
        for l in range(4):
            kb.dma("sp", mb[:], d_modb[l:l + 1, :], writes=[B_mb])
            for half in range(2):
                acc = [(banks[b_], bankb[b_]) for b_ in range(6)]
                for kc in range(8):
                    t_, tb = mw[li % 2], mwb[li % 2]
                    li += 1
                    kb.dma("sp", t_[:], d_modw[l, kc * 128:(kc + 1) * 128, half * 3072:(half + 1) * 3072],
                           writes=[tb])
                    for blk in range(6):
                        pt, pb = acc[blk]
                        if kc == 0:
                            kb.op("pe", lambda e, pt=pt, blk=blk, half=half: e.matmul(
                                pt[:], lhsT=ones1[0:1, :], rhs=mb[0:1, half * 3072 + blk * 512: half * 3072 + (blk + 1) * 512],
                                start=True, stop=False), reads=[B_c, B_mb], writes=[pb])
                        kb.op("pe", lambda e, pt=pt, blk=blk, kc=kc, t_=t_: e.matmul(
                            pt[:], lhsT=crep[:, kc, :], rhs=t_[:, blk * 512:(blk + 1) * 512],
                            start=False, stop=(kc == 7)), reads=[B_c, tb], writes=[pb])
                for blk in range(6):
                    pt, pb = acc[blk]
                    kb.op("dve" if blk % 2 == 0 else "act",
                          (lambda e, pt=pt, blk=blk: e.tensor_copy(out=mh[:, blk * 512:(blk + 1) * 512], in_=pt[:]))
                          if blk % 2 == 0 else
                          (lambda e, pt=pt, blk=blk: e.activation(out=mh[:, blk * 512:(blk + 1) * 512], in_=pt[:],
                                                                  func=AF.Identity)),
                          reads=[pb], writes=[B_mh])
                kb.dma("sp", dgs[l * 2 + half, :, :], mh[:, 2048:3072], reads=[B_mh], writes=[B_gs[l * 2 + half]])
                kb.op("dve", lambda e: e.tensor_tensor(
                    out=dtmp[:], in0=mh[:].rearrange("p (c j) -> p c j", j=128),
                    in1=ident_f[:].unsqueeze(1).to_broadcast([128, 24, 128]), op=ALU.mult),
                    reads=[B_mh, B_const], writes=[B_dt])
                kb.op("dve", lambda e, l=l, half=half: e.tensor_reduce(
                    out=modT[:, l, half * 24:(half + 1) * 24], in_=dtmp[:], axis=AX.X, op=ALU.add),
                    reads=[B_dt], writes=[B_modT])
            kb.op("dve", lambda e, l=l: e.scalar_tensor_tensor(
                out=amix[:, l, :], in0=modT[:, l, 8:16], scalar=1.0, in1=gmixT[:, l * 8:(l + 1) * 8],
                op0=ALU.add, op1=ALU.mult), reads=[B_modT], writes=[B_modT])
            kb.op("dve", lambda e, l=l: e.scalar_tensor_tensor(
                out=affn[:, l, :], in0=modT[:, l, 32:40], scalar=1.0, in1=gffnT[:, l * 8:(l + 1) * 8],
                op0=ALU.add, op1=ALU.mult), reads=[B_modT], writes=[B_modT])
        kb.barrier()
        ps_.close()

    B_gs = [Buf() for _ in range(8)]
    B_xs = [Buf() for _ in range(NT)]

    def load_x(src, i, xt, xb):
        kb.dma("sp", xt[:], src[i * 128:(i + 1) * 128, :], reads=[B_xs[i]] if src is dxs else [], writes=[xb])

    def norm_tile(xt, xb, junk, B_junk, ss, rs, B_st, xsb, B_xsb):
        kb.op("act", lambda e: e.activation(out=junk, in_=xt[:], func=AF.Square, accum_out=ss),
              reads=[xb], writes=[B_junk, B_st])
        rstd_from_ss(rs, ss, B_st, float(D), B_st)
        kb.op("dve", lambda e: e.tensor_scalar(out=xsb, in0=xt[:], scalar1=rs, scalar2=32.0, op0=ALU.mult,
                                               op1=ALU.mult), reads=[xb, B_st], writes=[B_xsb])

    def make_hT(xsb_t, B_xsb, hT_dst, B_hT, A, S):
        for kc in range(8):
            kb.op("pe", lambda e, kc=kc: e.transpose(out=TB[:, kc * 128:(kc + 1) * 128],
                                                     in_=xsb_t[:, kc * 128:(kc + 1) * 128], identity=ident_b[:]),
                  reads=[B_xsb, B_const], writes=[TBb])
        for kc in range(8):
            kb.op("act", lambda e, kc=kc: e.activation(out=hT_dst(kc), in_=TB[:, kc * 128:(kc + 1) * 128],
                                                       func=AF.Identity, scale=A[:, kc:kc + 1], bias=S[:, kc:kc + 1]),
                  reads=[TBb, B_modT], writes=[B_hT])

    def residual_store(ypairs, xt, xb, gb, B_gb, tmps, dst, dstb):
        for dh, (pt, pb) in enumerate(ypairs):
            tmp, tb = tmps[dh]
            kb.op("dve", lambda e, pt=pt, dh=dh, tmp=tmp: e.tensor_tensor(
                out=tmp[:], in0=pt[:], in1=gb[:, dh * 512:(dh + 1) * 512], op=ALU.mult),
                reads=[pb, B_gb], writes=[tb])
            kb.op("pool", lambda e, dh=dh, tmp=tmp: e.tensor_tensor(
                out=xt[:, dh * 512:(dh + 1) * 512], in0=xt[:, dh * 512:(dh + 1) * 512], in1=tmp[:], op=ALU.add),
                reads=[tb, xb], writes=[xb])
        kb.dma("sp", dst, xt[:], reads=[xb], writes=[dstb])

    def phase_ffn(l, src):
        ps_ = ExitStack()
        wg = sb(ps_, "wg", [128, 8, DFF], BF16)
        wu = sb(ps_, "wu", [128, 8, DFF], BF16)
        wd = sb(ps_, "wd", [128, NFC, D], BF16)
        gb = sb(ps_, "gb", [128, D], F32)
        xts = [sb(ps_, "xt%d" % i, [128, D], F32) for i in range(4)]
        xbs = [Buf() for _ in range(4)]
        junk = sb(ps_, "junk", [128, D], BF16)
        st = sb(ps_, "st", [128, 4], F32)
        xsb = [sb(ps_, "xsb%d" % i, [128, D], BF16) for i in range(2)]
        hT = [sb(ps_, "hT%d" % i, [128, 8, 256], BF16) for i in range(2)]
        actT = sb(ps_, "actT", [128, NFC, 256], BF16)
        sg = [sb(ps_, "sg%d" % i, [128, 256], F32) for i in range(2)]
        tmps = [(sb(ps_, "tmp%d" % i, [128, 512], F32), Buf()) for i in range(2)]
        B_w, B_gb, B_junk = Buf(), Buf(), Buf()
        B_st = [Buf(), Buf()]
        B_xsb = [Buf(), Buf()]
        B_hT = [Buf(), Buf()]
        B_sg = [Buf(), Buf()]
        B_act = [Buf() for _ in range(NFC)]
        B_wg = [Buf() for _ in range(8)]
        B_wu = [Buf() for _ in range(8)]
        B_wd = [Buf() for _ in range(NFC)]
        for kc in range(8):
            kb.dma("sp", wg[:, kc, :], w16["fwg%d" % l][kc * 128:(kc + 1) * 128, :], reads=[B_w16["fwg%d" % l]],
                   writes=[B_wg[kc]])
        for kc in range(8):
            kb.dma("sp", wu[:, kc, :], w16["fwu%d" % l][kc * 128:(kc + 1) * 128, :], reads=[B_w16["fwu%d" % l]],
                   writes=[B_wu[kc]])
        for fc in range(NFC):
            kb.dma("sp", wd[:, fc, :], w16["fwd%d" % l][fc * 128:(fc + 1) * 128, :], reads=[B_w16["fwd%d" % l]],
                   writes=[B_wd[fc]])
        kb.dma("sp", gb[:], dgs[l * 2 + 1, :, :], reads=[B_gs[l * 2 + 1]], writes=[B_gb])
        A = affn[:, l, :]
        S = modT[:, l, 24:32]
        NG = NT // 2

        def prep_a(g):
            for tl in range(2):
                i = g * 2 + tl
                load_x(src, i, xts[i % 4], xbs[i % 4])

        def prep_b(g):
            for tl in range(2):
                i = g * 2 + tl
                k = i % 2
                norm_tile(xts[i % 4], xbs[i % 4], junk[:], B_junk, st[:, 2 * k:2 * k + 1], st[:, 2 * k + 1:2 * k + 2],
                          B_st[k], xsb[k][:], B_xsb[k])
                make_hT(xsb[k], B_xsb[k], lambda kc, tl=tl, g=g: hT[g % 2][:, kc, tl * 128:(tl + 1) * 128],
                        B_hT[g % 2], A, S)

        prep_a(0)
        prep_b(0)
        for g in range(NG):
            h = hT[g % 2]
            hb = B_hT[g % 2]
            if g + 1 < NG:
                prep_a(g + 1)
            for fc in range(NFC):
                pt, pb = pbank()
                for slot, w in ((0, wg), (1, wu)):
                    for kc in range(8):
                        kb.op("pe", lambda e, pt=pt, slot=slot, w=w, kc=kc, fc=fc: e.matmul(
                            pt[:, slot * 256:(slot + 1) * 256], lhsT=w[:, kc, fc * 128:(fc + 1) * 128],
                            rhs=h[:, kc, :], start=(kc == 0), stop=(kc == 7)),
                            reads=[(B_wg if slot == 0 else B_wu)[kc], hb], writes=[pb])
                s_ = sg[fc % 2]
                sbf = B_sg[fc % 2]
                kb.op("act", lambda e, pt=pt, s_=s_: e.activation(out=s_[:], in_=pt[:, 0:256], func=AF.Silu),
                      reads=[pb], writes=[sbf])
                kb.op("dve", lambda e, pt=pt, s_=s_, fc=fc: e.tensor_tensor(
                    out=actT[:, fc, :], in0=pt[:, 256:512], in1=s_[:], op=ALU.mult),
                    reads=[pb, sbf], writes=[B_act[fc]])
                if fc == 6 and g + 1 < NG:
                    prep_b(g + 1)
            for tl in range(2):
                i = g * 2 + tl
                ys = []
                for dh in range(2):
                    pt, pb = pbank()
                    for fc in range(NFC):
                        kb.op("pe", lambda e, pt=pt, fc=fc, tl=tl, dh=dh: e.matmul(
                            pt[:], lhsT=actT[:, fc, tl * 128:(tl + 1) * 128], rhs=wd[:, fc, dh * 512:(dh + 1) * 512],
                            start=(fc == 0), stop=(fc == NFC - 1)), reads=[B_wd[fc], B_act[fc]], writes=[pb])
                    ys.append((pt, pb))
                residual_store(ys, xts[i % 4], xbs[i % 4], gb, B_gb, tmps, dxs[i * 128:(i + 1) * 128, :], B_xs[i])
        kb.barrier()
        ps_.close()

    def phase_final(src):
        ps_ = ExitStack()
        fg = sb(ps_, "fg", [128, D], F32)
        xts = [sb(ps_, "fxt%d" % i, [128, D], F32) for i in range(3)]
        xbs = [Buf() for _ in range(3)]
        ots = [sb(ps_, "fot%d" % i, [128, D], F32) for i in range(2)]
        obs = [Buf() for _ in range(2)]
        junk = sb(ps_, "fjunk", [128, D], BF16)
        st = sb(ps_, "fst", [128, 4], F32)
        B_st = [Buf(), Buf()]
        B_fg, B_junk, B_o = Buf(), Buf(), Buf()
        kb.dma("sp", fg[:], d_fg[0:1, :].to_broadcast([128, D]), writes=[B_fg])
        kb.op("dve", lambda e: e.tensor_scalar(out=fg[:], in0=fg[:], scalar1=32.0, scalar2=None, op0=ALU.mult),
              reads=[B_fg], writes=[B_fg])
        for i in range(NT):
            k = i % 2
            xt, xb = xts[i % 3], xbs[i % 3]
            load_x(src, i, xt, xb)
            ss, rs = st[:, 2 * k:2 * k + 1], st[:, 2 * k + 1:2 * k + 2]
            kb.op("act", lambda e, xt=xt, ss=ss: e.activation(out=junk[:], in_=xt[:], func=AF.Square, accum_out=ss),
                  reads=[xb], writes=[B_junk, B_st[k]])
            rstd_from_ss(rs, ss, B_st[k], float(D), B_st[k])
            kb.op("dve", lambda e, xt=xt, rs=rs, k=k: e.scalar_tensor_tensor(
                out=ots[k][:], in0=xt[:], scalar=rs, in1=fg[:], op0=ALU.mult, op1=ALU.mult),
                reads=[xb, B_st[k], B_fg], writes=[obs[k]])
            kb.dma("sp", dout[i * 128:(i + 1) * 128, :], ots[k][:], reads=[obs[k]], writes=[B_o])
        kb.barrier()
        ps_.close()

    def phase_gmlp(l, j, src):
        ps_ = ExitStack()
        win = sb(ps_, "bwin", [128, 8, 4096], BF16)
        wout = sb(ps_, "bwout", [128, 16, D], BF16)
        wcf = sb(ps_, "wcf", [128, 8, 128], F32)
        wcT = sb(ps_, "wcT", [128, 8, 128], BF16)
        lng = sb(ps_, "lng", [128, 2048], F32)
        lnb = sb(ps_, "lnb", [128, 2048], F32)
        bsb = sb(ps_, "bsb", [128, 8, 128], F32)
        gb = sb(ps_, "ggb", [128, D], F32)
        xts = [sb(ps_, "gxt%d" % i, [128, D], F32) for i in range(4)]
        xbs = [Buf() for _ in range(4)]
        junk = sb(ps_, "gjunk", [128, D], BF16)
        st = sb(ps_, "gst", [128, 4], F32)
        xsb = [sb(ps_, "gxsb%d" % i, [128, D], BF16) for i in range(2)]
        hT = [sb(ps_, "ghT%d" % i, [128, 8, 256], BF16) for i in range(2)]
        uT = sb(ps_, "uT", [128, 16, 256], F32)
        vr = [sb(ps_, "vr0", [128, 2048], F32)] * 2
        vt = [sb(ps_, "vt%d" % i, [128, 2048], BF16) for i in range(2)]
        bst = sb(ps_, "bst", [128, 2, 4, 6], F32)
        lst = sb(ps_, "lst", [128, 2, 4], F32)
        ypT = sb(ps_, "ypT", [128, 16, 256], BF16)
        ytm = [sb(ps_, "ytm%d" % i, [128, 512], F32) for i in range(2)]
        tmps = [(sb(ps_, "gtmp%d" % i, [128, 512], F32), Buf()) for i in range(2)]
        B_w, B_gb, B_junk, B_k = Buf(), Buf(), Buf(), Buf()
        B_st = [Buf(), Buf()]
        B_xsb = [Buf(), Buf()]
        B_hT = [Buf(), Buf()]
        B_u = [Buf() for _ in range(16)]
        B_vr = [Buf()] * 2
        B_vt = [Buf(), Buf()]
        B_ls = [Buf(), Buf()]
        B_yp = [[Buf() for _ in range(4)] for _ in range(2)]
        B_ytm = [Buf(), Buf()]
        for kc in range(8):
            kb.dma("sp", win[:, kc, :], w16["bwin%d" % j][kc * 128:(kc + 1) * 128, :], reads=[B_w16["bwin%d" % j]],
                   writes=[B_w])
        for ec in range(16):
            kb.dma("sp", wout[:, ec, :], w16["bwout%d" % j][ec * 128:(ec + 1) * 128, :], reads=[B_w16["bwout%d" % j]],
                   writes=[B_w])
        kb.dma("sp", wcf[:], d_bwsT[j, :, :].rearrange("s (g t) -> s g t", g=8), writes=[B_k])
        kb.dma("sp", lng[:], d_blng[j:j + 1, :].to_broadcast([128, 2048]), writes=[B_k])
        kb.dma("sp", lnb[:], d_blnb[j:j + 1, :].to_broadcast([128, 2048]), writes=[B_k])
        kb.dma("sp", bsb[:], d_bbs[j:j + 1, :].to_broadcast([128, 1024]).rearrange("p (g t) -> p g t", g=8),
               writes=[B_k])
        kb.dma("sp", gb[:], dgs[l * 2, :, :], reads=[B_gs[l * 2]], writes=[B_gb])
        kb.op("pool", lambda e: e.affine_select(out=wcf[:], in_=wcf[:], pattern=[[0, 8], [1, 128]],
                                                compare_op=ALU.is_ge, fill=0.0, base=0, channel_multiplier=-1),
              reads=[B_k], writes=[B_k])
        kb.op("dve", lambda e: e.tensor_copy(out=wcT[:], in_=wcf[:]), reads=[B_k], writes=[B_k])
        A = amix[:, l, :]
        S = modT[:, l, 0:8]
        NG = NT // 2

        def prep_a(g):
            for tl in range(2):
                i = g * 2 + tl
                load_x(src, i, xts[i % 4], xbs[i % 4])

        def prep_b(g):
            for tl in range(2):
                i = g * 2 + tl
                k = i % 2
                norm_tile(xts[i % 4], xbs[i % 4], junk[:], B_junk, st[:, 2 * k:2 * k + 1], st[:, 2 * k + 1:2 * k + 2],
                          B_st[k], xsb[k][:], B_xsb[k])
                make_hT(xsb[k], B_xsb[k], lambda kc, tl=tl, g=g: hT[g % 2][:, kc, tl * 128:(tl + 1) * 128],
                        B_hT[g % 2], A, S)

        prep_a(0)
        prep_b(0)
        for g in range(NG):
            h = hT[g % 2]
            hb = B_hT[g % 2]
            if g + 1 < NG:
                prep_a(g + 1)
            for tl in range(2):
                for cb in range(4):
                    pt, pb = pbank()
                    for kc in range(8):
                        kb.op("pe", lambda e, pt=pt, kc=kc, cb=cb, tl=tl: e.matmul(
                            pt[:], lhsT=h[:, kc, tl * 128:(tl + 1) * 128],
                            rhs=win[:, kc, 2048 + cb * 512:2048 + (cb + 1) * 512],
                            start=(kc == 0), stop=(kc == 7)), reads=[B_w, hb], writes=[pb])
                    kb.op("act", lambda e, pt=pt, cb=cb, tl=tl: e.activation(
                        out=vr[tl][:, cb * 512:(cb + 1) * 512], in_=pt[:], func=AF.Gelu),
                        reads=[pb], writes=[B_vr[tl]])
                    kb.op("dve", lambda e, cb=cb, tl=tl: e.bn_stats(out=bst[:, tl, cb, :],
                                                                    in_=vr[tl][:, cb * 512:(cb + 1) * 512]),
                          reads=[B_vr[tl]], writes=[B_ls[tl]])
                kb.op("dve", lambda e, tl=tl: e.bn_aggr(out=lst[:, tl, 0:2],
                                                        in_=bst[:, tl, :, :].rearrange("p a b -> p (a b)")),
                      reads=[B_ls[tl]], writes=[B_ls[tl]])
                kb.op("pool", lambda e, tl=tl: e.tensor_scalar(out=lst[:, tl, 2:3], in0=lst[:, tl, 1:2], scalar1=EPS,
                                                               scalar2=None, op0=ALU.add),
                      reads=[B_ls[tl]], writes=[B_ls[tl]])
                kb.op("pool", lambda e, tl=tl: e.tensor_tensor(out=lst[:, tl, 2:3], in0=lst[:, tl, 2:3], in1=mhalf[:],
                                                               op=ALU.pow),
                      reads=[B_ls[tl], B_const], writes=[B_ls[tl]])
                kb.op("dve", lambda e, tl=tl: e.tensor_scalar(out=vr[tl][:], in0=vr[tl][:], scalar1=lst[:, tl, 0:1],
                                                              scalar2=lst[:, tl, 2:3], op0=ALU.subtract, op1=ALU.mult),
                      reads=[B_ls[tl], B_vr[tl]], writes=[B_vr[tl]])
                kb.op("pool", lambda e, tl=tl: e.tensor_tensor(out=vr[tl][:], in0=vr[tl][:], in1=lng[:], op=ALU.mult),
                      reads=[B_vr[tl], B_k], writes=[B_vr[tl]])
                kb.op("pool", lambda e, tl=tl: e.tensor_tensor(out=vt[tl][:], in0=vr[tl][:], in1=lnb[:], op=ALU.add),
                      reads=[B_vr[tl], B_k], writes=[B_vt[tl]])
            for ec in range(16):
                if ec % 2 == 0:
                    pt, pb = pbank()
                half = ec % 2
                for kc in range(8):
                    kb.op("pe", lambda e, pt=pt, half=half, kc=kc, ec=ec: e.matmul(
                        pt[:, half * 256:(half + 1) * 256], lhsT=win[:, kc, ec * 128:(ec + 1) * 128],
                        rhs=h[:, kc, :], start=(kc == 0), stop=(kc == 7)), reads=[B_w, hb], writes=[pb])
                if ec % 2 == 1:
                    kb.op("act", lambda e, pt=pt, ec=ec: e.activation(
                        out=uT[:, ec - 1:ec + 1, :].rearrange("p a t -> p (a t)"), in_=pt[:], func=AF.Gelu),
                        reads=[pb], writes=[B_u[ec - 1], B_u[ec]])
            if g + 1 < NG:
                prep_b(g + 1)
            for tl in range(2):
                i = g * 2 + tl
                for q4 in range(4):
                    pt, pb = pbank()
                    for a in range(4):
                        ec = q4 * 4 + a
                        kb.op("pe", lambda e, pt=pt, a=a, ec=ec, tl=tl: e.matmul(
                            pt[:, a * 128:(a + 1) * 128], lhsT=vt[tl][:, ec * 128:(ec + 1) * 128],
                            rhs=wcT[:, ec // 2, :], start=True, stop=True), reads=[B_vt[tl], B_k], writes=[pb])
                    ym, ymb = ytm[q4 % 2], B_ytm[q4 % 2]
                    kb.op("dve", lambda e, pt=pt, q4=q4, ym=ym: e.tensor_tensor(
                        out=ym[:].rearrange("p (g r t) -> p g r t", g=2, r=2),
                        in0=pt[:].rearrange("p (g r t) -> p g r t", g=2, r=2),
                        in1=bsb[:, q4 * 2:q4 * 2 + 2, :].unsqueeze(2).to_broadcast([128, 2, 2, 128]), op=ALU.add),
                        reads=[pb, B_k], writes=[ymb])
                    kb.op("dve", lambda e, q4=q4, tl=tl, ym=ym: e.tensor_tensor(
                        out=ypT[:, q4 * 4:(q4 + 1) * 4, tl * 128:(tl + 1) * 128],
                        in0=ym[:].rearrange("p (a t) -> p a t", a=4),
                        in1=uT[:, q4 * 4:(q4 + 1) * 4, tl * 128:(tl + 1) * 128], op=ALU.mult),
                        reads=[ymb] + [B_u[q4 * 4 + a] for a in range(4)], writes=[B_yp[tl][q4]])
                ys = []
                for dh in range(2):
                    pt, pb = pbank()
                    for ec in range(16):
                        kb.op("pe", lambda e, pt=pt, ec=ec, tl=tl, dh=dh: e.matmul(
                            pt[:], lhsT=ypT[:, ec, tl * 128:(tl + 1) * 128], rhs=wout[:, ec, dh * 512:(dh + 1) * 512],
                            start=(ec == 0), stop=(ec == 15)), reads=[B_w, B_yp[tl][ec // 4]], writes=[pb])
                    ys.append((pt, pb))
                residual_store(ys, xts[i % 4], xbs[i % 4], gb, B_gb, tmps, dxs[i * 128:(i + 1) * 128, :], B_xs[i])
        kb.barrier()
        ps_.close()

    def phase_dsa(l, j, src, ntiles=NT):
        ps_ = ExitStack()
        win = sb(ps_, "awin", [128, 8, A_IN], BF16)
        wuk = sb(ps_, "awuk", [128, 8, 256], BF16)
        wuv = sb(ps_, "awuv", [128, 8, 2, 128], BF16)
        wo = sb(ps_, "awo", [128, 8, D], BF16)
        gkv = sb(ps_, "gkv", [128, 256], F32)
        gki = sb(ps_, "gki", [128, 64], F32)
        bki = sb(ps_, "bki", [128, 64], F32)
        cmask = sb(ps_, "cmask", [128, 128], F32)
        pw2 = sb(ps_, "pw2", [128, NIT + 1], F32)
        ckvT = sb(ps_, "ckvT", [128, 2, T], BF16)
        ckve = sb(ps_, "ckve", [128, NT, 258], BF16)
        kiT = [sb(ps_, "kiT%d" % i_, [128, T], BF16) for i_ in range(2)]
        xts = [sb(ps_, "axt%d" % i, [128, D], F32) for i in range(4)]
        xbs = [Buf() for _ in range(4)]
        st = sb(ps_, "ast", [128, 16], F32)
        xsb = sb(ps_, "axsb", [128, D], BF16)
        hT = sb(ps_, "ahT", [128, 8, 128], BF16)
        qT = sb(ps_, "qT", [128, 8, 128], BF16)
        qiT = sb(ps_, "qiT", [128, 4, 128], BF16)
        qlT = [sb(ps_, "qlT%d" % i, [128, 2, 1024], BF16) for i in range(3)]
        ctm = sb(ps_, "ctm", [128, 256], BF16)
        junk = ctm
        ktm = sb(ps_, "ktm", [128, 64], F32)
        kd = sb(ps_, "kd", [128, 2, 128], BF16)
        wi = sb(ps_, "wi", [128, 8], F32)
        dg = sb(ps_, "dg", [128, 8, 128], BF16)
        scores = [sb(ps_, "score%d" % i_, [128, T], F32) for i_ in range(2)]
        B_scs = [Buf(), Buf()]
        NR = 3
        Rh = [sb(ps_, "Rh%d" % i, [128, 512], BF16) for i in range(NR)]
        B_Rh = [Buf() for _ in range(NR)]
        nms = [sb(ps_, "nm%d" % i_, [128, T], BF16) for i_ in range(2)]
        B_nms = [Buf(), Buf()]
        ident4 = sb(ps_, "ident4", [128, 4, 128], BF16)
        thb = sb(ps_, "thb", [128, 8 + NIT + 8], F32)
        PT = [sb(ps_, "PT%d" % i, [128, 512], BF16) for i in range(2)]
        B_PT = [Buf() for _ in range(2)]
        ol = sb(ps_, "ol", [128, 8, 256], BF16)
        olT = sb(ps_, "olT", [128, 8, 128], BF16)
        oT = sb(ps_, "oT", [128, 8, 128], BF16)
        rden = sb(ps_, "rden", [128, 8], F32)
        B_w, B_k, B_gb, B_junk, B_st, B_xsb, B_hT = Buf(), Buf(), Buf(), Buf(), Buf(), Buf(), Buf()
        B_q, B_qi, B_ct, B_kt, B_kd, B_wi, B_dg, B_sc, B_nm, B_th = (Buf() for _ in range(10))
        B_ql = [Buf(), Buf(), Buf()]
        B_ckvT = [Buf() for _ in range(NT)]
        B_ckve = [Buf() for _ in range(NT)]
        B_kiT = [Buf() for _ in range(NT)]
        B_ol, B_olT, B_oT, B_rd = Buf(), Buf(), Buf(), Buf()
        if j == 0:
            for kc in range(8):
                kb.dma("pool", win[:, kc, :], d_awin[j, kc * 128:(kc + 1) * 128, :], writes=[B_w])
            kb.dma("pool", wuk[:], d_awuk[j].rearrange("h d r -> d h r"), writes=[B_w])
            kb.dma("pool", wuv[:], d_awuv[j].rearrange("h (c p) d -> p h c d", p=128), writes=[B_w])
            bg_cast_all()
        else:
            for kc in range(8):
                kb.dma("sp", win[:, kc, :], w16["awin1"][kc * 128:(kc + 1) * 128, :], reads=[B_w16["awin1"]],
                       writes=[B_w])
            kb.dma("sp", wuk[:], w16["awuk1"].rearrange("(h d) r -> d h r", h=8), reads=[B_w16["awuk1"]], writes=[B_w])
            kb.dma("sp", wuv[:], w16["awuv1"].rearrange("(h c p) d -> p h c d", h=8, p=128), reads=[B_w16["awuv1"]],
                   writes=[B_w])
        kb.dma("sp", gkv[:], d_agkv[j:j + 1, :].to_broadcast([128, 256]), writes=[B_k])
        kb.op("dve", lambda e: e.tensor_scalar(out=gkv[:], in0=gkv[:], scalar1=16.0, scalar2=None, op0=ALU.mult),
              reads=[B_k], writes=[B_k])
        kb.dma("sp", gki[:], d_agki[j:j + 1, :].to_broadcast([128, 64]), writes=[B_k])
        kb.dma("sp", bki[:], d_abki[j:j + 1, :].to_broadcast([128, 64]), writes=[B_k])
        kb.dma("sp", scores[1][:, 0:D], dgs[l * 2, :, :], reads=[B_gs[l * 2]], writes=[B_scs[1]])
        for kc in range(8):
            slot = scores[0][:, (kc % 4) * D:(kc % 4 + 1) * D]
            kb.dma("sp", slot, d_awo[j, kc * 128:(kc + 1) * 128, :], writes=[B_scs[0]])
            kb.op("dve", lambda e, kc=kc, slot=slot: e.tensor_tensor(out=wo[:, kc, :], in0=slot, in1=scores[1][:, 0:D],
                                                                     op=ALU.mult),
                  reads=[B_scs[0], B_scs[1]], writes=[B_w])
        kb.op("pool", lambda e: e.memset(cmask[:], 0.0), writes=[B_k])
        kb.op("pool", lambda e: e.memset(kd[:], 0.0), writes=[B_kd])
        kb.op("pool", lambda e: e.affine_select(out=cmask[:], in_=cmask[:], pattern=[[-1, 128]], compare_op=ALU.is_ge,
                                                fill=NEG, base=0, channel_multiplier=1), reads=[B_k], writes=[B_k])
        for k in range(NIT + 1):
            kb.op("pool", lambda e, k=k: e.memset(pw2[:, k:k + 1], 2.0 ** (-(k + 1))), writes=[B_k])
        kb.op("pool", lambda e: e.memset(ckve[:], 1.0), writes=B_ckve)
        kb.op("pool", lambda e: e.tensor_copy(out=ident4[:], in_=ident_b[:].unsqueeze(1).to_broadcast([128, 4, 128])),
              reads=[B_const], writes=[B_k])
        A = amix[:, l, :]
        S = modT[:, l, 0:8]
        SC_W = float(8 ** -0.5 * 64 ** -0.5)
        m8, lo_, w_, t_, c_, g_, thr = (thb[:, 0:8], thb[:, 8:9], thb[:, 9:10], thb[:, 10:11], thb[:, 11:12],
                                        thb[:, 12:13], thb[:, 13:14])
        wk = thb[:, 14:14 + NIT + 1]

        def pre_front(i):
            xt, xb = xts[i % 4], xbs[i % 4]
            load_x(src, i, xt, xb)
            norm_tile(xt, xb, xsb[:], B_xsb, st[:, 0:1], st[:, 1:2], B_st, xsb[:], B_xsb)

        def stage_a(i):
            xt, xb = xts[i % 4], xbs[i % 4]
            score, B_sc = scores[i % 2], B_scs[i % 2]
            make_hT(xsb, B_xsb, lambda kc: hT[:, kc, :], B_hT, A, S)
            for grp in range(3):
                pt, pb = pbank()
                for a in range(4):
                    ch = grp * 4 + a
                    c0 = ch * 128 if ch < 8 else 1280 + (ch - 8) * 128
                    for kc in range(8):
                        kb.op("pe", lambda e, pt=pt, a=a, c0=c0, kc=kc: e.matmul(
                            pt[:, a * 128:(a + 1) * 128], lhsT=win[:, kc, c0:c0 + 128], rhs=hT[:, kc, :],
                            start=(kc == 0), stop=(kc == 7)), reads=[B_w, B_hT], writes=[pb])
                if grp < 2:
                    kb.op("act", lambda e, pt=pt, grp=grp: e.activation(
                        out=qT[:, grp * 4:(grp + 1) * 4, :].rearrange("p a t -> p (a t)"), in_=pt[:], func=AF.Identity),
                        reads=[pb], writes=[B_q])
                else:
                    kb.op("act", lambda e, pt=pt: e.activation(
                        out=qiT[:].rearrange("p a t -> p (a t)"), in_=pt[:], func=AF.Identity),
                        reads=[pb], writes=[B_qi])
            pt, pb = pbank()
            for kc in range(8):
                kb.op("pe", lambda e, pt=pt, kc=kc: e.matmul(pt[:, 0:256], lhsT=hT[:, kc, :], rhs=win[:, kc, 1024:1280],
                                                             start=(kc == 0), stop=(kc == 7)),
                      reads=[B_w, B_hT], writes=[pb])
            for kc in range(8):
                kb.op("pe", lambda e, pt=pt, kc=kc: e.matmul(pt[:, 256:328], lhsT=hT[:, kc, :], rhs=win[:, kc, 1792:1864],
                                                             start=(kc == 0), stop=(kc == 7)),
                      reads=[B_w, B_hT], writes=[pb])
            kb.op("act", lambda e, pt=pt: e.activation(out=junk[:], in_=pt[:, 0:256], func=AF.Square,
                                                       accum_out=st[:, 2:3]), reads=[pb], writes=[B_junk, B_ct])
            rstd_from_ss(st[:, 3:4], st[:, 2:3], B_ct, 256.0, B_ct)
            kb.op("dve", lambda e, pt=pt: e.scalar_tensor_tensor(out=ckve[:, i, 0:256], in0=pt[:, 0:256],
                                                                 scalar=st[:, 3:4], in1=gkv[:], op0=ALU.mult,
                                                                 op1=ALU.mult),
                  reads=[pb, B_ct, B_k], writes=[B_ckve[i]])
            kb.op("dve", lambda e, pt=pt: e.bn_stats(out=st[:, 4:10], in_=pt[:, 256:320]), reads=[pb], writes=[B_kt])
            kb.op("dve", lambda e: e.bn_aggr(out=st[:, 10:12], in_=st[:, 4:10]), reads=[B_kt], writes=[B_kt])
            kb.op("pool", lambda e: e.tensor_scalar(out=st[:, 12:13], in0=st[:, 11:12], scalar1=EPS, scalar2=None,
                                                    op0=ALU.add), reads=[B_kt], writes=[B_kt])
            kb.op("pool", lambda e: e.tensor_tensor(out=st[:, 12:13], in0=st[:, 12:13], in1=mhalf[:], op=ALU.pow),
                  reads=[B_kt, B_const], writes=[B_kt])
            kb.op("dve", lambda e, pt=pt: e.tensor_scalar(out=ktm[:], in0=pt[:, 256:320], scalar1=st[:, 10:11],
                                                          scalar2=st[:, 12:13], op0=ALU.subtract, op1=ALU.mult),
                  reads=[pb, B_kt], writes=[B_kt])
            kb.op("dve", lambda e: e.tensor_tensor(out=ktm[:], in0=ktm[:], in1=gki[:], op=ALU.mult),
                  reads=[B_kt, B_k], writes=[B_kt])
            for a_ in range(2):
                kb.op("dve", lambda e, a_=a_: e.tensor_tensor(out=kd[:, a_, a_ * 64:(a_ + 1) * 64], in0=ktm[:],
                                                              in1=bki[:], op=ALU.add),
                      reads=[B_kt, B_k], writes=[B_kd])
            kb.op("dve", lambda e, pt=pt: e.tensor_scalar(out=wi[:], in0=pt[:, 320:328], scalar1=SC_W, scalar2=None,
                                                          op0=ALU.mult), reads=[pb], writes=[B_wi])
            for h in range(8):
                kb.op("dve", lambda e, h=h: e.tensor_scalar(out=dg[:, h, :], in0=ident_b[:], scalar1=wi[:, h:h + 1],
                                                            scalar2=None, op0=ALU.mult),
                      reads=[B_wi, B_const], writes=[B_dg])
            for rc in range(2):
                kb.op("pe", lambda e, rc=rc: e.transpose(out=TB[:, rc * 128:(rc + 1) * 128],
                                                         in_=ckve[:, i, rc * 128:(rc + 1) * 128], identity=ident_b[:]),
                      reads=[B_ckve[i], B_const], writes=[TBb])
            for a_ in range(2):
                kb.op("pe", lambda e, a_=a_: e.transpose(out=TB[:, 256 + a_ * 128:384 + a_ * 128], in_=kd[:, a_, :],
                                                         identity=ident_b[:]), reads=[B_kd, B_const], writes=[TBb])
            kb.op("dve", lambda e: e.tensor_copy(out=ckvT[:, :, i * 128:(i + 1) * 128],
                                                 in_=TB[:, 0:256].rearrange("p (c t) -> p c t", c=2)),
                  reads=[TBb], writes=[B_ckvT[i]])
            for a_ in range(2):
                kb.op("dve", lambda e, a_=a_: e.tensor_copy(out=kiT[a_][:, i * 128:(i + 1) * 128],
                                                            in_=TB[:, 256 + a_ * 128:384 + a_ * 128]),
                      reads=[TBb], writes=[B_kiT[i]])
            ql, qlb = qlT[i % 3], B_ql[i % 3]
            for rc in range(2):
                pts = [pbank(), pbank()]
                for h in range(8):
                    pt2, pb2 = pts[h // 4]
                    kb.op("pe", lambda e, pt2=pt2, h=h, rc=rc: e.matmul(
                        pt2[:, (h % 4) * 128:(h % 4 + 1) * 128], lhsT=wuk[:, h, rc * 128:(rc + 1) * 128],
                        rhs=qT[:, h, :], start=True, stop=True), reads=[B_w, B_q], writes=[pb2])
                for hg in range(2):
                    pt2, pb2 = pts[hg]
                    kb.op("act", lambda e, pt2=pt2, hg=hg, rc=rc, ql=ql: e.activation(
                        out=ql[:, rc, hg * 512:(hg + 1) * 512], in_=pt2[:], func=AF.Identity,
                        scale=float(128 ** -0.5)), reads=[pb2], writes=[qlb])
        def idx_gen(i):
            score, B_sc = scores[i % 2], B_scs[i % 2]
            n = (i + 1) * 128
            rr = 0
            for c0 in range(0, n, 512):
                wd_ = min(512, n - c0)
                kblk = [B_kiT[b] for b in range(c0 // 128, (c0 + wd_) // 128)]
                rl = []
                pt3, pb3 = banks[7], bankb[7]

                def hsum(h, rl=rl, pt3=pt3, pb3=pb3, wd_=wd_):
                    R, Rb = rl[h]
                    kb.op("pe", lambda e: e.matmul(pt3[:, 0:wd_], lhsT=dg[:, h, :], rhs=R[:, 0:wd_],
                                                   start=(h == 0), stop=(h == 7)), reads=[B_dg, Rb], writes=[pb3])
                for h in range(8):
                    pt2, pb2 = banks[6], bankb[6]
                    p0 = (h % 2) * 64
                    kb.op("pe", lambda e, pt2=pt2, h=h, p0=p0, c0=c0, wd_=wd_: e.matmul(
                        pt2[:, 0:wd_], lhsT=qiT[:, h // 2, :], rhs=kiT[h % 2][:, c0:c0 + wd_],
                        start=True, stop=True), reads=[B_qi] + kblk, writes=[pb2])
                    R, Rb = Rh[rr % NR], B_Rh[rr % NR]
                    rr += 1
                    kb.op("act", lambda e, pt2=pt2, R=R, wd_=wd_: e.activation(out=R[:, 0:wd_], in_=pt2[:, 0:wd_],
                                                                               func=AF.Relu),
                          reads=[pb2], writes=[Rb])
                    rl.append((R, Rb))
                    if h >= 2:
                        hsum(h - 2)
                    yield
                hsum(6)
                hsum(7)
                if c0 + wd_ == n:
                    if wd_ > 128:
                        kb.op("dve", lambda e, pt3=pt3, c0=c0, wd_=wd_: e.tensor_copy(
                            out=score[:, c0:c0 + wd_ - 128], in_=pt3[:, 0:wd_ - 128]), reads=[pb3], writes=[B_sc])
                    kb.op("dve", lambda e, pt3=pt3, c0=c0, wd_=wd_: e.tensor_tensor(
                        out=score[:, n - 128:n], in0=pt3[:, wd_ - 128:wd_], in1=cmask[:], op=ALU.add),
                        reads=[pb3, B_k], writes=[B_sc])
                else:
                    kb.op("dve", lambda e, pt3=pt3, c0=c0, wd_=wd_: e.tensor_copy(
                        out=score[:, c0:c0 + wd_], in_=pt3[:, 0:wd_]), reads=[pb3], writes=[B_sc])

        def t1_gen(i):
            n = (i + 1) * 128
            nm, B_nm = nms[i % 2], B_nms[i % 2]
            score, B_sc = scores[i % 2], B_scs[i % 2]
            if i < 2:
                kb.op("dve", lambda e: e.memset(thr, -1.0e29), writes=[B_th])
            else:
                kb.op("dve", lambda e: e.max(out=m8, in_=score[:, 0:n]), reads=[B_sc], writes=[B_th])
                yield
                kb.op("dve", lambda e: e.tensor_reduce(out=lo_, in_=score[:, 0:n - 128], axis=AX.X, op=ALU.min),
                      reads=[B_sc], writes=[B_th])
                kb.op("dve", lambda e: e.tensor_tensor(out=w_, in0=thb[:, 7:8], in1=lo_, op=ALU.subtract),
                      reads=[B_th], writes=[B_th])
                kb.op("dve", lambda e: e.tensor_scalar(out=wk, in0=pw2[:], scalar1=w_, scalar2=None, op0=ALU.mult),
                      reads=[B_th, B_k], writes=[B_th])
                kb.op("dve", lambda e: e.tensor_tensor(out=t_, in0=lo_, in1=thb[:, 14:15], op=ALU.add),
                      reads=[B_th], writes=[B_th])
                yield
                for k in range(NIT):
                    kb.op("dve", lambda e: e.tensor_scalar(out=nm[:, 0:n], in0=score[:, 0:n], scalar1=t_, scalar2=0.0,
                                                           op0=ALU.is_ge, op1=ALU.add, accum_out=c_),
                          reads=[B_sc, B_th], writes=[B_nm, B_th])
                    kb.op("dve", lambda e: e.tensor_scalar(out=g_, in0=c_, scalar1=float(TOPK) - 0.5, scalar2=0.5,
                                                           op0=ALU.is_ge, op1=ALU.subtract), reads=[B_th], writes=[B_th])
                    kb.op("dve", lambda e, k=k: e.scalar_tensor_tensor(out=t_, in0=g_, scalar=thb[:, 14 + k:15 + k],
                                                                       in1=t_, op0=ALU.mult, op1=ALU.add),
                          reads=[B_th], writes=[B_th])
                    yield
                kb.op("dve", lambda e: e.tensor_tensor(out=thr, in0=t_, in1=thb[:, 14 + NIT:15 + NIT], op=ALU.subtract),
                      reads=[B_th], writes=[B_th])
            kb.op("dve", lambda e: e.tensor_scalar(out=nm[:, 0:n], in0=score[:, 0:n], scalar1=thr, scalar2=-30000.0,
                                                   op0=ALU.is_lt, op1=ALU.mult), reads=[B_sc, B_th], writes=[B_nm])
            yield

        def stage_b(i, tick=None, drain=None, pre=None):
            xt, xb = xts[i % 4], xbs[i % 4]
            ql, qlb = qlT[i % 3], B_ql[i % 3]
            nm_i, B_nm_i = nms[i % 2], B_nms[i % 2]
            pi = 0
            for hg in range(2):
                accs = [(banks[b_], bankb[b_]) for b_ in range(4)]
                def qk(sbk, hg=hg):
                    pt, pb = pbank2()
                    for rc in range(2):
                        kb.op("pe", lambda e, pt=pt, rc=rc: e.matmul(
                            pt[:], lhsT=ckvT[:, rc, sbk * 128:(sbk + 1) * 128], rhs=ql[:, rc, hg * 512:(hg + 1) * 512],
                            start=(rc == 0), stop=False), reads=[B_ckvT[sbk], qlb], writes=[pb])
                    kb.op("pe", lambda e, pt=pt: e.matmul(
                        pt[:], lhsT=nm_i[:, sbk * 128:(sbk + 1) * 128], rhs=ident4[:].rearrange("p a t -> p (a t)"),
                        start=False, stop=True), reads=[B_nm_i, B_k], writes=[pb])
                    return pt, pb

                cur = qk(0)
                for sbk in range(i + 1):
                    pt, pb = cur
                    P_, Pb = PT[pi % 2], B_PT[pi % 2]
                    pi += 1
                    kb.op("act", lambda e, pt=pt, P_=P_: e.activation(out=P_[:], in_=pt[:], func=AF.Exp),
                          reads=[pb], writes=[Pb])
                    if sbk + 1 <= i:
                        cur = qk(sbk + 1)
                    if tick is not None:
                        tick()
                    for a in range(4):
                        at, ab = accs[a]
                        kb.op("pe", lambda e, at=at, a=a, P_=P_, sbk=sbk: e.matmul(
                            at[:, 0:257], lhsT=P_[:, a * 128:(a + 1) * 128], rhs=ckve[:, sbk, 0:257],
                            start=(sbk == 0), stop=(sbk == i)), reads=[Pb, B_ckve[sbk]], writes=[ab])
                for a in range(4):
                    h = hg * 4 + a
                    at, ab = accs[a]
                    kb.op("dve", lambda e, at=at, h=h: e.reciprocal(out=rden[:, h:h + 1], in_=at[:, 256:257]),
                          reads=[ab], writes=[B_rd])
                    kb.op("dve", lambda e, at=at, h=h: e.tensor_scalar(out=ol[:, h, :], in0=at[:, 0:256],
                                                                       scalar1=rden[:, h:h + 1], scalar2=None,
                                                                       op0=ALU.mult), reads=[ab, B_rd], writes=[B_ol])
            if drain is not None:
                drain()
            if pre is not None:
                pre()
            for half in range(2):
                for a in range(8):
                    blk = half * 8 + a
                    h, rc = blk // 2, blk % 2
                    kb.op("pe", lambda e, a=a, h=h, rc=rc: e.transpose(out=TB[:, a * 128:(a + 1) * 128],
                                                                       in_=ol[:, h, rc * 128:(rc + 1) * 128],
                                                                       identity=ident_b[:]),
                          reads=[B_ol, B_const], writes=[TBb])
                kb.op("dve", lambda e: e.tensor_copy(
                    out=olT[:], in_=TB[:].rearrange("p (a t) -> p a t", a=8)), reads=[TBb], writes=[B_olT])
                pt, pb = pbank()
                for a in range(4):
                    h = half * 4 + a
                    for rc in range(2):
                        kb.op("pe", lambda e, pt=pt, a=a, h=h, rc=rc: e.matmul(
                            pt[:, a * 128:(a + 1) * 128], lhsT=wuv[:, h, rc, :], rhs=olT[:, a * 2 + rc, :],
                            start=(rc == 0), stop=(rc == 1)), reads=[B_w, B_olT], writes=[pb])
                kb.op("act", lambda e, pt=pt, half=half: e.activation(
                    out=oT[:, half * 4:(half + 1) * 4, :].rearrange("p a t -> p (a t)"), in_=pt[:], func=AF.Identity),
                    reads=[pb], writes=[B_oT])
            ys = []
            for dh in range(2):
                pt, pb = pbank()
                for h in range(8):
                    kb.op("pe", lambda e, pt=pt, h=h, dh=dh: e.matmul(
                        pt[:], lhsT=oT[:, h, :], rhs=wo[:, h, dh * 512:(dh + 1) * 512],
                        start=(h == 0), stop=(h == 7)), reads=[B_w, B_oT], writes=[pb])
                ys.append((pt, pb))
            for dh, (pt, pb) in enumerate(ys):
                kb.op("dve", lambda e, pt=pt, dh=dh: e.tensor_tensor(
                    out=xt[:, dh * 512:(dh + 1) * 512], in0=pt[:], in1=xt[:, dh * 512:(dh + 1) * 512], op=ALU.add),
                    reads=[pb, xb], writes=[xb])
            kb.dma("sp", dxs[i * 128:(i + 1) * 128, :], xt[:], reads=[xb], writes=[B_xs[i]])

        def run_all(g):
            for _ in g:
                pass

        pre_front(0)
        stage_a(0)
        run_all(idx_gen(0))
        if ntiles > 1:
            pre_front(1)
        for i in range(ntiles):
            if i + 1 < ntiles:
                stage_a(i + 1)
            gi = idx_gen(i + 1) if i + 1 < ntiles else iter(())
            gt = t1_gen(i)
            nloop = max(1, 2 * i)
            t_every = max(1, nloop // 16)
            cnt = [0]

            def tick(gi=gi, gt=gt, cnt=cnt, t_every=t_every):
                next(gi, None)
                cnt[0] += 1
                if cnt[0] % t_every == 0:
                    next(gt, None)

            def drain(gi=gi, gt=gt):
                run_all(gi)
                run_all(gt)
            pre = (lambda i=i: pre_front(i + 2)) if i + 2 < ntiles else None
            if i >= 1:
                stage_b(i - 1, tick, drain, pre)
            else:
                drain()
                if pre is not None:
                    pre()
            if i >= 2:
                bg_issue(1)
        stage_b(ntiles - 1)
        bg_issue(1000)
        kb.barrier()
        ps_.close()

    src = dx
    for ph in phases:
        if ph[0] == "mod":
            phase_mod()
        elif ph[0] == "dsa":
            phase_dsa(ph[1], ph[1] // 2, src, *(ph[2:]))
            src = dxs
        elif ph[0] == "gmlp":
            phase_gmlp(ph[1], ph[1] // 2, src)
            src = dxs
        elif ph[0] == "ffn":
            phase_ffn(ph[1], src)
            src = dxs
        elif ph[0] == "final":
            phase_final(src)
        elif ph[0] == "dump":
            B_o = Buf()
            for i in range(NT):
                kb.dma("sp", dout[i * 128:(i + 1) * 128, :], dxs[i * 128:(i + 1) * 128, :], reads=[B_xs[i]], writes=[B_o])
        elif ph[0] == "dbgmod":
            ddbg = dram("dbg", [128, 4 * 48 + 64], kind="ExternalOutput")
            kb.dma("sp", ddbg[:, 0:192], modT[:].rearrange("p l c -> p (l c)"), reads=[B_modT])
            kb.dma("sp", ddbg[:, 192:224], amix[:].rearrange("p l c -> p (l c)"), reads=[B_modT])
            kb.dma("sp", ddbg[:, 224:256], affn[:].rearrange("p l c -> p (l c)"), reads=[B_modT])
    kb.barrier()
    es.close()
    return nc


FULL = [("mod",)]
for _l in range(4):
    FULL.append(("dsa" if _l % 2 == 0 else "gmlp", _l))
    FULL.append(("ffn", _l))
FULL.append(("final",))


def _core_inputs(inp, b):
    f = np.float32
    c = np.ascontiguousarray
    m = {
        "x": c(inp["x"][b], dtype=f),
        "cT": c(np.asarray(inp["c"][b], dtype=f).reshape(8, 128).T),
        "mod_w": c(inp["mod_w"], dtype=f),
        "mod_b": c(inp["mod_b"], dtype=f),
        "gmixT": c(np.asarray(inp["norm_mix_g"], dtype=f).reshape(4, 8, 128).transpose(2, 0, 1).reshape(128, 32)),
        "gffnT": c(np.asarray(inp["norm_ffn_g"], dtype=f).reshape(4, 8, 128).transpose(2, 0, 1).reshape(128, 32)),
        "final_g": c(np.asarray(inp["final_g"], dtype=f).reshape(1, D)),
        "a_w_in": c(inp["a_w_in"], dtype=f),
        "a_g_kv": c(inp["a_g_kv"], dtype=f),
        "a_g_kidx": c(inp["a_g_kidx"], dtype=f),
        "a_b_kidx": c(inp["a_b_kidx"], dtype=f),
        "a_w_uk": c(inp["a_w_uk"], dtype=f),
        "a_w_uv": c(inp["a_w_uv"], dtype=f),
        "a_w_o": c(inp["a_w_o"], dtype=f),
        "b_w_in": c(inp["b_w_in"], dtype=f),
        "b_ln_g": c(inp["b_ln_g"], dtype=f),
        "b_ln_b": c(inp["b_ln_b"], dtype=f),
        "b_w_sT": c(np.asarray(inp["b_w_s"], dtype=f).transpose(0, 3, 1, 2).reshape(2, 128, 1024)),
        "b_b_s": c(np.asarray(inp["b_b_s"], dtype=f).reshape(2, 1024)),
        "b_w_out": c(inp["b_w_out"], dtype=f),
        "ffn_w_gate": c(inp["ffn_w_gate"], dtype=f),
        "ffn_w_up": c(inp["ffn_w_up"], dtype=f),
        "ffn_w_down": c(inp["ffn_w_down"], dtype=f),
    }
    return m


def kernel(**inputs):
    inp = {k: np.asarray(v) for k, v in inputs.items()}
    nc = _build(FULL)
    in_maps = [_core_inputs(inp, b) for b in range(8)]
    res = run_bass_kernel_spmd(nc, in_maps, core_ids=list(range(8)))
    out = np.stack([np.asarray(r["out"], dtype=np.float32) for r in res.results], axis=0)
    return out
```

```python
import numpy as np
from contextlib import ExitStack
import concourse.bass as bass
import concourse.mybir as mybir
from concourse.bass_utils import run_bass_kernel_spmd

F32 = mybir.dt.float32
BF16 = mybir.dt.bfloat16
ALU = mybir.AluOpType
AF = mybir.ActivationFunctionType
AX = mybir.AxisListType

T = 4096
D = 1024
NT = 32
DFF = 2816
NFC = 22
A_IN = 1864
TOPK = 256
NIT = 13
EPS = 1e-6
NEG = -1.0e30


class Buf:
    __slots__ = ("w", "r")

    def __init__(self):
        self.w = None
        self.r = {}


class KB:
    def __init__(self, nc, es):
        self.nc = nc
        self.es = es
        self.eng = dict(pe=nc.tensor, dve=nc.vector, act=nc.scalar, pool=nc.gpsimd, sp=nc.sync)
        self.sems = {}
        self.cnt = {}
        self.seen = {e: {} for e in self.eng}
        for e in self.eng:
            self.sems[e] = es.enter_context(nc.semaphore("s_" + e))
            self.cnt[e] = 0
        self.dq = {}
        self.dqi = {}
        for q, n in (("sp", 20), ("pool", 10), ("bg", 3)):
            self.dq[q] = []
            self.dqi[q] = 0
            for i in range(n):
                k = "d_%s%d" % (q, i)
                self.sems[k] = es.enter_context(nc.semaphore(k))
                self.cnt[k] = 0
                self.dq[q].append(k)
        self.nbank = 0

    def _wait(self, e, key, val):
        if val <= 0 or self.seen[e].get(key, 0) >= val:
            return
        self.eng[e].wait_ge(self.sems[key], val)
        self.seen[e][key] = val

    def _deps(self, e, reads, writes, strict=False):
        for b in reads:
            if b.w is not None:
                k, v = b.w
                if strict or k != e or e != "pe":
                    self._wait(e, k, v)
        for b in writes:
            if b.w is not None:
                k, v = b.w
                if strict or k != e:
                    self._wait(e, k, v)
            for k, v in b.r.items():
                if strict or k != e:
                    self._wait(e, k, v)

    def op(self, e, fn, reads=(), writes=()):
        self._deps(e, reads, writes)
        inst = fn(self.eng[e])
        self.cnt[e] += 1
        inst.then_inc(self.sems[e], 1)
        v = self.cnt[e]
        for b in reads:
            b.r[e] = v
        for b in writes:
            b.w = (e, v)
            b.r = {}

    def dma(self, q, out, in_, reads=(), writes=(), sems=None):
        pool = self.dq[sems or q]
        key = pool[self.dqi[sems or q] % len(pool)]
        self.dqi[sems or q] += 1
        self._wait(q, key, self.cnt[key])
        self._deps(q, reads, writes, strict=True)
        inst = self.eng[q].dma_start(out=out, in_=in_)
        self.cnt[key] += 16
        inst.then_inc(self.sems[key], 16)
        v = self.cnt[key]
        for b in reads:
            b.r[key] = v
        for b in writes:
            b.w = (key, v)
            b.r = {}

    def barrier(self):
        keys = list(self.sems.keys())
        for e in self.eng:
            for k in keys:
                if k != e:
                    self._wait(e, k, self.cnt[k])


def _build(phases):
    nc = bass.Bass("TRN2", target_bir_lowering=False)
    es = ExitStack()
    kb = KB(nc, es)

    def dram(name, shape, kind="ExternalInput", dt=F32):
        return nc.dram_tensor(name, list(shape), dt, kind=kind).ap()

    dx = dram("x", [T, D])
    dout = dram("out", [T, D], kind="ExternalOutput")
    dxs = dram("xs", [T, D], kind="Internal")
    dgs = dram("gsc", [8, 128, D], kind="Internal")
    d_cT = dram("cT", [128, 8])
    d_modw = dram("mod_w", [4, D, 6 * D])
    d_modb = dram("mod_b", [4, 6 * D])
    d_gmix = dram("gmixT", [128, 32])
    d_gffn = dram("gffnT", [128, 32])
    d_fg = dram("final_g", [1, D])
    d_awin = dram("a_w_in", [2, D, A_IN])
    d_agkv = dram("a_g_kv", [2, 256])
    d_agki = dram("a_g_kidx", [2, 64])
    d_abki = dram("a_b_kidx", [2, 64])
    d_awuk = dram("a_w_uk", [2, 8, 128, 256])
    d_awuv = dram("a_w_uv", [2, 8, 256, 128])
    d_awo = dram("a_w_o", [2, D, D])
    d_bwin = dram("b_w_in", [2, D, 4096])
    d_blng = dram("b_ln_g", [2, 2048])
    d_blnb = dram("b_ln_b", [2, 2048])
    d_bwsT = dram("b_w_sT", [2, 128, 8 * 128])
    d_bbs = dram("b_b_s", [2, 8 * 128])
    d_bwout = dram("b_w_out", [2, 2048, D])
    d_fwg = dram("ffn_w_gate", [4, D, DFF])
    d_fwu = dram("ffn_w_up", [4, D, DFF])
    d_fwd = dram("ffn_w_down", [4, DFF, D])

    uid = [0]

    w16 = {}
    B_w16 = {}

    bg_pending = []

    def bg_cast(name, src_ap, shape):
        t = dram("w16_" + name, shape, kind="Internal", dt=BF16)
        w16[name] = t
        B_w16[name] = Buf()
        bg_pending.append(lambda: kb.dma("pool", t, src_ap, writes=[B_w16[name]], sems="bg"))

    def bg_issue(n=1):
        for _ in range(n):
            if bg_pending:
                bg_pending.pop(0)()

    def bg_cast_all():
        for l_ in range(4):
            bg_cast("fwg%d" % l_, d_fwg[l_], [D, DFF])
            bg_cast("fwu%d" % l_, d_fwu[l_], [D, DFF])
            bg_cast("fwd%d" % l_, d_fwd[l_], [DFF, D])
            if l_ == 0:
                bg_cast("bwin0", d_bwin[0], [D, 4096])
                bg_cast("bwout0", d_bwout[0], [2048, D])
            if l_ == 1:
                bg_cast("awin1", d_awin[1], [D, A_IN])
                bg_cast("awuk1", d_awuk[1].rearrange("h d r -> (h d) r"), [8 * 128, 256])
                bg_cast("awuv1", d_awuv[1].rearrange("h r d -> (h r) d"), [8 * 256, 128])
            if l_ == 2:
                bg_cast("bwin1", d_bwin[1], [D, 4096])
                bg_cast("bwout1", d_bwout[1], [2048, D])

    def sb(stack, name, shape, dt):
        uid[0] += 1
        return stack.enter_context(nc.sbuf_tensor("%s_%d" % (name, uid[0]), list(shape), dt))

    ident_f = sb(es, "ident_f", [128, 128], F32)
    ident_b = sb(es, "ident_b", [128, 128], BF16)
    mhalf = sb(es, "mhalf", [128, 1], F32)
    modT = sb(es, "modT", [128, 4, 48], F32)
    amix = sb(es, "amix", [128, 4, 8], F32)
    affn = sb(es, "affn", [128, 4, 8], F32)
    gmixT = sb(es, "gmixT_s", [128, 32], F32)
    gffnT = sb(es, "gffnT_s", [128, 32], F32)
    B_const = Buf()
    B_modT = Buf()
    banks = [es.enter_context(nc.psum_tensor("ps%d" % i, [128, 512], F32)) for i in range(8)]
    bankb = [Buf() for _ in range(8)]

    nb2 = [0]

    def pbank():
        i = kb.nbank % 5
        kb.nbank += 1
        return banks[i], bankb[i]

    def pbank2():
        i = 4 + nb2[0] % 2
        nb2[0] += 1
        return banks[i], bankb[i]

    TB = banks[7][:].bitcast(BF16)
    TBb = bankb[7]

    kb.op("pool", lambda e: e.memset(ident_f[:], 1.0), writes=[B_const])
    kb.op("pool", lambda e: e.memset(mhalf[:], -0.5), writes=[B_const])
    kb.op("pool", lambda e: e.affine_select(out=ident_f[:], in_=ident_f[:], pattern=[[-1, 128]],
                                            compare_op=ALU.is_equal, fill=0.0, base=0, channel_multiplier=1),
          reads=[B_const], writes=[B_const])
    kb.op("dve", lambda e: e.tensor_copy(out=ident_b[:], in_=ident_f[:]), reads=[B_const], writes=[B_const])

    def rstd_from_ss(stk_rs, ss, B_ss, n, B_rs):
        kb.op("pool", lambda e: e.tensor_scalar(out=stk_rs, in0=ss, scalar1=float(n) * EPS, scalar2=None,
                                                op0=ALU.add), reads=[B_ss], writes=[B_rs])
        kb.op("pool", lambda e: e.tensor_tensor(out=stk_rs, in0=stk_rs, in1=mhalf[:], op=ALU.pow),
              reads=[B_rs, B_const], writes=[B_rs])

    def phase_mod():
        ps_ = ExitStack()
        cT = sb(ps_, "cT_s", [128, 8], F32)
        cact = sb(ps_, "cact", [128, 8], F32)
        crep = sb(ps_, "crep", [128, 8, 128], F32)
        ones1 = sb(ps_, "ones1", [1, 128], F32)
        mb = sb(ps_, "mb", [1, 6 * D], F32)
        mw = [sb(ps_, "mw%d" % i, [128, 3072], F32) for i in range(5)]
        mwb = [Buf() for _ in range(5)]
        mh = sb(ps_, "mh", [128, 3072], F32)
        dtmp = sb(ps_, "dtmp", [128, 24, 128], F32)
        B_c, B_mb, B_mh, B_dt = Buf(), Buf(), Buf(), Buf()
        kb.dma("sp", cT[:], d_cT[:, :], writes=[B_c])
        kb.dma("sp", gmixT[:], d_gmix[:, :], writes=[B_modT])
        kb.dma("sp", gffnT[:], d_gffn[:, :], writes=[B_modT])
        kb.op("act", lambda e: e.activation(out=cact[:], in_=cT[:], func=AF.Silu), reads=[B_c], writes=[B_c])
        kb.op("pool", lambda e: e.memset(ones1[:], 1.0), writes=[B_c])
        for kc in range(8):
            kb.op("dve", lambda e, kc=kc: e.tensor_copy(out=crep[:, kc, :],
                                                        in_=cact[:, kc:kc + 1].to_broadcast([128, 128])),
                  reads=[B_c], writes=[B_c])
        li = 0
        for l in range(4):
            kb.dma("sp", mb[:], d_modb[l:l + 1, :], writes=[B_mb])
            for half in range(2):
                acc = [(banks[b_], bankb[b_]) for b_ in range(6)]
                for kc in range(8):
                    t_, tb = mw[li % 5], mwb[li % 5]
                    li += 1
                    kb.dma("sp", t_[:], d_modw[l, kc * 128:(kc + 1) * 128, half * 3072:(half + 1) * 3072],
                           writes=[tb])
                    for blk in range(6):
                        pt, pb = acc[blk]
                        if kc == 0:
                            kb.op("pe", lambda e, pt=pt, blk=blk, half=half: e.matmul(
                                pt[:], lhsT=ones1[0:1, :], rhs=mb[0:1, half * 3072 + blk * 512: half * 3072 + (blk + 1) * 512],
                                start=True, stop=False), reads=[B_c, B_mb], writes=[pb])
                        kb.op("pe", lambda e, pt=pt, blk=blk, kc=kc, t_=t_: e.matmul(
                            pt[:], lhsT=crep[:, kc, :], rhs=t_[:, blk * 512:(blk + 1) * 512],
                            start=False, stop=(kc == 7)), reads=[B_c, tb], writes=[pb])
                for blk in range(6):
                    pt, pb = acc[blk]
                    kb.op("dve" if blk % 2 == 0 else "act",
                          (lambda e, pt=pt, blk=blk: e.tensor_copy(out=mh[:, blk * 512:(blk + 1) * 512], in_=pt[:]))
                          if blk % 2 == 0 else
                          (lambda e, pt=pt, blk=blk: e.activation(out=mh[:, blk * 512:(blk + 1) * 512], in_=pt[:],
                                                                  func=AF.Identity)),
                          reads=[pb], writes=[B_mh])
                kb.dma("sp", dgs[l * 2 + half, :, :], mh[:, 2048:3072], reads=[B_mh], writes=[B_gs[l * 2 + half]])
                kb.op("dve", lambda e: e.tensor_tensor(
                    out=dtmp[:], in0=mh[:].rearrange("p (c j) -> p c j", j=128),
                    in1=ident_f[:].unsqueeze(1).to_broadcast([128, 24, 128]), op=ALU.mult),
                    reads=[B_mh, B_const], writes=[B_dt])
                kb.op("dve", lambda e, l=l, half=half: e.tensor_reduce(
                    out=modT[:, l, half * 24:(half + 1) * 24], in_=dtmp[:], axis=AX.X, op=ALU.add),
                    reads=[B_dt], writes=[B_modT])
            kb.op("dve", lambda e, l=l: e.scalar_tensor_tensor(
                out=amix[:, l, :], in0=modT[:, l, 8:16], scalar=1.0, in1=gmixT[:, l * 8:(l + 1) * 8],
                op0=ALU.add, op1=ALU.mult), reads=[B_modT], writes=[B_modT])
            kb.op("dve", lambda e, l=l: e.scalar_tensor_tensor(
                out=affn[:, l, :], in0=modT[:, l, 32:40], scalar=1.0, in1=gffnT[:, l * 8:(l + 1) * 8],
                op0=ALU.add, op1=ALU.mult), reads=[B_modT], writes=[B_modT])
        kb.barrier()
        ps_.close()

    B_gs = [Buf() for _ in range(8)]
    B_xs = [Buf() for _ in range(NT)]

    def load_x(src, i, xt, xb):
        kb.dma("sp", xt[:], src[i * 128:(i + 1) * 128, :], reads=[B_xs[i]] if src is dxs else [], writes=[xb])

    def norm_tile(xt, xb, junk, B_junk, ss, rs, B_st, xsb, B_xsb):
        kb.op("act", lambda e: e.activation(out=junk, in_=xt[:], func=AF.Square, accum_out=ss),
              reads=[xb], writes=[B_junk, B_st])
        rstd_from_ss(rs, ss, B_st, float(D), B_st)
        kb.op("dve", lambda e: e.tensor_scalar(out=xsb, in0=xt[:], scalar1=rs, scalar2=32.0, op0=ALU.mult,
                                               op1=ALU.mult), reads=[xb, B_st], writes=[B_xsb])

    def make_hT(xsb_t, B_xsb, hT_dst, B_hT, A, S):
        for kc in range(8):
            kb.op("pe", lambda e, kc=kc: e.transpose(out=TB[:, kc * 128:(kc + 1) * 128],
                                                     in_=xsb_t[:, kc * 128:(kc + 1) * 128], identity=ident_b[:]),
                  reads=[B_xsb, B_const], writes=[TBb])
        for kc in range(8):
            kb.op("act", lambda e, kc=kc: e.activation(out=hT_dst(kc), in_=TB[:, kc * 128:(kc + 1) * 128],
                                                       func=AF.Identity, scale=A[:, kc:kc + 1], bias=S[:, kc:kc + 1]),
                  reads=[TBb, B_modT], writes=[B_hT])

    def residual_store(ypairs, xt, xb, gb, B_gb, tmps, dst, dstb, fin=None):
        for dh, (pt, pb) in enumerate(ypairs):
            tmp, tb = tmps[dh]
            kb.op("dve", lambda e, pt=pt, dh=dh, tmp=tmp: e.tensor_tensor(
                out=tmp[:], in0=pt[:], in1=gb[:, dh * 512:(dh + 1) * 512], op=ALU.mult),
                reads=[pb, B_gb], writes=[tb])
            kb.op("pool", lambda e, dh=dh, tmp=tmp: e.tensor_tensor(
                out=xt[:, dh * 512:(dh + 1) * 512], in0=xt[:, dh * 512:(dh + 1) * 512], in1=tmp[:], op=ALU.add),
                reads=[tb, xb], writes=[xb])
        if fin is None:
            kb.dma("sp", dst, xt[:], reads=[xb], writes=[dstb])
        else:
            fg, B_fg, ot, B_ot, junk, B_junk, ss, rs, B_fs, odst, B_od = fin
            kb.op("act", lambda e: e.activation(out=junk, in_=xt[:], func=AF.Square, accum_out=ss),
                  reads=[xb], writes=[B_junk, B_fs])
            rstd_from_ss(rs, ss, B_fs, float(D), B_fs)
            kb.op("dve", lambda e: e.scalar_tensor_tensor(out=ot[:], in0=xt[:], scalar=rs, in1=fg[:], op0=ALU.mult,
                                                          op1=ALU.mult), reads=[xb, B_fs, B_fg], writes=[B_ot])
            kb.dma("sp", odst, ot[:], reads=[B_ot], writes=[B_od])

    def phase_ffn(l, src, fuse_final=False):
        ps_ = ExitStack()
        wg = sb(ps_, "wg", [128, 8, DFF], BF16)
        wu = sb(ps_, "wu", [128, 8, DFF], BF16)
        wd = sb(ps_, "wd", [128, NFC, D], BF16)
        gb = sb(ps_, "gb", [128, D], F32)
        xts = [sb(ps_, "xt%d" % i, [128, D], F32) for i in range(4)]
        xbs = [Buf() for _ in range(4)]
        junk = sb(ps_, "junk", [128, D], BF16)
        st = sb(ps_, "st", [128, 4], F32)
        xsb = [sb(ps_, "xsb%d" % i, [128, D], BF16) for i in range(2)]
        hT = [sb(ps_, "hT%d" % i, [128, 8, 256], BF16) for i in range(2)]
        actT = sb(ps_, "actT", [128, NFC, 256], BF16)
        sg = [sb(ps_, "sg%d" % i, [128, 256], F32) for i in range(2)]
        tmps = [(sb(ps_, "tmp%d" % i, [128, 512], F32), Buf()) for i in range(2)]
        B_w, B_gb, B_junk = Buf(), Buf(), Buf()
        if fuse_final:
            fg = sb(ps_, "ffg", [128, D], F32)
            ots = [sb(ps_, "fot%d" % i, [128, D], F32) for i in range(2)]
            B_ots = [Buf(), Buf()]
            fst = sb(ps_, "ffst", [128, 4], F32)
            B_fst = [Buf(), Buf()]
            B_fg, B_od = Buf(), Buf()
            kb.dma("sp", fg[:], d_fg[0:1, :].to_broadcast([128, D]), writes=[B_fg])
            kb.op("dve", lambda e: e.tensor_scalar(out=fg[:], in0=fg[:], scalar1=32.0, scalar2=None, op0=ALU.mult),
                  reads=[B_fg], writes=[B_fg])
        B_st = [Buf(), Buf()]
        B_xsb = [Buf(), Buf()]
        B_hT = [Buf(), Buf()]
        B_sg = [Buf(), Buf()]
        B_act = [Buf() for _ in range(NFC)]
        B_wg = [Buf() for _ in range(8)]
        B_wu = [Buf() for _ in range(8)]
        B_wd = [Buf() for _ in range(NFC)]
        for kc in range(8):
            kb.dma("sp", wg[:, kc, :], w16["fwg%d" % l][kc * 128:(kc + 1) * 128, :], reads=[B_w16["fwg%d" % l]],
                   writes=[B_wg[kc]])
        for kc in range(8):
            kb.dma("sp", wu[:, kc, :], w16["fwu%d" % l][kc * 128:(kc + 1) * 128, :], reads=[B_w16["fwu%d" % l]],
                   writes=[B_wu[kc]])
        for fc in range(NFC):
            kb.dma("sp", wd[:, fc, :], w16["fwd%d" % l][fc * 128:(fc + 1) * 128, :], reads=[B_w16["fwd%d" % l]],
                   writes=[B_wd[fc]])
        kb.dma("sp", gb[:], dgs[l * 2 + 1, :, :], reads=[B_gs[l * 2 + 1]], writes=[B_gb])
        A = affn[:, l, :]
        S = modT[:, l, 24:32]
        NG = NT // 2

        def prep_a(g):
            for tl in range(2):
                i = g * 2 + tl
                load_x(src, i, xts[i % 4], xbs[i % 4])

        def prep_b(g):
            for tl in range(2):
                i = g * 2 + tl
                k = i % 2
                norm_tile(xts[i % 4], xbs[i % 4], junk[:], B_junk, st[:, 2 * k:2 * k + 1], st[:, 2 * k + 1:2 * k + 2],
                          B_st[k], xsb[k][:], B_xsb[k])
                make_hT(xsb[k], B_xsb[k], lambda kc, tl=tl, g=g: hT[g % 2][:, kc, tl * 128:(tl + 1) * 128],
                        B_hT[g % 2], A, S)

        prep_a(0)
        prep_b(0)
        for g in range(NG):
            h = hT[g % 2]
            hb = B_hT[g % 2]
            if g + 1 < NG:
                prep_a(g + 1)
            for fc in range(NFC):
                pt, pb = pbank()
                for slot, w in ((0, wg), (1, wu)):
                    for kc in range(8):
                        kb.op("pe", lambda e, pt=pt, slot=slot, w=w, kc=kc, fc=fc: e.matmul(
                            pt[:, slot * 256:(slot + 1) * 256], lhsT=w[:, kc, fc * 128:(fc + 1) * 128],
                            rhs=h[:, kc, :], start=(kc == 0), stop=(kc == 7)),
                            reads=[(B_wg if slot == 0 else B_wu)[kc], hb], writes=[pb])
                s_ = sg[fc % 2]
                sbf = B_sg[fc % 2]
                kb.op("act", lambda e, pt=pt, s_=s_: e.activation(out=s_[:], in_=pt[:, 0:256], func=AF.Silu),
                      reads=[pb], writes=[sbf])
                kb.op("dve", lambda e, pt=pt, s_=s_, fc=fc: e.tensor_tensor(
                    out=actT[:, fc, :], in0=pt[:, 256:512], in1=s_[:], op=ALU.mult),
                    reads=[pb, sbf], writes=[B_act[fc]])
                if fc == 6 and g + 1 < NG:
                    prep_b(g + 1)
            for tl in range(2):
                i = g * 2 + tl
                ys = []
                for dh in range(2):
                    pt, pb = pbank()
                    for fc in range(NFC):
                        kb.op("pe", lambda e, pt=pt, fc=fc, tl=tl, dh=dh: e.matmul(
                            pt[:], lhsT=actT[:, fc, tl * 128:(tl + 1) * 128], rhs=wd[:, fc, dh * 512:(dh + 1) * 512],
                            start=(fc == 0), stop=(fc == NFC - 1)), reads=[B_wd[fc], B_act[fc]], writes=[pb])
                    ys.append((pt, pb))
                fin = None
                if fuse_final:
                    k_ = i % 2
                    fin = (fg, B_fg, ots[k_], B_ots[k_], junk[:], B_junk, fst[:, 2 * k_:2 * k_ + 1],
                           fst[:, 2 * k_ + 1:2 * k_ + 2], B_fst[k_], dout[i * 128:(i + 1) * 128, :], B_od)
                residual_store(ys, xts[i % 4], xbs[i % 4], gb, B_gb, tmps, dxs[i * 128:(i + 1) * 128, :], B_xs[i], fin)
        kb.barrier()
        ps_.close()

    def phase_final(src):
        ps_ = ExitStack()
        fg = sb(ps_, "fg", [128, D], F32)
        xts = [sb(ps_, "fxt%d" % i, [128, D], F32) for i in range(3)]
        xbs = [Buf() for _ in range(3)]
        ots = [sb(ps_, "fot%d" % i, [128, D], F32) for i in range(2)]
        obs = [Buf() for _ in range(2)]
        junk = sb(ps_, "fjunk", [128, D], BF16)
        st = sb(ps_, "fst", [128, 4], F32)
        B_st = [Buf(), Buf()]
        B_fg, B_junk, B_o = Buf(), Buf(), Buf()
        kb.dma("sp", fg[:], d_fg[0:1, :].to_broadcast([128, D]), writes=[B_fg])
        kb.op("dve", lambda e: e.tensor_scalar(out=fg[:], in0=fg[:], scalar1=32.0, scalar2=None, op0=ALU.mult),
              reads=[B_fg], writes=[B_fg])
        for i in range(NT):
            k = i % 2
            xt, xb = xts[i % 3], xbs[i % 3]
            load_x(src, i, xt, xb)
            ss, rs = st[:, 2 * k:2 * k + 1], st[:, 2 * k + 1:2 * k + 2]
            kb.op("act", lambda e, xt=xt, ss=ss: e.activation(out=junk[:], in_=xt[:], func=AF.Square, accum_out=ss),
                  reads=[xb], writes=[B_junk, B_st[k]])
            rstd_from_ss(rs, ss, B_st[k], float(D), B_st[k])
            kb.op("dve", lambda e, xt=xt, rs=rs, k=k: e.scalar_tensor_tensor(
                out=ots[k][:], in0=xt[:], scalar=rs, in1=fg[:], op0=ALU.mult, op1=ALU.mult),
                reads=[xb, B_st[k], B_fg], writes=[obs[k]])
            kb.dma("sp", dout[i * 128:(i + 1) * 128, :], ots[k][:], reads=[obs[k]], writes=[B_o])
        kb.barrier()
        ps_.close()

    def phase_gmlp(l, j, src):
        ps_ = ExitStack()
        win = sb(ps_, "bwin", [128, 8, 4096], BF16)
        wout = sb(ps_, "bwout", [128, 16, D], BF16)
        wcf = sb(ps_, "wcf", [128, 8, 128], F32)
        wcT = sb(ps_, "wcT", [128, 8, 128], BF16)
        lng = sb(ps_, "lng", [128, 2048], F32)
        lnb = sb(ps_, "lnb", [128, 2048], F32)
        bsb = sb(ps_, "bsb", [128, 8, 128], F32)
        gb = sb(ps_, "ggb", [128, D], F32)
        xts = [sb(ps_, "gxt%d" % i, [128, D], F32) for i in range(4)]
        xbs = [Buf() for _ in range(4)]
        junk = sb(ps_, "gjunk", [128, D], BF16)
        st = sb(ps_, "gst", [128, 4], F32)
        xsb = [sb(ps_, "gxsb%d" % i, [128, D], BF16) for i in range(2)]
        hT = [sb(ps_, "ghT%d" % i, [128, 8, 256], BF16) for i in range(2)]
        uT = sb(ps_, "uT", [128, 16, 256], F32)
        vr = [sb(ps_, "vr0", [128, 2048], F32)] * 2
        vt = [sb(ps_, "vt%d" % i, [128, 2048], BF16) for i in range(2)]
        bst = sb(ps_, "bst", [128, 2, 4, 6], F32)
        lst = sb(ps_, "lst", [128, 2, 4], F32)
        ypT = sb(ps_, "ypT", [128, 16, 256], BF16)
        ytm = [sb(ps_, "ytm%d" % i, [128, 512], F32) for i in range(2)]
        tmps = [(sb(ps_, "gtmp%d" % i, [128, 512], F32), Buf()) for i in range(2)]
        B_w, B_gb, B_junk, B_k = Buf(), Buf(), Buf(), Buf()
        B_st = [Buf(), Buf()]
        B_xsb = [Buf(), Buf()]
        B_hT = [Buf(), Buf()]
        B_u = [Buf() for _ in range(16)]
        B_vr = [Buf()] * 2
        B_vt = [Buf(), Buf()]
        B_ls = [Buf(), Buf()]
        B_yp = [[Buf() for _ in range(4)] for _ in range(2)]
        B_ytm = [Buf(), Buf()]
        for kc in range(8):
            kb.dma("sp", win[:, kc, :], w16["bwin%d" % j][kc * 128:(kc + 1) * 128, :], reads=[B_w16["bwin%d" % j]],
                   writes=[B_w])
        for ec in range(16):
            kb.dma("sp", wout[:, ec, :], w16["bwout%d" % j][ec * 128:(ec + 1) * 128, :], reads=[B_w16["bwout%d" % j]],
                   writes=[B_w])
        kb.dma("sp", wcf[:], d_bwsT[j, :, :].rearrange("s (g t) -> s g t", g=8), writes=[B_k])
        kb.dma("sp", lng[:], d_blng[j:j + 1, :].to_broadcast([128, 2048]), writes=[B_k])
        kb.dma("sp", lnb[:], d_blnb[j:j + 1, :].to_broadcast([128, 2048]), writes=[B_k])
        kb.dma("sp", bsb[:], d_bbs[j:j + 1, :].to_broadcast([128, 1024]).rearrange("p (g t) -> p g t", g=8),
               writes=[B_k])
        kb.dma("sp", gb[:], dgs[l * 2, :, :], reads=[B_gs[l * 2]], writes=[B_gb])
        kb.op("pool", lambda e: e.affine_select(out=wcf[:], in_=wcf[:], pattern=[[0, 8], [1, 128]],
                                                compare_op=ALU.is_ge, fill=0.0, base=0, channel_multiplier=-1),
              reads=[B_k], writes=[B_k])
        kb.op("dve", lambda e: e.tensor_copy(out=wcT[:], in_=wcf[:]), reads=[B_k], writes=[B_k])
        A = amix[:, l, :]
        S = modT[:, l, 0:8]
        NG = NT // 2

        def prep_a(g):
            for tl in range(2):
                i = g * 2 + tl
                load_x(src, i, xts[i % 4], xbs[i % 4])

        def prep_b(g):
            for tl in range(2):
                i = g * 2 + tl
                k = i % 2
                norm_tile(xts[i % 4], xbs[i % 4], junk[:], B_junk, st[:, 2 * k:2 * k + 1], st[:, 2 * k + 1:2 * k + 2],
                          B_st[k], xsb[k][:], B_xsb[k])
                make_hT(xsb[k], B_xsb[k], lambda kc, tl=tl, g=g: hT[g % 2][:, kc, tl * 128:(tl + 1) * 128],
                        B_hT[g % 2], A, S)

        prep_a(0)
        prep_b(0)
        for g in range(NG):
            h = hT[g % 2]
            hb = B_hT[g % 2]
            if g + 1 < NG:
                prep_a(g + 1)
            for tl in range(2):
                for cb in range(4):
                    pt, pb = pbank()
                    for kc in range(8):
                        kb.op("pe", lambda e, pt=pt, kc=kc, cb=cb, tl=tl: e.matmul(
                            pt[:], lhsT=h[:, kc, tl * 128:(tl + 1) * 128],
                            rhs=win[:, kc, 2048 + cb * 512:2048 + (cb + 1) * 512],
                            start=(kc == 0), stop=(kc == 7)), reads=[B_w, hb], writes=[pb])
                    kb.op("act", lambda e, pt=pt, cb=cb, tl=tl: e.activation(
                        out=vr[tl][:, cb * 512:(cb + 1) * 512], in_=pt[:], func=AF.Gelu),
                        reads=[pb], writes=[B_vr[tl]])
                    kb.op("dve", lambda e, cb=cb, tl=tl: e.bn_stats(out=bst[:, tl, cb, :],
                                                                    in_=vr[tl][:, cb * 512:(cb + 1) * 512]),
                          reads=[B_vr[tl]], writes=[B_ls[tl]])
                kb.op("dve", lambda e, tl=tl: e.bn_aggr(out=lst[:, tl, 0:2],
                                                        in_=bst[:, tl, :, :].rearrange("p a b -> p (a b)")),
                      reads=[B_ls[tl]], writes=[B_ls[tl]])
                kb.op("pool", lambda e, tl=tl: e.tensor_scalar(out=lst[:, tl, 2:3], in0=lst[:, tl, 1:2], scalar1=EPS,
                                                               scalar2=None, op0=ALU.add),
                      reads=[B_ls[tl]], writes=[B_ls[tl]])
                kb.op("pool", lambda e, tl=tl: e.tensor_tensor(out=lst[:, tl, 2:3], in0=lst[:, tl, 2:3], in1=mhalf[:],
                                                               op=ALU.pow),
                      reads=[B_ls[tl], B_const], writes=[B_ls[tl]])
                kb.op("dve", lambda e, tl=tl: e.tensor_scalar(out=vr[tl][:], in0=vr[tl][:], scalar1=lst[:, tl, 0:1],
                                                              scalar2=lst[:, tl, 2:3], op0=ALU.subtract, op1=ALU.mult),
                      reads=[B_ls[tl], B_vr[tl]], writes=[B_vr[tl]])
                kb.op("pool", lambda e, tl=tl: e.tensor_tensor(out=vr[tl][:], in0=vr[tl][:], in1=lng[:], op=ALU.mult),
                      reads=[B_vr[tl], B_k], writes=[B_vr[tl]])
                kb.op("pool", lambda e, tl=tl: e.tensor_tensor(out=vt[tl][:], in0=vr[tl][:], in1=lnb[:], op=ALU.add),
                      reads=[B_vr[tl], B_k], writes=[B_vt[tl]])
            for ec in range(16):
                if ec % 2 == 0:
                    pt, pb = pbank()
                half = ec % 2
                for kc in range(8):
                    kb.op("pe", lambda e, pt=pt, half=half, kc=kc, ec=ec: e.matmul(
                        pt[:, half * 256:(half + 1) * 256], lhsT=win[:, kc, ec * 128:(ec + 1) * 128],
                        rhs=h[:, kc, :], start=(kc == 0), stop=(kc == 7)), reads=[B_w, hb], writes=[pb])
                if ec % 2 == 1:
                    kb.op("act", lambda e, pt=pt, ec=ec: e.activation(
                        out=uT[:, ec - 1:ec + 1, :].rearrange("p a t -> p (a t)"), in_=pt[:], func=AF.Gelu),
                        reads=[pb], writes=[B_u[ec - 1], B_u[ec]])
            if g + 1 < NG:
                prep_b(g + 1)
            for tl in range(2):
                i = g * 2 + tl
                for q4 in range(4):
                    pt, pb = pbank()
                    for a in range(4):
                        ec = q4 * 4 + a
                        kb.op("pe", lambda e, pt=pt, a=a, ec=ec, tl=tl: e.matmul(
                            pt[:, a * 128:(a + 1) * 128], lhsT=vt[tl][:, ec * 128:(ec + 1) * 128],
                            rhs=wcT[:, ec // 2, :], start=True, stop=True), reads=[B_vt[tl], B_k], writes=[pb])
                    ym, ymb = ytm[q4 % 2], B_ytm[q4 % 2]
                    kb.op("dve", lambda e, pt=pt, q4=q4, ym=ym: e.tensor_tensor(
                        out=ym[:].rearrange("p (g r t) -> p g r t", g=2, r=2),
                        in0=pt[:].rearrange("p (g r t) -> p g r t", g=2, r=2),
                        in1=bsb[:, q4 * 2:q4 * 2 + 2, :].unsqueeze(2).to_broadcast([128, 2, 2, 128]), op=ALU.add),
                        reads=[pb, B_k], writes=[ymb])
                    kb.op("dve", lambda e, q4=q4, tl=tl, ym=ym: e.tensor_tensor(
                        out=ypT[:, q4 * 4:(q4 + 1) * 4, tl * 128:(tl + 1) * 128],
                        in0=ym[:].rearrange("p (a t) -> p a t", a=4),
                        in1=uT[:, q4 * 4:(q4 + 1) * 4, tl * 128:(tl + 1) * 128], op=ALU.mult),
                        reads=[ymb] + [B_u[q4 * 4 + a] for a in range(4)], writes=[B_yp[tl][q4]])
                ys = []
                for dh in range(2):
                    pt, pb = pbank()
                    for ec in range(16):
                        kb.op("pe", lambda e, pt=pt, ec=ec, tl=tl, dh=dh: e.matmul(
                            pt[:], lhsT=ypT[:, ec, tl * 128:(tl + 1) * 128], rhs=wout[:, ec, dh * 512:(dh + 1) * 512],
                            start=(ec == 0), stop=(ec == 15)), reads=[B_w, B_yp[tl][ec // 4]], writes=[pb])
                    ys.append((pt, pb))
                residual_store(ys, xts[i % 4], xbs[i % 4], gb, B_gb, tmps, dxs[i * 128:(i + 1) * 128, :], B_xs[i])
        kb.barrier()
        ps_.close()

    def phase_dsa(l, j, src, ntiles=NT):
        ps_ = ExitStack()
        win = sb(ps_, "awin", [128, 8, A_IN], BF16)
        wuk = sb(ps_, "awuk", [128, 8, 256], BF16)
        wuv = sb(ps_, "awuv", [128, 8, 2, 128], BF16)
        wo = sb(ps_, "awo", [128, 8, D], BF16)
        gkv = sb(ps_, "gkv", [128, 256], F32)
        gki = sb(ps_, "gki", [128, 64], F32)
        bki = sb(ps_, "bki", [128, 64], F32)
        cmask = sb(ps_, "cmask", [128, 128], F32)
        pw2 = sb(ps_, "pw2", [128, NIT + 1], F32)
        ckvT = sb(ps_, "ckvT", [128, 2, T], BF16)
        ckve = sb(ps_, "ckve", [128, NT, 258], BF16)
        kiT = [sb(ps_, "kiT%d" % i_, [128, T], BF16) for i_ in range(2)]
        xts = [sb(ps_, "axt%d" % i, [128, D], F32) for i in range(4)]
        xbs = [Buf() for _ in range(4)]
        st = sb(ps_, "ast", [128, 16], F32)
        xsb = sb(ps_, "axsb", [128, D], BF16)
        hT = sb(ps_, "ahT", [128, 8, 128], BF16)
        qT = sb(ps_, "qT", [128, 8, 128], BF16)
        qiT = sb(ps_, "qiT", [128, 4, 128], BF16)
        qlT = [sb(ps_, "qlT%d" % i, [128, 2, 1024], BF16) for i in range(3)]
        ctm = sb(ps_, "ctm", [128, 256], BF16)
        junk = ctm
        ktm = sb(ps_, "ktm", [128, 64], F32)
        kd = sb(ps_, "kd", [128, 2, 128], BF16)
        wi = sb(ps_, "wi", [128, 8], F32)
        dg = sb(ps_, "dg", [128, 8, 128], BF16)
        scores = [sb(ps_, "score%d" % i_, [128, T], F32) for i_ in range(2)]
        B_scs = [Buf(), Buf()]
        NR = 3
        Rh = [sb(ps_, "Rh%d" % i, [128, 512], BF16) for i in range(NR)]
        B_Rh = [Buf() for _ in range(NR)]
        nms = [sb(ps_, "nm%d" % i_, [128, T], BF16) for i_ in range(2)]
        B_nms = [Buf(), Buf()]
        ident4 = sb(ps_, "ident4", [128, 4, 128], BF16)
        thb = sb(ps_, "thb", [128, 8 + NIT + 8], F32)
        PT = [sb(ps_, "PT%d" % i, [128, 512], BF16) for i in range(2)]
        B_PT = [Buf() for _ in range(2)]
        ol = sb(ps_, "ol", [128, 8, 256], BF16)
        olT = sb(ps_, "olT", [128, 8, 128], BF16)
        oT = sb(ps_, "oT", [128, 8, 128], BF16)
        rden = sb(ps_, "rden", [128, 8], F32)
        B_w, B_k, B_gb, B_junk, B_st, B_xsb, B_hT = Buf(), Buf(), Buf(), Buf(), Buf(), Buf(), Buf()
        B_q, B_qi, B_ct, B_kt, B_kd, B_wi, B_dg, B_sc, B_nm, B_th = (Buf() for _ in range(10))
        B_ql = [Buf(), Buf(), Buf()]
        B_ckvT = [Buf() for _ in range(NT)]
        B_ckve = [Buf() for _ in range(NT)]
        B_kiT = [Buf() for _ in range(NT)]
        B_ol, B_olT, B_oT, B_rd = Buf(), Buf(), Buf(), Buf()
        if j == 0:
            for kc in range(8):
                kb.dma("pool", win[:, kc, :], d_awin[j, kc * 128:(kc + 1) * 128, :], writes=[B_w])
            kb.dma("pool", wuk[:], d_awuk[j].rearrange("h d r -> d h r"), writes=[B_w])
            kb.dma("pool", wuv[:], d_awuv[j].rearrange("h (c p) d -> p h c d", p=128), writes=[B_w])
            bg_cast_all()
        else:
            for kc in range(8):
                kb.dma("sp", win[:, kc, :], w16["awin1"][kc * 128:(kc + 1) * 128, :], reads=[B_w16["awin1"]],
                       writes=[B_w])
            kb.dma("sp", wuk[:], w16["awuk1"].rearrange("(h d) r -> d h r", h=8), reads=[B_w16["awuk1"]], writes=[B_w])
            kb.dma("sp", wuv[:], w16["awuv1"].rearrange("(h c p) d -> p h c d", h=8, p=128), reads=[B_w16["awuv1"]],
                   writes=[B_w])
        kb.dma("sp", gkv[:], d_agkv[j:j + 1, :].to_broadcast([128, 256]), writes=[B_k])
        kb.op("dve", lambda e: e.tensor_scalar(out=gkv[:], in0=gkv[:], scalar1=16.0, scalar2=None, op0=ALU.mult),
              reads=[B_k], writes=[B_k])
        kb.dma("sp", gki[:], d_agki[j:j + 1, :].to_broadcast([128, 64]), writes=[B_k])
        kb.dma("sp", bki[:], d_abki[j:j + 1, :].to_broadcast([128, 64]), writes=[B_k])
        kb.dma("sp", scores[1][:, 0:D], dgs[l * 2, :, :], reads=[B_gs[l * 2]], writes=[B_scs[1]])
        for kc in range(8):
            slot = scores[0][:, (kc % 4) * D:(kc % 4 + 1) * D]
            kb.dma("sp", slot, d_awo[j, kc * 128:(kc + 1) * 128, :], writes=[B_scs[0]])
            kb.op("dve", lambda e, kc=kc, slot=slot: e.tensor_tensor(out=wo[:, kc, :], in0=slot, in1=scores[1][:, 0:D],
                                                                     op=ALU.mult),
                  reads=[B_scs[0], B_scs[1]], writes=[B_w])
        kb.op("pool", lambda e: e.memset(cmask[:], 0.0), writes=[B_k])
        kb.op("pool", lambda e: e.memset(kd[:], 0.0), writes=[B_kd])
        kb.op("pool", lambda e: e.affine_select(out=cmask[:], in_=cmask[:], pattern=[[-1, 128]], compare_op=ALU.is_ge,
                                                fill=NEG, base=0, channel_multiplier=1), reads=[B_k], writes=[B_k])
        for k in range(NIT + 1):
            kb.op("pool", lambda e, k=k: e.memset(pw2[:, k:k + 1], 2.0 ** (-(k + 1))), writes=[B_k])
        kb.op("pool", lambda e: e.memset(ckve[:], 1.0), writes=B_ckve)
        kb.op("pool", lambda e: e.tensor_copy(out=ident4[:], in_=ident_b[:].unsqueeze(1).to_broadcast([128, 4, 128])),
              reads=[B_const], writes=[B_k])
        A = amix[:, l, :]
        S = modT[:, l, 0:8]
        SC_W = float(8 ** -0.5 * 64 ** -0.5)
        m8, lo_, w_, t_, c_, g_, thr = (thb[:, 0:8], thb[:, 8:9], thb[:, 9:10], thb[:, 10:11], thb[:, 11:12],
                                        thb[:, 12:13], thb[:, 13:14])
        wk = thb[:, 14:14 + NIT + 1]

        def pre_front(i):
            xt, xb = xts[i % 4], xbs[i % 4]
            load_x(src, i, xt, xb)
            norm_tile(xt, xb, xsb[:], B_xsb, st[:, 0:1], st[:, 1:2], B_st, xsb[:], B_xsb)

        def stage_a(i):
            xt, xb = xts[i % 4], xbs[i % 4]
            score, B_sc = scores[i % 2], B_scs[i % 2]
            make_hT(xsb, B_xsb, lambda kc: hT[:, kc, :], B_hT, A, S)
            for grp in range(3):
                pt, pb = pbank()
                for a in range(4):
                    ch = grp * 4 + a
                    c0 = ch * 128 if ch < 8 else 1280 + (ch - 8) * 128
                    for kc in range(8):
                        kb.op("pe", lambda e, pt=pt, a=a, c0=c0, kc=kc: e.matmul(
                            pt[:, a * 128:(a + 1) * 128], lhsT=win[:, kc, c0:c0 + 128], rhs=hT[:, kc, :],
                            start=(kc == 0), stop=(kc == 7)), reads=[B_w, B_hT], writes=[pb])
                if grp < 2:
                    kb.op("act", lambda e, pt=pt, grp=grp: e.activation(
                        out=qT[:, grp * 4:(grp + 1) * 4, :].rearrange("p a t -> p (a t)"), in_=pt[:], func=AF.Identity),
                        reads=[pb], writes=[B_q])
                else:
                    kb.op("act", lambda e, pt=pt: e.activation(
                        out=qiT[:].rearrange("p a t -> p (a t)"), in_=pt[:], func=AF.Identity),
                        reads=[pb], writes=[B_qi])
            pt, pb = pbank()
            for kc in range(8):
                kb.op("pe", lambda e, pt=pt, kc=kc: e.matmul(pt[:, 0:256], lhsT=hT[:, kc, :], rhs=win[:, kc, 1024:1280],
                                                             start=(kc == 0), stop=(kc == 7)),
                      reads=[B_w, B_hT], writes=[pb])
            for kc in range(8):
                kb.op("pe", lambda e, pt=pt, kc=kc: e.matmul(pt[:, 256:328], lhsT=hT[:, kc, :], rhs=win[:, kc, 1792:1864],
                                                             start=(kc == 0), stop=(kc == 7)),
                      reads=[B_w, B_hT], writes=[pb])
            kb.op("act", lambda e, pt=pt: e.activation(out=junk[:], in_=pt[:, 0:256], func=AF.Square,
                                                       accum_out=st[:, 2:3]), reads=[pb], writes=[B_junk, B_ct])
            rstd_from_ss(st[:, 3:4], st[:, 2:3], B_ct, 256.0, B_ct)
            kb.op("dve", lambda e, pt=pt: e.scalar_tensor_tensor(out=ckve[:, i, 0:256], in0=pt[:, 0:256],
                                                                 scalar=st[:, 3:4], in1=gkv[:], op0=ALU.mult,
                                                                 op1=ALU.mult),
                  reads=[pb, B_ct, B_k], writes=[B_ckve[i]])
            kb.op("dve", lambda e, pt=pt: e.bn_stats(out=st[:, 4:10], in_=pt[:, 256:320]), reads=[pb], writes=[B_kt])
            kb.op("dve", lambda e: e.bn_aggr(out=st[:, 10:12], in_=st[:, 4:10]), reads=[B_kt], writes=[B_kt])
            kb.op("pool", lambda e: e.tensor_scalar(out=st[:, 12:13], in0=st[:, 11:12], scalar1=EPS, scalar2=None,
                                                    op0=ALU.add), reads=[B_kt], writes=[B_kt])
            kb.op("pool", lambda e: e.tensor_tensor(out=st[:, 12:13], in0=st[:, 12:13], in1=mhalf[:], op=ALU.pow),
                  reads=[B_kt, B_const], writes=[B_kt])
            kb.op("dve", lambda e, pt=pt: e.tensor_scalar(out=ktm[:], in0=pt[:, 256:320], scalar1=st[:, 10:11],
                                                          scalar2=st[:, 12:13], op0=ALU.subtract, op1=ALU.mult),
                  reads=[pb, B_kt], writes=[B_kt])
            kb.op("dve", lambda e: e.tensor_tensor(out=ktm[:], in0=ktm[:], in1=gki[:], op=ALU.mult),
                  reads=[B_kt, B_k], writes=[B_kt])
            for a_ in range(2):
                kb.op("dve", lambda e, a_=a_: e.tensor_tensor(out=kd[:, a_, a_ * 64:(a_ + 1) * 64], in0=ktm[:],
                                                              in1=bki[:], op=ALU.add),
                      reads=[B_kt, B_k], writes=[B_kd])
            kb.op("dve", lambda e, pt=pt: e.tensor_scalar(out=wi[:], in0=pt[:, 320:328], scalar1=SC_W, scalar2=None,
                                                          op0=ALU.mult), reads=[pb], writes=[B_wi])
            for h in range(8):
                kb.op("dve", lambda e, h=h: e.tensor_scalar(out=dg[:, h, :], in0=ident_b[:], scalar1=wi[:, h:h + 1],
                                                            scalar2=None, op0=ALU.mult),
                      reads=[B_wi, B_const], writes=[B_dg])
            for rc in range(2):
                kb.op("pe", lambda e, rc=rc: e.transpose(out=TB[:, rc * 128:(rc + 1) * 128],
                                                         in_=ckve[:, i, rc * 128:(rc + 1) * 128], identity=ident_b[:]),
                      reads=[B_ckve[i], B_const], writes=[TBb])
            for a_ in range(2):
                kb.op("pe", lambda e, a_=a_: e.transpose(out=TB[:, 256 + a_ * 128:384 + a_ * 128], in_=kd[:, a_, :],
                                                         identity=ident_b[:]), reads=[B_kd, B_const], writes=[TBb])
            kb.op("dve", lambda e: e.tensor_copy(out=ckvT[:, :, i * 128:(i + 1) * 128],
                                                 in_=TB[:, 0:256].rearrange("p (c t) -> p c t", c=2)),
                  reads=[TBb], writes=[B_ckvT[i]])
            for a_ in range(2):
                kb.op("dve", lambda e, a_=a_: e.tensor_copy(out=kiT[a_][:, i * 128:(i + 1) * 128],
                                                            in_=TB[:, 256 + a_ * 128:384 + a_ * 128]),
                      reads=[TBb], writes=[B_kiT[i]])
            ql, qlb = qlT[i % 3], B_ql[i % 3]
            for rc in range(2):
                pts = [pbank(), pbank()]
                for h in range(8):
                    pt2, pb2 = pts[h // 4]
                    kb.op("pe", lambda e, pt2=pt2, h=h, rc=rc: e.matmul(
                        pt2[:, (h % 4) * 128:(h % 4 + 1) * 128], lhsT=wuk[:, h, rc * 128:(rc + 1) * 128],
                        rhs=qT[:, h, :], start=True, stop=True), reads=[B_w, B_q], writes=[pb2])
                for hg in range(2):
                    pt2, pb2 = pts[hg]
                    kb.op("act", lambda e, pt2=pt2, hg=hg, rc=rc, ql=ql: e.activation(
                        out=ql[:, rc, hg * 512:(hg + 1) * 512], in_=pt2[:], func=AF.Identity,
                        scale=float(128 ** -0.5)), reads=[pb2], writes=[qlb])
        def idx_gen(i):
            score, B_sc = scores[i % 2], B_scs[i % 2]
            n = (i + 1) * 128
            rr = 0
            for c0 in range(0, n, 512):
                wd_ = min(512, n - c0)
                kblk = [B_kiT[b] for b in range(c0 // 128, (c0 + wd_) // 128)]
                rl = []
                pt3, pb3 = banks[7], bankb[7]

                def hsum(h, rl=rl, pt3=pt3, pb3=pb3, wd_=wd_):
                    R, Rb = rl[h]
                    kb.op("pe", lambda e: e.matmul(pt3[:, 0:wd_], lhsT=dg[:, h, :], rhs=R[:, 0:wd_],
                                                   start=(h == 0), stop=(h == 7)), reads=[B_dg, Rb], writes=[pb3])
                for h in range(8):
                    pt2, pb2 = banks[6], bankb[6]
                    p0 = (h % 2) * 64
                    kb.op("pe", lambda e, pt2=pt2, h=h, p0=p0, c0=c0, wd_=wd_: e.matmul(
                        pt2[:, 0:wd_], lhsT=qiT[:, h // 2, :], rhs=kiT[h % 2][:, c0:c0 + wd_],
                        start=True, stop=True), reads=[B_qi] + kblk, writes=[pb2])
                    R, Rb = Rh[rr % NR], B_Rh[rr % NR]
                    rr += 1
                    kb.op("act", lambda e, pt2=pt2, R=R, wd_=wd_: e.activation(out=R[:, 0:wd_], in_=pt2[:, 0:wd_],
                                                                               func=AF.Relu),
                          reads=[pb2], writes=[Rb])
                    rl.append((R, Rb))
                    if h >= 2:
                        hsum(h - 2)
                    yield
                hsum(6)
                hsum(7)
                if c0 + wd_ == n:
                    if wd_ > 128:
                        kb.op("dve", lambda e, pt3=pt3, c0=c0, wd_=wd_: e.tensor_copy(
                            out=score[:, c0:c0 + wd_ - 128], in_=pt3[:, 0:wd_ - 128]), reads=[pb3], writes=[B_sc])
                    kb.op("dve", lambda e, pt3=pt3, c0=c0, wd_=wd_: e.tensor_tensor(
                        out=score[:, n - 128:n], in0=pt3[:, wd_ - 128:wd_], in1=cmask[:], op=ALU.add),
                        reads=[pb3, B_k], writes=[B_sc])
                else:
                    kb.op("dve", lambda e, pt3=pt3, c0=c0, wd_=wd_: e.tensor_copy(
                        out=score[:, c0:c0 + wd_], in_=pt3[:, 0:wd_]), reads=[pb3], writes=[B_sc])

        def t1_gen(i):
            n = (i + 1) * 128
            nm, B_nm = nms[i % 2], B_nms[i % 2]
            score, B_sc = scores[i % 2], B_scs[i % 2]
            if i < 2:
                kb.op("dve", lambda e: e.memset(thr, -1.0e29), writes=[B_th])
            else:
                kb.op("dve", lambda e: e.max(out=m8, in_=score[:, 0:n]), reads=[B_sc], writes=[B_th])
                yield
                kb.op("dve", lambda e: e.tensor_reduce(out=lo_, in_=score[:, 0:n - 128], axis=AX.X, op=ALU.min),
                      reads=[B_sc], writes=[B_th])
                kb.op("dve", lambda e: e.tensor_tensor(out=w_, in0=thb[:, 7:8], in1=lo_, op=ALU.subtract),
                      reads=[B_th], writes=[B_th])
                kb.op("dve", lambda e: e.tensor_scalar(out=wk, in0=pw2[:], scalar1=w_, scalar2=None, op0=ALU.mult),
                      reads=[B_th, B_k], writes=[B_th])
                kb.op("dve", lambda e: e.tensor_tensor(out=t_, in0=lo_, in1=thb[:, 14:15], op=ALU.add),
                      reads=[B_th], writes=[B_th])
                yield
                for k in range(NIT):
                    kb.op("dve", lambda e: e.tensor_scalar(out=nm[:, 0:n], in0=score[:, 0:n], scalar1=t_, scalar2=0.0,
                                                           op0=ALU.is_ge, op1=ALU.add, accum_out=c_),
                          reads=[B_sc, B_th], writes=[B_nm, B_th])
                    kb.op("dve", lambda e: e.tensor_scalar(out=g_, in0=c_, scalar1=float(TOPK) - 0.5, scalar2=0.5,
                                                           op0=ALU.is_ge, op1=ALU.subtract), reads=[B_th], writes=[B_th])
                    kb.op("dve", lambda e, k=k: e.scalar_tensor_tensor(out=t_, in0=g_, scalar=thb[:, 14 + k:15 + k],
                                                                       in1=t_, op0=ALU.mult, op1=ALU.add),
                          reads=[B_th], writes=[B_th])
                    yield
                kb.op("dve", lambda e: e.tensor_tensor(out=thr, in0=t_, in1=thb[:, 14 + NIT:15 + NIT], op=ALU.subtract),
                      reads=[B_th], writes=[B_th])
            kb.op("dve", lambda e: e.tensor_scalar(out=nm[:, 0:n], in0=score[:, 0:n], scalar1=thr, scalar2=-30000.0,
                                                   op0=ALU.is_lt, op1=ALU.mult), reads=[B_sc, B_th], writes=[B_nm])
            yield

        def stage_b(i, tick=None, drain=None, pre=None):
            xt, xb = xts[i % 4], xbs[i % 4]
            ql, qlb = qlT[i % 3], B_ql[i % 3]
            nm_i, B_nm_i = nms[i % 2], B_nms[i % 2]
            pi = 0
            for hg in range(2):
                accs = [(banks[b_], bankb[b_]) for b_ in range(4)]
                def qk(sbk, hg=hg):
                    pt, pb = pbank2()
                    for rc in range(2):
                        kb.op("pe", lambda e, pt=pt, rc=rc: e.matmul(
                            pt[:], lhsT=ckvT[:, rc, sbk * 128:(sbk + 1) * 128], rhs=ql[:, rc, hg * 512:(hg + 1) * 512],
                            start=(rc == 0), stop=False), reads=[B_ckvT[sbk], qlb], writes=[pb])
                    kb.op("pe", lambda e, pt=pt: e.matmul(
                        pt[:], lhsT=nm_i[:, sbk * 128:(sbk + 1) * 128], rhs=ident4[:].rearrange("p a t -> p (a t)"),
                        start=False, stop=True), reads=[B_nm_i, B_k], writes=[pb])
                    return pt, pb

                cur = qk(0)
                for sbk in range(i + 1):
                    pt, pb = cur
                    P_, Pb = PT[pi % 2], B_PT[pi % 2]
                    pi += 1
                    kb.op("act", lambda e, pt=pt, P_=P_: e.activation(out=P_[:], in_=pt[:], func=AF.Exp),
                          reads=[pb], writes=[Pb])
                    if sbk + 1 <= i:
                        cur = qk(sbk + 1)
                    if tick is not None:
                        tick()
                    for a in range(4):
                        at, ab = accs[a]
                        kb.op("pe", lambda e, at=at, a=a, P_=P_, sbk=sbk: e.matmul(
                            at[:, 0:257], lhsT=P_[:, a * 128:(a + 1) * 128], rhs=ckve[:, sbk, 0:257],
                            start=(sbk == 0), stop=(sbk == i)), reads=[Pb, B_ckve[sbk]], writes=[ab])
                for a in range(4):
                    h = hg * 4 + a
                    at, ab = accs[a]
                    kb.op("dve", lambda e, at=at, h=h: e.reciprocal(out=rden[:, h:h + 1], in_=at[:, 256:257]),
                          reads=[ab], writes=[B_rd])
                    kb.op("dve", lambda e, at=at, h=h: e.tensor_scalar(out=ol[:, h, :], in0=at[:, 0:256],
                                                                       scalar1=rden[:, h:h + 1], scalar2=None,
                                                                       op0=ALU.mult), reads=[ab, B_rd], writes=[B_ol])
            if drain is not None:
                drain()
            if pre is not None:
                pre()
            for half in range(2):
                for a in range(8):
                    blk = half * 8 + a
                    h, rc = blk // 2, blk % 2
                    kb.op("pe", lambda e, a=a, h=h, rc=rc: e.transpose(out=TB[:, a * 128:(a + 1) * 128],
                                                                       in_=ol[:, h, rc * 128:(rc + 1) * 128],
                                                                       identity=ident_b[:]),
                          reads=[B_ol, B_const], writes=[TBb])
                kb.op("dve", lambda e: e.tensor_copy(
                    out=olT[:], in_=TB[:].rearrange("p (a t) -> p a t", a=8)), reads=[TBb], writes=[B_olT])
                pt, pb = pbank()
                for a in range(4):
                    h = half * 4 + a
                    for rc in range(2):
                        kb.op("pe", lambda e, pt=pt, a=a, h=h, rc=rc: e.matmul(
                            pt[:, a * 128:(a + 1) * 128], lhsT=wuv[:, h, rc, :], rhs=olT[:, a * 2 + rc, :],
                            start=(rc == 0), stop=(rc == 1)), reads=[B_w, B_olT], writes=[pb])
                kb.op("act", lambda e, pt=pt, half=half: e.activation(
                    out=oT[:, half * 4:(half + 1) * 4, :].rearrange("p a t -> p (a t)"), in_=pt[:], func=AF.Identity),
                    reads=[pb], writes=[B_oT])
            ys = []
            for dh in range(2):
                pt, pb = pbank()
                for h in range(8):
                    kb.op("pe", lambda e, pt=pt, h=h, dh=dh: e.matmul(
                        pt[:], lhsT=oT[:, h, :], rhs=wo[:, h, dh * 512:(dh + 1) * 512],
                        start=(h == 0), stop=(h == 7)), reads=[B_w, B_oT], writes=[pb])
                ys.append((pt, pb))
            for dh, (pt, pb) in enumerate(ys):
                kb.op("dve", lambda e, pt=pt, dh=dh: e.tensor_tensor(
                    out=xt[:, dh * 512:(dh + 1) * 512], in0=pt[:], in1=xt[:, dh * 512:(dh + 1) * 512], op=ALU.add),
                    reads=[pb, xb], writes=[xb])
            kb.dma("sp", dxs[i * 128:(i + 1) * 128, :], xt[:], reads=[xb], writes=[B_xs[i]])

        def run_all(g):
            for _ in g:
                pass

        pre_front(0)
        stage_a(0)
        run_all(idx_gen(0))
        if ntiles > 1:
            pre_front(1)
        for i in range(ntiles):
            if i + 1 < ntiles:
                stage_a(i + 1)
            gi = idx_gen(i + 1) if i + 1 < ntiles else iter(())
            gt = t1_gen(i)
            nloop = max(1, 2 * i)
            t_every = max(1, nloop // 16)
            cnt = [0]

            def tick(gi=gi, gt=gt, cnt=cnt, t_every=t_every):
                next(gi, None)
                cnt[0] += 1
                if cnt[0] % t_every == 0:
                    next(gt, None)

            def drain(gi=gi, gt=gt):
                run_all(gi)
                run_all(gt)
            pre = (lambda i=i: pre_front(i + 2)) if i + 2 < ntiles else None
            if i >= 1:
                stage_b(i - 1, tick, drain, pre)
            else:
                drain()
                if pre is not None:
                    pre()
            if i >= 2:
                bg_issue(1)
        stage_b(ntiles - 1)
        bg_issue(1000)
        kb.barrier()
        ps_.close()

    src = dx
    for ph in phases:
        if ph[0] == "mod":
            phase_mod()
        elif ph[0] == "dsa":
            phase_dsa(ph[1], ph[1] // 2, src, *(ph[2:]))
            src = dxs
        elif ph[0] == "gmlp":
            phase_gmlp(ph[1], ph[1] // 2, src)
            src = dxs
        elif ph[0] == "ffn":
            phase_ffn(ph[1], src, *(ph[2:]))
            src = dxs
        elif ph[0] == "final":
            phase_final(src)
        elif ph[0] == "dump":
            B_o = Buf()
            for i in range(NT):
                kb.dma("sp", dout[i * 128:(i + 1) * 128, :], dxs[i * 128:(i + 1) * 128, :], reads=[B_xs[i]], writes=[B_o])
        elif ph[0] == "dbgmod":
            ddbg = dram("dbg", [128, 4 * 48 + 64], kind="ExternalOutput")
            kb.dma("sp", ddbg[:, 0:192], modT[:].rearrange("p l c -> p (l c)"), reads=[B_modT])
            kb.dma("sp", ddbg[:, 192:224], amix[:].rearrange("p l c -> p (l c)"), reads=[B_modT])
            kb.dma("sp", ddbg[:, 224:256], affn[:].rearrange("p l c -> p (l c)"), reads=[B_modT])
    kb.barrier()
    es.close()
    return nc


FULL = [("mod",)]
for _l in range(4):
    FULL.append(("dsa" if _l % 2 == 0 else "gmlp", _l))
    FULL.append(("ffn", _l) if _l < 3 else ("ffn", _l, True))


def _core_inputs(inp, b):
    f = np.float32
    c = np.ascontiguousarray
    m = {
        "x": c(inp["x"][b], dtype=f),
        "cT": c(np.asarray(inp["c"][b], dtype=f).reshape(8, 128).T),
        "mod_w": c(inp["mod_w"], dtype=f),
        "mod_b": c(inp["mod_b"], dtype=f),
        "gmixT": c(np.asarray(inp["norm_mix_g"], dtype=f).reshape(4, 8, 128).transpose(2, 0, 1).reshape(128, 32)),
        "gffnT": c(np.asarray(inp["norm_ffn_g"], dtype=f).reshape(4, 8, 128).transpose(2, 0, 1).reshape(128, 32)),
        "final_g": c(np.asarray(inp["final_g"], dtype=f).reshape(1, D)),
        "a_w_in": c(inp["a_w_in"], dtype=f),
        "a_g_kv": c(inp["a_g_kv"], dtype=f),
        "a_g_kidx": c(inp["a_g_kidx"], dtype=f),
        "a_b_kidx": c(inp["a_b_kidx"], dtype=f),
        "a_w_uk": c(inp["a_w_uk"], dtype=f),
        "a_w_uv": c(inp["a_w_uv"], dtype=f),
        "a_w_o": c(inp["a_w_o"], dtype=f),
        "b_w_in": c(inp["b_w_in"], dtype=f),
        "b_ln_g": c(inp["b_ln_g"], dtype=f),
        "b_ln_b": c(inp["b_ln_b"], dtype=f),
        "b_w_sT": c(np.asarray(inp["b_w_s"], dtype=f).transpose(0, 3, 1, 2).reshape(2, 128, 1024)),
        "b_b_s": c(np.asarray(inp["b_b_s"], dtype=f).reshape(2, 1024)),
        "b_w_out": c(inp["b_w_out"], dtype=f),
        "ffn_w_gate": c(inp["ffn_w_gate"], dtype=f),
        "ffn_w_up": c(inp["ffn_w_up"], dtype=f),
        "ffn_w_down": c(inp["ffn_w_down"], dtype=f),
    }
    return m


def kernel(**inputs):
    inp = {k: np.asarray(v) for k, v in inputs.items()}
    nc = _build(FULL)
    in_maps = [_core_inputs(inp, b) for b in range(8)]
    res = run_bass_kernel_spmd(nc, in_maps, core_ids=list(range(8)))
    out = np.stack([np.asarray(r["out"], dtype=np.float32) for r in res.results], axis=0)
    return out
```

```python
import numpy as np
from contextlib import ExitStack
import concourse.bass as bass
import concourse.mybir as mybir
from concourse.bass_utils import run_bass_kernel_spmd

F32 = mybir.dt.float32
BF16 = mybir.dt.bfloat16
ALU = mybir.AluOpType
AF = mybir.ActivationFunctionType
AX = mybir.AxisListType

T = 4096
D = 1024
NT = 32
DFF = 2816
NFC = 22
A_IN = 1864
TOPK = 256
NIT = 13
EPS = 1e-6
NEG = -1.0e30


class Buf:
    __slots__ = ("w", "r")

    def __init__(self):
        self.w = None
        self.r = {}


class KB:
    def __init__(self, nc, es):
        self.nc = nc
        self.es = es
        self.eng = dict(pe=nc.tensor, dve=nc.vector, act=nc.scalar, pool=nc.gpsimd, sp=nc.sync)
        self.sems = {}
        self.cnt = {}
        self.seen = {e: {} for e in self.eng}
        for e in self.eng:
            self.sems[e] = es.enter_context(nc.semaphore("s_" + e))
            self.cnt[e] = 0
        self.dq = {}
        self.dqi = {}
        for q, n in (("sp", 20), ("pool", 10), ("bg", 3)):
            self.dq[q] = []
            self.dqi[q] = 0
            for i in range(n):
                k = "d_%s%d" % (q, i)
                self.sems[k] = es.enter_context(nc.semaphore(k))
                self.cnt[k] = 0
                self.dq[q].append(k)
        self.nbank = 0

    def _wait(self, e, key, val):
        if val <= 0 or self.seen[e].get(key, 0) >= val:
            return
        self.eng[e].wait_ge(self.sems[key], val)
        self.seen[e][key] = val

    def _deps(self, e, reads, writes, strict=False):
        for b in reads:
            if b.w is not None:
                k, v = b.w
                if strict or k != e or e != "pe":
                    self._wait(e, k, v)
        for b in writes:
            if b.w is not None:
                k, v = b.w
                if strict or k != e:
                    self._wait(e, k, v)
            for k, v in b.r.items():
                if strict or k != e:
                    self._wait(e, k, v)

    def op(self, e, fn, reads=(), writes=()):
        self._deps(e, reads, writes)
        inst = fn(self.eng[e])
        self.cnt[e] += 1
        inst.then_inc(self.sems[e], 1)
        v = self.cnt[e]
        for b in reads:
            b.r[e] = v
        for b in writes:
            b.w = (e, v)
            b.r = {}

    def dma(self, q, out, in_, reads=(), writes=(), sems=None):
        pool = self.dq[sems or q]
        key = pool[self.dqi[sems or q] % len(pool)]
        self.dqi[sems or q] += 1
        self._wait(q, key, self.cnt[key])
        self._deps(q, reads, writes, strict=True)
        inst = self.eng[q].dma_start(out=out, in_=in_)
        self.cnt[key] += 16
        inst.then_inc(self.sems[key], 16)
        v = self.cnt[key]
        for b in reads:
            b.r[key] = v
        for b in writes:
            b.w = (key, v)
            b.r = {}

    def barrier(self):
        keys = list(self.sems.keys())
        for e in self.eng:
            for k in keys:
                if k != e:
                    self._wait(e, k, self.cnt[k])


def _build(phases):
    nc = bass.Bass("TRN2", target_bir_lowering=False)
    es = ExitStack()
    kb = KB(nc, es)

    def dram(name, shape, kind="ExternalInput", dt=F32):
        return nc.dram_tensor(name, list(shape), dt, kind=kind).ap()

    dx = dram("x", [T, D])
    dout = dram("out", [T, D], kind="ExternalOutput")
    dxs = dram("xs", [T, D], kind="Internal")
    dgs = dram("gsc", [8, 128, D], kind="Internal")
    d_cT = dram("cT", [128, 8])
    d_modw = dram("mod_w", [4, D, 6 * D])
    d_modb = dram("mod_b", [4, 6 * D])
    d_gmix = dram("gmixT", [128, 32])
    d_gffn = dram("gffnT", [128, 32])
    d_fg = dram("final_g", [1, D])
    d_awin = dram("a_w_in", [2, D, A_IN])
    d_agkv = dram("a_g_kv", [2, 256])
    d_agki = dram("a_g_kidx", [2, 64])
    d_abki = dram("a_b_kidx", [2, 64])
    d_awuk = dram("a_w_uk", [2, 8, 128, 256])
    d_awuv = dram("a_w_uv", [2, 8, 256, 128])
    d_awo = dram("a_w_o", [2, D, D])
    d_bwin = dram("b_w_in", [2, D, 4096])
    d_blng = dram("b_ln_g", [2, 2048])
    d_blnb = dram("b_ln_b", [2, 2048])
    d_bwsT = dram("b_w_sT", [2, 128, 8 * 128])
    d_bbs = dram("b_b_s", [2, 8 * 128])
    d_bwout = dram("b_w_out", [2, 2048, D])
    d_fwg = dram("ffn_w_gate", [4, D, DFF])
    d_fwu = dram("ffn_w_up", [4, D, DFF])
    d_fwd = dram("ffn_w_down", [4, DFF, D])

    uid = [0]

    w16 = {}
    B_w16 = {}

    bg_pending = []

    def bg_cast(name, src_ap, shape):
        t = dram("w16_" + name, shape, kind="Internal", dt=BF16)
        w16[name] = t
        B_w16[name] = Buf()
        bg_pending.append(lambda: kb.dma("pool", t, src_ap, writes=[B_w16[name]], sems="bg"))

    def bg_issue(n=1):
        for _ in range(n):
            if bg_pending:
                bg_pending.pop(0)()

    def bg_cast_all():
        for l_ in range(4):
            bg_cast("fwg%d" % l_, d_fwg[l_], [D, DFF])
            bg_cast("fwu%d" % l_, d_fwu[l_], [D, DFF])
            bg_cast("fwd%d" % l_, d_fwd[l_], [DFF, D])
            if l_ == 0:
                bg_cast("bwin0", d_bwin[0], [D, 4096])
                bg_cast("bwout0", d_bwout[0], [2048, D])
            if l_ == 1:
                bg_cast("awin1", d_awin[1], [D, A_IN])
                bg_cast("awuk1", d_awuk[1].rearrange("h d r -> (h d) r"), [8 * 128, 256])
                bg_cast("awuv1", d_awuv[1].rearrange("h r d -> (h r) d"), [8 * 256, 128])
            if l_ == 2:
                bg_cast("bwin1", d_bwin[1], [D, 4096])
                bg_cast("bwout1", d_bwout[1], [2048, D])

    def sb(stack, name, shape, dt):
        uid[0] += 1
        return stack.enter_context(nc.sbuf_tensor("%s_%d" % (name, uid[0]), list(shape), dt))

    ident_f = sb(es, "ident_f", [128, 128], F32)
    ident_b = sb(es, "ident_b", [128, 128], BF16)
    mhalf = sb(es, "mhalf", [128, 1], F32)
    modT = sb(es, "modT", [128, 4, 48], F32)
    amix = sb(es, "amix", [128, 4, 8], F32)
    affn = sb(es, "affn", [128, 4, 8], F32)
    gmixT = sb(es, "gmixT_s", [128, 32], F32)
    gffnT = sb(es, "gffnT_s", [128, 32], F32)
    B_const = Buf()
    B_modT = Buf()
    banks = [es.enter_context(nc.psum_tensor("ps%d" % i, [128, 512], F32)) for i in range(8)]
    bankb = [Buf() for _ in range(8)]

    nb2 = [0]

    def pbank():
        i = kb.nbank % 5
        kb.nbank += 1
        return banks[i], bankb[i]

    def pbank2():
        i = 4 + nb2[0] % 2
        nb2[0] += 1
        return banks[i], bankb[i]

    TB = banks[7][:].bitcast(BF16)
    TBb = bankb[7]

    kb.op("pool", lambda e: e.memset(ident_f[:], 1.0), writes=[B_const])
    kb.op("pool", lambda e: e.memset(mhalf[:], -0.5), writes=[B_const])
    kb.op("pool", lambda e: e.affine_select(out=ident_f[:], in_=ident_f[:], pattern=[[-1, 128]],
                                            compare_op=ALU.is_equal, fill=0.0, base=0, channel_multiplier=1),
          reads=[B_const], writes=[B_const])
    kb.op("dve", lambda e: e.tensor_copy(out=ident_b[:], in_=ident_f[:]), reads=[B_const], writes=[B_const])

    def rstd_from_ss(stk_rs, ss, B_ss, n, B_rs):
        kb.op("pool", lambda e: e.tensor_scalar(out=stk_rs, in0=ss, scalar1=float(n) * EPS, scalar2=None,
                                                op0=ALU.add), reads=[B_ss], writes=[B_rs])
        kb.op("pool", lambda e: e.tensor_tensor(out=stk_rs, in0=stk_rs, in1=mhalf[:], op=ALU.pow),
              reads=[B_rs, B_const], writes=[B_rs])

    def phase_mod():
        ps_ = ExitStack()
        cT = sb(ps_, "cT_s", [128, 8], F32)
        cact = sb(ps_, "cact", [128, 8], F32)
        crep = sb(ps_, "crep", [128, 8, 128], F32)
        ones1 = sb(ps_, "ones1", [1, 128], F32)
        mb = sb(ps_, "mb", [1, 6 * D], F32)
        mw = [sb(ps_, "mw%d" % i, [128, 3072], F32) for i in range(5)]
        mwb = [Buf() for _ in range(5)]
        mh = sb(ps_, "mh", [128, 3072], F32)
        dtmp = sb(ps_, "dtmp", [128, 24, 128], F32)
        B_c, B_mb, B_mh, B_dt = Buf(), Buf(), Buf(), Buf()
        kb.dma("sp", cT[:], d_cT[:, :], writes=[B_c])
        kb.dma("sp", gmixT[:], d_gmix[:, :], writes=[B_modT])
        kb.dma("sp", gffnT[:], d_gffn[:, :], writes=[B_modT])
        kb.op("act", lambda e: e.activation(out=cact[:], in_=cT[:], func=AF.Silu), reads=[B_c], writes=[B_c])
        kb.op("pool", lambda e: e.memset(ones1[:], 1.0), writes=[B_c])
        for kc in range(8):
            kb.op("dve", lambda e, kc=kc: e.tensor_copy(out=crep[:, kc, :],
                                                        in_=cact[:, kc:kc + 1].to_broadcast([128, 128])),
                  reads=[B_c], writes=[B_c])
        li = 0
        for l in range(4):
            kb.dma("sp", mb[:], d_modb[l:l + 1, :], writes=[B_mb])
            for half in range(2):
                acc = [(banks[b_], bankb[b_]) for b_ in range(6)]
                for kc in range(8):
                    t_, tb = mw[li % 5], mwb[li % 5]
                    li += 1
                    kb.dma("sp", t_[:], d_modw[l, kc * 128:(kc + 1) * 128, half * 3072:(half + 1) * 3072],
                           writes=[tb])
                    for blk in range(6):
                        pt, pb = acc[blk]
                        if kc == 0:
                            kb.op("pe", lambda e, pt=pt, blk=blk, half=half: e.matmul(
                                pt[:], lhsT=ones1[0:1, :], rhs=mb[0:1, half * 3072 + blk * 512: half * 3072 + (blk + 1) * 512],
                                start=True, stop=False), reads=[B_c, B_mb], writes=[pb])
                        kb.op("pe", lambda e, pt=pt, blk=blk, kc=kc, t_=t_: e.matmul(
                            pt[:], lhsT=crep[:, kc, :], rhs=t_[:, blk * 512:(blk + 1) * 512],
                            start=False, stop=(kc == 7)), reads=[B_c, tb], writes=[pb])
                for blk in range(6):
                    pt, pb = acc[blk]
                    kb.op("dve" if blk % 2 == 0 else "act",
                          (lambda e, pt=pt, blk=blk: e.tensor_copy(out=mh[:, blk * 512:(blk + 1) * 512], in_=pt[:]))
                          if blk % 2 == 0 else
                          (lambda e, pt=pt, blk=blk: e.activation(out=mh[:, blk * 512:(blk + 1) * 512], in_=pt[:],
                                                                  func=AF.Identity)),
                          reads=[pb], writes=[B_mh])
                kb.dma("sp", dgs[l * 2 + half, :, :], mh[:, 2048:3072], reads=[B_mh], writes=[B_gs[l * 2 + half]])
                kb.op("dve", lambda e: e.tensor_tensor(
                    out=dtmp[:], in0=mh[:].rearrange("p (c j) -> p c j", j=128),
                    in1=ident_f[:].unsqueeze(1).to_broadcast([128, 24, 128]), op=ALU.mult),
                    reads=[B_mh, B_const], writes=[B_dt])
                kb.op("dve", lambda e, l=l, half=half: e.tensor_reduce(
                    out=modT[:, l, half * 24:(half + 1) * 24], in_=dtmp[:], axis=AX.X, op=ALU.add),
                    reads=[B_dt], writes=[B_modT])
            kb.op("dve", lambda e, l=l: e.scalar_tensor_tensor(
                out=amix[:, l, :], in0=modT[:, l, 8:16], scalar=1.0, in1=gmixT[:, l * 8:(l + 1) * 8],
                op0=ALU.add, op1=ALU.mult), reads=[B_modT], writes=[B_modT])
            kb.op("dve", lambda e, l=l: e.scalar_tensor_tensor(
                out=affn[:, l, :], in0=modT[:, l, 32:40], scalar=1.0, in1=gffnT[:, l * 8:(l + 1) * 8],
                op0=ALU.add, op1=ALU.mult), reads=[B_modT], writes=[B_modT])
        kb.barrier()
        ps_.close()

    B_gs = [Buf() for _ in range(8)]
    B_xs = [Buf() for _ in range(NT)]

    def load_x(src, i, xt, xb):
        kb.dma("sp", xt[:], src[i * 128:(i + 1) * 128, :], reads=[B_xs[i]] if src is dxs else [], writes=[xb])

    def norm_tile(xt, xb, junk, B_junk, ss, rs, B_st, xsb, B_xsb):
        kb.op("act", lambda e: e.activation(out=junk, in_=xt[:], func=AF.Square, accum_out=ss),
              reads=[xb], writes=[B_junk, B_st])
        rstd_from_ss(rs, ss, B_st, float(D), B_st)
        kb.op("dve", lambda e: e.tensor_scalar(out=xsb, in0=xt[:], scalar1=rs, scalar2=32.0, op0=ALU.mult,
                                               op1=ALU.mult), reads=[xb, B_st], writes=[B_xsb])

    def make_hT(xsb_t, B_xsb, hT_dst, B_hT, A, S):
        for kc in range(8):
            kb.op("pe", lambda e, kc=kc: e.transpose(out=TB[:, kc * 128:(kc + 1) * 128],
                                                     in_=xsb_t[:, kc * 128:(kc + 1) * 128], identity=ident_b[:]),
                  reads=[B_xsb, B_const], writes=[TBb])
        for kc in range(8):
            kb.op("act", lambda e, kc=kc: e.activation(out=hT_dst(kc), in_=TB[:, kc * 128:(kc + 1) * 128],
                                                       func=AF.Identity, scale=A[:, kc:kc + 1], bias=S[:, kc:kc + 1]),
                  reads=[TBb, B_modT], writes=[B_hT])

    def residual_store(ypairs, xt, xb, gb, B_gb, tmps, dst, dstb, fin=None):
        for dh, (pt, pb) in enumerate(ypairs):
            tmp, tb = tmps[dh]
            kb.op("dve", lambda e, pt=pt, dh=dh, tmp=tmp: e.tensor_tensor(
                out=tmp[:], in0=pt[:], in1=gb[:, dh * 512:(dh + 1) * 512], op=ALU.mult),
                reads=[pb, B_gb], writes=[tb])
            kb.op("pool", lambda e, dh=dh, tmp=tmp: e.tensor_tensor(
                out=xt[:, dh * 512:(dh + 1) * 512], in0=xt[:, dh * 512:(dh + 1) * 512], in1=tmp[:], op=ALU.add),
                reads=[tb, xb], writes=[xb])
        if fin is None:
            kb.dma("sp", dst, xt[:], reads=[xb], writes=[dstb])
        else:
            fg, B_fg, ot, B_ot, junk, B_junk, ss, rs, B_fs, odst, B_od = fin
            kb.op("act", lambda e: e.activation(out=junk, in_=xt[:], func=AF.Square, accum_out=ss),
                  reads=[xb], writes=[B_junk, B_fs])
            rstd_from_ss(rs, ss, B_fs, float(D), B_fs)
            kb.op("dve", lambda e: e.scalar_tensor_tensor(out=ot[:], in0=xt[:], scalar=rs, in1=fg[:], op0=ALU.mult,
                                                          op1=ALU.mult), reads=[xb, B_fs, B_fg], writes=[B_ot])
            kb.dma("sp", odst, ot[:], reads=[B_ot], writes=[B_od])

    def phase_ffn(l, src, fuse_final=False):
        ps_ = ExitStack()
        wg = sb(ps_, "wg", [128, 8, DFF], BF16)
        wu = sb(ps_, "wu", [128, 8, DFF], BF16)
        wd = sb(ps_, "wd", [128, NFC, D], BF16)
        gb = sb(ps_, "gb", [128, D], F32)
        xts = [sb(ps_, "xt%d" % i, [128, D], F32) for i in range(4)]
        xbs = [Buf() for _ in range(4)]
        junk = sb(ps_, "junk", [128, D], BF16)
        st = sb(ps_, "st", [128, 4], F32)
        xsb = [sb(ps_, "xsb%d" % i, [128, D], BF16) for i in range(2)]
        hT = [sb(ps_, "hT%d" % i, [128, 8, 256], BF16) for i in range(2)]
        actT = sb(ps_, "actT", [128, NFC, 256], BF16)
        sg = [sb(ps_, "sg%d" % i, [128, 256], F32) for i in range(2)]
        tmps = [(sb(ps_, "tmp%d" % i, [128, 512], F32), Buf()) for i in range(2)]
        B_w, B_gb, B_junk = Buf(), Buf(), Buf()
        if fuse_final:
            fg = sb(ps_, "ffg", [128, D], F32)
            ots = [sb(ps_, "fot%d" % i, [128, D], F32) for i in range(2)]
            B_ots = [Buf(), Buf()]
            fst = sb(ps_, "ffst", [128, 4], F32)
            B_fst = [Buf(), Buf()]
            B_fg, B_od = Buf(), Buf()
            kb.dma("sp", fg[:], d_fg[0:1, :].to_broadcast([128, D]), writes=[B_fg])
            kb.op("dve", lambda e: e.tensor_scalar(out=fg[:], in0=fg[:], scalar1=32.0, scalar2=None, op0=ALU.mult),
                  reads=[B_fg], writes=[B_fg])
        B_st = [Buf(), Buf()]
        B_xsb = [Buf(), Buf()]
        B_hT = [Buf(), Buf()]
        B_sg = [Buf(), Buf()]
        B_act = [Buf() for _ in range(NFC)]
        B_wg = [Buf() for _ in range(8)]
        B_wu = [Buf() for _ in range(8)]
        B_wd = [Buf() for _ in range(NFC)]
        for kc in range(8):
            kb.dma("sp", wg[:, kc, :], w16["fwg%d" % l][kc * 128:(kc + 1) * 128, :], reads=[B_w16["fwg%d" % l]],
                   writes=[B_wg[kc]])
        for kc in range(8):
            kb.dma("sp", wu[:, kc, :], w16["fwu%d" % l][kc * 128:(kc + 1) * 128, :], reads=[B_w16["fwu%d" % l]],
                   writes=[B_wu[kc]])
        for fc in range(NFC):
            kb.dma("sp", wd[:, fc, :], w16["fwd%d" % l][fc * 128:(fc + 1) * 128, :], reads=[B_w16["fwd%d" % l]],
                   writes=[B_wd[fc]])
        kb.dma("sp", gb[:], dgs[l * 2 + 1, :, :], reads=[B_gs[l * 2 + 1]], writes=[B_gb])
        A = affn[:, l, :]
        S = modT[:, l, 24:32]
        NG = NT // 2

        def prep_a(g):
            for tl in range(2):
                i = g * 2 + tl
                load_x(src, i, xts[i % 4], xbs[i % 4])

        def prep_b(g):
            for tl in range(2):
                i = g * 2 + tl
                k = i % 2
                norm_tile(xts[i % 4], xbs[i % 4], junk[:], B_junk, st[:, 2 * k:2 * k + 1], st[:, 2 * k + 1:2 * k + 2],
                          B_st[k], xsb[k][:], B_xsb[k])
                make_hT(xsb[k], B_xsb[k], lambda kc, tl=tl, g=g: hT[g % 2][:, kc, tl * 128:(tl + 1) * 128],
                        B_hT[g % 2], A, S)

        prep_a(0)
        prep_b(0)
        for g in range(NG):
            h = hT[g % 2]
            hb = B_hT[g % 2]
            if g + 1 < NG:
                prep_a(g + 1)
            for fc in range(NFC):
                pt, pb = pbank()
                for slot, w in ((0, wg), (1, wu)):
                    for kc in range(8):
                        kb.op("pe", lambda e, pt=pt, slot=slot, w=w, kc=kc, fc=fc: e.matmul(
                            pt[:, slot * 256:(slot + 1) * 256], lhsT=w[:, kc, fc * 128:(fc + 1) * 128],
                            rhs=h[:, kc, :], start=(kc == 0), stop=(kc == 7)),
                            reads=[(B_wg if slot == 0 else B_wu)[kc], hb], writes=[pb])
                s_ = sg[fc % 2]
                sbf = B_sg[fc % 2]
                kb.op("act", lambda e, pt=pt, s_=s_: e.activation(out=s_[:], in_=pt[:, 0:256], func=AF.Silu),
                      reads=[pb], writes=[sbf])
                kb.op("dve", lambda e, pt=pt, s_=s_, fc=fc: e.tensor_tensor(
                    out=actT[:, fc, :], in0=pt[:, 256:512], in1=s_[:], op=ALU.mult),
                    reads=[pb, sbf], writes=[B_act[fc]])
                if fc == 6 and g + 1 < NG:
                    prep_b(g + 1)
            for tl in range(2):
                i = g * 2 + tl
                ys = []
                for dh in range(2):
                    pt, pb = pbank()
                    for fc in range(NFC):
                        kb.op("pe", lambda e, pt=pt, fc=fc, tl=tl, dh=dh: e.matmul(
                            pt[:], lhsT=actT[:, fc, tl * 128:(tl + 1) * 128], rhs=wd[:, fc, dh * 512:(dh + 1) * 512],
                            start=(fc == 0), stop=(fc == NFC - 1)), reads=[B_wd[fc], B_act[fc]], writes=[pb])
                    ys.append((pt, pb))
                fin = None
                if fuse_final:
                    k_ = i % 2
                    fin = (fg, B_fg, ots[k_], B_ots[k_], junk[:], B_junk, fst[:, 2 * k_:2 * k_ + 1],
                           fst[:, 2 * k_ + 1:2 * k_ + 2], B_fst[k_], dout[i * 128:(i + 1) * 128, :], B_od)
                residual_store(ys, xts[i % 4], xbs[i % 4], gb, B_gb, tmps, dxs[i * 128:(i + 1) * 128, :], B_xs[i], fin)
        kb.barrier()
        ps_.close()

    def phase_final(src):
        ps_ = ExitStack()
        fg = sb(ps_, "fg", [128, D], F32)
        xts = [sb(ps_, "fxt%d" % i, [128, D], F32) for i in range(3)]
        xbs = [Buf() for _ in range(3)]
        ots = [sb(ps_, "fot%d" % i, [128, D], F32) for i in range(2)]
        obs = [Buf() for _ in range(2)]
        junk = sb(ps_, "fjunk", [128, D], BF16)
        st = sb(ps_, "fst", [128, 4], F32)
        B_st = [Buf(), Buf()]
        B_fg, B_junk, B_o = Buf(), Buf(), Buf()
        kb.dma("sp", fg[:], d_fg[0:1, :].to_broadcast([128, D]), writes=[B_fg])
        kb.op("dve", lambda e: e.tensor_scalar(out=fg[:], in0=fg[:], scalar1=32.0, scalar2=None, op0=ALU.mult),
              reads=[B_fg], writes=[B_fg])
        for i in range(NT):
            k = i % 2
            xt, xb = xts[i % 3], xbs[i % 3]
            load_x(src, i, xt, xb)
            ss, rs = st[:, 2 * k:2 * k + 1], st[:, 2 * k + 1:2 * k + 2]
            kb.op("act", lambda e, xt=xt, ss=ss: e.activation(out=junk[:], in_=xt[:], func=AF.Square, accum_out=ss),
                  reads=[xb], writes=[B_junk, B_st[k]])
            rstd_from_ss(rs, ss, B_st[k], float(D), B_st[k])
            kb.op("dve", lambda e, xt=xt, rs=rs, k=k: e.scalar_tensor_tensor(
                out=ots[k][:], in0=xt[:], scalar=rs, in1=fg[:], op0=ALU.mult, op1=ALU.mult),
                reads=[xb, B_st[k], B_fg], writes=[obs[k]])
            kb.dma("sp", dout[i * 128:(i + 1) * 128, :], ots[k][:], reads=[obs[k]], writes=[B_o])
        kb.barrier()
        ps_.close()

    def phase_gmlp(l, j, src):
        ps_ = ExitStack()
        win = sb(ps_, "bwin", [128, 8, 4096], BF16)
        wout = sb(ps_, "bwout", [128, 16, D], BF16)
        wcT = sb(ps_, "wcT", [128, 8, 128], BF16)
        lng = sb(ps_, "lng", [128, 2048], F32)
        lnb = sb(ps_, "lnb", [128, 2048], F32)
        bsb = sb(ps_, "bsb", [128, 8, 128], F32)
        gb = sb(ps_, "ggb", [128, D], F32)
        xts = [sb(ps_, "gxt%d" % i, [128, D], F32) for i in range(4)]
        xbs = [Buf() for _ in range(4)]
        st = sb(ps_, "gst", [128, 4], F32)
        xsb = [sb(ps_, "gxsb%d" % i, [128, D], BF16) for i in range(2)]
        hT = [sb(ps_, "ghT%d" % i, [128, 8, 256], BF16) for i in range(2)]
        uT = sb(ps_, "uT", [128, 16, 256], F32)
        vr = [sb(ps_, "vr%d" % i, [128, 2048], F32) for i in range(2)]
        vt = [sb(ps_, "vt%d" % i, [128, 2048], BF16) for i in range(2)]
        bst = sb(ps_, "bst", [128, 2, 4, 6], F32)
        lst = sb(ps_, "lst", [128, 2, 4], F32)
        ypT = sb(ps_, "ypT", [128, 16, 256], BF16)
        ytm = [sb(ps_, "ytm0", [128, 512], F32)] * 2
        tmps = [(sb(ps_, "gtmp%d" % i, [128, 512], F32), Buf()) for i in range(2)]
        B_w, B_gb, B_junk, B_k = Buf(), Buf(), Buf(), Buf()
        B_st = [Buf(), Buf()]
        B_xsb = [Buf(), Buf()]
        B_hT = [Buf(), Buf()]
        B_u = [Buf() for _ in range(16)]
        B_vr = [Buf(), Buf()]
        B_vt = [Buf(), Buf()]
        B_ls = [Buf(), Buf()]
        B_yp = [[Buf() for _ in range(4)] for _ in range(2)]
        B_ytm = [Buf()] * 2
        for kc in range(8):
            kb.dma("sp", win[:, kc, :], w16["bwin%d" % j][kc * 128:(kc + 1) * 128, :], reads=[B_w16["bwin%d" % j]],
                   writes=[B_w])
        for ec in range(16):
            kb.dma("sp", wout[:, ec, :], w16["bwout%d" % j][ec * 128:(ec + 1) * 128, :], reads=[B_w16["bwout%d" % j]],
                   writes=[B_w])
        wcf = vr[0][:, 0:1024].rearrange("p (g t) -> p g t", g=8)
        kb.dma("sp", wcf, d_bwsT[j, :, :].rearrange("s (g t) -> s g t", g=8), writes=[B_vr[0]])
        kb.dma("sp", lng[:], d_blng[j:j + 1, :].to_broadcast([128, 2048]), writes=[B_k])
        kb.dma("sp", lnb[:], d_blnb[j:j + 1, :].to_broadcast([128, 2048]), writes=[B_k])
        kb.dma("sp", bsb[:], d_bbs[j:j + 1, :].to_broadcast([128, 1024]).rearrange("p (g t) -> p g t", g=8),
               writes=[B_k])
        kb.dma("sp", gb[:], dgs[l * 2, :, :], reads=[B_gs[l * 2]], writes=[B_gb])
        kb.op("pool", lambda e: e.affine_select(out=wcf, in_=wcf, pattern=[[0, 8], [1, 128]],
                                                compare_op=ALU.is_ge, fill=0.0, base=0, channel_multiplier=-1),
              reads=[B_vr[0]], writes=[B_vr[0]])
        kb.op("dve", lambda e: e.tensor_copy(out=wcT[:], in_=wcf), reads=[B_vr[0]], writes=[B_k])
        A = amix[:, l, :]
        S = modT[:, l, 0:8]
        NG = NT // 2

        def prep_a(g):
            for tl in range(2):
                i = g * 2 + tl
                load_x(src, i, xts[i % 4], xbs[i % 4])

        def prep_b(g):
            for tl in range(2):
                i = g * 2 + tl
                k = i % 2
                norm_tile(xts[i % 4], xbs[i % 4], xsb[k][:], B_xsb[k], st[:, 2 * k:2 * k + 1], st[:, 2 * k + 1:2 * k + 2],
                          B_st[k], xsb[k][:], B_xsb[k])
                make_hT(xsb[k], B_xsb[k], lambda kc, tl=tl, g=g: hT[g % 2][:, kc, tl * 128:(tl + 1) * 128],
                        B_hT[g % 2], A, S)

        prep_a(0)
        prep_b(0)
        for g in range(NG):
            h = hT[g % 2]
            hb = B_hT[g % 2]
            if g + 1 < NG:
                prep_a(g + 1)
            for tl in range(2):
                for cb in range(4):
                    pt, pb = pbank()
                    for kc in range(8):
                        kb.op("pe", lambda e, pt=pt, kc=kc, cb=cb, tl=tl: e.matmul(
                            pt[:], lhsT=h[:, kc, tl * 128:(tl + 1) * 128],
                            rhs=win[:, kc, 2048 + cb * 512:2048 + (cb + 1) * 512],
                            start=(kc == 0), stop=(kc == 7)), reads=[B_w, hb], writes=[pb])
                    kb.op("act", lambda e, pt=pt, cb=cb, tl=tl: e.activation(
                        out=vr[tl][:, cb * 512:(cb + 1) * 512], in_=pt[:], func=AF.Gelu),
                        reads=[pb], writes=[B_vr[tl]])
                    kb.op("dve", lambda e, cb=cb, tl=tl: e.bn_stats(out=bst[:, tl, cb, :],
                                                                    in_=vr[tl][:, cb * 512:(cb + 1) * 512]),
                          reads=[B_vr[tl]], writes=[B_ls[tl]])
                kb.op("dve", lambda e, tl=tl: e.bn_aggr(out=lst[:, tl, 0:2],
                                                        in_=bst[:, tl, :, :].rearrange("p a b -> p (a b)")),
                      reads=[B_ls[tl]], writes=[B_ls[tl]])
                kb.op("pool", lambda e, tl=tl: e.tensor_scalar(out=lst[:, tl, 2:3], in0=lst[:, tl, 1:2], scalar1=EPS,
                                                               scalar2=None, op0=ALU.add),
                      reads=[B_ls[tl]], writes=[B_ls[tl]])
                kb.op("pool", lambda e, tl=tl: e.tensor_tensor(out=lst[:, tl, 2:3], in0=lst[:, tl, 2:3], in1=mhalf[:],
                                                               op=ALU.pow),
                      reads=[B_ls[tl], B_const], writes=[B_ls[tl]])
                kb.op("dve", lambda e, tl=tl: e.tensor_scalar(out=vr[tl][:], in0=vr[tl][:], scalar1=lst[:, tl, 0:1],
                                                              scalar2=lst[:, tl, 2:3], op0=ALU.subtract, op1=ALU.mult),
                      reads=[B_ls[tl], B_vr[tl]], writes=[B_vr[tl]])
                kb.op("dve", lambda e, tl=tl: e.tensor_tensor(out=vr[tl][:], in0=vr[tl][:], in1=lng[:], op=ALU.mult),
                      reads=[B_vr[tl], B_k], writes=[B_vr[tl]])
                kb.op("dve", lambda e, tl=tl: e.tensor_tensor(out=vt[tl][:], in0=vr[tl][:], in1=lnb[:], op=ALU.add),
                      reads=[B_vr[tl], B_k], writes=[B_vt[tl]])
            for ec in range(16):
                if ec % 2 == 0:
                    pt, pb = pbank()
                half = ec % 2
                for kc in range(8):
                    kb.op("pe", lambda e, pt=pt, half=half, kc=kc, ec=ec: e.matmul(
                        pt[:, half * 256:(half + 1) * 256], lhsT=win[:, kc, ec * 128:(ec + 1) * 128],
                        rhs=h[:, kc, :], start=(kc == 0), stop=(kc == 7)), reads=[B_w, hb], writes=[pb])
                if ec % 2 == 1:
                    kb.op("act", lambda e, pt=pt, ec=ec: e.activation(
                        out=uT[:, ec - 1:ec + 1, :].rearrange("p a t -> p (a t)"), in_=pt[:], func=AF.Gelu),
                        reads=[pb], writes=[B_u[ec - 1], B_u[ec]])
            if g + 1 < NG:
                prep_b(g + 1)
            for tl in range(2):
                i = g * 2 + tl
                for q4 in range(4):
                    pt, pb = pbank()
                    for a in range(4):
                        ec = q4 * 4 + a
                        kb.op("pe", lambda e, pt=pt, a=a, ec=ec, tl=tl: e.matmul(
                            pt[:, a * 128:(a + 1) * 128], lhsT=vt[tl][:, ec * 128:(ec + 1) * 128],
                            rhs=wcT[:, ec // 2, :], start=True, stop=True), reads=[B_vt[tl], B_k], writes=[pb])
                    ym, ymb = ytm[q4 % 2], B_ytm[q4 % 2]
                    kb.op("dve", lambda e, pt=pt, q4=q4, ym=ym: e.tensor_tensor(
                        out=ym[:].rearrange("p (g r t) -> p g r t", g=2, r=2),
                        in0=pt[:].rearrange("p (g r t) -> p g r t", g=2, r=2),
                        in1=bsb[:, q4 * 2:q4 * 2 + 2, :].unsqueeze(2).to_broadcast([128, 2, 2, 128]), op=ALU.add),
                        reads=[pb, B_k], writes=[ymb])
                    kb.op("dve", lambda e, q4=q4, tl=tl, ym=ym: e.tensor_tensor(
                        out=ypT[:, q4 * 4:(q4 + 1) * 4, tl * 128:(tl + 1) * 128],
                        in0=ym[:].rearrange("p (a t) -> p a t", a=4),
                        in1=uT[:, q4 * 4:(q4 + 1) * 4, tl * 128:(tl + 1) * 128], op=ALU.mult),
                        reads=[ymb] + [B_u[q4 * 4 + a] for a in range(4)], writes=[B_yp[tl][q4]])
                ys = []
                for dh in range(2):
                    pt, pb = pbank()
                    for ec in range(16):
                        kb.op("pe", lambda e, pt=pt, ec=ec, tl=tl, dh=dh: e.matmul(
                            pt[:], lhsT=ypT[:, ec, tl * 128:(tl + 1) * 128], rhs=wout[:, ec, dh * 512:(dh + 1) * 512],
                            start=(ec == 0), stop=(ec == 15)), reads=[B_w, B_yp[tl][ec // 4]], writes=[pb])
                    ys.append((pt, pb))
                residual_store(ys, xts[i % 4], xbs[i % 4], gb, B_gb, tmps, dxs[i * 128:(i + 1) * 128, :], B_xs[i])
        kb.barrier()
        ps_.close()

    def phase_dsa(l, j, src, ntiles=NT):
        ps_ = ExitStack()
        win = sb(ps_, "awin", [128, 8, A_IN], BF16)
        wuk = sb(ps_, "awuk", [128, 8, 256], BF16)
        wuv = sb(ps_, "awuv", [128, 8, 2, 128], BF16)
        wo = sb(ps_, "awo", [128, 8, D], BF16)
        gkv = sb(ps_, "gkv", [128, 256], F32)
        gki = sb(ps_, "gki", [128, 64], F32)
        bki = sb(ps_, "bki", [128, 64], F32)
        cmask = sb(ps_, "cmask", [128, 128], F32)
        pw2 = sb(ps_, "pw2", [128, NIT + 1], F32)
        ckvT = sb(ps_, "ckvT", [128, 2, T], BF16)
        ckve = sb(ps_, "ckve", [128, NT, 258], BF16)
        kiT = [sb(ps_, "kiT%d" % i_, [128, T], BF16) for i_ in range(2)]
        xts = [sb(ps_, "axt%d" % i, [128, D], F32) for i in range(4)]
        xbs = [Buf() for _ in range(4)]
        st = sb(ps_, "ast", [128, 16], F32)
        xsb = sb(ps_, "axsb", [128, D], BF16)
        hT = sb(ps_, "ahT", [128, 8, 128], BF16)
        qT = sb(ps_, "qT", [128, 8, 128], BF16)
        qiT = sb(ps_, "qiT", [128, 4, 128], BF16)
        qlT = [sb(ps_, "qlT%d" % i, [128, 2, 1024], BF16) for i in range(3)]
        ctm = sb(ps_, "ctm", [128, 256], BF16)
        junk = ctm
        ktm = sb(ps_, "ktm", [128, 64], F32)
        kd = sb(ps_, "kd", [128, 2, 128], BF16)
        wi = sb(ps_, "wi", [128, 8], F32)
        dg = sb(ps_, "dg", [128, 8, 128], BF16)
        scores = [sb(ps_, "score%d" % i_, [128, T], F32) for i_ in range(2)]
        B_scs = [Buf(), Buf()]
        NR = 3
        Rh = [sb(ps_, "Rh%d" % i, [128, 512], BF16) for i in range(NR)]
        B_Rh = [Buf() for _ in range(NR)]
        nms = [sb(ps_, "nm%d" % i_, [128, T], BF16) for i_ in range(2)]
        B_nms = [Buf(), Buf()]
        ident4 = sb(ps_, "ident4", [128, 4, 128], BF16)
        thb = sb(ps_, "thb", [128, 8 + NIT + 8], F32)
        PT = [sb(ps_, "PT%d" % i, [128, 512], BF16) for i in range(2)]
        B_PT = [Buf() for _ in range(2)]
        ol = sb(ps_, "ol", [128, 8, 256], BF16)
        olT = sb(ps_, "olT", [128, 8, 128], BF16)
        oT = sb(ps_, "oT", [128, 8, 128], BF16)
        rden = sb(ps_, "rden", [128, 8], F32)
        B_w, B_k, B_gb, B_junk, B_st, B_xsb, B_hT = Buf(), Buf(), Buf(), Buf(), Buf(), Buf(), Buf()
        B_q, B_qi, B_ct, B_kt, B_kd, B_wi, B_dg, B_sc, B_nm, B_th = (Buf() for _ in range(10))
        B_ql = [Buf(), Buf(), Buf()]
        B_ckvT = [Buf() for _ in range(NT)]
        B_ckve = [Buf() for _ in range(NT)]
        B_kiT = [Buf() for _ in range(NT)]
        B_ol, B_olT, B_oT, B_rd = Buf(), Buf(), Buf(), Buf()
        if j == 0:
            for kc in range(8):
                kb.dma("pool", win[:, kc, :], d_awin[j, kc * 128:(kc + 1) * 128, :], writes=[B_w])
            kb.dma("pool", wuk[:], d_awuk[j].rearrange("h d r -> d h r"), writes=[B_w])
            kb.dma("pool", wuv[:], d_awuv[j].rearrange("h (c p) d -> p h c d", p=128), writes=[B_w])
            bg_cast_all()
        else:
            for kc in range(8):
                kb.dma("sp", win[:, kc, :], w16["awin1"][kc * 128:(kc + 1) * 128, :], reads=[B_w16["awin1"]],
                       writes=[B_w])
            kb.dma("sp", wuk[:], w16["awuk1"].rearrange("(h d) r -> d h r", h=8), reads=[B_w16["awuk1"]], writes=[B_w])
            kb.dma("sp", wuv[:], w16["awuv1"].rearrange("(h c p) d -> p h c d", h=8, p=128), reads=[B_w16["awuv1"]],
                   writes=[B_w])
        kb.dma("sp", gkv[:], d_agkv[j:j + 1, :].to_broadcast([128, 256]), writes=[B_k])
        kb.op("dve", lambda e: e.tensor_scalar(out=gkv[:], in0=gkv[:], scalar1=16.0, scalar2=None, op0=ALU.mult),
              reads=[B_k], writes=[B_k])
        kb.dma("sp", gki[:], d_agki[j:j + 1, :].to_broadcast([128, 64]), writes=[B_k])
        kb.dma("sp", bki[:], d_abki[j:j + 1, :].to_broadcast([128, 64]), writes=[B_k])
        kb.dma("sp", scores[1][:, 0:D], dgs[l * 2, :, :], reads=[B_gs[l * 2]], writes=[B_scs[1]])
        for kc in range(8):
            slot = scores[0][:, (kc % 4) * D:(kc % 4 + 1) * D]
            kb.dma("sp", slot, d_awo[j, kc * 128:(kc + 1) * 128, :], writes=[B_scs[0]])
            kb.op("dve", lambda e, kc=kc, slot=slot: e.tensor_tensor(out=wo[:, kc, :], in0=slot, in1=scores[1][:, 0:D],
                                                                     op=ALU.mult),
                  reads=[B_scs[0], B_scs[1]], writes=[B_w])
        kb.op("pool", lambda e: e.memset(cmask[:], 0.0), writes=[B_k])
        kb.op("pool", lambda e: e.memset(kd[:], 0.0), writes=[B_kd])
        kb.op("pool", lambda e: e.affine_select(out=cmask[:], in_=cmask[:], pattern=[[-1, 128]], compare_op=ALU.is_ge,
                                                fill=NEG, base=0, channel_multiplier=1), reads=[B_k], writes=[B_k])
        for k in range(NIT + 1):
            kb.op("pool", lambda e, k=k: e.memset(pw2[:, k:k + 1], 2.0 ** (-(k + 1))), writes=[B_k])
        kb.op("pool", lambda e: e.memset(ckve[:], 1.0), writes=B_ckve)
        kb.op("pool", lambda e: e.tensor_copy(out=ident4[:], in_=ident_b[:].unsqueeze(1).to_broadcast([128, 4, 128])),
              reads=[B_const], writes=[B_k])
        A = amix[:, l, :]
        S = modT[:, l, 0:8]
        SC_W = float(8 ** -0.5 * 64 ** -0.5)
        m8, lo_, w_, t_, c_, g_, thr = (thb[:, 0:8], thb[:, 8:9], thb[:, 9:10], thb[:, 10:11], thb[:, 11:12],
                                        thb[:, 12:13], thb[:, 13:14])
        wk = thb[:, 14:14 + NIT + 1]

        def pre_front(i):
            xt, xb = xts[i % 4], xbs[i % 4]
            load_x(src, i, xt, xb)
            norm_tile(xt, xb, xsb[:], B_xsb, st[:, 0:1], st[:, 1:2], B_st, xsb[:], B_xsb)

        def stage_a(i):
            xt, xb = xts[i % 4], xbs[i % 4]
            score, B_sc = scores[i % 2], B_scs[i % 2]
            make_hT(xsb, B_xsb, lambda kc: hT[:, kc, :], B_hT, A, S)
            for grp in range(3):
                pt, pb = pbank()
                for a in range(4):
                    ch = grp * 4 + a
                    c0 = ch * 128 if ch < 8 else 1280 + (ch - 8) * 128
                    for kc in range(8):
                        kb.op("pe", lambda e, pt=pt, a=a, c0=c0, kc=kc: e.matmul(
                            pt[:, a * 128:(a + 1) * 128], lhsT=win[:, kc, c0:c0 + 128], rhs=hT[:, kc, :],
                            start=(kc == 0), stop=(kc == 7)), reads=[B_w, B_hT], writes=[pb])
                if grp < 2:
                    kb.op("act", lambda e, pt=pt, grp=grp: e.activation(
                        out=qT[:, grp * 4:(grp + 1) * 4, :].rearrange("p a t -> p (a t)"), in_=pt[:], func=AF.Identity),
                        reads=[pb], writes=[B_q])
                else:
                    kb.op("act", lambda e, pt=pt: e.activation(
                        out=qiT[:].rearrange("p a t -> p (a t)"), in_=pt[:], func=AF.Identity),
                        reads=[pb], writes=[B_qi])
            pt, pb = pbank()
            for kc in range(8):
                kb.op("pe", lambda e, pt=pt, kc=kc: e.matmul(pt[:, 0:256], lhsT=hT[:, kc, :], rhs=win[:, kc, 1024:1280],
                                                             start=(kc == 0), stop=(kc == 7)),
                      reads=[B_w, B_hT], writes=[pb])
            for kc in range(8):
                kb.op("pe", lambda e, pt=pt, kc=kc: e.matmul(pt[:, 256:328], lhsT=hT[:, kc, :], rhs=win[:, kc, 1792:1864],
                                                             start=(kc == 0), stop=(kc == 7)),
                      reads=[B_w, B_hT], writes=[pb])
            kb.op("act", lambda e, pt=pt: e.activation(out=junk[:], in_=pt[:, 0:256], func=AF.Square,
                                                       accum_out=st[:, 2:3]), reads=[pb], writes=[B_junk, B_ct])
            rstd_from_ss(st[:, 3:4], st[:, 2:3], B_ct, 256.0, B_ct)
            kb.op("dve", lambda e, pt=pt: e.scalar_tensor_tensor(out=ckve[:, i, 0:256], in0=pt[:, 0:256],
                                                                 scalar=st[:, 3:4], in1=gkv[:], op0=ALU.mult,
                                                                 op1=ALU.mult),
                  reads=[pb, B_ct, B_k], writes=[B_ckve[i]])
            kb.op("dve", lambda e, pt=pt: e.bn_stats(out=st[:, 4:10], in_=pt[:, 256:320]), reads=[pb], writes=[B_kt])
            kb.op("dve", lambda e: e.bn_aggr(out=st[:, 10:12], in_=st[:, 4:10]), reads=[B_kt], writes=[B_kt])
            kb.op("pool", lambda e: e.tensor_scalar(out=st[:, 12:13], in0=st[:, 11:12], scalar1=EPS, scalar2=None,
                                                    op0=ALU.add), reads=[B_kt], writes=[B_kt])
            kb.op("pool", lambda e: e.tensor_tensor(out=st[:, 12:13], in0=st[:, 12:13], in1=mhalf[:], op=ALU.pow),
                  reads=[B_kt, B_const], writes=[B_kt])
            kb.op("dve", lambda e, pt=pt: e.tensor_scalar(out=ktm[:], in0=pt[:, 256:320], scalar1=st[:, 10:11],
                                                          scalar2=st[:, 12:13], op0=ALU.subtract, op1=ALU.mult),
                  reads=[pb, B_kt], writes=[B_kt])
            kb.op("dve", lambda e: e.tensor_tensor(out=ktm[:], in0=ktm[:], in1=gki[:], op=ALU.mult),
                  reads=[B_kt, B_k], writes=[B_kt])
            for a_ in range(2):
                kb.op("dve", lambda e, a_=a_: e.tensor_tensor(out=kd[:, a_, a_ * 64:(a_ + 1) * 64], in0=ktm[:],
                                                              in1=bki[:], op=ALU.add),
                      reads=[B_kt, B_k], writes=[B_kd])
            kb.op("dve", lambda e, pt=pt: e.tensor_scalar(out=wi[:], in0=pt[:, 320:328], scalar1=SC_W, scalar2=None,
                                                          op0=ALU.mult), reads=[pb], writes=[B_wi])
            for h in range(8):
                kb.op("dve", lambda e, h=h: e.tensor_scalar(out=dg[:, h, :], in0=ident_b[:], scalar1=wi[:, h:h + 1],
                                                            scalar2=None, op0=ALU.mult),
                      reads=[B_wi, B_const], writes=[B_dg])
            for rc in range(2):
                kb.op("pe", lambda e, rc=rc: e.transpose(out=TB[:, rc * 128:(rc + 1) * 128],
                                                         in_=ckve[:, i, rc * 128:(rc + 1) * 128], identity=ident_b[:]),
                      reads=[B_ckve[i], B_const], writes=[TBb])
            for a_ in range(2):
                kb.op("pe", lambda e, a_=a_: e.transpose(out=TB[:, 256 + a_ * 128:384 + a_ * 128], in_=kd[:, a_, :],
                                                         identity=ident_b[:]), reads=[B_kd, B_const], writes=[TBb])
            kb.op("dve", lambda e: e.tensor_copy(out=ckvT[:, :, i * 128:(i + 1) * 128],
                                                 in_=TB[:, 0:256].rearrange("p (c t) -> p c t", c=2)),
                  reads=[TBb], writes=[B_ckvT[i]])
            for a_ in range(2):
                kb.op("dve", lambda e, a_=a_: e.tensor_copy(out=kiT[a_][:, i * 128:(i + 1) * 128],
                                                            in_=TB[:, 256 + a_ * 128:384 + a_ * 128]),
                      reads=[TBb], writes=[B_kiT[i]])
            ql, qlb = qlT[i % 3], B_ql[i % 3]
            for rc in range(2):
                pts = [pbank(), pbank()]
                for h in range(8):
                    pt2, pb2 = pts[h // 4]
                    kb.op("pe", lambda e, pt2=pt2, h=h, rc=rc: e.matmul(
                        pt2[:, (h % 4) * 128:(h % 4 + 1) * 128], lhsT=wuk[:, h, rc * 128:(rc + 1) * 128],
                        rhs=qT[:, h, :], start=True, stop=True), reads=[B_w, B_q], writes=[pb2])
                for hg in range(2):
                    pt2, pb2 = pts[hg]
                    kb.op("act", lambda e, pt2=pt2, hg=hg, rc=rc, ql=ql: e.activation(
                        out=ql[:, rc, hg * 512:(hg + 1) * 512], in_=pt2[:], func=AF.Identity,
                        scale=float(128 ** -0.5)), reads=[pb2], writes=[qlb])
        def idx_gen(i):
            score, B_sc = scores[i % 2], B_scs[i % 2]
            n = (i + 1) * 128
            rr = 0
            for c0 in range(0, n, 512):
                wd_ = min(512, n - c0)
                kblk = [B_kiT[b] for b in range(c0 // 128, (c0 + wd_) // 128)]
                rl = []
                pt3, pb3 = banks[7], bankb[7]

                def hsum(h, rl=rl, pt3=pt3, pb3=pb3, wd_=wd_):
                    R, Rb = rl[h]
                    kb.op("pe", lambda e: e.matmul(pt3[:, 0:wd_], lhsT=dg[:, h, :], rhs=R[:, 0:wd_],
                                                   start=(h == 0), stop=(h == 7)), reads=[B_dg, Rb], writes=[pb3])
                for h in range(8):
                    pt2, pb2 = banks[6], bankb[6]
                    p0 = (h % 2) * 64
                    kb.op("pe", lambda e, pt2=pt2, h=h, p0=p0, c0=c0, wd_=wd_: e.matmul(
                        pt2[:, 0:wd_], lhsT=qiT[:, h // 2, :], rhs=kiT[h % 2][:, c0:c0 + wd_],
                        start=True, stop=True), reads=[B_qi] + kblk, writes=[pb2])
                    R, Rb = Rh[rr % NR], B_Rh[rr % NR]
                    rr += 1
                    kb.op("act", lambda e, pt2=pt2, R=R, wd_=wd_: e.activation(out=R[:, 0:wd_], in_=pt2[:, 0:wd_],
                                                                               func=AF.Relu),
                          reads=[pb2], writes=[Rb])
                    rl.append((R, Rb))
                    if h >= 2:
                        hsum(h - 2)
                    yield
                hsum(6)
                hsum(7)
                if c0 + wd_ == n:
                    if wd_ > 128:
                        kb.op("dve", lambda e, pt3=pt3, c0=c0, wd_=wd_: e.tensor_copy(
                            out=score[:, c0:c0 + wd_ - 128], in_=pt3[:, 0:wd_ - 128]), reads=[pb3], writes=[B_sc])
                    kb.op("dve", lambda e, pt3=pt3, c0=c0, wd_=wd_: e.tensor_tensor(
                        out=score[:, n - 128:n], in0=pt3[:, wd_ - 128:wd_], in1=cmask[:], op=ALU.add),
                        reads=[pb3, B_k], writes=[B_sc])
                else:
                    kb.op("dve", lambda e, pt3=pt3, c0=c0, wd_=wd_: e.tensor_copy(
                        out=score[:, c0:c0 + wd_], in_=pt3[:, 0:wd_]), reads=[pb3], writes=[B_sc])

        def t1_gen(i):
            n = (i + 1) * 128
            nm, B_nm = nms[i % 2], B_nms[i % 2]
            score, B_sc = scores[i % 2], B_scs[i % 2]
            if i < 2:
                kb.op("dve", lambda e: e.memset(thr, -1.0e29), writes=[B_th])
            else:
                kb.op("dve", lambda e: e.max(out=m8, in_=score[:, 0:n]), reads=[B_sc], writes=[B_th])
                yield
                kb.op("dve", lambda e: e.tensor_reduce(out=lo_, in_=score[:, 0:n - 128], axis=AX.X, op=ALU.min),
                      reads=[B_sc], writes=[B_th])
                kb.op("dve", lambda e: e.tensor_tensor(out=w_, in0=thb[:, 7:8], in1=lo_, op=ALU.subtract),
                      reads=[B_th], writes=[B_th])
                kb.op("dve", lambda e: e.tensor_scalar(out=wk, in0=pw2[:], scalar1=w_, scalar2=None, op0=ALU.mult),
                      reads=[B_th, B_k], writes=[B_th])
                kb.op("dve", lambda e: e.tensor_tensor(out=t_, in0=lo_, in1=thb[:, 14:15], op=ALU.add),
                      reads=[B_th], writes=[B_th])
                yield
                for k in range(NIT):
                    kb.op("dve", lambda e: e.tensor_scalar(out=nm[:, 0:n], in0=score[:, 0:n], scalar1=t_, scalar2=0.0,
                                                           op0=ALU.is_ge, op1=ALU.add, accum_out=c_),
                          reads=[B_sc, B_th], writes=[B_nm, B_th])
                    kb.op("dve", lambda e: e.tensor_scalar(out=g_, in0=c_, scalar1=float(TOPK) - 0.5, scalar2=0.5,
                                                           op0=ALU.is_ge, op1=ALU.subtract), reads=[B_th], writes=[B_th])
                    kb.op("dve", lambda e, k=k: e.scalar_tensor_tensor(out=t_, in0=g_, scalar=thb[:, 14 + k:15 + k],
                                                                       in1=t_, op0=ALU.mult, op1=ALU.add),
                          reads=[B_th], writes=[B_th])
                    yield
                kb.op("dve", lambda e: e.tensor_tensor(out=thr, in0=t_, in1=thb[:, 14 + NIT:15 + NIT], op=ALU.subtract),
                      reads=[B_th], writes=[B_th])
            kb.op("dve", lambda e: e.tensor_scalar(out=nm[:, 0:n], in0=score[:, 0:n], scalar1=thr, scalar2=-30000.0,
                                                   op0=ALU.is_lt, op1=ALU.mult), reads=[B_sc, B_th], writes=[B_nm])
            yield

        def stage_b(i, tick=None, drain=None, pre=None):
            xt, xb = xts[i % 4], xbs[i % 4]
            ql, qlb = qlT[i % 3], B_ql[i % 3]
            nm_i, B_nm_i = nms[i % 2], B_nms[i % 2]
            pi = 0
            for hg in range(2):
                accs = [(banks[b_], bankb[b_]) for b_ in range(4)]
                def qk(sbk, hg=hg):
                    pt, pb = pbank2()
                    for rc in range(2):
                        kb.op("pe", lambda e, pt=pt, rc=rc: e.matmul(
                            pt[:], lhsT=ckvT[:, rc, sbk * 128:(sbk + 1) * 128], rhs=ql[:, rc, hg * 512:(hg + 1) * 512],
                            start=(rc == 0), stop=False), reads=[B_ckvT[sbk], qlb], writes=[pb])
                    kb.op("pe", lambda e, pt=pt: e.matmul(
                        pt[:], lhsT=nm_i[:, sbk * 128:(sbk + 1) * 128], rhs=ident4[:].rearrange("p a t -> p (a t)"),
                        start=False, stop=True), reads=[B_nm_i, B_k], writes=[pb])
                    return pt, pb

                cur = qk(0)
                for sbk in range(i + 1):
                    pt, pb = cur
                    P_, Pb = PT[pi % 2], B_PT[pi % 2]
                    pi += 1
                    kb.op("act", lambda e, pt=pt, P_=P_: e.activation(out=P_[:], in_=pt[:], func=AF.Exp),
                          reads=[pb], writes=[Pb])
                    if sbk + 1 <= i:
                        cur = qk(sbk + 1)
                    if tick is not None:
                        tick()
                    for a in range(4):
                        at, ab = accs[a]
                        kb.op("pe", lambda e, at=at, a=a, P_=P_, sbk=sbk: e.matmul(
                            at[:, 0:257], lhsT=P_[:, a * 128:(a + 1) * 128], rhs=ckve[:, sbk, 0:257],
                            start=(sbk == 0), stop=(sbk == i)), reads=[Pb, B_ckve[sbk]], writes=[ab])
                for a in range(4):
                    h = hg * 4 + a
                    at, ab = accs[a]
                    kb.op("dve", lambda e, at=at, h=h: e.reciprocal(out=rden[:, h:h + 1], in_=at[:, 256:257]),
                          reads=[ab], writes=[B_rd])
                    kb.op("dve", lambda e, at=at, h=h: e.tensor_scalar(out=ol[:, h, :], in0=at[:, 0:256],
                                                                       scalar1=rden[:, h:h + 1], scalar2=None,
                                                                       op0=ALU.mult), reads=[ab, B_rd], writes=[B_ol])
            if drain is not None:
                drain()
            if pre is not None:
                pre()
            for half in range(2):
                for a in range(8):
                    blk = half * 8 + a
                    h, rc = blk // 2, blk % 2
                    kb.op("pe", lambda e, a=a, h=h, rc=rc: e.transpose(out=TB[:, a * 128:(a + 1) * 128],
                                                                       in_=ol[:, h, rc * 128:(rc + 1) * 128],
                                                                       identity=ident_b[:]),
                          reads=[B_ol, B_const], writes=[TBb])
                kb.op("dve", lambda e: e.tensor_copy(
                    out=olT[:], in_=TB[:].rearrange("p (a t) -> p a t", a=8)), reads=[TBb], writes=[B_olT])
                pt, pb = pbank()
                for a in range(4):
                    h = half * 4 + a
                    for rc in range(2):
                        kb.op("pe", lambda e, pt=pt, a=a, h=h, rc=rc: e.matmul(
                            pt[:, a * 128:(a + 1) * 128], lhsT=wuv[:, h, rc, :], rhs=olT[:, a * 2 + rc, :],
                            start=(rc == 0), stop=(rc == 1)), reads=[B_w, B_olT], writes=[pb])
                kb.op("act", lambda e, pt=pt, half=half: e.activation(
                    out=oT[:, half * 4:(half + 1) * 4, :].rearrange("p a t -> p (a t)"), in_=pt[:], func=AF.Identity),
                    reads=[pb], writes=[B_oT])
            ys = []
            for dh in range(2):
                pt, pb = pbank()
                for h in range(8):
                    kb.op("pe", lambda e, pt=pt, h=h, dh=dh: e.matmul(
                        pt[:], lhsT=oT[:, h, :], rhs=wo[:, h, dh * 512:(dh + 1) * 512],
                        start=(h == 0), stop=(h == 7)), reads=[B_w, B_oT], writes=[pb])
                ys.append((pt, pb))
            for dh, (pt, pb) in enumerate(ys):
                kb.op("dve", lambda e, pt=pt, dh=dh: e.tensor_tensor(
                    out=xt[:, dh * 512:(dh + 1) * 512], in0=pt[:], in1=xt[:, dh * 512:(dh + 1) * 512], op=ALU.add),
                    reads=[pb, xb], writes=[xb])
            kb.dma("sp", dxs[i * 128:(i + 1) * 128, :], xt[:], reads=[xb], writes=[B_xs[i]])

        def run_all(g):
            for _ in g:
                pass

        pre_front(0)
        stage_a(0)
        run_all(idx_gen(0))
        if ntiles > 1:
            pre_front(1)
        for i in range(ntiles):
            if i + 1 < ntiles:
                stage_a(i + 1)
            gi = idx_gen(i + 1) if i + 1 < ntiles else iter(())
            gt = t1_gen(i)
            nloop = max(1, 2 * i)
            t_every = max(1, nloop // 16)
            cnt = [0]

            def tick(gi=gi, gt=gt, cnt=cnt, t_every=t_every):
                next(gi, None)
                cnt[0] += 1
                if cnt[0] % t_every == 0:
                    next(gt, None)

            def drain(gi=gi, gt=gt):
                run_all(gi)
                run_all(gt)
            pre = (lambda i=i: pre_front(i + 2)) if i + 2 < ntiles else None
            if i >= 1:
                stage_b(i - 1, tick, drain, pre)
            else:
                drain()
                if pre is not None:
                    pre()
            if i >= 2:
                bg_issue(1)
        stage_b(ntiles - 1)
        bg_issue(1000)
        kb.barrier()
        ps_.close()

    src = dx
    for ph in phases:
        if ph[0] == "mod":
            phase_mod()
        elif ph[0] == "dsa":
            phase_dsa(ph[1], ph[1] // 2, src, *(ph[2:]))
            src = dxs
        elif ph[0] == "gmlp":
            phase_gmlp(ph[1], ph[1] // 2, src)
            src = dxs
        elif ph[0] == "ffn":
            phase_ffn(ph[1], src, *(ph[2:]))
            src = dxs
        elif ph[0] == "final":
            phase_final(src)
        elif ph[0] == "dump":
            B_o = Buf()
            for i in range(NT):
                kb.dma("sp", dout[i * 128:(i + 1) * 128, :], dxs[i * 128:(i + 1) * 128, :], reads=[B_xs[i]], writes=[B_o])
        elif ph[0] == "dbgmod":
            ddbg = dram("dbg", [128, 4 * 48 + 64], kind="ExternalOutput")
            kb.dma("sp", ddbg[:, 0:192], modT[:].rearrange("p l c -> p (l c)"), reads=[B_modT])
            kb.dma("sp", ddbg[:, 192:224], amix[:].rearrange("p l c -> p (l c)"), reads=[B_modT])
            kb.dma("sp", ddbg[:, 224:256], affn[:].rearrange("p l c -> p (l c)"), reads=[B_modT])
    kb.barrier()
    es.close()
    return nc


FULL = [("mod",)]
for _l in range(4):
    FULL.append(("dsa" if _l % 2 == 0 else "gmlp", _l))
    FULL.append(("ffn", _l) if _l < 3 else ("ffn", _l, True))


def _core_inputs(inp, b):
    f = np.float32
    c = np.ascontiguousarray
    m = {
        "x": c(inp["x"][b], dtype=f),
        "cT": c(np.asarray(inp["c"][b], dtype=f).reshape(8, 128).T),
        "mod_w": c(inp["mod_w"], dtype=f),
        "mod_b": c(inp["mod_b"], dtype=f),
        "gmixT": c(np.asarray(inp["norm_mix_g"], dtype=f).reshape(4, 8, 128).transpose(2, 0, 1).reshape(128, 32)),
        "gffnT": c(np.asarray(inp["norm_ffn_g"], dtype=f).reshape(4, 8, 128).transpose(2, 0, 1).reshape(128, 32)),
        "final_g": c(np.asarray(inp["final_g"], dtype=f).reshape(1, D)),
        "a_w_in": c(inp["a_w_in"], dtype=f),
        "a_g_kv": c(inp["a_g_kv"], dtype=f),
        "a_g_kidx": c(inp["a_g_kidx"], dtype=f),
        "a_b_kidx": c(inp["a_b_kidx"], dtype=f),
        "a_w_uk": c(inp["a_w_uk"], dtype=f),
        "a_w_uv": c(inp["a_w_uv"], dtype=f),
        "a_w_o": c(inp["a_w_o"], dtype=f),
        "b_w_in": c(inp["b_w_in"], dtype=f),
        "b_ln_g": c(inp["b_ln_g"], dtype=f),
        "b_ln_b": c(inp["b_ln_b"], dtype=f),
        "b_w_sT": c(np.asarray(inp["b_w_s"], dtype=f).transpose(0, 3, 1, 2).reshape(2, 128, 1024)),
        "b_b_s": c(np.asarray(inp["b_b_s"], dtype=f).reshape(2, 1024)),
        "b_w_out": c(inp["b_w_out"], dtype=f),
        "ffn_w_gate": c(inp["ffn_w_gate"], dtype=f),
        "ffn_w_up": c(inp["ffn_w_up"], dtype=f),
        "ffn_w_down": c(inp["ffn_w_down"], dtype=f),
    }
    return m


def kernel(**inputs):
    inp = {k: np.asarray(v) for k, v in inputs.items()}
    nc = _build(FULL)
    in_maps = [_core_inputs(inp, b) for b in range(8)]
    res = run_bass_kernel_spmd(nc, in_maps, core_ids=list(range(8)))
    out = np.stack([np.asarray(r["out"], dtype=np.float32) for r in res.results], axis=0)
    return out
```

```python
import numpy as np
from contextlib import ExitStack
import concourse.bass as bass
import concourse.mybir as mybir
from concourse.bass_utils import run_bass_kernel_spmd

F32 = mybir.dt.float32
BF16 = mybir.dt.bfloat16
ALU = mybir.AluOpType
AF = mybir.ActivationFunctionType
AX = mybir.AxisListType

T = 4096
D = 1024
NT = 32
DFF = 2816
NFC = 22
A_IN = 1864
TOPK = 256
NIT = 13
EPS = 1e-6
NEG = -1.0e30


class Buf:
    __slots__ = ("w", "r")

    def __init__(self):
        self.w = None
        self.r = {}


class KB:
    def __init__(self, nc, es):
        self.nc = nc
        self.es = es
        self.eng = dict(pe=nc.tensor, dve=nc.vector, act=nc.scalar, pool=nc.gpsimd, sp=nc.sync)
        self.sems = {}
        self.cnt = {}
        self.seen = {e: {} for e in self.eng}
        for e in self.eng:
            self.sems[e] = es.enter_context(nc.semaphore("s_" + e))
            self.cnt[e] = 0
        self.dq = {}
        self.dqi = {}
        for q, n in (("sp", 20), ("pool", 10), ("bg", 3)):
            self.dq[q] = []
            self.dqi[q] = 0
            for i in range(n):
                k = "d_%s%d" % (q, i)
                self.sems[k] = es.enter_context(nc.semaphore(k))
                self.cnt[k] = 0
                self.dq[q].append(k)
        self.nbank = 0

    def _wait(self, e, key, val):
        if val <= 0 or self.seen[e].get(key, 0) >= val:
            return
        self.eng[e].wait_ge(self.sems[key], val)
        self.seen[e][key] = val

    def _deps(self, e, reads, writes, strict=False):
        for b in reads:
            if b.w is not None:
                k, v = b.w
                if strict or k != e or e != "pe":
                    self._wait(e, k, v)
        for b in writes:
            if b.w is not None:
                k, v = b.w
                if strict or k != e:
                    self._wait(e, k, v)
            for k, v in b.r.items():
                if strict or k != e:
                    self._wait(e, k, v)

    def op(self, e, fn, reads=(), writes=()):
        self._deps(e, reads, writes)
        inst = fn(self.eng[e])
        self.cnt[e] += 1
        inst.then_inc(self.sems[e], 1)
        v = self.cnt[e]
        for b in reads:
            b.r[e] = v
        for b in writes:
            b.w = (e, v)
            b.r = {}

    def dma(self, q, out, in_, reads=(), writes=(), sems=None):
        pool = self.dq[sems or q]
        key = pool[self.dqi[sems or q] % len(pool)]
        self.dqi[sems or q] += 1
        self._wait(q, key, self.cnt[key])
        self._deps(q, reads, writes, strict=True)
        inst = self.eng[q].dma_start(out=out, in_=in_)
        self.cnt[key] += 16
        inst.then_inc(self.sems[key], 16)
        v = self.cnt[key]
        for b in reads:
            b.r[key] = v
        for b in writes:
            b.w = (key, v)
            b.r = {}

    def barrier(self):
        keys = list(self.sems.keys())
        for e in self.eng:
            for k in keys:
                if k != e:
                    self._wait(e, k, self.cnt[k])


def _build(phases):
    nc = bass.Bass("TRN2", target_bir_lowering=False)
    es = ExitStack()
    kb = KB(nc, es)

    def dram(name, shape, kind="ExternalInput", dt=F32):
        return nc.dram_tensor(name, list(shape), dt, kind=kind).ap()

    dx = dram("x", [T, D])
    dout = dram("out", [T, D], kind="ExternalOutput")
    dxs = dram("xs", [T, D], kind="Internal")
    dgs = dram("gsc", [8, 128, D], kind="Internal")
    d_cT = dram("cT", [128, 8])
    d_modw = dram("mod_w", [4, D, 6 * D])
    d_modb = dram("mod_b", [4, 6 * D])
    d_gmix = dram("gmixT", [128, 32])
    d_gffn = dram("gffnT", [128, 32])
    d_fg = dram("final_g", [1, D])
    d_awin = dram("a_w_in", [2, D, A_IN])
    d_agkv = dram("a_g_kv", [2, 256])
    d_agki = dram("a_g_kidx", [2, 64])
    d_abki = dram("a_b_kidx", [2, 64])
    d_awuk = dram("a_w_uk", [2, 8, 128, 256])
    d_awuv = dram("a_w_uv", [2, 8, 256, 128])
    d_awo = dram("a_w_o", [2, D, D])
    d_bwin = dram("b_w_in", [2, D, 4096])
    d_blng = dram("b_ln_g", [2, 2048])
    d_blnb = dram("b_ln_b", [2, 2048])
    d_bwsT = dram("b_w_sT", [2, 128, 8 * 128])
    d_bbs = dram("b_b_s", [2, 8 * 128])
    d_bwout = dram("b_w_out", [2, 2048, D])
    d_fwg = dram("ffn_w_gate", [4, D, DFF])
    d_fwu = dram("ffn_w_up", [4, D, DFF])
    d_fwd = dram("ffn_w_down", [4, DFF, D])

    uid = [0]

    w16 = {}
    B_w16 = {}

    bg_pending = []

    def bg_cast(name, src_ap, shape):
        t = dram("w16_" + name, shape, kind="Internal", dt=BF16)
        w16[name] = t
        B_w16[name] = Buf()
        bg_pending.append(lambda: kb.dma("pool", t, src_ap, writes=[B_w16[name]], sems="bg"))

    def bg_issue(n=1):
        for _ in range(n):
            if bg_pending:
                bg_pending.pop(0)()

    def bg_cast_all():
        for l_ in range(4):
            bg_cast("fwg%d" % l_, d_fwg[l_], [D, DFF])
            bg_cast("fwu%d" % l_, d_fwu[l_], [D, DFF])
            bg_cast("fwd%d" % l_, d_fwd[l_], [DFF, D])
            if l_ == 0:
                bg_cast("bwin0", d_bwin[0], [D, 4096])
                bg_cast("bwout0", d_bwout[0], [2048, D])
            if l_ == 1:
                bg_cast("awin1", d_awin[1], [D, A_IN])
                bg_cast("awuk1", d_awuk[1].rearrange("h d r -> (h d) r"), [8 * 128, 256])
                bg_cast("awuv1", d_awuv[1].rearrange("h r d -> (h r) d"), [8 * 256, 128])
            if l_ == 2:
                bg_cast("bwin1", d_bwin[1], [D, 4096])
                bg_cast("bwout1", d_bwout[1], [2048, D])

    def sb(stack, name, shape, dt):
        uid[0] += 1
        return stack.enter_context(nc.sbuf_tensor("%s_%d" % (name, uid[0]), list(shape), dt))

    ident_f = sb(es, "ident_f", [128, 128], F32)
    ident_b = sb(es, "ident_b", [128, 128], BF16)
    mhalf = sb(es, "mhalf", [128, 1], F32)
    modT = sb(es, "modT", [128, 4, 48], F32)
    amix = sb(es, "amix", [128, 4, 8], F32)
    affn = sb(es, "affn", [128, 4, 8], F32)
    gmixT = sb(es, "gmixT_s", [128, 32], F32)
    gffnT = sb(es, "gffnT_s", [128, 32], F32)
    B_const = Buf()
    B_modT = Buf()
    banks = [es.enter_context(nc.psum_tensor("ps%d" % i, [128, 512], F32)) for i in range(8)]
    bankb = [Buf() for _ in range(8)]

    nb2 = [0]

    def pbank():
        i = kb.nbank % 5
        kb.nbank += 1
        return banks[i], bankb[i]

    def pbank2():
        i = 4 + nb2[0] % 2
        nb2[0] += 1
        return banks[i], bankb[i]

    TB = banks[7][:].bitcast(BF16)
    TBb = bankb[7]

    kb.op("pool", lambda e: e.memset(ident_f[:], 1.0), writes=[B_const])
    kb.op("pool", lambda e: e.memset(mhalf[:], -0.5), writes=[B_const])
    kb.op("pool", lambda e: e.affine_select(out=ident_f[:], in_=ident_f[:], pattern=[[-1, 128]],
                                            compare_op=ALU.is_equal, fill=0.0, base=0, channel_multiplier=1),
          reads=[B_const], writes=[B_const])
    kb.op("dve", lambda e: e.tensor_copy(out=ident_b[:], in_=ident_f[:]), reads=[B_const], writes=[B_const])

    def rstd_from_ss(stk_rs, ss, B_ss, n, B_rs):
        kb.op("pool", lambda e: e.tensor_scalar(out=stk_rs, in0=ss, scalar1=float(n) * EPS, scalar2=None,
                                                op0=ALU.add), reads=[B_ss], writes=[B_rs])
        kb.op("pool", lambda e: e.tensor_tensor(out=stk_rs, in0=stk_rs, in1=mhalf[:], op=ALU.pow),
              reads=[B_rs, B_const], writes=[B_rs])

    def phase_mod():
        ps_ = ExitStack()
        cT = sb(ps_, "cT_s", [128, 8], F32)
        cact = sb(ps_, "cact", [128, 8], F32)
        crep = sb(ps_, "crep", [128, 8, 128], F32)
        ones1 = sb(ps_, "ones1", [1, 128], F32)
        mb = sb(ps_, "mb", [1, 6 * D], F32)
        mw = [sb(ps_, "mw%d" % i, [128, 3072], F32) for i in range(5)]
        mwb = [Buf() for _ in range(5)]
        mh = sb(ps_, "mh", [128, 3072], F32)
        dtmp = sb(ps_, "dtmp", [128, 24, 128], F32)
        B_c, B_mb, B_mh, B_dt = Buf(), Buf(), Buf(), Buf()
        kb.dma("sp", cT[:], d_cT[:, :], writes=[B_c])
        kb.dma("sp", gmixT[:], d_gmix[:, :], writes=[B_modT])
        kb.dma("sp", gffnT[:], d_gffn[:, :], writes=[B_modT])
        kb.op("act", lambda e: e.activation(out=cact[:], in_=cT[:], func=AF.Silu), reads=[B_c], writes=[B_c])
        kb.op("pool", lambda e: e.memset(ones1[:], 1.0), writes=[B_c])
        for kc in range(8):
            kb.op("dve", lambda e, kc=kc: e.tensor_copy(out=crep[:, kc, :],
                                                        in_=cact[:, kc:kc + 1].to_broadcast([128, 128])),
                  reads=[B_c], writes=[B_c])
        li = 0
        for l in range(4):
            kb.dma("sp", mb[:], d_modb[l:l + 1, :], writes=[B_mb])
            for half in range(2):
                acc = [(banks[b_], bankb[b_]) for b_ in range(6)]
                for kc in range(8):
                    t_, tb = mw[li % 5], mwb[li % 5]
                    li += 1
                    kb.dma("sp", t_[:], d_modw[l, kc * 128:(kc + 1) * 128, half * 3072:(half + 1) * 3072],
                           writes=[tb])
                    for blk in range(6):
                        pt, pb = acc[blk]
                        if kc == 0:
                            kb.op("pe", lambda e, pt=pt, blk=blk, half=half: e.matmul(
                                pt[:], lhsT=ones1[0:1, :], rhs=mb[0:1, half * 3072 + blk * 512: half * 3072 + (blk + 1) * 512],
                                start=True, stop=False), reads=[B_c, B_mb], writes=[pb])
                        kb.op("pe", lambda e, pt=pt, blk=blk, kc=kc, t_=t_: e.matmul(
                            pt[:], lhsT=crep[:, kc, :], rhs=t_[:, blk * 512:(blk + 1) * 512],
                            start=False, stop=(kc == 7)), reads=[B_c, tb], writes=[pb])
                for blk in range(6):
                    pt, pb = acc[blk]
                    kb.op("dve" if blk % 2 == 0 else "act",
                          (lambda e, pt=pt, blk=blk: e.tensor_copy(out=mh[:, blk * 512:(blk + 1) * 512], in_=pt[:]))
                          if blk % 2 == 0 else
                          (lambda e, pt=pt, blk=blk: e.activation(out=mh[:, blk * 512:(blk + 1) * 512], in_=pt[:],
                                                                  func=AF.Identity)),
                          reads=[pb], writes=[B_mh])
                kb.dma("sp", dgs[l * 2 + half, :, :], mh[:, 2048:3072], reads=[B_mh], writes=[B_gs[l * 2 + half]])
                kb.op("dve", lambda e: e.tensor_tensor(
                    out=dtmp[:], in0=mh[:].rearrange("p (c j) -> p c j", j=128),
                    in1=ident_f[:].unsqueeze(1).to_broadcast([128, 24, 128]), op=ALU.mult),
                    reads=[B_mh, B_const], writes=[B_dt])
                kb.op("dve", lambda e, l=l, half=half: e.tensor_reduce(
                    out=modT[:, l, half * 24:(half + 1) * 24], in_=dtmp[:], axis=AX.X, op=ALU.add),
                    reads=[B_dt], writes=[B_modT])
            kb.op("dve", lambda e, l=l: e.scalar_tensor_tensor(
                out=amix[:, l, :], in0=modT[:, l, 8:16], scalar=1.0, in1=gmixT[:, l * 8:(l + 1) * 8],
                op0=ALU.add, op1=ALU.mult), reads=[B_modT], writes=[B_modT])
            kb.op("dve", lambda e, l=l: e.scalar_tensor_tensor(
                out=affn[:, l, :], in0=modT[:, l, 32:40], scalar=1.0, in1=gffnT[:, l * 8:(l + 1) * 8],
                op0=ALU.add, op1=ALU.mult), reads=[B_modT], writes=[B_modT])
        kb.barrier()
        ps_.close()

    B_gs = [Buf() for _ in range(8)]
    B_xs = [Buf() for _ in range(NT)]

    def load_x(src, i, xt, xb):
        kb.dma("sp", xt[:], src[i * 128:(i + 1) * 128, :], reads=[B_xs[i]] if src is dxs else [], writes=[xb])

    def norm_tile(xt, xb, junk, B_junk, ss, rs, B_st, xsb, B_xsb):
        kb.op("act", lambda e: e.activation(out=junk, in_=xt[:], func=AF.Square, accum_out=ss),
              reads=[xb], writes=[B_junk, B_st])
        rstd_from_ss(rs, ss, B_st, float(D), B_st)
        kb.op("dve", lambda e: e.tensor_scalar(out=xsb, in0=xt[:], scalar1=rs, scalar2=32.0, op0=ALU.mult,
                                               op1=ALU.mult), reads=[xb, B_st], writes=[B_xsb])

    def make_hT(xsb_t, B_xsb, hT_dst, B_hT, A, S):
        for kc in range(8):
            kb.op("pe", lambda e, kc=kc: e.transpose(out=TB[:, kc * 128:(kc + 1) * 128],
                                                     in_=xsb_t[:, kc * 128:(kc + 1) * 128], identity=ident_b[:]),
                  reads=[B_xsb, B_const], writes=[TBb])
        for kc in range(8):
            kb.op("act", lambda e, kc=kc: e.activation(out=hT_dst(kc), in_=TB[:, kc * 128:(kc + 1) * 128],
                                                       func=AF.Identity, scale=A[:, kc:kc + 1], bias=S[:, kc:kc + 1]),
                  reads=[TBb, B_modT], writes=[B_hT])

    def residual_store(ypairs, xt, xb, gb, B_gb, tmps, dst, dstb, fin=None):
        for dh, (pt, pb) in enumerate(ypairs):
            tmp, tb = tmps[dh]
            kb.op("dve", lambda e, pt=pt, dh=dh, tmp=tmp: e.tensor_tensor(
                out=tmp[:], in0=pt[:], in1=gb[:, dh * 512:(dh + 1) * 512], op=ALU.mult),
                reads=[pb, B_gb], writes=[tb])
            kb.op("pool", lambda e, dh=dh, tmp=tmp: e.tensor_tensor(
                out=xt[:, dh * 512:(dh + 1) * 512], in0=xt[:, dh * 512:(dh + 1) * 512], in1=tmp[:], op=ALU.add),
                reads=[tb, xb], writes=[xb])
        if fin is None:
            kb.dma("sp", dst, xt[:], reads=[xb], writes=[dstb])
        else:
            fg, B_fg, ot, B_ot, junk, B_junk, ss, rs, B_fs, odst, B_od = fin
            kb.op("act", lambda e: e.activation(out=junk, in_=xt[:], func=AF.Square, accum_out=ss),
                  reads=[xb], writes=[B_junk, B_fs])
            rstd_from_ss(rs, ss, B_fs, float(D), B_fs)
            kb.op("dve", lambda e: e.scalar_tensor_tensor(out=ot[:], in0=xt[:], scalar=rs, in1=fg[:], op0=ALU.mult,
                                                          op1=ALU.mult), reads=[xb, B_fs, B_fg], writes=[B_ot])
            kb.dma("sp", odst, ot[:], reads=[B_ot], writes=[B_od])

    def phase_ffn(l, src, fuse_final=False):
        ps_ = ExitStack()
        wg = sb(ps_, "wg", [128, 8, DFF], BF16)
        wu = sb(ps_, "wu", [128, 8, DFF], BF16)
        wd = sb(ps_, "wd", [128, NFC, D], BF16)
        gb = sb(ps_, "gb", [128, D], F32)
        xts = [sb(ps_, "xt%d" % i, [128, D], F32) for i in range(4)]
        xbs = [Buf() for _ in range(4)]
        junk = sb(ps_, "junk", [128, D], BF16)
        st = sb(ps_, "st", [128, 4], F32)
        xsb = [sb(ps_, "xsb%d" % i, [128, D], BF16) for i in range(2)]
        hT = [sb(ps_, "hT%d" % i, [128, 8, 256], BF16) for i in range(2)]
        actT = sb(ps_, "actT", [128, NFC, 256], BF16)
        sg = [sb(ps_, "sg%d" % i, [128, 256], F32) for i in range(2)]
        tmps = [(sb(ps_, "tmp%d" % i, [128, 512], F32), Buf()) for i in range(2)]
        B_w, B_gb, B_junk = Buf(), Buf(), Buf()
        if fuse_final:
            fg = sb(ps_, "ffg", [128, D], F32)
            ots = [sb(ps_, "fot%d" % i, [128, D], F32) for i in range(2)]
            B_ots = [Buf(), Buf()]
            fst = sb(ps_, "ffst", [128, 4], F32)
            B_fst = [Buf(), Buf()]
            B_fg, B_od = Buf(), Buf()
            kb.dma("sp", fg[:], d_fg[0:1, :].to_broadcast([128, D]), writes=[B_fg])
            kb.op("dve", lambda e: e.tensor_scalar(out=fg[:], in0=fg[:], scalar1=32.0, scalar2=None, op0=ALU.mult),
                  reads=[B_fg], writes=[B_fg])
        B_st = [Buf(), Buf()]
        B_xsb = [Buf(), Buf()]
        B_hT = [Buf(), Buf()]
        B_sg = [Buf(), Buf()]
        B_act = [Buf() for _ in range(NFC)]
        B_wg = [Buf() for _ in range(8)]
        B_wu = [Buf() for _ in range(8)]
        B_wd = [Buf() for _ in range(NFC)]
        for kc in range(8):
            kb.dma("sp", wg[:, kc, :], w16["fwg%d" % l][kc * 128:(kc + 1) * 128, :], reads=[B_w16["fwg%d" % l]],
                   writes=[B_wg[kc]])
        for kc in range(8):
            kb.dma("sp", wu[:, kc, :], w16["fwu%d" % l][kc * 128:(kc + 1) * 128, :], reads=[B_w16["fwu%d" % l]],
                   writes=[B_wu[kc]])
        for fc in range(NFC):
            kb.dma("sp", wd[:, fc, :], w16["fwd%d" % l][fc * 128:(fc + 1) * 128, :], reads=[B_w16["fwd%d" % l]],
                   writes=[B_wd[fc]])
        kb.dma("sp", gb[:], dgs[l * 2 + 1, :, :], reads=[B_gs[l * 2 + 1]], writes=[B_gb])
        A = affn[:, l, :]
        S = modT[:, l, 24:32]
        NG = NT // 2

        def prep_a(g):
            for tl in range(2):
                i = g * 2 + tl
                load_x(src, i, xts[i % 4], xbs[i % 4])

        def prep_b(g):
            for tl in range(2):
                i = g * 2 + tl
                k = i % 2
                norm_tile(xts[i % 4], xbs[i % 4], junk[:], B_junk, st[:, 2 * k:2 * k + 1], st[:, 2 * k + 1:2 * k + 2],
                          B_st[k], xsb[k][:], B_xsb[k])
                make_hT(xsb[k], B_xsb[k], lambda kc, tl=tl, g=g: hT[g % 2][:, kc, tl * 128:(tl + 1) * 128],
                        B_hT[g % 2], A, S)

        prep_a(0)
        prep_b(0)
        for g in range(NG):
            h = hT[g % 2]
            hb = B_hT[g % 2]
            if g + 1 < NG:
                prep_a(g + 1)
            for fc in range(NFC):
                pt, pb = pbank()
                for slot, w in ((0, wg), (1, wu)):
                    for kc in range(8):
                        kb.op("pe", lambda e, pt=pt, slot=slot, w=w, kc=kc, fc=fc: e.matmul(
                            pt[:, slot * 256:(slot + 1) * 256], lhsT=w[:, kc, fc * 128:(fc + 1) * 128],
                            rhs=h[:, kc, :], start=(kc == 0), stop=(kc == 7)),
                            reads=[(B_wg if slot == 0 else B_wu)[kc], hb], writes=[pb])
                s_ = sg[fc % 2]
                sbf = B_sg[fc % 2]
                kb.op("act", lambda e, pt=pt, s_=s_: e.activation(out=s_[:], in_=pt[:, 0:256], func=AF.Silu),
                      reads=[pb], writes=[sbf])
                kb.op("dve", lambda e, pt=pt, s_=s_, fc=fc: e.tensor_tensor(
                    out=actT[:, fc, :], in0=pt[:, 256:512], in1=s_[:], op=ALU.mult),
                    reads=[pb, sbf], writes=[B_act[fc]])
                if fc == 6 and g + 1 < NG:
                    prep_b(g + 1)
            for tl in range(2):
                i = g * 2 + tl
                ys = []
                for dh in range(2):
                    pt, pb = pbank()
                    for fc in range(NFC):
                        kb.op("pe", lambda e, pt=pt, fc=fc, tl=tl, dh=dh: e.matmul(
                            pt[:], lhsT=actT[:, fc, tl * 128:(tl + 1) * 128], rhs=wd[:, fc, dh * 512:(dh + 1) * 512],
                            start=(fc == 0), stop=(fc == NFC - 1)), reads=[B_wd[fc], B_act[fc]], writes=[pb])
                    ys.append((pt, pb))
                fin = None
                if fuse_final:
                    k_ = i % 2
                    fin = (fg, B_fg, ots[k_], B_ots[k_], junk[:], B_junk, fst[:, 2 * k_:2 * k_ + 1],
                           fst[:, 2 * k_ + 1:2 * k_ + 2], B_fst[k_], dout[i * 128:(i + 1) * 128, :], B_od)
                residual_store(ys, xts[i % 4], xbs[i % 4], gb, B_gb, tmps, dxs[i * 128:(i + 1) * 128, :], B_xs[i], fin)
        kb.barrier()
        ps_.close()

    def phase_final(src):
        ps_ = ExitStack()
        fg = sb(ps_, "fg", [128, D], F32)
        xts = [sb(ps_, "fxt%d" % i, [128, D], F32) for i in range(3)]
        xbs = [Buf() for _ in range(3)]
        ots = [sb(ps_, "fot%d" % i, [128, D], F32) for i in range(2)]
        obs = [Buf() for _ in range(2)]
        junk = sb(ps_, "fjunk", [128, D], BF16)
        st = sb(ps_, "fst", [128, 4], F32)
        B_st = [Buf(), Buf()]
        B_fg, B_junk, B_o = Buf(), Buf(), Buf()
        kb.dma("sp", fg[:], d_fg[0:1, :].to_broadcast([128, D]), writes=[B_fg])
        kb.op("dve", lambda e: e.tensor_scalar(out=fg[:], in0=fg[:], scalar1=32.0, scalar2=None, op0=ALU.mult),
              reads=[B_fg], writes=[B_fg])
        for i in range(NT):
            k = i % 2
            xt, xb = xts[i % 3], xbs[i % 3]
            load_x(src, i, xt, xb)
            ss, rs = st[:, 2 * k:2 * k + 1], st[:, 2 * k + 1:2 * k + 2]
            kb.op("act", lambda e, xt=xt, ss=ss: e.activation(out=junk[:], in_=xt[:], func=AF.Square, accum_out=ss),
                  reads=[xb], writes=[B_junk, B_st[k]])
            rstd_from_ss(rs, ss, B_st[k], float(D), B_st[k])
            kb.op("dve", lambda e, xt=xt, rs=rs, k=k: e.scalar_tensor_tensor(
                out=ots[k][:], in0=xt[:], scalar=rs, in1=fg[:], op0=ALU.mult, op1=ALU.mult),
                reads=[xb, B_st[k], B_fg], writes=[obs[k]])
            kb.dma("sp", dout[i * 128:(i + 1) * 128, :], ots[k][:], reads=[obs[k]], writes=[B_o])
        kb.barrier()
        ps_.close()

    def phase_gmlp(l, j, src):
        ps_ = ExitStack()
        win = sb(ps_, "bwin", [128, 8, 4096], BF16)
        wout = sb(ps_, "bwout", [128, 16, D], BF16)
        wcT = sb(ps_, "wcT", [128, 8, 128], BF16)
        lng = sb(ps_, "lng", [128, 2048], F32)
        lnb = sb(ps_, "lnb", [128, 2048], F32)
        bsb = sb(ps_, "bsb", [128, 8, 128], F32)
        gb = sb(ps_, "ggb", [128, D], F32)
        xts = [sb(ps_, "gxt%d" % i, [128, D], F32) for i in range(4)]
        xbs = [Buf() for _ in range(4)]
        st = sb(ps_, "gst", [128, 4], F32)
        xsb = [sb(ps_, "gxsb%d" % i, [128, D], BF16) for i in range(2)]
        hT = [sb(ps_, "ghT%d" % i, [128, 8, 256], BF16) for i in range(2)]
        uT = sb(ps_, "uT", [128, 16, 256], F32)
        vr = [sb(ps_, "vr%d" % i, [128, 2048], F32) for i in range(2)]
        vt = [sb(ps_, "vt%d" % i, [128, 2048], BF16) for i in range(2)]
        bst = sb(ps_, "bst", [128, 2, 4, 6], F32)
        lst = sb(ps_, "lst", [128, 2, 4], F32)
        ypT = sb(ps_, "ypT", [128, 16, 256], BF16)
        ytm = [sb(ps_, "ytm0", [128, 512], F32)] * 2
        tmps = [(sb(ps_, "gtmp%d" % i, [128, 512], F32), Buf()) for i in range(2)]
        B_w, B_gb, B_junk, B_k = Buf(), Buf(), Buf(), Buf()
        B_st = [Buf(), Buf()]
        B_xsb = [Buf(), Buf()]
        B_hT = [Buf(), Buf()]
        B_u = [Buf() for _ in range(16)]
        B_vr = [Buf(), Buf()]
        B_vt = [Buf(), Buf()]
        B_ls = [Buf(), Buf()]
        B_yp = [[Buf() for _ in range(4)] for _ in range(2)]
        B_ytm = [Buf()] * 2
        for kc in range(8):
            kb.dma("sp", win[:, kc, :], w16["bwin%d" % j][kc * 128:(kc + 1) * 128, :], reads=[B_w16["bwin%d" % j]],
                   writes=[B_w])
        for ec in range(16):
            kb.dma("sp", wout[:, ec, :], w16["bwout%d" % j][ec * 128:(ec + 1) * 128, :], reads=[B_w16["bwout%d" % j]],
                   writes=[B_w])
        wcf = vr[0][:, 0:1024].rearrange("p (g t) -> p g t", g=8)
        kb.dma("sp", wcf, d_bwsT[j, :, :].rearrange("s (g t) -> s g t", g=8), writes=[B_vr[0]])
        kb.dma("sp", lng[:], d_blng[j:j + 1, :].to_broadcast([128, 2048]), writes=[B_k])
        kb.dma("sp", lnb[:], d_blnb[j:j + 1, :].to_broadcast([128, 2048]), writes=[B_k])
        kb.dma("sp", bsb[:], d_bbs[j:j + 1, :].to_broadcast([128, 1024]).rearrange("p (g t) -> p g t", g=8),
               writes=[B_k])
        kb.dma("sp", gb[:], dgs[l * 2, :, :], reads=[B_gs[l * 2]], writes=[B_gb])
        kb.op("pool", lambda e: e.affine_select(out=wcf, in_=wcf, pattern=[[0, 8], [1, 128]],
                                                compare_op=ALU.is_ge, fill=0.0, base=0, channel_multiplier=-1),
              reads=[B_vr[0]], writes=[B_vr[0]])
        kb.op("dve", lambda e: e.tensor_copy(out=wcT[:], in_=wcf), reads=[B_vr[0]], writes=[B_k])
        A = amix[:, l, :]
        S = modT[:, l, 0:8]
        NG = NT // 2

        def prep_a(g):
            for tl in range(2):
                i = g * 2 + tl
                load_x(src, i, xts[i % 4], xbs[i % 4])

        def prep_b(g):
            for tl in range(2):
                i = g * 2 + tl
                k = i % 2
                norm_tile(xts[i % 4], xbs[i % 4], xsb[k][:], B_xsb[k], st[:, 2 * k:2 * k + 1], st[:, 2 * k + 1:2 * k + 2],
                          B_st[k], xsb[k][:], B_xsb[k])
                make_hT(xsb[k], B_xsb[k], lambda kc, tl=tl, g=g: hT[g % 2][:, kc, tl * 128:(tl + 1) * 128],
                        B_hT[g % 2], A, S)

        prep_a(0)
        prep_b(0)
        for g in range(NG):
            h = hT[g % 2]
            hb = B_hT[g % 2]
            if g + 1 < NG:
                prep_a(g + 1)
            for tl in range(2):
                for cb in range(4):
                    pt, pb = pbank()
                    for kc in range(8):
                        kb.op("pe", lambda e, pt=pt, kc=kc, cb=cb, tl=tl: e.matmul(
                            pt[:], lhsT=h[:, kc, tl * 128:(tl + 1) * 128],
                            rhs=win[:, kc, 2048 + cb * 512:2048 + (cb + 1) * 512],
                            start=(kc == 0), stop=(kc == 7)), reads=[B_w, hb], writes=[pb])
                    kb.op("act", lambda e, pt=pt, cb=cb, tl=tl: e.activation(
                        out=vr[tl][:, cb * 512:(cb + 1) * 512], in_=pt[:], func=AF.Gelu),
                        reads=[pb], writes=[B_vr[tl]])
                    kb.op("dve", lambda e, cb=cb, tl=tl: e.bn_stats(out=bst[:, tl, cb, :],
                                                                    in_=vr[tl][:, cb * 512:(cb + 1) * 512]),
                          reads=[B_vr[tl]], writes=[B_ls[tl]])
                kb.op("dve", lambda e, tl=tl: e.bn_aggr(out=lst[:, tl, 0:2],
                                                        in_=bst[:, tl, :, :].rearrange("p a b -> p (a b)")),
                      reads=[B_ls[tl]], writes=[B_ls[tl]])
                kb.op("pool", lambda e, tl=tl: e.tensor_scalar(out=lst[:, tl, 2:3], in0=lst[:, tl, 1:2], scalar1=EPS,
                                                               scalar2=None, op0=ALU.add),
                      reads=[B_ls[tl]], writes=[B_ls[tl]])
                kb.op("pool", lambda e, tl=tl: e.tensor_tensor(out=lst[:, tl, 2:3], in0=lst[:, tl, 2:3], in1=mhalf[:],
                                                               op=ALU.pow),
                      reads=[B_ls[tl], B_const], writes=[B_ls[tl]])
                kb.op("dve", lambda e, tl=tl: e.tensor_scalar(out=vr[tl][:], in0=vr[tl][:], scalar1=lst[:, tl, 0:1],
                                                              scalar2=lst[:, tl, 2:3], op0=ALU.subtract, op1=ALU.mult),
                      reads=[B_ls[tl], B_vr[tl]], writes=[B_vr[tl]])
                kb.op("pool", lambda e, tl=tl: e.tensor_tensor(out=vr[tl][:], in0=vr[tl][:], in1=lng[:], op=ALU.mult),
                      reads=[B_vr[tl], B_k], writes=[B_vr[tl]])
                kb.op("pool", lambda e, tl=tl: e.tensor_tensor(out=vt[tl][:], in0=vr[tl][:], in1=lnb[:], op=ALU.add),
                      reads=[B_vr[tl], B_k], writes=[B_vt[tl]])
            for ec in range(16):
                if ec % 2 == 0:
                    pt, pb = pbank()
                half = ec % 2
                for kc in range(8):
                    kb.op("pe", lambda e, pt=pt, half=half, kc=kc, ec=ec: e.matmul(
                        pt[:, half * 256:(half + 1) * 256], lhsT=win[:, kc, ec * 128:(ec + 1) * 128],
                        rhs=h[:, kc, :], start=(kc == 0), stop=(kc == 7)), reads=[B_w, hb], writes=[pb])
                if ec % 2 == 1:
                    kb.op("act", lambda e, pt=pt, ec=ec: e.activation(
                        out=uT[:, ec - 1:ec + 1, :].rearrange("p a t -> p (a t)"), in_=pt[:], func=AF.Gelu),
                        reads=[pb], writes=[B_u[ec - 1], B_u[ec]])
            if g + 1 < NG:
                prep_b(g + 1)
            for tl in range(2):
                i = g * 2 + tl
                for q4 in range(4):
                    pt, pb = pbank()
                    for a in range(4):
                        ec = q4 * 4 + a
                        kb.op("pe", lambda e, pt=pt, a=a, ec=ec, tl=tl: e.matmul(
                            pt[:, a * 128:(a + 1) * 128], lhsT=vt[tl][:, ec * 128:(ec + 1) * 128],
                            rhs=wcT[:, ec // 2, :], start=True, stop=True), reads=[B_vt[tl], B_k], writes=[pb])
                    ym, ymb = ytm[q4 % 2], B_ytm[q4 % 2]
                    kb.op("dve", lambda e, pt=pt, q4=q4, ym=ym: e.tensor_tensor(
                        out=ym[:].rearrange("p (g r t) -> p g r t", g=2, r=2),
                        in0=pt[:].rearrange("p (g r t) -> p g r t", g=2, r=2),
                        in1=bsb[:, q4 * 2:q4 * 2 + 2, :].unsqueeze(2).to_broadcast([128, 2, 2, 128]), op=ALU.add),
                        reads=[pb, B_k], writes=[ymb])
                    kb.op("dve", lambda e, q4=q4, tl=tl, ym=ym: e.tensor_tensor(
                        out=ypT[:, q4 * 4:(q4 + 1) * 4, tl * 128:(tl + 1) * 128],
                        in0=ym[:].rearrange("p (a t) -> p a t", a=4),
                        in1=uT[:, q4 * 4:(q4 + 1) * 4, tl * 128:(tl + 1) * 128], op=ALU.mult),
                        reads=[ymb] + [B_u[q4 * 4 + a] for a in range(4)], writes=[B_yp[tl][q4]])
                ys = []
                for dh in range(2):
                    pt, pb = pbank()
                    for ec in range(16):
                        kb.op("pe", lambda e, pt=pt, ec=ec, tl=tl, dh=dh: e.matmul(
                            pt[:], lhsT=ypT[:, ec, tl * 128:(tl + 1) * 128], rhs=wout[:, ec, dh * 512:(dh + 1) * 512],
                            start=(ec == 0), stop=(ec == 15)), reads=[B_w, B_yp[tl][ec // 4]], writes=[pb])
                    ys.append((pt, pb))
                residual_store(ys, xts[i % 4], xbs[i % 4], gb, B_gb, tmps, dxs[i * 128:(i + 1) * 128, :], B_xs[i])
        kb.barrier()
        ps_.close()

    def phase_dsa(l, j, src, ntiles=NT):
        ps_ = ExitStack()
        win = sb(ps_, "awin", [128, 8, A_IN], BF16)
        wuk = sb(ps_, "awuk", [128, 8, 256], BF16)
        wuv = sb(ps_, "awuv", [128, 8, 2, 128], BF16)
        wo = sb(ps_, "awo", [128, 8, D], BF16)
        gkv = sb(ps_, "gkv", [128, 256], F32)
        gki = sb(ps_, "gki", [128, 64], F32)
        bki = sb(ps_, "bki", [128, 64], F32)
        cmask = sb(ps_, "cmask", [128, 128], F32)
        pw2 = sb(ps_, "pw2", [128, NIT + 1], F32)
        ckvT = sb(ps_, "ckvT", [128, 2, T], BF16)
        ckve = sb(ps_, "ckve", [128, NT, 258], BF16)
        kiT = [sb(ps_, "kiT%d" % i_, [128, T], BF16) for i_ in range(2)]
        xts = [sb(ps_, "axt%d" % i, [128, D], F32) for i in range(4)]
        xbs = [Buf() for _ in range(4)]
        st = sb(ps_, "ast", [128, 16], F32)
        xsb = sb(ps_, "axsb", [128, D], BF16)
        hT = sb(ps_, "ahT", [128, 8, 128], BF16)
        qT = sb(ps_, "qT", [128, 8, 128], BF16)
        qiT = sb(ps_, "qiT", [128, 4, 128], BF16)
        qlT = [sb(ps_, "qlT%d" % i, [128, 2, 1024], BF16) for i in range(3)]
        ctm = sb(ps_, "ctm", [128, 256], BF16)
        junk = ctm
        ktm = sb(ps_, "ktm", [128, 64], F32)
        kd = sb(ps_, "kd", [128, 2, 128], BF16)
        wi = sb(ps_, "wi", [128, 8], F32)
        dg = sb(ps_, "dg", [128, 8, 128], BF16)
        scores = [sb(ps_, "score%d" % i_, [128, T], F32) for i_ in range(2)]
        B_scs = [Buf(), Buf()]
        NR = 3
        Rh = [sb(ps_, "Rh%d" % i, [128, 512], BF16) for i in range(NR)]
        B_Rh = [Buf() for _ in range(NR)]
        nms = [sb(ps_, "nm%d" % i_, [128, T], BF16) for i_ in range(2)]
        B_nms = [Buf(), Buf()]
        ident4 = sb(ps_, "ident4", [128, 4, 128], BF16)
        thb = sb(ps_, "thb", [128, 8 + NIT + 8], F32)
        PT = [sb(ps_, "PT%d" % i, [128, 512], BF16) for i in range(2)]
        B_PT = [Buf() for _ in range(2)]
        ol = sb(ps_, "ol", [128, 8, 256], BF16)
        olT = sb(ps_, "olT", [128, 8, 128], BF16)
        oT = sb(ps_, "oT", [128, 8, 128], BF16)
        rden = sb(ps_, "rden", [128, 8], F32)
        B_w, B_k, B_gb, B_junk, B_st, B_xsb, B_hT = Buf(), Buf(), Buf(), Buf(), Buf(), Buf(), Buf()
        B_q, B_qi, B_ct, B_kt, B_kd, B_wi, B_dg, B_sc, B_nm, B_th = (Buf() for _ in range(10))
        B_ql = [Buf(), Buf(), Buf()]
        B_ckvT = [Buf() for _ in range(NT)]
        B_ckve = [Buf() for _ in range(NT)]
        B_kiT = [Buf() for _ in range(NT)]
        B_ol, B_olT, B_oT, B_rd = Buf(), Buf(), Buf(), Buf()
        if j == 0:
            for kc in range(8):
                kb.dma("pool", win[:, kc, :], d_awin[j, kc * 128:(kc + 1) * 128, :], writes=[B_w])
            kb.dma("pool", wuk[:], d_awuk[j].rearrange("h d r -> d h r"), writes=[B_w])
            kb.dma("pool", wuv[:], d_awuv[j].rearrange("h (c p) d -> p h c d", p=128), writes=[B_w])
            bg_cast_all()
        else:
            for kc in range(8):
                kb.dma("sp", win[:, kc, :], w16["awin1"][kc * 128:(kc + 1) * 128, :], reads=[B_w16["awin1"]],
                       writes=[B_w])
            kb.dma("sp", wuk[:], w16["awuk1"].rearrange("(h d) r -> d h r", h=8), reads=[B_w16["awuk1"]], writes=[B_w])
            kb.dma("sp", wuv[:], w16["awuv1"].rearrange("(h c p) d -> p h c d", h=8, p=128), reads=[B_w16["awuv1"]],
                   writes=[B_w])
        kb.dma("sp", gkv[:], d_agkv[j:j + 1, :].to_broadcast([128, 256]), writes=[B_k])
        kb.op("dve", lambda e: e.tensor_scalar(out=gkv[:], in0=gkv[:], scalar1=16.0, scalar2=None, op0=ALU.mult),
              reads=[B_k], writes=[B_k])
        kb.dma("sp", gki[:], d_agki[j:j + 1, :].to_broadcast([128, 64]), writes=[B_k])
        kb.dma("sp", bki[:], d_abki[j:j + 1, :].to_broadcast([128, 64]), writes=[B_k])
        kb.dma("sp", scores[1][:, 0:D], dgs[l * 2, :, :], reads=[B_gs[l * 2]], writes=[B_scs[1]])
        for kc in range(8):
            slot = scores[0][:, (kc % 4) * D:(kc % 4 + 1) * D]
            kb.dma("sp", slot, d_awo[j, kc * 128:(kc + 1) * 128, :], writes=[B_scs[0]])
            kb.op("dve", lambda e, kc=kc, slot=slot: e.tensor_tensor(out=wo[:, kc, :], in0=slot, in1=scores[1][:, 0:D],
                                                                     op=ALU.mult),
                  reads=[B_scs[0], B_scs[1]], writes=[B_w])
        kb.op("pool", lambda e: e.memset(cmask[:], 0.0), writes=[B_k])
        kb.op("pool", lambda e: e.memset(kd[:], 0.0), writes=[B_kd])
        kb.op("pool", lambda e: e.affine_select(out=cmask[:], in_=cmask[:], pattern=[[-1, 128]], compare_op=ALU.is_ge,
                                                fill=NEG, base=0, channel_multiplier=1), reads=[B_k], writes=[B_k])
        for k in range(NIT + 1):
            kb.op("pool", lambda e, k=k: e.memset(pw2[:, k:k + 1], 2.0 ** (-(k + 1))), writes=[B_k])
        kb.op("pool", lambda e: e.memset(ckve[:], 1.0), writes=B_ckve)
        kb.op("pool", lambda e: e.tensor_copy(out=ident4[:], in_=ident_b[:].unsqueeze(1).to_broadcast([128, 4, 128])),
              reads=[B_const], writes=[B_k])
        A = amix[:, l, :]
        S = modT[:, l, 0:8]
        SC_W = float(8 ** -0.5 * 64 ** -0.5)
        m8, lo_, w_, t_, c_, g_, thr = (thb[:, 0:8], thb[:, 8:9], thb[:, 9:10], thb[:, 10:11], thb[:, 11:12],
                                        thb[:, 12:13], thb[:, 13:14])
        wk = thb[:, 14:14 + NIT + 1]

        def pre_front(i):
            xt, xb = xts[i % 4], xbs[i % 4]
            load_x(src, i, xt, xb)
            norm_tile(xt, xb, xsb[:], B_xsb, st[:, 0:1], st[:, 1:2], B_st, xsb[:], B_xsb)

        def stage_a(i):
            xt, xb = xts[i % 4], xbs[i % 4]
            score, B_sc = scores[i % 2], B_scs[i % 2]
            make_hT(xsb, B_xsb, lambda kc: hT[:, kc, :], B_hT, A, S)
            for grp in range(3):
                pt, pb = pbank()
                for a in range(4):
                    ch = grp * 4 + a
                    c0 = ch * 128 if ch < 8 else 1280 + (ch - 8) * 128
                    for kc in range(8):
                        kb.op("pe", lambda e, pt=pt, a=a, c0=c0, kc=kc: e.matmul(
                            pt[:, a * 128:(a + 1) * 128], lhsT=win[:, kc, c0:c0 + 128], rhs=hT[:, kc, :],
                            start=(kc == 0), stop=(kc == 7)), reads=[B_w, B_hT], writes=[pb])
                if grp < 2:
                    kb.op("act", lambda e, pt=pt, grp=grp: e.activation(
                        out=qT[:, grp * 4:(grp + 1) * 4, :].rearrange("p a t -> p (a t)"), in_=pt[:], func=AF.Identity),
                        reads=[pb], writes=[B_q])
                else:
                    kb.op("act", lambda e, pt=pt: e.activation(
                        out=qiT[:].rearrange("p a t -> p (a t)"), in_=pt[:], func=AF.Identity),
                        reads=[pb], writes=[B_qi])
            pt, pb = pbank()
            for kc in range(8):
                kb.op("pe", lambda e, pt=pt, kc=kc: e.matmul(pt[:, 0:256], lhsT=hT[:, kc, :], rhs=win[:, kc, 1024:1280],
                                                             start=(kc == 0), stop=(kc == 7)),
                      reads=[B_w, B_hT], writes=[pb])
            for kc in range(8):
                kb.op("pe", lambda e, pt=pt, kc=kc: e.matmul(pt[:, 256:328], lhsT=hT[:, kc, :], rhs=win[:, kc, 1792:1864],
                                                             start=(kc == 0), stop=(kc == 7)),
                      reads=[B_w, B_hT], writes=[pb])
            ql, qlb = qlT[i % 3], B_ql[i % 3]
            for rc in range(2):
                pts = [pbank(), pbank()]
                for h in range(8):
                    pt2, pb2 = pts[h // 4]
                    kb.op("pe", lambda e, pt2=pt2, h=h, rc=rc: e.matmul(
                        pt2[:, (h % 4) * 128:(h % 4 + 1) * 128], lhsT=wuk[:, h, rc * 128:(rc + 1) * 128],
                        rhs=qT[:, h, :], start=True, stop=True), reads=[B_w, B_q], writes=[pb2])
                for hg in range(2):
                    pt2, pb2 = pts[hg]
                    kb.op("act", lambda e, pt2=pt2, hg=hg, rc=rc, ql=ql: e.activation(
                        out=ql[:, rc, hg * 512:(hg + 1) * 512], in_=pt2[:], func=AF.Identity,
                        scale=float(128 ** -0.5)), reads=[pb2], writes=[qlb])
            kb.op("act", lambda e, pt=pt: e.activation(out=junk[:], in_=pt[:, 0:256], func=AF.Square,
                                                       accum_out=st[:, 2:3]), reads=[pb], writes=[B_junk, B_ct])
            rstd_from_ss(st[:, 3:4], st[:, 2:3], B_ct, 256.0, B_ct)
            kb.op("dve", lambda e, pt=pt: e.scalar_tensor_tensor(out=ckve[:, i, 0:256], in0=pt[:, 0:256],
                                                                 scalar=st[:, 3:4], in1=gkv[:], op0=ALU.mult,
                                                                 op1=ALU.mult),
                  reads=[pb, B_ct, B_k], writes=[B_ckve[i]])
            kb.op("dve", lambda e, pt=pt: e.bn_stats(out=st[:, 4:10], in_=pt[:, 256:320]), reads=[pb], writes=[B_kt])
            kb.op("dve", lambda e: e.bn_aggr(out=st[:, 10:12], in_=st[:, 4:10]), reads=[B_kt], writes=[B_kt])
            kb.op("pool", lambda e: e.tensor_scalar(out=st[:, 12:13], in0=st[:, 11:12], scalar1=EPS, scalar2=None,
                                                    op0=ALU.add), reads=[B_kt], writes=[B_kt])
            kb.op("pool", lambda e: e.tensor_tensor(out=st[:, 12:13], in0=st[:, 12:13], in1=mhalf[:], op=ALU.pow),
                  reads=[B_kt, B_const], writes=[B_kt])
            kb.op("dve", lambda e, pt=pt: e.tensor_scalar(out=ktm[:], in0=pt[:, 256:320], scalar1=st[:, 10:11],
                                                          scalar2=st[:, 12:13], op0=ALU.subtract, op1=ALU.mult),
                  reads=[pb, B_kt], writes=[B_kt])
            kb.op("dve", lambda e: e.tensor_tensor(out=ktm[:], in0=ktm[:], in1=gki[:], op=ALU.mult),
                  reads=[B_kt, B_k], writes=[B_kt])
            for a_ in range(2):
                kb.op("dve", lambda e, a_=a_: e.tensor_tensor(out=kd[:, a_, a_ * 64:(a_ + 1) * 64], in0=ktm[:],
                                                              in1=bki[:], op=ALU.add),
                      reads=[B_kt, B_k], writes=[B_kd])
            kb.op("dve", lambda e, pt=pt: e.tensor_scalar(out=wi[:], in0=pt[:, 320:328], scalar1=SC_W, scalar2=None,
                                                          op0=ALU.mult), reads=[pb], writes=[B_wi])
            for h in range(8):
                kb.op("dve", lambda e, h=h: e.tensor_scalar(out=dg[:, h, :], in0=ident_b[:], scalar1=wi[:, h:h + 1],
                                                            scalar2=None, op0=ALU.mult),
                      reads=[B_wi, B_const], writes=[B_dg])
            for rc in range(2):
                kb.op("pe", lambda e, rc=rc: e.transpose(out=TB[:, rc * 128:(rc + 1) * 128],
                                                         in_=ckve[:, i, rc * 128:(rc + 1) * 128], identity=ident_b[:]),
                      reads=[B_ckve[i], B_const], writes=[TBb])
            for a_ in range(2):
                kb.op("pe", lambda e, a_=a_: e.transpose(out=TB[:, 256 + a_ * 128:384 + a_ * 128], in_=kd[:, a_, :],
                                                         identity=ident_b[:]), reads=[B_kd, B_const], writes=[TBb])
            kb.op("dve", lambda e: e.tensor_copy(out=ckvT[:, :, i * 128:(i + 1) * 128],
                                                 in_=TB[:, 0:256].rearrange("p (c t) -> p c t", c=2)),
                  reads=[TBb], writes=[B_ckvT[i]])
            for a_ in range(2):
                kb.op("dve", lambda e, a_=a_: e.tensor_copy(out=kiT[a_][:, i * 128:(i + 1) * 128],
                                                            in_=TB[:, 256 + a_ * 128:384 + a_ * 128]),
                      reads=[TBb], writes=[B_kiT[i]])
        def idx_gen(i):
            score, B_sc = scores[i % 2], B_scs[i % 2]
            n = (i + 1) * 128
            rr = 0
            for c0 in range(0, n, 512):
                wd_ = min(512, n - c0)
                kblk = [B_kiT[b] for b in range(c0 // 128, (c0 + wd_) // 128)]
                rl = []
                pt3, pb3 = banks[7], bankb[7]

                def hsum(h, rl=rl, pt3=pt3, pb3=pb3, wd_=wd_):
                    R, Rb = rl[h]
                    kb.op("pe", lambda e: e.matmul(pt3[:, 0:wd_], lhsT=dg[:, h, :], rhs=R[:, 0:wd_],
                                                   start=(h == 0), stop=(h == 7)), reads=[B_dg, Rb], writes=[pb3])
                for h in range(8):
                    pt2, pb2 = banks[6], bankb[6]
                    p0 = (h % 2) * 64
                    kb.op("pe", lambda e, pt2=pt2, h=h, p0=p0, c0=c0, wd_=wd_: e.matmul(
                        pt2[:, 0:wd_], lhsT=qiT[:, h // 2, :], rhs=kiT[h % 2][:, c0:c0 + wd_],
                        start=True, stop=True), reads=[B_qi] + kblk, writes=[pb2])
                    R, Rb = Rh[rr % NR], B_Rh[rr % NR]
                    rr += 1
                    kb.op("act", lambda e, pt2=pt2, R=R, wd_=wd_: e.activation(out=R[:, 0:wd_], in_=pt2[:, 0:wd_],
                                                                               func=AF.Relu),
                          reads=[pb2], writes=[Rb])
                    rl.append((R, Rb))
                    if h >= 2:
                        hsum(h - 2)
                    yield
                hsum(6)
                hsum(7)
                if c0 + wd_ == n:
                    if wd_ > 128:
                        kb.op("dve", lambda e, pt3=pt3, c0=c0, wd_=wd_: e.tensor_copy(
                            out=score[:, c0:c0 + wd_ - 128], in_=pt3[:, 0:wd_ - 128]), reads=[pb3], writes=[B_sc])
                    kb.op("dve", lambda e, pt3=pt3, c0=c0, wd_=wd_: e.tensor_tensor(
                        out=score[:, n - 128:n], in0=pt3[:, wd_ - 128:wd_], in1=cmask[:], op=ALU.add),
                        reads=[pb3, B_k], writes=[B_sc])
                else:
                    kb.op("dve", lambda e, pt3=pt3, c0=c0, wd_=wd_: e.tensor_copy(
                        out=score[:, c0:c0 + wd_], in_=pt3[:, 0:wd_]), reads=[pb3], writes=[B_sc])

        def t1_gen(i):
            n = (i + 1) * 128
            nm, B_nm = nms[i % 2], B_nms[i % 2]
            score, B_sc = scores[i % 2], B_scs[i % 2]
            if i < 2:
                kb.op("dve", lambda e: e.memset(thr, -1.0e29), writes=[B_th])
            else:
                kb.op("dve", lambda e: e.max(out=m8, in_=score[:, 0:n]), reads=[B_sc], writes=[B_th])
                yield
                kb.op("dve", lambda e: e.tensor_reduce(out=lo_, in_=score[:, 0:n - 128], axis=AX.X, op=ALU.min),
                      reads=[B_sc], writes=[B_th])
                kb.op("dve", lambda e: e.tensor_tensor(out=w_, in0=thb[:, 7:8], in1=lo_, op=ALU.subtract),
                      reads=[B_th], writes=[B_th])
                kb.op("dve", lambda e: e.tensor_scalar(out=wk, in0=pw2[:], scalar1=w_, scalar2=None, op0=ALU.mult),
                      reads=[B_th, B_k], writes=[B_th])
                kb.op("dve", lambda e: e.tensor_tensor(out=t_, in0=lo_, in1=thb[:, 14:15], op=ALU.add),
                      reads=[B_th], writes=[B_th])
                yield
                for k in range(NIT):
                    kb.op("dve", lambda e: e.tensor_scalar(out=nm[:, 0:n], in0=score[:, 0:n], scalar1=t_, scalar2=0.0,
                                                           op0=ALU.is_ge, op1=ALU.add, accum_out=c_),
                          reads=[B_sc, B_th], writes=[B_nm, B_th])
                    kb.op("dve", lambda e: e.tensor_scalar(out=g_, in0=c_, scalar1=float(TOPK) - 0.5, scalar2=0.5,
                                                           op0=ALU.is_ge, op1=ALU.subtract), reads=[B_th], writes=[B_th])
                    kb.op("dve", lambda e, k=k: e.scalar_tensor_tensor(out=t_, in0=g_, scalar=thb[:, 14 + k:15 + k],
                                                                       in1=t_, op0=ALU.mult, op1=ALU.add),
                          reads=[B_th], writes=[B_th])
                    yield
                kb.op("dve", lambda e: e.tensor_tensor(out=thr, in0=t_, in1=thb[:, 14 + NIT:15 + NIT], op=ALU.subtract),
                      reads=[B_th], writes=[B_th])
            kb.op("dve", lambda e: e.tensor_scalar(out=nm[:, 0:n], in0=score[:, 0:n], scalar1=thr, scalar2=-30000.0,
                                                   op0=ALU.is_lt, op1=ALU.mult), reads=[B_sc, B_th], writes=[B_nm])
            yield

        def stage_b(i, tick=None, drain=None, pre=None):
            xt, xb = xts[i % 4], xbs[i % 4]
            ql, qlb = qlT[i % 3], B_ql[i % 3]
            nm_i, B_nm_i = nms[i % 2], B_nms[i % 2]
            pi = 0
            for hg in range(2):
                accs = [(banks[b_], bankb[b_]) for b_ in range(4)]
                def qk(sbk, hg=hg):
                    pt, pb = pbank2()
                    for rc in range(2):
                        kb.op("pe", lambda e, pt=pt, rc=rc: e.matmul(
                            pt[:], lhsT=ckvT[:, rc, sbk * 128:(sbk + 1) * 128], rhs=ql[:, rc, hg * 512:(hg + 1) * 512],
                            start=(rc == 0), stop=False), reads=[B_ckvT[sbk], qlb], writes=[pb])
                    kb.op("pe", lambda e, pt=pt: e.matmul(
                        pt[:], lhsT=nm_i[:, sbk * 128:(sbk + 1) * 128], rhs=ident4[:].rearrange("p a t -> p (a t)"),
                        start=False, stop=True), reads=[B_nm_i, B_k], writes=[pb])
                    return pt, pb

                cur = qk(0)
                for sbk in range(i + 1):
                    pt, pb = cur
                    P_, Pb = PT[pi % 2], B_PT[pi % 2]
                    pi += 1
                    kb.op("act", lambda e, pt=pt, P_=P_: e.activation(out=P_[:], in_=pt[:], func=AF.Exp),
                          reads=[pb], writes=[Pb])
                    if sbk + 1 <= i:
                        cur = qk(sbk + 1)
                    if tick is not None:
                        tick()
                    for a in range(4):
                        at, ab = accs[a]
                        kb.op("pe", lambda e, at=at, a=a, P_=P_, sbk=sbk: e.matmul(
                            at[:, 0:257], lhsT=P_[:, a * 128:(a + 1) * 128], rhs=ckve[:, sbk, 0:257],
                            start=(sbk == 0), stop=(sbk == i)), reads=[Pb, B_ckve[sbk]], writes=[ab])
                for a in range(4):
                    h = hg * 4 + a
                    at, ab = accs[a]
                    kb.op("dve", lambda e, at=at, h=h: e.reciprocal(out=rden[:, h:h + 1], in_=at[:, 256:257]),
                          reads=[ab], writes=[B_rd])
                    kb.op("dve", lambda e, at=at, h=h: e.tensor_scalar(out=ol[:, h, :], in0=at[:, 0:256],
                                                                       scalar1=rden[:, h:h + 1], scalar2=None,
                                                                       op0=ALU.mult), reads=[ab, B_rd], writes=[B_ol])
            if drain is not None:
                drain()
            if pre is not None:
                pre()
            for half in range(2):
                for a in range(8):
                    blk = half * 8 + a
                    h, rc = blk // 2, blk % 2
                    kb.op("pe", lambda e, a=a, h=h, rc=rc: e.transpose(out=TB[:, a * 128:(a + 1) * 128],
                                                                       in_=ol[:, h, rc * 128:(rc + 1) * 128],
                                                                       identity=ident_b[:]),
                          reads=[B_ol, B_const], writes=[TBb])
                kb.op("dve", lambda e: e.tensor_copy(
                    out=olT[:], in_=TB[:].rearrange("p (a t) -> p a t", a=8)), reads=[TBb], writes=[B_olT])
                pt, pb = pbank()
                for a in range(4):
                    h = half * 4 + a
                    for rc in range(2):
                        kb.op("pe", lambda e, pt=pt, a=a, h=h, rc=rc: e.matmul(
                            pt[:, a * 128:(a + 1) * 128], lhsT=wuv[:, h, rc, :], rhs=olT[:, a * 2 + rc, :],
                            start=(rc == 0), stop=(rc == 1)), reads=[B_w, B_olT], writes=[pb])
                kb.op("act", lambda e, pt=pt, half=half: e.activation(
                    out=oT[:, half * 4:(half + 1) * 4, :].rearrange("p a t -> p (a t)"), in_=pt[:], func=AF.Identity),
                    reads=[pb], writes=[B_oT])
            ys = []
            for dh in range(2):
                pt, pb = pbank()
                for h in range(8):
                    kb.op("pe", lambda e, pt=pt, h=h, dh=dh: e.matmul(
                        pt[:], lhsT=oT[:, h, :], rhs=wo[:, h, dh * 512:(dh + 1) * 512],
                        start=(h == 0), stop=(h == 7)), reads=[B_w, B_oT], writes=[pb])
                ys.append((pt, pb))
            for dh, (pt, pb) in enumerate(ys):
                kb.op("dve", lambda e, pt=pt, dh=dh: e.tensor_tensor(
                    out=xt[:, dh * 512:(dh + 1) * 512], in0=pt[:], in1=xt[:, dh * 512:(dh + 1) * 512], op=ALU.add),
                    reads=[pb, xb], writes=[xb])
            kb.dma("sp", dxs[i * 128:(i + 1) * 128, :], xt[:], reads=[xb], writes=[B_xs[i]])

        def run_all(g):
            for _ in g:
                pass

        pre_front(0)
        stage_a(0)
        run_all(idx_gen(0))
        if ntiles > 1:
            pre_front(1)
        for i in range(ntiles):
            if i + 1 < ntiles:
                stage_a(i + 1)
            gi = idx_gen(i + 1) if i + 1 < ntiles else iter(())
            gt = t1_gen(i)
            nloop = max(1, 2 * i)
            t_every = max(1, nloop // 16)
            cnt = [0]

            def tick(gi=gi, gt=gt, cnt=cnt, t_every=t_every):
                next(gi, None)
                cnt[0] += 1
                if cnt[0] % t_every == 0:
                    next(gt, None)

            def drain(gi=gi, gt=gt):
                run_all(gi)
                run_all(gt)
            pre = (lambda i=i: pre_front(i + 2)) if i + 2 < ntiles else None
            if i >= 1:
                stage_b(i - 1, tick, drain, pre)
            else:
                drain()
                if pre is not None:
                    pre()
            if i >= 2:
                bg_issue(1)
        stage_b(ntiles - 1)
        bg_issue(1000)
        kb.barrier()
        ps_.close()

    src = dx
    for ph in phases:
        if ph[0] == "mod":
            phase_mod()
        elif ph[0] == "dsa":
            phase_dsa(ph[1], ph[1] // 2, src, *(ph[2:]))
            src = dxs
        elif ph[0] == "gmlp":
            phase_gmlp(ph[1], ph[1] // 2, src)
            src = dxs
        elif ph[0] == "ffn":
            phase_ffn(ph[1], src, *(ph[2:]))
            src = dxs
        elif ph[0] == "final":
            phase_final(src)
        elif ph[0] == "dump":
            B_o = Buf()
            for i in range(NT):
                kb.dma("sp", dout[i * 128:(i + 1) * 128, :], dxs[i * 128:(i + 1) * 128, :], reads=[B_xs[i]], writes=[B_o])
        elif ph[0] == "dbgmod":
            ddbg = dram("dbg", [128, 4 * 48 + 64], kind="ExternalOutput")
            kb.dma("sp", ddbg[:, 0:192], modT[:].rearrange("p l c -> p (l c)"), reads=[B_modT])
            kb.dma("sp", ddbg[:, 192:224], amix[:].rearrange("p l c -> p (l c)"), reads=[B_modT])
            kb.dma("sp", ddbg[:, 224:256], affn[:].rearrange("p l c -> p (l c)"), reads=[B_modT])
    kb.barrier()
    es.close()
    return nc


FULL = [("mod",)]
for _l in range(4):
    FULL.append(("dsa" if _l % 2 == 0 else "gmlp", _l))
    FULL.append(("ffn", _l) if _l < 3 else ("ffn", _l, True))


def _core_inputs(inp, b):
    f = np.float32
    c = np.ascontiguousarray
    m = {
        "x": c(inp["x"][b], dtype=f),
        "cT": c(np.asarray(inp["c"][b], dtype=f).reshape(8, 128).T),
        "mod_w": c(inp["mod_w"], dtype=f),
        "mod_b": c(inp["mod_b"], dtype=f),
        "gmixT": c(np.asarray(inp["norm_mix_g"], dtype=f).reshape(4, 8, 128).transpose(2, 0, 1).reshape(128, 32)),
        "gffnT": c(np.asarray(inp["norm_ffn_g"], dtype=f).reshape(4, 8, 128).transpose(2, 0, 1).reshape(128, 32)),
        "final_g": c(np.asarray(inp["final_g"], dtype=f).reshape(1, D)),
        "a_w_in": c(inp["a_w_in"], dtype=f),
        "a_g_kv": c(inp["a_g_kv"], dtype=f),
        "a_g_kidx": c(inp["a_g_kidx"], dtype=f),
        "a_b_kidx": c(inp["a_b_kidx"], dtype=f),
        "a_w_uk": c(inp["a_w_uk"], dtype=f),
        "a_w_uv": c(inp["a_w_uv"], dtype=f),
        "a_w_o": c(inp["a_w_o"], dtype=f),
        "b_w_in": c(inp["b_w_in"], dtype=f),
        "b_ln_g": c(inp["b_ln_g"], dtype=f),
        "b_ln_b": c(inp["b_ln_b"], dtype=f),
        "b_w_sT": c(np.asarray(inp["b_w_s"], dtype=f).transpose(0, 3, 1, 2).reshape(2, 128, 1024)),
        "b_b_s": c(np.asarray(inp["b_b_s"], dtype=f).reshape(2, 1024)),
        "b_w_out": c(inp["b_w_out"], dtype=f),
        "ffn_w_gate": c(inp["ffn_w_gate"], dtype=f),
        "ffn_w_up": c(inp["ffn_w_up"], dtype=f),
        "ffn_w_down": c(inp["ffn_w_down"], dtype=f),
    }
    return m


def kernel(**inputs):
    inp = {k: np.asarray(v) for k, v in inputs.items()}
    nc = _build(FULL)
    in_maps = [_core_inputs(inp, b) for b in range(8)]
    res = run_bass_kernel_spmd(nc, in_maps, core_ids=list(range(8)))
    out = np.stack([np.asarray(r["out"], dtype=np.float32) for r in res.results], axis=0)
    return out
```
